# Optimizing a Trainium2 kernel written in Bass

```python
import jax
import jax.numpy as jnp
from jax import lax
import numpy as np

D_MODEL = 1024
BATCH = 32
SEQ = 2048
DEPTH = 4

N_MIXERS = 4
GROUP_WIDTH = D_MODEL // N_MIXERS
HEADS = 4
HEAD_DIM = GROUP_WIDTH // HEADS
CONV_WIDTH = 3
SGU_CHUNK = 128
GLA_KEY_DIM = HEAD_DIM // 2
GLA_GATE_RANK = 16
GLA_GATE_NORMALIZER = 16.0
LA_CHUNK = 16
D_FF = 2816
N_EXPERTS = 8
TOP_K = 2
D_FF_EXPERT = 3584
N_MOD = 6
N_DENSE_LAYERS = (DEPTH + 1) // 2
N_MOE_LAYERS = DEPTH // 2
EPS = 1e-6
PROJ_SIZES = (
    GROUP_WIDTH, GROUP_WIDTH, GROUP_WIDTH,
    GROUP_WIDTH, GROUP_WIDTH,
    HEADS * GLA_KEY_DIM, HEADS * GLA_KEY_DIM, GROUP_WIDTH,
    GLA_GATE_RANK, GROUP_WIDTH,
    GROUP_WIDTH, GROUP_WIDTH, GROUP_WIDTH, GROUP_WIDTH,
)
IN_PROJ_WIDTH = sum(PROJ_SIZES)

kernel_name = "hybrid_parallel_groups_adaln_moe_trunk"


def rms_norm(x, g):
    xf = x.astype(jnp.float32)
    y = xf * lax.rsqrt(jnp.mean(xf * xf, axis=-1, keepdims=True) + EPS)
    return y.astype(x.dtype) * g


def causal_depthwise_conv(x, w):
    s = x.shape[1]
    xp = jnp.pad(x, ((0, 0), (CONV_WIDTH - 1, 0), (0, 0)))
    return sum(xp[:, k:k + s] * w[k] for k in range(CONV_WIDTH))


def short_conv_mixer(b_gate, c_gate, x_in, w_conv):
    return b_gate * causal_depthwise_conv(c_gate * x_in, w_conv)


def spatial_gating_mixer(u, v, norm_g, w_s, b_s):
    bn, s, _ = v.shape
    n = s // SGU_CHUNK
    vf = v.astype(jnp.float32)
    mu = jnp.mean(vf, axis=-1, keepdims=True)
    var = jnp.mean(jnp.square(vf - mu), axis=-1, keepdims=True)
    vn = ((vf - mu) * lax.rsqrt(var + EPS)).astype(v.dtype) * norm_g
    vc = vn.reshape(bn, n, SGU_CHUNK, HEADS, HEAD_DIM)
    causal = jnp.tril(jnp.ones((SGU_CHUNK, SGU_CHUNK), dtype=bool))
    ws = jnp.where(causal[None], w_s, 0.0)
    mixed = jnp.einsum('hts,bnshd->bnthd', ws, vc) + b_s.T[None, None, :, :, None]
    return u * mixed.reshape(bn, s, GROUP_WIDTH)


def chunk_gated_linear_attention(q, k, v, log_a):
    bn, s, h, kd = q.shape
    vd = v.shape[-1]
    n = s // LA_CHUNK
    f32 = jnp.float32
    q, k, v, log_a = (t.astype(f32).reshape(bn, n, LA_CHUNK, h, t.shape[-1]) for t in (q, k, v, log_a))
    b = jnp.cumsum(log_a, axis=2)
    mid = LA_CHUNK // 2
    b_ref = b[:, :, mid:mid + 1]
    scores = jnp.einsum('bnihk,bnjhk->bnhij', q * jnp.exp(b - b_ref), k * jnp.exp(b_ref - b))
    causal = jnp.tril(jnp.ones((LA_CHUNK, LA_CHUNK), dtype=bool))
    scores = jnp.where(causal, scores, 0.0)
    o_intra = jnp.einsum('bnhij,bnjhv->bnihv', scores, v)
    b_last = b[:, :, -1:]
    k_upd = k * jnp.exp(b_last - b)
    decay = jnp.exp(b_last[:, :, 0])

    def step(state, xs):
        d, kc, vc = xs
        new_state = d[..., None] * state + jnp.einsum('bjhk,bjhv->bhkv', kc, vc)
        return new_state, state

    s0 = jnp.zeros((bn, h, kd, vd), f32)
    _, s_prev = lax.scan(step, s0, (jnp.moveaxis(decay, 1, 0), jnp.moveaxis(k_upd, 1, 0), jnp.moveaxis(v, 1, 0)))
    o_inter = jnp.einsum('bnihk,nbhkv->bnihv', q * jnp.exp(b), s_prev)
    return (o_intra + o_inter).reshape(bn, s, h, vd)


def gla_mixer(q, k, v, g_lowrank, r, w_gate, b_gate, norm_g):
    bn, s, _ = q.shape
    log_a = jax.nn.log_sigmoid((g_lowrank @ w_gate + b_gate).astype(jnp.float32)) / GLA_GATE_NORMALIZER
    o = chunk_gated_linear_attention(
        q.reshape(bn, s, HEADS, GLA_KEY_DIM) * (GLA_KEY_DIM ** -0.5),
        k.reshape(bn, s, HEADS, GLA_KEY_DIM),
        v.reshape(bn, s, HEADS, HEAD_DIM),
        log_a.reshape(bn, s, HEADS, GLA_KEY_DIM))
    o = rms_norm(o.astype(q.dtype), norm_g)
    return o.reshape(bn, s, GROUP_WIDTH) * jax.nn.silu(r)


def hgrn2_mixer(q, f_logit, i_in, g, lower_bound, norm_g):
    bn, s, _ = q.shape
    f = lower_bound + (1.0 - lower_bound) * jax.nn.sigmoid(f_logit.astype(jnp.float32))
    o = chunk_gated_linear_attention(
        q.reshape(bn, s, HEADS, HEAD_DIM),
        (1.0 - f).reshape(bn, s, HEADS, HEAD_DIM),
        i_in.reshape(bn, s, HEADS, HEAD_DIM),
        jnp.log(f).reshape(bn, s, HEADS, HEAD_DIM))
    o = rms_norm(o.astype(q.dtype), norm_g)
    return o.reshape(bn, s, GROUP_WIDTH) * jax.nn.silu(g)


def swiglu(x, w1, w3, w2):
    return (jax.nn.silu(x @ w1) * (x @ w3)) @ w2


def moe_swiglu(x, w_router, w1, w3, w2):
    bn, s, d = x.shape
    t = x.reshape(bn * s, d)
    logits = (t @ w_router).astype(jnp.float32)
    top_val, top_idx = lax.top_k(logits, TOP_K)
    top_w = jax.nn.softmax(top_val, axis=-1)
    combine = jnp.sum(jax.nn.one_hot(top_idx, N_EXPERTS, dtype=jnp.float32) * top_w[..., None], axis=1)
    out = jnp.zeros_like(t)
    for e in range(N_EXPERTS):
        out = out + combine[:, e:e + 1].astype(t.dtype) * swiglu(t, w1[e], w3[e], w2[e])
    return out.reshape(bn, s, d)


def setup_inputs(seed: int = 0) -> dict:
    key = jax.random.key(seed)
    ks = jax.random.split(key, 25)
    f32 = jnp.float32

    def normal(k, shape, scale):
        return scale * jax.random.normal(k, shape, f32)

    def gain(k, shape):
        return 1.0 + normal(k, shape, 0.02)

    L = DEPTH
    return {
        "x": normal(ks[0], (BATCH, SEQ, D_MODEL), 1.0),
        "c": normal(ks[1], (BATCH, D_MODEL), 1.0),
        "norm_mix_g": gain(ks[2], (L, D_MODEL)),
        "norm_ffn_g": gain(ks[3], (L, D_MODEL)),
        "final_norm_g": gain(ks[4], (D_MODEL,)),
        "w_ada": normal(ks[5], (L, D_MODEL, N_MOD * D_MODEL), 0.5 * D_MODEL ** -0.5),
        "b_ada": normal(ks[6], (L, N_MOD * D_MODEL), 0.02),
        "w_in": normal(ks[7], (L, D_MODEL, IN_PROJ_WIDTH), D_MODEL ** -0.5),
        "w_out": normal(ks[8], (L, N_MIXERS * GROUP_WIDTH, D_MODEL), (N_MIXERS * GROUP_WIDTH) ** -0.5),
        "conv_w": normal(ks[9], (L, CONV_WIDTH, GROUP_WIDTH), CONV_WIDTH ** -0.5),
        "sgu_norm_g": gain(ks[10], (L, GROUP_WIDTH)),
        "sgu_w": normal(ks[11], (L, HEADS, SGU_CHUNK, SGU_CHUNK), SGU_CHUNK ** -0.5),
        "sgu_b": gain(ks[12], (L, HEADS, SGU_CHUNK)),
        "gla_w_gate": normal(ks[13], (L, GLA_GATE_RANK, HEADS * GLA_KEY_DIM), GLA_GATE_RANK ** -0.5),
        "gla_b_gate": normal(ks[14], (L, HEADS * GLA_KEY_DIM), 0.1),
        "gla_norm_g": gain(ks[15], (L, HEAD_DIM)),
        "hgrn_lower_bounds": normal(ks[16], (L, GROUP_WIDTH), 0.1),
        "hgrn_norm_g": gain(ks[17], (L, HEAD_DIM)),
        "ffn_w1": normal(ks[18], (N_DENSE_LAYERS, D_MODEL, D_FF), D_MODEL ** -0.5),
        "ffn_w3": normal(ks[19], (N_DENSE_LAYERS, D_MODEL, D_FF), D_MODEL ** -0.5),
        "ffn_w2": normal(ks[20], (N_DENSE_LAYERS, D_FF, D_MODEL), D_FF ** -0.5),
        "moe_router": normal(ks[21], (N_MOE_LAYERS, D_MODEL, N_EXPERTS), D_MODEL ** -0.5),
        "moe_w1": normal(ks[22], (N_MOE_LAYERS, N_EXPERTS, D_MODEL, D_FF_EXPERT), D_MODEL ** -0.5),
        "moe_w3": normal(ks[23], (N_MOE_LAYERS, N_EXPERTS, D_MODEL, D_FF_EXPERT), D_MODEL ** -0.5),
        "moe_w2": normal(ks[24], (N_MOE_LAYERS, N_EXPERTS, D_FF_EXPERT, D_MODEL), D_FF_EXPERT ** -0.5),
    }


def reference(x, c, norm_mix_g, norm_ffn_g, final_norm_g, w_ada, b_ada, w_in, w_out, conv_w,
              sgu_norm_g, sgu_w, sgu_b, gla_w_gate, gla_b_gate, gla_norm_g, hgrn_lower_bounds,
              hgrn_norm_g, ffn_w1, ffn_w3, ffn_w2, moe_router, moe_w1, moe_w3, moe_w2):
    split_points = np.cumsum(PROJ_SIZES)[:-1].tolist()
    cond = jax.nn.silu(c)
    lb_cum = jnp.cumsum(jax.nn.softmax(hgrn_lower_bounds.astype(jnp.float32), axis=0), axis=0)
    lower_bounds = lb_cum - lb_cum[0]
    for layer in range(DEPTH):
        mod = cond @ w_ada[layer] + b_ada[layer]
        shift_m, scale_m, gate_m, shift_f, scale_f, gate_f = [m[:, None, :] for m in jnp.split(mod, N_MOD, axis=-1)]
        h = rms_norm(x, norm_mix_g[layer]) * (1.0 + scale_m) + shift_m
        (cb, cc, cx, su, sv, aq, ak, av, ag, ar, hq, hf, hi, hg) = jnp.split(h @ w_in[layer], split_points, axis=-1)
        y_conv = short_conv_mixer(cb, cc, cx, conv_w[layer])
        y_sgu = spatial_gating_mixer(su, sv, sgu_norm_g[layer], sgu_w[layer], sgu_b[layer])
        y_gla = gla_mixer(aq, ak, av, ag, ar, gla_w_gate[layer], gla_b_gate[layer], gla_norm_g[layer])
        y_hgrn = hgrn2_mixer(hq, hf, hi, hg, lower_bounds[layer], hgrn_norm_g[layer])
        mixed = jnp.concatenate([y_conv, y_sgu, y_gla, y_hgrn], axis=-1) @ w_out[layer]
        x = x + gate_m * mixed
        h = rms_norm(x, norm_ffn_g[layer]) * (1.0 + scale_f) + shift_f
        idx = layer // 2
        if layer % 2 == 0:
            y = swiglu(h, ffn_w1[idx], ffn_w3[idx], ffn_w2[idx])
        else:
            y = moe_swiglu(h, moe_router[idx], moe_w1[idx], moe_w3[idx], moe_w2[idx])
        x = x + gate_f * y
    return rms_norm(x, final_norm_g)
```

```python
import contextlib
import types
import numpy as np
import concourse.bass as bass
import concourse.mybir as mybir
from concourse.bass_utils import run_bass_kernel_spmd

F32 = mybir.dt.float32
BF16 = mybir.dt.bfloat16
AF = mybir.ActivationFunctionType
ALU = mybir.AluOpType
AX = mybir.AxisListType

D = 1024
DC = 8
GW = 256
HD = 64
NE = 8
DFF = 2816
DFE = 3584
INW = 3088
EPS = 1e-6
TT = 512
CH = 32
SLAB = 256


class Dep:
    __slots__ = ("name", "last_writer", "readers", "dma_sem", "dma_count")

    def __init__(self, name="", after=None):
        self.name = name
        self.last_writer = after
        self.readers = []
        self.dma_sem = None
        self.dma_count = 0


class Op:
    __slots__ = ("eng", "fn", "deps", "signaled", "is_dma", "dep_obj", "sem_val")

    def __init__(self, eng, fn, is_dma=False):
        self.eng = eng
        self.fn = fn
        self.deps = []
        self.signaled = False
        self.is_dma = is_dma
        self.dep_obj = None
        self.sem_val = None


ENGS = ("pe", "act", "dve", "pool", "sp")


def _freeze(fn):
    if fn.__closure__ is None:
        return fn
    cells = []
    for c in fn.__closure__:
        try:
            cells.append(types.CellType(c.cell_contents))
        except ValueError:
            cells.append(c)
    g = types.FunctionType(fn.__code__, fn.__globals__, fn.__name__, fn.__defaults__, tuple(cells))
    g.__kwdefaults__ = fn.__kwdefaults__
    return g


class Prog:
    def __init__(self, nc):
        self.nc = nc
        self.ops = {e: [] for e in ENGS}
        self.n_dma_sems = 0
        self.final_waits = []

    def _collect(self, op, reads, writes, same_engine_sync=True):
        deps = []
        for d in reads:
            w = d.last_writer
            if w is not None:
                deps.append(w)
        for d in writes:
            w = d.last_writer
            if w is not None:
                if not (op.is_dma and w.is_dma):
                    deps.append(w)
            deps.extend(d.readers)
        out = []
        seen = set()
        for w in deps:
            if w is op or id(w) in seen:
                continue
            if (not w.is_dma) and w.eng == op.eng and not same_engine_sync:
                continue
            seen.add(id(w))
            out.append(w)
        op.deps = out
        for w in out:
            w.signaled = True
        for d in writes:
            d.last_writer = op
            d.readers = []
        for d in reads:
            d.readers.append(op)

    def op(self, eng, fn, reads=(), writes=()):
        o = Op(eng, _freeze(fn))
        self._collect(o, reads, writes, same_engine_sync=(eng != "pe"))
        self.ops[eng].append(o)
        return o

    def dma(self, queue, fn, reads=(), writes=(), dep=None):
        o = Op(queue, _freeze(fn), is_dma=True)
        d = dep if dep is not None else writes[0]
        if d.dma_sem is None:
            d.dma_sem = self.n_dma_sems
            self.n_dma_sems += 1
        d.dma_count += 1
        o.dep_obj = d
        o.sem_val = 16 * d.dma_count
        o.signaled = True
        self._collect(o, reads, writes)
        self.ops[queue].append(o)
        return o

    def pe(self, fn, reads=(), writes=()):
        return self.op("pe", fn, reads, writes)

    def act(self, fn, reads=(), writes=()):
        return self.op("act", fn, reads, writes)

    def dve(self, fn, reads=(), writes=()):
        return self.op("dve", fn, reads, writes)

    def pool(self, fn, reads=(), writes=()):
        return self.op("pool", fn, reads, writes)

    def barrier(self, deps):
        return self.op("dve", lambda e: e.memset(self.scratch, 0.0), reads=(), writes=list(deps) + [self.scratch_dep])

    def emit(self):
        nc = self.nc
        for e in ENGS:
            cnt = 0
            for o in self.ops[e]:
                if o.is_dma:
                    continue
                if o.signaled:
                    cnt += 1
                    o.sem_val = cnt
        with contextlib.ExitStack() as st:
            eng_sems = {e: st.enter_context(nc.semaphore("s_" + e)) for e in ENGS}
            dma_sems = [st.enter_context(nc.semaphore("d%d" % i)) for i in range(self.n_dma_sems)]
            block = st.enter_context(nc.Block())

            def tok(o):
                if o.is_dma:
                    return ("d", o.dep_obj.dma_sem), dma_sems[o.dep_obj.dma_sem], o.sem_val
                return ("e", o.eng), eng_sems[o.eng], o.sem_val

            def run(ename, engine):
                known = {}
                for o in self.ops[ename]:
                    need = {}
                    for w in o.deps:
                        k, s, v = tok(w)
                        if known.get(k, 0) >= v:
                            continue
                        if k not in need or need[k][1] < v:
                            need[k] = (s, v)
                    for k, (s, v) in need.items():
                        engine.wait_ge(s, v)
                        known[k] = v
                    ins = o.fn(engine)
                    if o.is_dma:
                        ins.then_inc(dma_sems[o.dep_obj.dma_sem], 16)
                    elif o.signaled:
                        ins.then_inc(eng_sems[ename], 1)
                if ename == "sp":
                    for w in self.final_waits:
                        k, s, v = tok(w)
                        engine.wait_ge(s, v)

            @block.tensor
            def _(t):
                run("pe", t)

            @block.scalar
            def _(t):
                run("act", t)

            @block.vector
            def _(t):
                run("dve", t)

            @block.gpsimd
            def _(t):
                run("pool", t)

            @block.sync
            def _(t):
                run("sp", t)


class Arena:
    def __init__(self, tensor, words):
        self.t = tensor
        self.words = words
        self.off = 0
        self.marks = []

    def alloc(self, shape, dtype=F32):
        n = int(np.prod(shape))
        w = n if dtype == F32 else (n + 1) // 2
        w = (w + 7) // 8 * 8
        assert self.off + w <= self.words, ("arena overflow", self.off, w, self.words)
        v = self.t[:, self.off:self.off + w]
        self.off += w
        if dtype != F32:
            v = v.bitcast(dtype)
        v = v[:, 0:n]
        if len(shape) == 2:
            v = v.rearrange("p (a b) -> p a b", a=shape[0])
        elif len(shape) == 3:
            v = v.rearrange("p (a b c) -> p a b c", a=shape[0], b=shape[1])
        elif len(shape) == 4:
            v = v.rearrange("p (a b c d) -> p a b c d", a=shape[0], b=shape[1], c=shape[2])
        return v

    def mark(self):
        return self.off

    def reset(self, m):
        self.off = m


def bc_last(ap, n):
    shp = list(ap.shape)
    return ap.unsqueeze(len(shp)).to_broadcast(shp + [n])


def bc_mid(ap, n):
    shp = list(ap.shape)
    return ap.unsqueeze(1).to_broadcast([shp[0], n] + shp[1:])


def build_program(NB, T, L, n_moe, diag_skip_mixers=False):
    NTT = T // TT
    n_dense = (L + 1) // 2
    nc = bass.Bass("TRN2", target_bir_lowering=False)

    def din(name, shape):
        return nc.dram_tensor(name, list(shape), F32, kind="ExternalInput").ap()

    xT = din("xT", [NB, D, T])
    cT = din("cT", [128, DC, NB])
    g_mix = din("g_mix", [128, L, DC])
    g_ffn = din("g_ffn", [128, L, DC])
    g_fin = din("g_fin", [128, DC])
    w_ada = din("w_ada", [L, D, 6 * D])
    b_ada = din("b_ada", [128, L, 48])
    w_in = din("w_in", [L, D, INW])
    w_out = din("w_out", [L, D, D])
    conv_w = din("conv_w", [128, L, 3, 2])
    sgu_g = din("sgu_g", [128, L, 2])
    sgu_wT = din("sgu_wT", [128, L, 4, 128])
    sgu_bf = din("sgu_bf", [128, L, 2, 128])
    gla_wg = din("gla_wg", [16, L, 128])
    gla_bg = din("gla_bg", [128, L])
    gla_ng = din("gla_ng", [128, L])
    hg_lb = din("hg_lb", [128, 2, L])
    hg_ng = din("hg_ng", [128, L])
    ffn_w1 = din("ffn_w1", [n_dense, D, DFF])
    ffn_w3 = din("ffn_w3", [n_dense, D, DFF])
    ffn_w2 = din("ffn_w2", [n_dense, DFF, D])
    moe_r = din("moe_r", [max(n_moe, 1), D, NE])
    moe_w1 = din("moe_w1", [max(n_moe, 1), NE, D, DFE])
    moe_w3 = din("moe_w3", [max(n_moe, 1), NE, D, DFE])
    moe_w2 = din("moe_w2", [max(n_moe, 1), NE, DFE, D])
    outT = nc.dram_tensor("outT", [NB, D, T], F32, kind="ExternalOutput").ap()

    P = Prog(nc)
    with contextlib.ExitStack() as st:
        def sb(name, shape, dt=F32):
            return st.enter_context(nc.sbuf_tensor(name, list(shape), dt))

        x = sb("x", [128, DC, T])
        h = sb("h", [128, DC, T], BF16)
        RINGW = 4352
        ring = [sb("ring%d" % i, [128, RINGW]) for i in range(2)]
        UW = 13696
        ureg = sb("ureg", [128, UW])
        ones_bf = sb("ones_bf", [128, 128], BF16)
        blk_bf = sb("blk_bf", [128, 128], BF16)
        ident_bf = sb("ident_bf", [128, 128], BF16)
        ident_f = sb("ident_f", [128, 128])
        ones_f = sb("ones_f", [128, 128])
        maskBD = sb("maskBD", [128, 128])
        m32 = sb("m32", [128, TT])
        hmask4 = sb("hmask4", [128, 4])
        hmask2 = sb("hmask2", [128, 2])
        cmask = sb("cmask", [128, 4])
        sel = sb("sel", [8, NE, 128])
        scratch = sb("scratch", [128, 8])
        P.scratch = scratch[:, 0:1]
        P.scratch_dep = Dep("scratch")
        cond = sb("cond", [128, DC, NB])
        gmix_s = sb("gmix_s", [128, L, DC])
        gffn_s = sb("gffn_s", [128, L, DC])
        gfin_s = sb("gfin_s", [128, DC])
        bada_s = sb("bada_s", [128, L, 48])
        modv = sb("modv", [128, L, 48, NB])
        gsm = sb("gsm", [128, L, DC, NB])
        gsf = sb("gsf", [128, L, DC, NB])
        convw_s = sb("convw_s", [128, L, 3, 2])
        sgug_s = sb("sgug_s", [128, L, 2])
        sguw_s = sb("sguw_s", [128, L, 4, 128], BF16)
        sgub_s = sb("sgub_s", [128, L, 2, 128])
        wg_s = sb("wg_s", [16, L, 128])
        nbg_s = sb("nbg_s", [128, L])
        glang_s = sb("glang_s", [128, L])
        hgng_s = sb("hgng_s", [128, L])
        lbe = sb("lbe", [128, 2, L])
        lb_s = sb("lb_s", [128, 2, L])
        oml_s = sb("oml_s", [128, 2, L])
        lbt = sb("lbt", [128, 2, 2])
        wr_s = sb("wr_s", [128, max(n_moe, 1), DC, NE], BF16)

        ps = [st.enter_context(nc.psum_tensor("ps%d" % i, [128, 512], F32)) for i in range(7)]
        psT = st.enter_context(nc.psum_tensor("psT", [128, 1024], BF16))
        d_ps = [Dep("ps%d" % i) for i in range(7)]
        d_psT = Dep("psT")

        d_x = [[Dep("x%d_%d" % (c, t)) for t in range(NTT)] for c in range(DC)]
        d_h = [[Dep("h%d_%d" % (c, t)) for t in range(NTT)] for c in range(DC)]
        d_ring = [[Dep("ring%d_%d" % (i, j)) for j in range(3)] for i in range(2)]
        d_const = Dep("const")
        d_par = Dep("par")
        d_setup = Dep("setup")

        C = [d_const]
        P.pool(lambda e: e.memset(ones_bf[:], 1.0), writes=C)
        P.pool(lambda e: e.memset(ones_f[:], 1.0), writes=C)
        P.pool(lambda e: e.memset(blk_bf[:], 0.0), writes=C)
        P.pool(lambda e: e.memset(blk_bf[0:64, 0:64], 1.0), reads=C, writes=C)
        P.pool(lambda e: e.memset(blk_bf[64:128, 64:128], 1.0), reads=C, writes=C)
        P.pool(lambda e: e.affine_select(out=ident_bf[:], in_=ones_bf[:], pattern=[[-1, 128]], compare_op=ALU.is_equal,
                                          fill=0.0, base=0, channel_multiplier=1), reads=C, writes=C)
        P.pool(lambda e: e.affine_select(out=ident_f[:], in_=ones_f[:], pattern=[[-1, 128]], compare_op=ALU.is_equal,
                                          fill=0.0, base=0, channel_multiplier=1), reads=C, writes=C)
        P.pool(lambda e: e.affine_select(out=maskBD[:], in_=ones_f[:], pattern=[[1, 128]], compare_op=ALU.is_ge,
                                          fill=0.0, base=0, channel_multiplier=-1), reads=C, writes=C)
        for bl in range(1, 4):
            P.pool(lambda e, bl=bl: e.affine_select(out=maskBD[:, 32 * bl:32 * bl + 32], in_=maskBD[:, 32 * bl:32 * bl + 32],
                                                    pattern=[[0, 32]], compare_op=ALU.is_ge, fill=0.0, base=-32 * bl,
                                                    channel_multiplier=1), reads=C, writes=C)
        P.pool(lambda e: e.memset(m32[:], 1.0), reads=C, writes=C)
        P.pool(lambda e: e.memset(m32[:].rearrange("p (n k) -> p n k", k=CH)[:, :, 0:1], 0.0), reads=C, writes=C)
        for (msk, nh, dk) in ((hmask4, 4, 32), (hmask2, 2, 64), (cmask, 4, 32)):
            P.pool(lambda e, msk=msk, nh=nh, dk=dk: e.affine_select(out=msk[:], in_=ones_f[:, 0:nh], pattern=[[-dk, nh]], compare_op=ALU.is_ge,
                                                                    fill=0.0, base=0, channel_multiplier=1), reads=C, writes=C)
            P.pool(lambda e, msk=msk, nh=nh, dk=dk: e.affine_select(out=msk[:], in_=msk[:], pattern=[[dk, nh]], compare_op=ALU.is_ge,
                                                                    fill=0.0, base=dk - 1, channel_multiplier=-1), reads=C, writes=C)
        P.pool(lambda e: e.affine_select(out=sel[:], in_=bc_mid(ones_f[0:8, :], NE), pattern=[[-1, NE], [0, 128]], compare_op=ALU.is_equal,
                                          fill=0.0, base=0, channel_multiplier=1), reads=C, writes=C)

        Wp = [d_par]
        for dst, src in ((cond, cT), (gmix_s, g_mix), (gffn_s, g_ffn), (gfin_s, g_fin), (bada_s, b_ada), (convw_s, conv_w),
                         (sgug_s, sgu_g), (sgub_s, sgu_bf), (wg_s, gla_wg), (nbg_s, gla_bg), (glang_s, gla_ng),
                         (hgng_s, hg_ng), (lbe, hg_lb)):
            P.dma("sp", lambda e, dst=dst, src=src: e.dma_start(out=dst[:], in_=src), writes=Wp)
        P.dma("pool", lambda e: e.dma_start(out=sguw_s[:], in_=sgu_wT), writes=Wp)
        if n_moe:
            for m in range(n_moe):
                P.dma("pool", lambda e, m=m: e.dma_start(out=wr_s[:, m], in_=moe_r[m].rearrange("(c p) e -> p c e", p=128)), writes=Wp)
        S = [d_setup]
        R_ = [d_par, d_const]
        for l in range(L):
            for hh in range(4):
                P.pool(lambda e, l=l, hh=hh: e.affine_select(out=sguw_s[:, l, hh, :], in_=sguw_s[:, l, hh, :], pattern=[[1, 128]],
                                                             compare_op=ALU.is_ge, fill=0.0, base=0, channel_multiplier=-1),
                       reads=R_, writes=S)
        P.act(lambda e: e.activation(out=cond[:], in_=cond[:], func=AF.Silu), reads=R_, writes=S)
        P.dve(lambda e: e.tensor_scalar(out=nbg_s[:], in0=nbg_s[:], scalar1=-1.0, scalar2=None, op0=ALU.mult), reads=R_, writes=S)
        P.act(lambda e: e.activation(out=lbe[:], in_=lbe[:], func=AF.Exp), reads=R_ + S, writes=S)
        P.dve(lambda e: e.tensor_reduce(out=lbt[:, :, 0:1], in_=lbe[:], axis=AX.X, op=ALU.add), reads=S, writes=S)
        P.dve(lambda e: e.reciprocal(out=lbt[:, :, 1:2], in_=lbt[:, :, 0:1]), reads=S, writes=S)
        P.dve(lambda e: e.tensor_tensor(out=lbe[:], in0=lbe[:], in1=lbt[:, :, 1:2].to_broadcast([128, 2, L]), op=ALU.mult), reads=S, writes=S)
        P.dve(lambda e: e.memset(lb_s[:, :, 0:1], 0.0), reads=S, writes=S)
        for l in range(1, L):
            P.dve(lambda e, l=l: e.tensor_tensor(out=lb_s[:, :, l:l + 1], in0=lb_s[:, :, l - 1:l], in1=lbe[:, :, l:l + 1], op=ALU.add), reads=S, writes=S)
        P.dve(lambda e: e.tensor_scalar(out=oml_s[:], in0=lb_s[:], scalar1=-1.0, scalar2=1.0, op0=ALU.mult, op1=ALU.add), reads=S, writes=S)

        ADW = 512
        rv = [ring[i][:, 0:DC * ADW].rearrange("p (c n) -> p c n", c=DC) for i in range(2)]
        step = 0
        d_ada = [Dep("ada0"), Dep("ada1")]
        for l in range(L):
            for pc in range(6 * D // ADW):
                s_ = step % 2
                step += 1
                P.dma("sp", lambda e, l=l, pc=pc, s_=s_: e.dma_start(out=rv[s_], in_=w_ada[l][:, pc * ADW:(pc + 1) * ADW].rearrange("(c p) n -> p c n", p=128)),
                      writes=[d_ada[s_]])
                for jj in range(ADW // 128):
                    j = pc * (ADW // 128) + jj
                    for c in range(DC):
                        P.pe(lambda e, s_=s_, jj=jj, j=j, c=c: e.matmul(ps[0][:, j * NB:(j + 1) * NB], lhsT=rv[s_][:, c, jj * 128:(jj + 1) * 128],
                                                                      rhs=cond[:, c, :], start=(c == 0), stop=(c == DC - 1)),
                             reads=[d_ada[s_], d_setup], writes=[d_ps[0]])
            P.dve(lambda e, l=l: e.tensor_tensor(out=modv[:, l], in0=ps[0][:, 0:48 * NB].rearrange("p (j b) -> p j b", b=NB),
                                                 in1=bc_last(bada_s[:, l, :], NB), op=ALU.add), reads=[d_ps[0], d_par], writes=S)
            for (gdst, gsrc, j0) in ((gsm, gmix_s, 8), (gsf, gffn_s, 32)):
                P.dve(lambda e, l=l, gdst=gdst, j0=j0: e.tensor_scalar(out=gdst[:, l], in0=modv[:, l, j0:j0 + 8, :], scalar1=1.0, scalar2=None, op0=ALU.add),
                      reads=S, writes=S)
                P.dve(lambda e, l=l, gdst=gdst, gsrc=gsrc: e.tensor_tensor(out=gdst[:, l], in0=gdst[:, l], in1=bc_last(gsrc[:, l, :], NB), op=ALU.mult),
                      reads=S + [d_par], writes=S)

        ada_done = P.barrier(d_ada)
        for i in range(2):
            for j in range(3):
                d_ring[i][j].last_writer = ada_done
        CONSTS = [d_const, d_par, d_setup]

        ua = Arena(ureg, UW)
        phase = {"deps": [], "after": None}

        def newdep(name=""):
            d_ = Dep(name, after=phase["after"])
            phase["deps"].append(d_)
            return d_

        def phase_switch():
            if phase["deps"]:
                phase["after"] = P.barrier(phase["deps"])
            phase["deps"] = []
            ua.reset(0)

        rot = {"i": 0}

        def rot_bank(n=3):
            i = rot["i"] % n
            rot["i"] += 1
            return i

        def rmsnorm_to_h(l, b, gs_t, sh_j0):
            phase_switch()
            sq = [ua.alloc([TT], BF16) for _ in range(3)]
            d_sq = [newdep() for _ in range(3)]
            lnv = ua.alloc([TT]); rstd = ua.alloc([TT])
            d_ln = newdep(); d_rs = newdep()
            tmp = [ua.alloc([TT]) for _ in range(3)]
            d_tmp = [newdep() for _ in range(3)]
            k = 0
            for tt in range(NTT):
                tsl = slice(tt * TT, (tt + 1) * TT)
                bk = rot_bank()
                for c in range(DC):
                    i = k % 3
                    k += 1
                    P.act(lambda e, c=c, i=i, tsl=tsl: e.activation(out=sq[i], in_=x[:, c, tsl], func=AF.Square),
                          reads=[d_x[c][tt]], writes=[d_sq[i]])
                    P.pe(lambda e, c=c, i=i, bk=bk: e.matmul(ps[bk][:, :], lhsT=ones_bf[:], rhs=sq[i], start=(c == 0), stop=(c == DC - 1)),
                         reads=[d_sq[i], d_const], writes=[d_ps[bk]])
                P.act(lambda e, bk=bk: e.activation(out=lnv, in_=ps[bk][:, :], func=AF.Ln, scale=1.0 / D, bias=EPS),
                      reads=[d_ps[bk]], writes=[d_ln])
                P.act(lambda e: e.activation(out=rstd, in_=lnv, func=AF.Exp, scale=-0.5), reads=[d_ln], writes=[d_rs])
                for c in range(DC):
                    i = k % 3
                    k += 1
                    P.dve(lambda e, c=c, i=i, tsl=tsl: e.tensor_tensor(out=tmp[i], in0=x[:, c, tsl], in1=rstd, op=ALU.mult),
                          reads=[d_x[c][tt], d_rs], writes=[d_tmp[i]])
                    P.act(lambda e, c=c, i=i, tsl=tsl: e.activation(out=h[:, c, tsl], in_=tmp[i], func=AF.Identity,
                                                                    scale=gs_t[:, l, c, b:b + 1], bias=modv[:, l, sh_j0 + c, b:b + 1]),
                          reads=[d_tmp[i], d_setup], writes=[d_h[c][tt]])

        ring_state = {"n": 0}

        def ring_next():
            s_ = ring_state["n"] % 2
            ring_state["n"] += 1
            return s_

        WO_OFF = 3328

        def load_mixer_weights(l, ranges, r0, nrows):
            s_ = ring_next()
            ncols = sum(n for _, n in ranges)
            assert DC * ncols // 2 <= WO_OFF
            wv = ring[s_][:, 0:DC * ncols // 2].bitcast(BF16).rearrange("p (c n) -> p c n", c=DC)
            nkc = nrows // 128
            wo = ring[s_][:, WO_OFF:WO_OFF + nkc * D // 2].bitcast(BF16).rearrange("p (c n) -> p c n", c=nkc)
            nwords = DC * ncols // 2
            span = [d_ring[s_][j] for j in range(3) if nwords > (0, 1024, 2048)[j]]
            off = 0
            for (c0, n) in ranges:
                P.dma("pool", lambda e, c0=c0, n=n, off=off: e.dma_start(out=wv[:, :, off:off + n],
                                                                        in_=w_in[l][:, c0:c0 + n].rearrange("(c p) n -> p c n", p=128)),
                      writes=span, dep=d_ring[s_][0])
                off += n
            P.dma("pool", lambda e: e.dma_start(out=wo, in_=w_out[l][r0:r0 + nrows, :].rearrange("(c p) n -> p c n", p=128)),
                  writes=[d_ring[s_][2]], dep=d_ring[s_][2])
            dwo_ = [d_ring[s_][2]]
            return wv, wo, span, dwo_

        def proj_fm(wv, dw, col, tt, bk, M=128):
            tsl = slice(tt * TT, (tt + 1) * TT)
            for c in range(DC):
                P.pe(lambda e, c=c: e.matmul(ps[bk][0:M, :], lhsT=wv[:, c, col:col + M], rhs=h[:, c, tsl], start=(c == 0), stop=(c == DC - 1)),
                     reads=dw + [d_h[c][tt]], writes=[d_ps[bk]])

        def proj_tok(wv, dw, col, ncols, tt, sub, bk, off):
            t0 = tt * TT + sub * 128
            for c in range(DC):
                P.pe(lambda e, c=c: e.matmul(ps[bk][:, off:off + ncols], lhsT=h[:, c, t0:t0 + 128], rhs=wv[:, c, col:col + ncols],
                                             start=(c == 0), stop=(c == DC - 1)),
                     reads=dw + [d_h[c][tt]], writes=[d_ps[bk]])

        def out_proj(l, b, wo, dwo, y, d_y, tt, nk=2):
            tsl = slice(tt * TT, (tt + 1) * TT)
            for co in range(DC):
                bk = rot_bank()
                for kc in range(nk):
                    P.pe(lambda e, co=co, kc=kc, bk=bk: e.matmul(ps[bk][:, :], lhsT=wo[:, kc, co * 128:(co + 1) * 128], rhs=y[:, kc, :],
                                                                 start=(kc == 0), stop=(kc == nk - 1)),
                         reads=dwo + [d_y], writes=[d_ps[bk]])
                P.dve(lambda e, co=co, bk=bk: e.scalar_tensor_tensor(out=x[:, co, tsl], in0=ps[bk][:, :], scalar=modv[:, l, 16 + co, b:b + 1],
                                                                     in1=x[:, co, tsl], op0=ALU.mult, op1=ALU.add),
                      reads=[d_ps[bk], d_x[co][tt], d_setup], writes=[d_x[co][tt]])

        def conv_pass(l, b):
            phase_switch()
            wv, wo, dw, dwo = load_mixer_weights(l, [(0, 768)], 0, 256)
            z = ua.alloc([2, TT + 2]); d_z = [newdep(), newdep()]
            cxs = [ua.alloc([TT]) for _ in range(2)]; d_cxs = [newdep(), newdep()]
            acc = [ua.alloc([TT]) for _ in range(2)]; d_acc = [newdep(), newdep()]
            y = [ua.alloc([2, TT], BF16) for _ in range(2)]; d_y = [newdep(), newdep()]
            for ch in range(2):
                P.dve(lambda e, ch=ch: e.memset(z[:, ch, 0:2], 0.0), writes=[d_z[ch]])
            for tt in range(NTT):
                yi = tt % 2
                for ch in range(2):
                    b_cc = rot_bank(); proj_fm(wv, dw, 256 + ch * 128, tt, b_cc)
                    b_cx = rot_bank(); proj_fm(wv, dw, 512 + ch * 128, tt, b_cx)
                    P.act(lambda e, ch=ch, b_cx=b_cx: e.activation(out=cxs[ch], in_=ps[b_cx][:, :], func=AF.Copy),
                          reads=[d_ps[b_cx]], writes=[d_cxs[ch]])
                    P.dve(lambda e, ch=ch, b_cc=b_cc: e.tensor_tensor(out=z[:, ch, 2:TT + 2], in0=ps[b_cc][:, :], in1=cxs[ch], op=ALU.mult),
                          reads=[d_ps[b_cc], d_cxs[ch]], writes=[d_z[ch]])
                    P.dve(lambda e, ch=ch: e.tensor_scalar(out=acc[ch], in0=z[:, ch, 0:TT], scalar1=convw_s[:, l, 0, ch:ch + 1], scalar2=None, op0=ALU.mult),
                          reads=[d_z[ch], d_par], writes=[d_acc[ch]])
                    for kk in (1, 2):
                        P.dve(lambda e, ch=ch, kk=kk: e.scalar_tensor_tensor(out=acc[ch], in0=z[:, ch, kk:TT + kk], scalar=convw_s[:, l, kk, ch:ch + 1],
                                                                             in1=acc[ch], op0=ALU.mult, op1=ALU.add),
                              reads=[d_z[ch], d_par, d_acc[ch]], writes=[d_acc[ch]])
                    P.dve(lambda e, ch=ch: e.tensor_copy(out=z[:, ch, 0:2], in_=z[:, ch, TT:TT + 2]), reads=[d_z[ch]], writes=[d_z[ch]])
                    b_cb = rot_bank(); proj_fm(wv, dw, ch * 128, tt, b_cb)
                    P.dve(lambda e, ch=ch, b_cb=b_cb, yi=yi: e.tensor_tensor(out=y[yi][:, ch, :], in0=ps[b_cb][:, :], in1=acc[ch], op=ALU.mult),
                          reads=[d_ps[b_cb], d_acc[ch]], writes=[d_y[yi]])
                out_proj(l, b, wo, dwo, y[yi], d_y[yi], tt)

        def sgu_pass(l, b):
            phase_switch()
            wv, wo, dw, dwo = load_mixer_weights(l, [(768, 512)], 256, 256)
            vn = [ua.alloc([256], BF16) for _ in range(2)]; d_vn = [newdep(), newdep()]
            stats = ua.alloc([2, 6]); mv = ua.alloc([2, 2]); rs = ua.alloc([2, 2]); d_st = [newdep(), newdep()]
            t1 = [ua.alloc([TT]) for _ in range(2)]; d_t1 = [newdep(), newdep()]
            y = [ua.alloc([2, TT], BF16) for _ in range(2)]; d_y = [newdep(), newdep()]
            k = 0
            for tt in range(NTT):
                yi = tt % 2
                bm = [3, 4]
                for sub in range(4):
                    i = k % 2
                    k += 1
                    bv = rot_bank()
                    proj_tok(wv, dw, 256, 256, tt, sub, bv, 0)
                    P.dve(lambda e, i=i, bv=bv: e.bn_stats(out=stats[:, i, :], in_=ps[bv][:, 0:256]), reads=[d_ps[bv]], writes=[d_st[i]])
                    P.dve(lambda e, i=i: e.bn_aggr(out=mv[:, i, :], in_=stats[:, i, :]), reads=[d_st[i]], writes=[d_st[i]])
                    P.act(lambda e, i=i: e.activation(out=rs[:, i, 0:1], in_=mv[:, i, 1:2], func=AF.Ln, bias=EPS, scale=1.0), reads=[d_st[i]], writes=[d_st[i]])
                    P.act(lambda e, i=i: e.activation(out=rs[:, i, 1:2], in_=rs[:, i, 0:1], func=AF.Exp, scale=-0.5), reads=[d_st[i]], writes=[d_st[i]])
                    P.dve(lambda e, i=i, bv=bv: e.tensor_scalar(out=vn[i], in0=ps[bv][:, 0:256], scalar1=mv[:, i, 0:1], scalar2=rs[:, i, 1:2],
                                                                op0=ALU.subtract, op1=ALU.mult),
                          reads=[d_ps[bv], d_st[i]], writes=[d_vn[i]])
                    for hh in range(4):
                        cc, hl = hh // 2, hh % 2
                        P.pe(lambda e, i=i, hh=hh, cc=cc, hl=hl, sub=sub: e.matmul(ps[bm[cc]][64 * hl:64 * hl + 64, sub * 128:(sub + 1) * 128],
                                                                                   lhsT=vn[i][:, hh * 64:(hh + 1) * 64], rhs=sguw_s[:, l, hh, :],
                                                                                   start=True, stop=True),
                             reads=[d_vn[i], d_setup], writes=[d_ps[bm[cc]]])
                for cc in range(2):
                    bu = rot_bank(); proj_fm(wv, dw, cc * 128, tt, bu)
                    P.dve(lambda e, cc=cc: e.scalar_tensor_tensor(out=t1[cc].rearrange("p (a t) -> p a t", a=4),
                                                                  in0=ps[bm[cc]][:, :].rearrange("p (a t) -> p a t", a=4),
                                                                  scalar=sgug_s[:, l, cc:cc + 1], in1=bc_mid(sgub_s[:, l, cc, :], 4),
                                                                  op0=ALU.mult, op1=ALU.add),
                          reads=[d_ps[bm[cc]], d_par], writes=[d_t1[cc]])
                    P.dve(lambda e, cc=cc, bu=bu, yi=yi: e.tensor_tensor(out=y[yi][:, cc, :], in0=ps[bu][:, :], in1=t1[cc], op=ALU.mult),
                          reads=[d_ps[bu], d_t1[cc]], writes=[d_y[yi]])
                out_proj(l, b, wo, dwo, y[yi], d_y[yi], tt)

        def recur_pass(l, b, kind, g=0):
            phase_switch()
            if kind == "gla":
                wv, wo, dw, dwo = load_mixer_weights(l, [(1280, 784)], 512, 256)
                nh, dk, sc, hmask, ng = 4, 32, -1.0 / 16.0, hmask4, glang_s
                qc, kc, vc, gate_col, noc = 0, 128, 256, 528, 2
            else:
                base = 2064
                wv, wo, dw, dwo = load_mixer_weights(l, [(base + g * 128, 128), (base + 256 + g * 128, 128), (base + 512 + g * 128, 128),
                                                        (base + 768 + g * 128, 128)], 768 + g * 128, 128)
                nh, dk, sc, hmask, ng = 2, 64, 1.0, hmask2, hgng_s
                qc, fcol, vc, gate_col, noc = 0, 128, 256, 384, 1
            BO = [5, 6]
            BA, BU = 3, 4
            ext = ua.alloc([64, 9]); d_ext = newdep()
            dece = ua.alloc([2, 9]); d_dece = newdep()
            y = [ua.alloc([noc, TT], BF16) for _ in range(2)]; d_y = [newdep(), newdep()]
            sgate = ua.alloc([noc, TT]); d_sg = [newdep() for _ in range(noc)]
            qt = ua.alloc([TT], BF16); d_qt = newdep()
            kt = ua.alloc([TT], BF16); d_kt = newdep()
            ktm = ua.alloc([nh, TT], BF16); d_ktm = newdep()
            ktok = ua.alloc([4, 128], BF16); d_ktok = newdep()
            V = ua.alloc([4, nh * 64], BF16); d_V = newdep()
            Vbd = ua.alloc([2, nh, 4, 64], BF16); d_Vbd = newdep()
            A = [ua.alloc([nh, 128], BF16) for _ in range(2)]; d_A = [newdep(), newdep()]
            T1 = ua.alloc([TT]); d_T1 = newdep()
            T2 = ua.alloc([TT]); d_T2 = newdep()
            T3 = ua.alloc([TT]); d_T3 = newdep()
            T4 = ua.alloc([TT]); d_T4 = newdep()
            T5 = ua.alloc([TT]); d_T5 = newdep()
            T6 = ua.alloc([TT]); d_T6 = newdep()
            sm = ua.alloc([4, 16]); d_sm = newdep()
            emh = ua.alloc([4, 16]); d_emh = newdep()
            cUe = ua.alloc([64, 9]); d_cUe = newdep()
            decf = ua.alloc([64, 9]); d_decf = newdep()
            Sbd = ua.alloc([8, nh * 64], BF16); d_Sbd = newdep()
            osq = ua.alloc([TT], BF16); d_osq = newdep()
            P.dve(lambda e: e.memset(dece, 0.0), writes=[d_dece])
            P.dve(lambda e: e.memset(ext, 0.0), writes=[d_ext])
            ai = 0
            for tt in range(NTT):
                tsl = slice(tt * TT, (tt + 1) * TT)
                yi = tt % 2
                for oc in range(noc):
                    bg_ = rot_bank(); proj_fm(wv, dw, gate_col + oc * 128, tt, bg_)
                    P.act(lambda e, bg_=bg_: e.activation(out=T1, in_=ps[bg_][:, :], func=AF.Exp, scale=-1.0), reads=[d_ps[bg_]], writes=[d_T1])
                    P.act(lambda e: e.activation(out=T2, in_=T1, func=AF.Ln, bias=1.0, scale=1.0), reads=[d_T1], writes=[d_T2])
                    P.act(lambda e: e.activation(out=T3, in_=T2, func=AF.Exp, scale=-1.0), reads=[d_T2], writes=[d_T3])
                    P.dve(lambda e, oc=oc, bg_=bg_: e.tensor_tensor(out=sgate[:, oc, :], in0=ps[bg_][:, :], in1=T3, op=ALU.mult),
                          reads=[d_ps[bg_], d_T3], writes=[d_sg[oc]])
                if kind == "gla":
                    bgl = rot_bank(); proj_fm(wv, dw, 512, tt, bgl, M=16)
                    P.act(lambda e, bgl=bgl: e.activation(out=T5[0:16, :], in_=ps[bgl][0:16, :], func=AF.Copy), reads=[d_ps[bgl]], writes=[d_T5])
                    bz = rot_bank()
                    P.pe(lambda e, bz=bz: e.matmul(ps[bz][:, :], lhsT=wg_s[0:16, l, :], rhs=T5[0:16, :], start=True, stop=True),
                         reads=[d_T5, d_par], writes=[d_ps[bz]])
                    P.act(lambda e, bz=bz: e.activation(out=T1, in_=ps[bz][:, :], func=AF.Exp, scale=-1.0, bias=nbg_s[:, l:l + 1]),
                          reads=[d_ps[bz], d_setup], writes=[d_T1])
                    P.act(lambda e: e.activation(out=T2, in_=T1, func=AF.Ln, bias=1.0, scale=1.0), reads=[d_T1], writes=[d_T2])
                    src_l, d_src = T2, d_T2
                else:
                    bf_ = rot_bank(); proj_fm(wv, dw, fcol, tt, bf_)
                    P.act(lambda e, bf_=bf_: e.activation(out=T1, in_=ps[bf_][:, :], func=AF.Exp, scale=-1.0), reads=[d_ps[bf_]], writes=[d_T1])
                    P.act(lambda e: e.activation(out=T2, in_=T1, func=AF.Ln, bias=1.0, scale=1.0), reads=[d_T1], writes=[d_T2])
                    P.act(lambda e: e.activation(out=T3, in_=T1, func=AF.Ln, bias=1.0, scale=lb_s[:, g, l:l + 1]), reads=[d_T1, d_setup], writes=[d_T3])
                    P.dve(lambda e, bf_=bf_: e.tensor_tensor(out=T6, in0=ps[bf_][:, :], in1=T2, op=ALU.add), reads=[d_ps[bf_], d_T2], writes=[d_T6])
                    P.act(lambda e: e.activation(out=T6, in_=T6, func=AF.Exp, scale=-1.0), reads=[d_T6], writes=[d_T6])
                    P.dve(lambda e: e.tensor_tensor(out=T3, in0=T3, in1=T2, op=ALU.subtract), reads=[d_T3, d_T2], writes=[d_T3])
                    src_l, d_src = T3, d_T3
                P.dve(lambda e, src_l=src_l: e.tensor_tensor_scan(out=T4, data0=m32[:], data1=src_l, initial=0.0, op0=ALU.mult, op1=ALU.add),
                      reads=[d_src, d_const], writes=[d_T4])
                b3 = T4.rearrange("p (n k) -> p n k", k=CH)
                b4 = T4.rearrange("p (a n k) -> p a n k", a=2, k=CH)
                P.dve(lambda e, b3=b3: e.tensor_tensor(out=T5.rearrange("p (n k) -> p n k", k=CH), in0=b3,
                                                       in1=b3[:, :, CH // 2:CH // 2 + 1].to_broadcast([128, 16, CH]), op=ALU.subtract),
                      reads=[d_T4, d_T5], writes=[d_T5])
                P.act(lambda e: e.activation(out=T1, in_=T5, func=AF.Exp, scale=sc), reads=[d_T5, d_T1], writes=[d_T1])
                P.act(lambda e: e.activation(out=T2, in_=T5, func=AF.Exp, scale=-sc), reads=[d_T5, d_T2], writes=[d_T2])
                bmid, blast = b3[:, :, CH // 2], b3[:, :, CH - 1]
                P.act(lambda e, bmid=bmid: e.activation(out=sm[:, 0, :], in_=bmid, func=AF.Exp, scale=sc), reads=[d_T4], writes=[d_sm])
                P.act(lambda e, b4=b4: e.activation(out=dece[:, :, 1:9], in_=b4[:, :, :, CH - 1], func=AF.Exp, scale=sc), reads=[d_T4], writes=[d_dece])
                P.dve(lambda e, bmid=bmid, blast=blast: e.tensor_tensor(out=sm[:, 2, :], in0=blast, in1=bmid, op=ALU.subtract), reads=[d_T4, d_sm], writes=[d_sm])
                P.act(lambda e: e.activation(out=sm[:, 1, :], in_=sm[:, 2, :], func=AF.Exp, scale=sc), reads=[d_sm], writes=[d_sm])
                P.dve(lambda e: e.tensor_tensor(out=emh[:, 0:nh, :], in0=bc_mid(sm[:, 0, :], nh), in1=bc_last(hmask[:, 0:nh], 16), op=ALU.mult),
                      reads=[d_sm, d_const], writes=[d_emh])
                bq = rot_bank(); proj_fm(wv, dw, qc, tt, bq)
                if kind == "gla":
                    P.dve(lambda e, bq=bq: e.scalar_tensor_tensor(out=qt, in0=ps[bq][:, :], scalar=float(32 ** -0.5), in1=T1, op0=ALU.mult, op1=ALU.mult),
                          reads=[d_ps[bq], d_T1], writes=[d_qt])
                    bk_ = rot_bank(); proj_fm(wv, dw, kc, tt, bk_)
                    P.dve(lambda e, bk_=bk_: e.tensor_tensor(out=kt, in0=ps[bk_][:, :], in1=T2, op=ALU.mult), reads=[d_ps[bk_], d_T2], writes=[d_kt])
                else:
                    P.dve(lambda e, bq=bq: e.tensor_tensor(out=qt, in0=ps[bq][:, :], in1=T1, op=ALU.mult), reads=[d_ps[bq], d_T1], writes=[d_qt])
                    P.dve(lambda e: e.scalar_tensor_tensor(out=kt, in0=T6, scalar=oml_s[:, g, l:l + 1], in1=T2, op0=ALU.mult, op1=ALU.mult),
                          reads=[d_T6, d_T2, d_setup], writes=[d_kt])
                for hh in range(nh):
                    P.dve(lambda e, hh=hh: e.tensor_scalar(out=ktm[:, hh, :], in0=kt, scalar1=hmask[:, hh:hh + 1], scalar2=None, op0=ALU.mult),
                          reads=[d_kt, d_const], writes=[d_ktm])
                for sub in range(4):
                    P.pe(lambda e, sub=sub: e.transpose(psT[:, sub * 128:(sub + 1) * 128], kt[:, sub * 128:(sub + 1) * 128], ident_bf[:]),
                         reads=[d_kt, d_const], writes=[d_psT])
                P.act(lambda e: e.activation(out=ktok.rearrange("p a t -> p (a t)"), in_=psT[:, 0:512], func=AF.Copy), reads=[d_psT], writes=[d_ktok])
                for half in range(2):
                    bv = rot_bank()
                    for s2 in range(2):
                        proj_tok(wv, dw, vc, nh * 64, tt, half * 2 + s2, bv, s2 * 256)
                    P.act(lambda e, bv=bv, half=half: e.activation(out=V[:, half * 2:half * 2 + 2, :],
                                                                   in_=ps[bv][:, :].rearrange("p (s c) -> p s c", s=2)[:, :, 0:nh * 64], func=AF.Copy),
                          reads=[d_ps[bv]], writes=[d_V])
                    for s2 in range(2):
                        for n_ in range(4):
                            P.pool(lambda e, s2=s2, n_=n_, half=half: e.tensor_scalar(
                                out=Vbd[:, s2, :, n_, :], in0=V[:, half * 2 + s2, :].rearrange("p (h v) -> p h v", h=nh),
                                scalar1=cmask[:, n_:n_ + 1], scalar2=1.0, op0=ALU.mult, op1=ALU.mult),
                                reads=[d_V, d_const], writes=[d_Vbd])
                    for s2 in range(2):
                        sub = half * 2 + s2
                        for hh in range(nh):
                            kw = {"tile_position": (0, 96)} if hh * dk == 96 else {}
                            P.pe(lambda e, s2=s2, sub=sub, hh=hh, kw=kw: e.matmul(
                                ps[BU][hh * dk:(hh + 1) * dk, s2 * 256:(s2 + 1) * 256], lhsT=ktok[:, sub, hh * dk:(hh + 1) * dk],
                                rhs=Vbd[:, s2, hh, :, :].rearrange("p n v -> p (n v)"), start=True, stop=True, **kw),
                                reads=[d_ktok, d_Vbd], writes=[d_ps[BU]])
                    n0 = half * 8
                    P.dve(lambda e, n0=n0: e.tensor_tensor(out=cUe[:, :, 1:9], in0=ps[BU][:, :].rearrange("p (n v) -> p v n", v=64),
                                                           in1=bc_mid(sm[:, 1, n0:n0 + 8], 64), op=ALU.mult),
                          reads=[d_ps[BU], d_sm], writes=[d_cUe])
                    P.dve(lambda e: e.tensor_copy(out=cUe[:, :, 0:1], in_=ext[:, :, 8:9]), reads=[d_ext, d_cUe], writes=[d_cUe])
                    P.dve(lambda e, half=half: e.tensor_copy(out=decf, in_=bc_mid(dece[:, half, :], 64)), reads=[d_dece], writes=[d_decf])
                    P.dve(lambda e: e.tensor_tensor_scan(out=ext.rearrange("p v n -> p (v n)"), data0=decf.rearrange("p v n -> p (v n)"),
                                                         data1=cUe.rearrange("p v n -> p (v n)"), initial=0.0, op0=ALU.mult, op1=ALU.add),
                          reads=[d_cUe, d_decf], writes=[d_ext])
                    for hh in range(nh):
                        P.dve(lambda e, hh=hh, n0=n0: e.tensor_tensor(out=Sbd[:, :, hh * 64:(hh + 1) * 64], in0=ext[:, :, 0:8].rearrange("p v n -> p n v"),
                                                                      in1=bc_last(emh[:, hh, n0:n0 + 8], 64), op=ALU.mult),
                              reads=[d_ext, d_emh], writes=[d_Sbd])
                    for s2 in range(2):
                        sub = half * 2 + s2
                        ssl = slice(sub * 128, (sub + 1) * 128)
                        for hh in range(nh):
                            P.pe(lambda e, hh=hh, ssl=ssl: e.matmul(ps[BA][:, hh * 128:(hh + 1) * 128], lhsT=ktm[:, hh, ssl], rhs=qt[:, ssl], start=True, stop=True),
                                 reads=[d_ktm, d_qt], writes=[d_ps[BA]])
                        a_ = ai % 2
                        ai += 1
                        P.dve(lambda e, a_=a_: e.tensor_tensor(out=A[a_], in0=ps[BA][:, 0:nh * 128].rearrange("p (h i) -> p h i", h=nh),
                                                               in1=bc_mid(maskBD[:], nh), op=ALU.mult),
                              reads=[d_ps[BA], d_const], writes=[d_A[a_]])
                        for hh in range(nh):
                            oc, hl = hh // 2, hh % 2
                            P.pe(lambda e, a_=a_, hh=hh, oc=oc, hl=hl, sub=sub, ssl=ssl: e.matmul(
                                ps[BO[oc]][64 * hl:64 * hl + 64, ssl], lhsT=V[:, sub, hh * 64:(hh + 1) * 64], rhs=A[a_][:, hh, :], start=True, stop=False),
                                reads=[d_V, d_A[a_]], writes=[d_ps[BO[oc]]])
                        for n_ in range(4):
                            nn = s2 * 4 + n_
                            csl = slice(sub * 128 + n_ * 32, sub * 128 + n_ * 32 + 32)
                            for oc in range(noc):
                                P.pe(lambda e, nn=nn, oc=oc, csl=csl, n_=n_: e.matmul(ps[BO[oc]][:, csl], lhsT=Sbd[:, nn, oc * 128:(oc + 1) * 128], rhs=qt[:, csl],
                                                                                  start=False, stop=(n_ == 3)),
                                     reads=[d_Sbd, d_qt], writes=[d_ps[BO[oc]]])
                for oc in range(noc):
                    P.act(lambda e, oc=oc: e.activation(out=osq, in_=ps[BO[oc]][:, :], func=AF.Square), reads=[d_ps[BO[oc]]], writes=[d_osq])
                    bs_ = rot_bank()
                    P.pe(lambda e, bs_=bs_: e.matmul(ps[bs_][:, :], lhsT=blk_bf[:], rhs=osq, start=True, stop=True), reads=[d_osq, d_const], writes=[d_ps[bs_]])
                    P.act(lambda e, bs_=bs_: e.activation(out=T1, in_=ps[bs_][:, :], func=AF.Ln, scale=1.0 / HD, bias=EPS), reads=[d_ps[bs_], d_T1], writes=[d_T1])
                    P.act(lambda e: e.activation(out=T2, in_=T1, func=AF.Exp, scale=-0.5), reads=[d_T1, d_T2], writes=[d_T2])
                    P.dve(lambda e, oc=oc: e.tensor_tensor(out=T3, in0=ps[BO[oc]][:, :], in1=T2, op=ALU.mult), reads=[d_ps[BO[oc]], d_T2, d_T3], writes=[d_T3])
                    P.dve(lambda e, oc=oc, yi=yi: e.scalar_tensor_tensor(out=y[yi][:, oc, :], in0=T3, scalar=ng[:, l:l + 1], in1=sgate[:, oc, :],
                                                                         op0=ALU.mult, op1=ALU.mult),
                          reads=[d_T3, d_sg[oc], d_par], writes=[d_y[yi]])
                out_proj(l, b, wo, dwo, y[yi], d_y[yi], tt, nk=noc)

        def ffn_slabs(l, b, w1d, w3d, w2d, dff, comb=None, st_=None):
            nsl = (dff + SLAB - 1) // SLAB
            hid, d_hid, sa, d_sa, tq, d_tq = st_["hid"], st_["d_hid"], st_["sa"], st_["d_sa"], st_["tq"], st_["d_tq"]
            pending = None

            def issue(si):
                f0 = si * SLAB
                sw = min(SLAB, dff - f0)
                s_ = ring_next()
                w1v = ring[s_][:, 0:1024].bitcast(BF16).rearrange("p (c n) -> p c n", c=DC)[:, :, 0:sw]
                w3v = ring[s_][:, 1024:2048].bitcast(BF16).rearrange("p (c n) -> p c n", c=DC)[:, :, 0:sw]
                w2v = ring[s_][:, 2048:3072].bitcast(BF16).rearrange("p (c n) -> p c n", c=2)[:, 0:sw // 128, :]
                P.dma("pool", lambda e: e.dma_start(out=w1v, in_=w1d[:, f0:f0 + sw].rearrange("(c p) n -> p c n", p=128)), writes=[d_ring[s_][0]])
                P.dma("pool", lambda e: e.dma_start(out=w3v, in_=w3d[:, f0:f0 + sw].rearrange("(c p) n -> p c n", p=128)), writes=[d_ring[s_][1]])
                P.dma("pool", lambda e: e.dma_start(out=w2v, in_=w2d[f0:f0 + sw, :].rearrange("(c p) n -> p c n", p=128)), writes=[d_ring[s_][2]])
                return (s_, sw, w1v, w3v, w2v)

            nxt = issue(0)
            for si in range(nsl):
                s_, sw, w1v, w3v, w2v = nxt
                if si + 1 < nsl:
                    nxt = issue(si + 1)
                nfc = sw // 128
                for tt in range(NTT):
                    tsl = slice(tt * TT, (tt + 1) * TT)
                    hi = st_["k"] % 2
                    st_["k"] += 1
                    for fc in range(nfc):
                        pa = (st_["p"] % 2) * 2
                        st_["p"] += 1
                        for (wv_, dr, bk) in ((w1v, d_ring[s_][0], pa), (w3v, d_ring[s_][1], pa + 1)):
                            for c in range(DC):
                                P.pe(lambda e, wv_=wv_, c=c, fc=fc, bk=bk: e.matmul(ps[bk][:, :], lhsT=wv_[:, c, fc * 128:(fc + 1) * 128], rhs=h[:, c, tsl],
                                                                                  start=(c == 0), stop=(c == DC - 1)),
                                     reads=[dr, d_h[c][tt]], writes=[d_ps[bk]])
                        qi = st_["q"] % 3
                        st_["q"] += 1
                        P.act(lambda e, qi=qi, pa=pa: e.activation(out=sa[qi], in_=ps[pa][:, :], func=AF.Silu), reads=[d_ps[pa]], writes=[d_sa[qi]])
                        if comb is not None:
                            cb_, d_cb = comb
                            P.pool(lambda e, qi=qi: e.tensor_tensor(out=tq[qi], in0=sa[qi], in1=cb_[:, tsl], op=ALU.mult), reads=[d_sa[qi], d_cb], writes=[d_tq[qi]])
                            src, dsrc = tq[qi], d_tq[qi]
                        else:
                            src, dsrc = sa[qi], d_sa[qi]
                        P.dve(lambda e, hi=hi, fc=fc, pa=pa, src=src: e.tensor_tensor(out=hid[hi][:, fc, :], in0=ps[pa + 1][:, :], in1=src, op=ALU.mult),
                              reads=[d_ps[pa + 1], dsrc], writes=[d_hid[hi][fc]])
                    for co in range(DC):
                        bk = 4 + st_["o"] % 3
                        st_["o"] += 1
                        for fc in range(nfc):
                            P.pe(lambda e, co=co, fc=fc, bk=bk, hi=hi: e.matmul(ps[bk][:, :], lhsT=w2v[:, fc, co * 128:(co + 1) * 128], rhs=hid[hi][:, fc, :],
                                                                               start=(fc == 0), stop=(fc == nfc - 1)),
                                 reads=[d_ring[s_][2], d_hid[hi][fc]], writes=[d_ps[bk]])
                        P.dve(lambda e, co=co, bk=bk: e.scalar_tensor_tensor(out=x[:, co, tsl], in0=ps[bk][:, :], scalar=modv[:, l, 40 + co, b:b + 1],
                                                                             in1=x[:, co, tsl], op0=ALU.mult, op1=ALU.add),
                              reads=[d_ps[bk], d_x[co][tt], d_setup], writes=[d_x[co][tt]])

        def ffn_state():
            st_ = dict(k=0, p=0, q=0, o=0)
            st_["hid"] = [ua.alloc([SLAB // 128, TT], BF16) for _ in range(2)]
            st_["d_hid"] = [[newdep() for _ in range(4)] for _ in range(2)]
            st_["sa"] = [ua.alloc([TT]) for _ in range(3)]
            st_["d_sa"] = [newdep() for _ in range(3)]
            st_["tq"] = [ua.alloc([TT]) for _ in range(3)]
            st_["d_tq"] = [newdep() for _ in range(3)]
            return st_

        def dense_ffn(l, b):
            phase_switch()
            st_ = ffn_state()
            idx = l // 2
            ffn_slabs(l, b, ffn_w1[idx], ffn_w3[idx], ffn_w2[idx], DFF, None, st_)

        def moe_ffn(l, b):
            phase_switch()
            idx = l // 2
            st_ = ffn_state()
            NS = T // 128
            lg = ua.alloc([NS, NE]); d_lg = newdep()
            top = ua.alloc([NS, 8]); d_top = newdep()
            wts = ua.alloc([4, NS]); d_w = newdep()
            cmb = ua.alloc([NS, NE]); d_cmb = newdep()
            cm2 = ua.alloc([NS, NE]); d_cm2 = newdep()
            combT = ua.alloc([T]); d_cT = newdep()
            cbc = [ua.alloc([T]) for _ in range(2)]; d_cbc = [newdep(), newdep()]
            for s in range(NS):
                tt = (s * 128) // TT
                for c in range(DC):
                    P.pe(lambda e, s=s, c=c: e.matmul(ps[0][:, s * NE:(s + 1) * NE], lhsT=h[:, c, s * 128:(s + 1) * 128], rhs=wr_s[:, idx, c, :],
                                                      start=(c == 0), stop=(c == DC - 1)),
                         reads=[d_h[c][tt], d_par], writes=[d_ps[0]])
            P.dve(lambda e: e.tensor_copy(out=lg.rearrange("p s e -> p (s e)"), in_=ps[0][:, 0:NS * NE]), reads=[d_ps[0]], writes=[d_lg])
            for s in range(NS):
                P.dve(lambda e, s=s: e.max(out=top[:, s, :], in_=lg[:, s, :]), reads=[d_lg], writes=[d_top])
            m1 = top[:, :, 0]
            m2 = top[:, :, 1]
            P.dve(lambda e: e.tensor_tensor(out=wts[:, 0, :], in0=m2, in1=m1, op=ALU.subtract), reads=[d_top], writes=[d_w])
            P.act(lambda e: e.activation(out=wts[:, 0, :], in_=wts[:, 0, :], func=AF.Exp), reads=[d_w], writes=[d_w])
            P.dve(lambda e: e.tensor_scalar(out=wts[:, 1, :], in0=wts[:, 0, :], scalar1=1.0, scalar2=None, op0=ALU.add), reads=[d_w], writes=[d_w])
            P.dve(lambda e: e.reciprocal(out=wts[:, 1, :], in_=wts[:, 1, :]), reads=[d_w], writes=[d_w])
            P.dve(lambda e: e.tensor_tensor(out=wts[:, 2, :], in0=wts[:, 0, :], in1=wts[:, 1, :], op=ALU.mult), reads=[d_w], writes=[d_w])
            P.dve(lambda e: e.tensor_tensor(out=cmb, in0=lg, in1=bc_last(m1, NE), op=ALU.is_equal), reads=[d_lg, d_top], writes=[d_cmb])
            P.dve(lambda e: e.tensor_tensor(out=cmb, in0=cmb, in1=bc_last(wts[:, 1, :], NE), op=ALU.mult), reads=[d_cmb, d_w], writes=[d_cmb])
            P.dve(lambda e: e.tensor_tensor(out=cm2, in0=lg, in1=bc_last(m2, NE), op=ALU.is_equal), reads=[d_lg, d_top], writes=[d_cm2])
            P.dve(lambda e: e.tensor_tensor(out=cm2, in0=cm2, in1=bc_last(wts[:, 2, :], NE), op=ALU.mult), reads=[d_cm2, d_w], writes=[d_cm2])
            P.dve(lambda e: e.tensor_tensor(out=cmb, in0=cmb, in1=cm2, op=ALU.add), reads=[d_cmb, d_cm2], writes=[d_cmb])
            for s in range(NS):
                bk = 1 + (s // 4) % 2
                P.pe(lambda e, s=s, bk=bk: e.transpose(ps[bk][0:NE, (s % 4) * 128:(s % 4 + 1) * 128], cmb[:, s, :], ident_f[:]),
                     reads=[d_cmb, d_const], writes=[d_ps[bk]])
                if s % 4 == 3:
                    P.act(lambda e, s=s, bk=bk: e.activation(out=combT[0:NE, (s - 3) * 128:(s + 1) * 128], in_=ps[bk][0:NE, :], func=AF.Copy),
                          reads=[d_ps[bk]], writes=[d_cT])
            for ex in range(NE):
                ci = ex % 2
                for tt in range(NTT):
                    bk = 1 + tt % 2
                    P.pe(lambda e, ex=ex, tt=tt, bk=bk: e.matmul(ps[bk][:, :], lhsT=sel[0:NE, ex, :], rhs=combT[0:NE, tt * TT:(tt + 1) * TT], start=True, stop=True),
                         reads=[d_cT, d_const], writes=[d_ps[bk]])
                    P.act(lambda e, ci=ci, tt=tt, bk=bk: e.activation(out=cbc[ci][:, tt * TT:(tt + 1) * TT], in_=ps[bk][:, :], func=AF.Copy),
                          reads=[d_ps[bk]], writes=[d_cbc[ci]])
                ffn_slabs(l, b, moe_w1[idx, ex], moe_w3[idx, ex], moe_w2[idx, ex], DFE, (cbc[ci], d_cbc[ci]), st_)

        for b in range(NB):
            for c in range(DC):
                for tt in range(NTT):
                    P.dma("sp", lambda e, b=b, c=c, tt=tt: e.dma_start(out=x[:, c, tt * TT:(tt + 1) * TT], in_=xT[b, c * 128:(c + 1) * 128, tt * TT:(tt + 1) * TT]),
                          writes=[d_x[c][tt]])
            for l in range(L):
                if not diag_skip_mixers:
                    rmsnorm_to_h(l, b, gsm, 0)
                    conv_pass(l, b)
                    sgu_pass(l, b)
                    recur_pass(l, b, "gla")
                    recur_pass(l, b, "hgrn", 0)
                    recur_pass(l, b, "hgrn", 1)
                rmsnorm_to_h(l, b, gsf, 24)
                if l % 2 == 0:
                    dense_ffn(l, b)
                else:
                    moe_ffn(l, b)
            phase_switch()
            sq = [ua.alloc([TT], BF16) for _ in range(3)]; d_sq = [newdep() for _ in range(3)]
            lnv = ua.alloc([TT]); rstd = ua.alloc([TT]); d_ln = newdep(); d_rs = newdep()
            k = 0
            for tt in range(NTT):
                tsl = slice(tt * TT, (tt + 1) * TT)
                bk = rot_bank()
                for c in range(DC):
                    i = k % 3
                    k += 1
                    P.act(lambda e, c=c, i=i, tsl=tsl: e.activation(out=sq[i], in_=x[:, c, tsl], func=AF.Square), reads=[d_x[c][tt]], writes=[d_sq[i]])
                    P.pe(lambda e, c=c, i=i, bk=bk: e.matmul(ps[bk][:, :], lhsT=ones_bf[:], rhs=sq[i], start=(c == 0), stop=(c == DC - 1)),
                         reads=[d_sq[i], d_const], writes=[d_ps[bk]])
                P.act(lambda e, bk=bk: e.activation(out=lnv, in_=ps[bk][:, :], func=AF.Ln, scale=1.0 / D, bias=EPS), reads=[d_ps[bk]], writes=[d_ln])
                P.act(lambda e: e.activation(out=rstd, in_=lnv, func=AF.Exp, scale=-0.5), reads=[d_ln], writes=[d_rs])
                for c in range(DC):
                    P.dve(lambda e, c=c, tsl=tsl: e.scalar_tensor_tensor(out=x[:, c, tsl], in0=x[:, c, tsl], scalar=gfin_s[:, c:c + 1], in1=rstd,
                                                                         op0=ALU.mult, op1=ALU.mult),
                          reads=[d_x[c][tt], d_rs, d_par], writes=[d_x[c][tt]])
                    o = P.dma("sp", lambda e, b=b, c=c, tsl=tsl: e.dma_start(out=outT[b, c * 128:(c + 1) * 128, tsl], in_=x[:, c, tsl]),
                              reads=[d_x[c][tt]], writes=[Dep()], dep=d_x[c][tt])
                    P.final_waits.append(o)
        P.emit()
    return nc


def _fm(v):
    v = np.asarray(v, np.float32)
    lead = v.shape[:-1]
    n = v.shape[-1] // 128
    v = v.reshape(lead + (n, 128))
    return np.ascontiguousarray(np.moveaxis(v, -1, 0))


_PROG_CACHE = {}


def kernel(x, c, norm_mix_g, norm_ffn_g, final_norm_g, w_ada, b_ada, w_in, w_out, conv_w,
           sgu_norm_g, sgu_w, sgu_b, gla_w_gate, gla_b_gate, gla_norm_g, hgrn_lower_bounds,
           hgrn_norm_g, ffn_w1, ffn_w3, ffn_w2, moe_router, moe_w1, moe_w3, moe_w2, n_cores=8):
    f = lambda a: np.ascontiguousarray(np.asarray(a, dtype=np.float32))
    x = f(x)
    B, T, _ = x.shape
    L = w_in.shape[0]
    n_moe = L // 2
    NB = B // n_cores
    key = (NB, T, L, n_moe)
    if key not in _PROG_CACHE:
        _PROG_CACHE[key] = build_program(NB, T, L, n_moe)
    nc = _PROG_CACHE[key]
    c = f(c)
    shared = {
        "g_mix": _fm(norm_mix_g), "g_ffn": _fm(norm_ffn_g), "g_fin": _fm(final_norm_g),
        "w_ada": f(w_ada), "b_ada": _fm(b_ada), "w_in": f(w_in), "w_out": f(w_out),
        "conv_w": _fm(conv_w), "sgu_g": _fm(sgu_norm_g),
        "sgu_wT": np.ascontiguousarray(f(sgu_w).transpose(3, 0, 1, 2)),
        "sgu_bf": np.ascontiguousarray(np.repeat(f(sgu_b).reshape(L, 2, 2, 1, 128), 64, axis=3).reshape(L, 2, 128, 128).transpose(2, 0, 1, 3)),
        "gla_wg": np.ascontiguousarray(f(gla_w_gate).transpose(1, 0, 2)),
        "gla_bg": np.ascontiguousarray(f(gla_b_gate).T),
        "gla_ng": np.ascontiguousarray(np.tile(f(gla_norm_g), (1, 2)).T),
        "hg_lb": np.ascontiguousarray(f(hgrn_lower_bounds).reshape(L, 2, 128).transpose(2, 1, 0)),
        "hg_ng": np.ascontiguousarray(np.tile(f(hgrn_norm_g), (1, 2)).T),
        "ffn_w1": f(ffn_w1), "ffn_w3": f(ffn_w3), "ffn_w2": f(ffn_w2),
        "moe_r": f(moe_router), "moe_w1": f(moe_w1), "moe_w3": f(moe_w3), "moe_w2": f(moe_w2),
    }
    in_maps = []
    for i in range(n_cores):
        xb = x[i * NB:(i + 1) * NB]
        m = dict(shared)
        m["xT"] = np.ascontiguousarray(xb.transpose(0, 2, 1))
        m["cT"] = np.ascontiguousarray(c[i * NB:(i + 1) * NB].T.reshape(DC, 128, NB).transpose(1, 0, 2))
        in_maps.append(m)
    res = run_bass_kernel_spmd(nc, in_maps, core_ids=list(range(n_cores)))
    out = np.empty((B, T, D), np.float32)
    for i in range(n_cores):
        out[i * NB:(i + 1) * NB] = res.results[i]["outT"].transpose(0, 2, 1)
    return out
```

```python
import contextlib
import types
import numpy as np
import concourse.bass as bass
import concourse.mybir as mybir
from concourse.bass_utils import run_bass_kernel_spmd

F32 = mybir.dt.float32
BF16 = mybir.dt.bfloat16
AF = mybir.ActivationFunctionType
ALU = mybir.AluOpType
AX = mybir.AxisListType

D = 1024
DC = 8
GW = 256
HD = 64
NE = 8
DFF = 2816
DFE = 3584
INW = 3088
EPS = 1e-6
TT = 512
CH = 32
SLAB = 256


class Dep:
    __slots__ = ("name", "last_writer", "readers", "dma_sem", "dma_count")

    def __init__(self, name="", after=None):
        self.name = name
        self.last_writer = after
        self.readers = []
        self.dma_sem = None
        self.dma_count = 0


class Op:
    __slots__ = ("eng", "fn", "deps", "signaled", "is_dma", "dep_obj", "sem_val")

    def __init__(self, eng, fn, is_dma=False):
        self.eng = eng
        self.fn = fn
        self.deps = []
        self.signaled = False
        self.is_dma = is_dma
        self.dep_obj = None
        self.sem_val = None


ENGS = ("pe", "act", "dve", "pool", "sp")


def _freeze(fn):
    if fn.__closure__ is None:
        return fn
    cells = []
    for c in fn.__closure__:
        try:
            cells.append(types.CellType(c.cell_contents))
        except ValueError:
            cells.append(c)
    g = types.FunctionType(fn.__code__, fn.__globals__, fn.__name__, fn.__defaults__, tuple(cells))
    g.__kwdefaults__ = fn.__kwdefaults__
    return g


class Prog:
    def __init__(self, nc):
        self.nc = nc
        self.ops = {e: [] for e in ENGS}
        self.n_dma_sems = 0
        self.final_waits = []

    def _collect(self, op, reads, writes, same_engine_sync=True):
        deps = []
        for d in reads:
            w = d.last_writer
            if w is not None:
                deps.append(w)
        for d in writes:
            w = d.last_writer
            if w is not None:
                if not (op.is_dma and w.is_dma):
                    deps.append(w)
            deps.extend(d.readers)
        out = []
        seen = set()
        for w in deps:
            if w is op or id(w) in seen:
                continue
            if (not w.is_dma) and w.eng == op.eng and not same_engine_sync:
                continue
            seen.add(id(w))
            out.append(w)
        op.deps = out
        for w in out:
            w.signaled = True
        for d in writes:
            d.last_writer = op
            d.readers = []
        for d in reads:
            if not op.is_dma and d.readers:
                d.readers = [r for r in d.readers if r.is_dma or r.eng != op.eng]
            d.readers.append(op)

    def op(self, eng, fn, reads=(), writes=()):
        o = Op(eng, _freeze(fn))
        self._collect(o, reads, writes, same_engine_sync=(eng != "pe"))
        self.ops[eng].append(o)
        return o

    def dma(self, queue, fn, reads=(), writes=(), dep=None):
        o = Op(queue, _freeze(fn), is_dma=True)
        d = dep if dep is not None else writes[0]
        if d.dma_sem is None:
            d.dma_sem = self.n_dma_sems
            self.n_dma_sems += 1
        d.dma_count += 1
        o.dep_obj = d
        o.sem_val = 16 * d.dma_count
        o.signaled = True
        self._collect(o, reads, writes)
        self.ops[queue].append(o)
        return o

    def pe(self, fn, reads=(), writes=()):
        return self.op("pe", fn, reads, writes)

    def act(self, fn, reads=(), writes=()):
        return self.op("act", fn, reads, writes)

    def dve(self, fn, reads=(), writes=()):
        return self.op("dve", fn, reads, writes)

    def pool(self, fn, reads=(), writes=()):
        return self.op("pool", fn, reads, writes)

    def barrier(self, deps):
        return self.op("dve", lambda e: e.memset(self.scratch, 0.0), reads=(), writes=list(deps) + [self.scratch_dep])

    def emit(self):
        nc = self.nc
        for e in ENGS:
            cnt = 0
            for o in self.ops[e]:
                if o.is_dma:
                    continue
                if o.signaled:
                    cnt += 1
                    o.sem_val = cnt
        with contextlib.ExitStack() as st:
            eng_sems = {e: st.enter_context(nc.semaphore("s_" + e)) for e in ENGS}
            dma_sems = [st.enter_context(nc.semaphore("d%d" % i)) for i in range(self.n_dma_sems)]
            block = st.enter_context(nc.Block())

            def tok(o):
                if o.is_dma:
                    return ("d", o.dep_obj.dma_sem), dma_sems[o.dep_obj.dma_sem], o.sem_val
                return ("e", o.eng), eng_sems[o.eng], o.sem_val

            def run(ename, engine):
                known = {}
                for o in self.ops[ename]:
                    need = {}
                    for w in o.deps:
                        k, s, v = tok(w)
                        if known.get(k, 0) >= v:
                            continue
                        if k not in need or need[k][1] < v:
                            need[k] = (s, v)
                    for k, (s, v) in need.items():
                        engine.wait_ge(s, v)
                        known[k] = v
                    ins = o.fn(engine)
                    if o.is_dma:
                        ins.then_inc(dma_sems[o.dep_obj.dma_sem], 16)
                    elif o.signaled:
                        ins.then_inc(eng_sems[ename], 1)
                if ename == "sp":
                    for w in self.final_waits:
                        k, s, v = tok(w)
                        engine.wait_ge(s, v)

            @block.tensor
            def _(t):
                run("pe", t)

            @block.scalar
            def _(t):
                run("act", t)

            @block.vector
            def _(t):
                run("dve", t)

            @block.gpsimd
            def _(t):
                run("pool", t)

            @block.sync
            def _(t):
                run("sp", t)


class Arena:
    def __init__(self, tensor, words):
        self.t = tensor
        self.words = words
        self.off = 0
        self.marks = []

    def alloc(self, shape, dtype=F32):
        n = int(np.prod(shape))
        w = n if dtype == F32 else (n + 1) // 2
        w = (w + 7) // 8 * 8
        assert self.off + w <= self.words, ("arena overflow", self.off, w, self.words)
        v = self.t[:, self.off:self.off + w]
        self.off += w
        if dtype != F32:
            v = v.bitcast(dtype)
        v = v[:, 0:n]
        if len(shape) == 2:
            v = v.rearrange("p (a b) -> p a b", a=shape[0])
        elif len(shape) == 3:
            v = v.rearrange("p (a b c) -> p a b c", a=shape[0], b=shape[1])
        elif len(shape) == 4:
            v = v.rearrange("p (a b c d) -> p a b c d", a=shape[0], b=shape[1], c=shape[2])
        return v

    def mark(self):
        return self.off

    def reset(self, m):
        self.off = m


def bc_last(ap, n):
    shp = list(ap.shape)
    return ap.unsqueeze(len(shp)).to_broadcast(shp + [n])


def bc_mid(ap, n):
    shp = list(ap.shape)
    return ap.unsqueeze(1).to_broadcast([shp[0], n] + shp[1:])


def build_program(NB, T, L, n_moe, diag_skip_mixers=False):
    NTT = T // TT
    n_dense = (L + 1) // 2
    nc = bass.Bass("TRN2", target_bir_lowering=False)

    def din(name, shape):
        return nc.dram_tensor(name, list(shape), F32, kind="ExternalInput").ap()

    xT = din("xT", [NB, D, T])
    cT = din("cT", [128, DC, NB])
    g_mix = din("g_mix", [128, L, DC])
    g_ffn = din("g_ffn", [128, L, DC])
    g_fin = din("g_fin", [128, DC])
    w_ada = din("w_ada", [L, D, 6 * D])
    b_ada = din("b_ada", [128, L, 48])
    w_in = din("w_in", [L, D, INW])
    w_out = din("w_out", [L, D, D])
    conv_w = din("conv_w", [128, L, 3, 2])
    sgu_g = din("sgu_g", [128, L, 2])
    sgu_wT = din("sgu_wT", [128, L, 4, 128])
    sgu_bf = din("sgu_bf", [128, L, 2, 128])
    gla_wg = din("gla_wg", [16, L, 128])
    gla_bg = din("gla_bg", [128, L])
    gla_ng = din("gla_ng", [128, L])
    hg_lb = din("hg_lb", [128, 2, L])
    hg_ng = din("hg_ng", [128, L])
    ffn_w1 = din("ffn_w1", [n_dense, D, DFF])
    ffn_w3 = din("ffn_w3", [n_dense, D, DFF])
    ffn_w2 = din("ffn_w2", [n_dense, DFF, D])
    moe_r = din("moe_r", [max(n_moe, 1), D, NE])
    moe_w1 = din("moe_w1", [max(n_moe, 1), NE, D, DFE])
    moe_w3 = din("moe_w3", [max(n_moe, 1), NE, D, DFE])
    moe_w2 = din("moe_w2", [max(n_moe, 1), NE, DFE, D])
    outT = nc.dram_tensor("outT", [NB, D, T], F32, kind="ExternalOutput").ap()

    P = Prog(nc)
    with contextlib.ExitStack() as st:
        def sb(name, shape, dt=F32):
            return st.enter_context(nc.sbuf_tensor(name, list(shape), dt))

        x = sb("x", [128, DC, T])
        h = sb("h", [128, DC, T], BF16)
        RINGW = 4352
        ring = [sb("ring%d" % i, [128, RINGW]) for i in range(2)]
        UW = 13696
        ureg = sb("ureg", [128, UW])
        ones_bf = sb("ones_bf", [128, 128], BF16)
        blk_bf = sb("blk_bf", [128, 128], BF16)
        ident_bf = sb("ident_bf", [128, 128], BF16)
        ident_f = sb("ident_f", [128, 128])
        ones_f = sb("ones_f", [128, 128])
        maskBD = sb("maskBD", [128, 128])
        m32 = sb("m32", [128, TT])
        hmask4 = sb("hmask4", [128, 4])
        hmask2 = sb("hmask2", [128, 2])
        cmask = sb("cmask", [128, 4])
        sel = sb("sel", [8, NE, 128])
        scratch = sb("scratch", [128, 8])
        P.scratch = scratch[:, 0:1]
        P.scratch_dep = Dep("scratch")
        cond = sb("cond", [128, DC, NB])
        gmix_s = sb("gmix_s", [128, L, DC])
        gffn_s = sb("gffn_s", [128, L, DC])
        gfin_s = sb("gfin_s", [128, DC])
        bada_s = sb("bada_s", [128, L, 48])
        modv = sb("modv", [128, L, 48, NB])
        gsm = sb("gsm", [128, L, DC, NB])
        gsf = sb("gsf", [128, L, DC, NB])
        convw_s = sb("convw_s", [128, L, 3, 2])
        sgug_s = sb("sgug_s", [128, L, 2])
        sguw_s = sb("sguw_s", [128, L, 4, 128], BF16)
        sgub_s = sb("sgub_s", [128, L, 2, 128])
        wg_s = sb("wg_s", [16, L, 128])
        nbg_s = sb("nbg_s", [128, L])
        glang_s = sb("glang_s", [128, L])
        hgng_s = sb("hgng_s", [128, L])
        lbe = sb("lbe", [128, 2, L])
        lb_s = sb("lb_s", [128, 2, L])
        oml_s = sb("oml_s", [128, 2, L])
        lbt = sb("lbt", [128, 2, 2])
        wr_s = sb("wr_s", [128, max(n_moe, 1), DC, NE], BF16)

        ps = [st.enter_context(nc.psum_tensor("ps%d" % i, [128, 512], F32)) for i in range(7)]
        psT = st.enter_context(nc.psum_tensor("psT", [128, 1024], BF16))
        d_ps = [Dep("ps%d" % i) for i in range(7)]
        d_psT = Dep("psT")

        d_x = [[Dep("x%d_%d" % (c, t)) for t in range(NTT)] for c in range(DC)]
        d_h = [[Dep("h%d_%d" % (c, t)) for t in range(NTT)] for c in range(DC)]
        d_ring = [[Dep("ring%d_%d" % (i, j)) for j in range(3)] for i in range(2)]
        d_const = Dep("const")
        d_par = Dep("par")
        d_setup = Dep("setup")

        C = [d_const]
        P.pool(lambda e: e.memset(ones_bf[:], 1.0), writes=C)
        P.pool(lambda e: e.memset(ones_f[:], 1.0), writes=C)
        P.pool(lambda e: e.memset(blk_bf[:], 0.0), writes=C)
        P.pool(lambda e: e.memset(blk_bf[0:64, 0:64], 1.0), reads=C, writes=C)
        P.pool(lambda e: e.memset(blk_bf[64:128, 64:128], 1.0), reads=C, writes=C)
        P.pool(lambda e: e.affine_select(out=ident_bf[:], in_=ones_bf[:], pattern=[[-1, 128]], compare_op=ALU.is_equal,
                                          fill=0.0, base=0, channel_multiplier=1), reads=C, writes=C)
        P.pool(lambda e: e.affine_select(out=ident_f[:], in_=ones_f[:], pattern=[[-1, 128]], compare_op=ALU.is_equal,
                                          fill=0.0, base=0, channel_multiplier=1), reads=C, writes=C)
        P.pool(lambda e: e.affine_select(out=maskBD[:], in_=ones_f[:], pattern=[[1, 128]], compare_op=ALU.is_ge,
                                          fill=0.0, base=0, channel_multiplier=-1), reads=C, writes=C)
        for bl in range(1, 4):
            P.pool(lambda e, bl=bl: e.affine_select(out=maskBD[:, 32 * bl:32 * bl + 32], in_=maskBD[:, 32 * bl:32 * bl + 32],
                                                    pattern=[[0, 32]], compare_op=ALU.is_ge, fill=0.0, base=-32 * bl,
                                                    channel_multiplier=1), reads=C, writes=C)
        P.pool(lambda e: e.memset(m32[:], 1.0), reads=C, writes=C)
        P.pool(lambda e: e.memset(m32[:].rearrange("p (n k) -> p n k", k=CH)[:, :, 0:1], 0.0), reads=C, writes=C)
        for (msk, nh, dk) in ((hmask4, 4, 32), (hmask2, 2, 64), (cmask, 4, 32)):
            P.pool(lambda e, msk=msk, nh=nh, dk=dk: e.affine_select(out=msk[:], in_=ones_f[:, 0:nh], pattern=[[-dk, nh]], compare_op=ALU.is_ge,
                                                                    fill=0.0, base=0, channel_multiplier=1), reads=C, writes=C)
            P.pool(lambda e, msk=msk, nh=nh, dk=dk: e.affine_select(out=msk[:], in_=msk[:], pattern=[[dk, nh]], compare_op=ALU.is_ge,
                                                                    fill=0.0, base=dk - 1, channel_multiplier=-1), reads=C, writes=C)
        P.pool(lambda e: e.affine_select(out=sel[:], in_=bc_mid(ones_f[0:8, :], NE), pattern=[[-1, NE], [0, 128]], compare_op=ALU.is_equal,
                                          fill=0.0, base=0, channel_multiplier=1), reads=C, writes=C)

        Wp = [d_par]
        for dst, src in ((cond, cT), (gmix_s, g_mix), (gffn_s, g_ffn), (gfin_s, g_fin), (bada_s, b_ada), (convw_s, conv_w),
                         (sgug_s, sgu_g), (sgub_s, sgu_bf), (wg_s, gla_wg), (nbg_s, gla_bg), (glang_s, gla_ng),
                         (hgng_s, hg_ng), (lbe, hg_lb)):
            P.dma("sp", lambda e, dst=dst, src=src: e.dma_start(out=dst[:], in_=src), writes=Wp)
        P.dma("pool", lambda e: e.dma_start(out=sguw_s[:], in_=sgu_wT), writes=Wp)
        if n_moe:
            for m in range(n_moe):
                P.dma("pool", lambda e, m=m: e.dma_start(out=wr_s[:, m], in_=moe_r[m].rearrange("(c p) e -> p c e", p=128)), writes=Wp)
        S = [d_setup]
        R_ = [d_par, d_const]
        for l in range(L):
            for hh in range(4):
                P.pool(lambda e, l=l, hh=hh: e.affine_select(out=sguw_s[:, l, hh, :], in_=sguw_s[:, l, hh, :], pattern=[[1, 128]],
                                                             compare_op=ALU.is_ge, fill=0.0, base=0, channel_multiplier=-1),
                       reads=R_, writes=S)
        P.act(lambda e: e.activation(out=cond[:], in_=cond[:], func=AF.Silu), reads=R_, writes=S)
        P.dve(lambda e: e.tensor_scalar(out=nbg_s[:], in0=nbg_s[:], scalar1=-1.0, scalar2=None, op0=ALU.mult), reads=R_, writes=S)
        P.act(lambda e: e.activation(out=lbe[:], in_=lbe[:], func=AF.Exp), reads=R_ + S, writes=S)
        P.dve(lambda e: e.tensor_reduce(out=lbt[:, :, 0:1], in_=lbe[:], axis=AX.X, op=ALU.add), reads=S, writes=S)
        P.dve(lambda e: e.reciprocal(out=lbt[:, :, 1:2], in_=lbt[:, :, 0:1]), reads=S, writes=S)
        P.dve(lambda e: e.tensor_tensor(out=lbe[:], in0=lbe[:], in1=lbt[:, :, 1:2].to_broadcast([128, 2, L]), op=ALU.mult), reads=S, writes=S)
        P.dve(lambda e: e.memset(lb_s[:, :, 0:1], 0.0), reads=S, writes=S)
        for l in range(1, L):
            P.dve(lambda e, l=l: e.tensor_tensor(out=lb_s[:, :, l:l + 1], in0=lb_s[:, :, l - 1:l], in1=lbe[:, :, l:l + 1], op=ALU.add), reads=S, writes=S)
        P.dve(lambda e: e.tensor_scalar(out=oml_s[:], in0=lb_s[:], scalar1=-1.0, scalar2=1.0, op0=ALU.mult, op1=ALU.add), reads=S, writes=S)

        ADW = 512
        rv = [ring[i][:, 0:DC * ADW].rearrange("p (c n) -> p c n", c=DC) for i in range(2)]
        step = 0
        d_ada = [Dep("ada0"), Dep("ada1")]
        for l in range(L):
            for pc in range(6 * D // ADW):
                s_ = step % 2
                step += 1
                P.dma("sp", lambda e, l=l, pc=pc, s_=s_: e.dma_start(out=rv[s_], in_=w_ada[l][:, pc * ADW:(pc + 1) * ADW].rearrange("(c p) n -> p c n", p=128)),
                      writes=[d_ada[s_]])
                for jj in range(ADW // 128):
                    j = pc * (ADW // 128) + jj
                    for c in range(DC):
                        P.pe(lambda e, s_=s_, jj=jj, j=j, c=c: e.matmul(ps[0][:, j * NB:(j + 1) * NB], lhsT=rv[s_][:, c, jj * 128:(jj + 1) * 128],
                                                                      rhs=cond[:, c, :], start=(c == 0), stop=(c == DC - 1)),
                             reads=[d_ada[s_], d_setup], writes=[d_ps[0]])
            P.dve(lambda e, l=l: e.tensor_tensor(out=modv[:, l], in0=ps[0][:, 0:48 * NB].rearrange("p (j b) -> p j b", b=NB),
                                                 in1=bc_last(bada_s[:, l, :], NB), op=ALU.add), reads=[d_ps[0], d_par], writes=S)
            for (gdst, gsrc, j0) in ((gsm, gmix_s, 8), (gsf, gffn_s, 32)):
                P.dve(lambda e, l=l, gdst=gdst, j0=j0: e.tensor_scalar(out=gdst[:, l], in0=modv[:, l, j0:j0 + 8, :], scalar1=1.0, scalar2=None, op0=ALU.add),
                      reads=S, writes=S)
                P.dve(lambda e, l=l, gdst=gdst, gsrc=gsrc: e.tensor_tensor(out=gdst[:, l], in0=gdst[:, l], in1=bc_last(gsrc[:, l, :], NB), op=ALU.mult),
                      reads=S + [d_par], writes=S)

        ada_done = P.barrier(d_ada)
        for i in range(2):
            for j in range(3):
                d_ring[i][j].last_writer = ada_done
        CONSTS = [d_const, d_par, d_setup]

        ua = Arena(ureg, UW)
        phase = {"deps": [], "after": None}

        def newdep(name=""):
            d_ = Dep(name, after=phase["after"])
            phase["deps"].append(d_)
            return d_

        def phase_switch():
            if phase["deps"]:
                phase["after"] = P.barrier(phase["deps"])
            phase["deps"] = []
            ua.reset(0)

        rot = {"i": 0}

        def rot_bank(n=3):
            i = rot["i"] % n
            rot["i"] += 1
            return i

        def rmsnorm_to_h(l, b, gs_t, sh_j0):
            phase_switch()
            sq = [ua.alloc([TT], BF16) for _ in range(3)]
            d_sq = [newdep() for _ in range(3)]
            lnv = ua.alloc([TT]); rstd = ua.alloc([TT])
            d_ln = newdep(); d_rs = newdep()
            tmp = [ua.alloc([TT]) for _ in range(3)]
            d_tmp = [newdep() for _ in range(3)]
            k = 0
            for tt in range(NTT):
                tsl = slice(tt * TT, (tt + 1) * TT)
                bk = rot_bank()
                for c in range(DC):
                    i = k % 3
                    k += 1
                    P.act(lambda e, c=c, i=i, tsl=tsl: e.activation(out=sq[i], in_=x[:, c, tsl], func=AF.Square),
                          reads=[d_x[c][tt]], writes=[d_sq[i]])
                    P.pe(lambda e, c=c, i=i, bk=bk: e.matmul(ps[bk][:, :], lhsT=ones_bf[:], rhs=sq[i], start=(c == 0), stop=(c == DC - 1)),
                         reads=[d_sq[i], d_const], writes=[d_ps[bk]])
                P.act(lambda e, bk=bk: e.activation(out=lnv, in_=ps[bk][:, :], func=AF.Ln, scale=1.0 / D, bias=EPS),
                      reads=[d_ps[bk]], writes=[d_ln])
                P.act(lambda e: e.activation(out=rstd, in_=lnv, func=AF.Exp, scale=-0.5), reads=[d_ln], writes=[d_rs])
                for c in range(DC):
                    i = k % 3
                    k += 1
                    P.dve(lambda e, c=c, i=i, tsl=tsl: e.tensor_tensor(out=tmp[i], in0=x[:, c, tsl], in1=rstd, op=ALU.mult),
                          reads=[d_x[c][tt], d_rs], writes=[d_tmp[i]])
                    P.act(lambda e, c=c, i=i, tsl=tsl: e.activation(out=h[:, c, tsl], in_=tmp[i], func=AF.Identity,
                                                                    scale=gs_t[:, l, c, b:b + 1], bias=modv[:, l, sh_j0 + c, b:b + 1]),
                          reads=[d_tmp[i], d_setup], writes=[d_h[c][tt]])

        ring_state = {"n": 0}

        def ring_next():
            s_ = ring_state["n"] % 2
            ring_state["n"] += 1
            return s_

        WO_OFF = 3328

        def load_mixer_weights(l, ranges, r0, nrows):
            s_ = ring_next()
            ncols = sum(n for _, n in ranges)
            assert DC * ncols // 2 <= WO_OFF
            wv = ring[s_][:, 0:DC * ncols // 2].bitcast(BF16).rearrange("p (c n) -> p c n", c=DC)
            nkc = nrows // 128
            wo = ring[s_][:, WO_OFF:WO_OFF + nkc * D // 2].bitcast(BF16).rearrange("p (c n) -> p c n", c=nkc)
            nwords = DC * ncols // 2
            span = [d_ring[s_][j] for j in range(3) if nwords > (0, 1024, 2048)[j]]
            off = 0
            for (c0, n) in ranges:
                P.dma("pool", lambda e, c0=c0, n=n, off=off: e.dma_start(out=wv[:, :, off:off + n],
                                                                        in_=w_in[l][:, c0:c0 + n].rearrange("(c p) n -> p c n", p=128)),
                      writes=span, dep=d_ring[s_][0])
                off += n
            P.dma("pool", lambda e: e.dma_start(out=wo, in_=w_out[l][r0:r0 + nrows, :].rearrange("(c p) n -> p c n", p=128)),
                  writes=[d_ring[s_][2]], dep=d_ring[s_][2])
            dwo_ = [d_ring[s_][2]]
            return wv, wo, span, dwo_

        def proj_fm(wv, dw, col, tt, bk, M=128):
            tsl = slice(tt * TT, (tt + 1) * TT)
            for c in range(DC):
                P.pe(lambda e, c=c: e.matmul(ps[bk][0:M, :], lhsT=wv[:, c, col:col + M], rhs=h[:, c, tsl], start=(c == 0), stop=(c == DC - 1)),
                     reads=dw + [d_h[c][tt]], writes=[d_ps[bk]])

        def proj_tok(wv, dw, col, ncols, tt, sub, bk, off):
            t0 = tt * TT + sub * 128
            for c in range(DC):
                P.pe(lambda e, c=c: e.matmul(ps[bk][:, off:off + ncols], lhsT=h[:, c, t0:t0 + 128], rhs=wv[:, c, col:col + ncols],
                                             start=(c == 0), stop=(c == DC - 1)),
                     reads=dw + [d_h[c][tt]], writes=[d_ps[bk]])

        def out_proj(l, b, wo, dwo, y, d_y, tt, nk=2):
            tsl = slice(tt * TT, (tt + 1) * TT)
            for co in range(DC):
                bk = rot_bank()
                for kc in range(nk):
                    P.pe(lambda e, co=co, kc=kc, bk=bk: e.matmul(ps[bk][:, :], lhsT=wo[:, kc, co * 128:(co + 1) * 128], rhs=y[:, kc, :],
                                                                 start=(kc == 0), stop=(kc == nk - 1)),
                         reads=dwo + [d_y], writes=[d_ps[bk]])
                P.dve(lambda e, co=co, bk=bk: e.scalar_tensor_tensor(out=x[:, co, tsl], in0=ps[bk][:, :], scalar=modv[:, l, 16 + co, b:b + 1],
                                                                     in1=x[:, co, tsl], op0=ALU.mult, op1=ALU.add),
                      reads=[d_ps[bk], d_x[co][tt], d_setup], writes=[d_x[co][tt]])

        def conv_pass(l, b):
            phase_switch()
            wv, wo, dw, dwo = load_mixer_weights(l, [(0, 768)], 0, 256)
            z = ua.alloc([2, TT + 2]); d_z = [newdep(), newdep()]
            cxs = [ua.alloc([TT]) for _ in range(2)]; d_cxs = [newdep(), newdep()]
            acc = [ua.alloc([TT]) for _ in range(2)]; d_acc = [newdep(), newdep()]
            y = [ua.alloc([2, TT], BF16) for _ in range(2)]; d_y = [newdep(), newdep()]
            for ch in range(2):
                P.dve(lambda e, ch=ch: e.memset(z[:, ch, 0:2], 0.0), writes=[d_z[ch]])
            for tt in range(NTT):
                yi = tt % 2
                for ch in range(2):
                    b_cc = rot_bank(); proj_fm(wv, dw, 256 + ch * 128, tt, b_cc)
                    b_cx = rot_bank(); proj_fm(wv, dw, 512 + ch * 128, tt, b_cx)
                    P.act(lambda e, ch=ch, b_cx=b_cx: e.activation(out=cxs[ch], in_=ps[b_cx][:, :], func=AF.Copy),
                          reads=[d_ps[b_cx]], writes=[d_cxs[ch]])
                    P.dve(lambda e, ch=ch, b_cc=b_cc: e.tensor_tensor(out=z[:, ch, 2:TT + 2], in0=ps[b_cc][:, :], in1=cxs[ch], op=ALU.mult),
                          reads=[d_ps[b_cc], d_cxs[ch]], writes=[d_z[ch]])
                    P.dve(lambda e, ch=ch: e.tensor_scalar(out=acc[ch], in0=z[:, ch, 0:TT], scalar1=convw_s[:, l, 0, ch:ch + 1], scalar2=None, op0=ALU.mult),
                          reads=[d_z[ch], d_par], writes=[d_acc[ch]])
                    for kk in (1, 2):
                        P.dve(lambda e, ch=ch, kk=kk: e.scalar_tensor_tensor(out=acc[ch], in0=z[:, ch, kk:TT + kk], scalar=convw_s[:, l, kk, ch:ch + 1],
                                                                             in1=acc[ch], op0=ALU.mult, op1=ALU.add),
                              reads=[d_z[ch], d_par, d_acc[ch]], writes=[d_acc[ch]])
                    P.dve(lambda e, ch=ch: e.tensor_copy(out=z[:, ch, 0:2], in_=z[:, ch, TT:TT + 2]), reads=[d_z[ch]], writes=[d_z[ch]])
                    b_cb = rot_bank(); proj_fm(wv, dw, ch * 128, tt, b_cb)
                    P.dve(lambda e, ch=ch, b_cb=b_cb, yi=yi: e.tensor_tensor(out=y[yi][:, ch, :], in0=ps[b_cb][:, :], in1=acc[ch], op=ALU.mult),
                          reads=[d_ps[b_cb], d_acc[ch]], writes=[d_y[yi]])
                out_proj(l, b, wo, dwo, y[yi], d_y[yi], tt)

        def sgu_pass(l, b):
            phase_switch()
            wv, wo, dw, dwo = load_mixer_weights(l, [(768, 512)], 256, 256)
            vn = [ua.alloc([256], BF16) for _ in range(2)]; d_vn = [newdep(), newdep()]
            stats = ua.alloc([2, 6]); mv = ua.alloc([2, 2]); rs = ua.alloc([2, 2]); d_st = [newdep(), newdep()]
            t1 = [ua.alloc([TT]) for _ in range(2)]; d_t1 = [newdep(), newdep()]
            y = [ua.alloc([2, TT], BF16) for _ in range(2)]; d_y = [newdep(), newdep()]
            k = 0
            for tt in range(NTT):
                yi = tt % 2
                bm = [3, 4]
                for sub in range(4):
                    i = k % 2
                    k += 1
                    bv = rot_bank()
                    proj_tok(wv, dw, 256, 256, tt, sub, bv, 0)
                    P.dve(lambda e, i=i, bv=bv: e.bn_stats(out=stats[:, i, :], in_=ps[bv][:, 0:256]), reads=[d_ps[bv]], writes=[d_st[i]])
                    P.dve(lambda e, i=i: e.bn_aggr(out=mv[:, i, :], in_=stats[:, i, :]), reads=[d_st[i]], writes=[d_st[i]])
                    P.act(lambda e, i=i: e.activation(out=rs[:, i, 0:1], in_=mv[:, i, 1:2], func=AF.Ln, bias=EPS, scale=1.0), reads=[d_st[i]], writes=[d_st[i]])
                    P.act(lambda e, i=i: e.activation(out=rs[:, i, 1:2], in_=rs[:, i, 0:1], func=AF.Exp, scale=-0.5), reads=[d_st[i]], writes=[d_st[i]])
                    P.dve(lambda e, i=i, bv=bv: e.tensor_scalar(out=vn[i], in0=ps[bv][:, 0:256], scalar1=mv[:, i, 0:1], scalar2=rs[:, i, 1:2],
                                                                op0=ALU.subtract, op1=ALU.mult),
                          reads=[d_ps[bv], d_st[i]], writes=[d_vn[i]])
                    for hh in range(4):
                        cc, hl = hh // 2, hh % 2
                        P.pe(lambda e, i=i, hh=hh, cc=cc, hl=hl, sub=sub: e.matmul(ps[bm[cc]][64 * hl:64 * hl + 64, sub * 128:(sub + 1) * 128],
                                                                                   lhsT=vn[i][:, hh * 64:(hh + 1) * 64], rhs=sguw_s[:, l, hh, :],
                                                                                   start=True, stop=True),
                             reads=[d_vn[i], d_setup], writes=[d_ps[bm[cc]]])
                for cc in range(2):
                    bu = rot_bank(); proj_fm(wv, dw, cc * 128, tt, bu)
                    P.dve(lambda e, cc=cc: e.scalar_tensor_tensor(out=t1[cc].rearrange("p (a t) -> p a t", a=4),
                                                                  in0=ps[bm[cc]][:, :].rearrange("p (a t) -> p a t", a=4),
                                                                  scalar=sgug_s[:, l, cc:cc + 1], in1=bc_mid(sgub_s[:, l, cc, :], 4),
                                                                  op0=ALU.mult, op1=ALU.add),
                          reads=[d_ps[bm[cc]], d_par], writes=[d_t1[cc]])
                    P.dve(lambda e, cc=cc, bu=bu, yi=yi: e.tensor_tensor(out=y[yi][:, cc, :], in0=ps[bu][:, :], in1=t1[cc], op=ALU.mult),
                          reads=[d_ps[bu], d_t1[cc]], writes=[d_y[yi]])
                out_proj(l, b, wo, dwo, y[yi], d_y[yi], tt)

        def recur_pass(l, b, kind, g=0):
            phase_switch()
            if kind == "gla":
                wv, wo, dw, dwo = load_mixer_weights(l, [(1280, 784)], 512, 256)
                nh, dk, sc, hmask, ng = 4, 32, -1.0 / 16.0, hmask4, glang_s
                qc, kc, vc, gate_col, noc = 0, 128, 256, 528, 2
            else:
                base = 2064
                wv, wo, dw, dwo = load_mixer_weights(l, [(base + g * 128, 128), (base + 256 + g * 128, 128), (base + 512 + g * 128, 128),
                                                        (base + 768 + g * 128, 128)], 768 + g * 128, 128)
                nh, dk, sc, hmask, ng = 2, 64, 1.0, hmask2, hgng_s
                qc, fcol, vc, gate_col, noc = 0, 128, 256, 384, 1
            BO = [5, 6]
            BA, BU = 3, 4
            ext = ua.alloc([64, 9]); d_ext = newdep()
            dece = ua.alloc([2, 9]); d_dece = newdep()
            y = [ua.alloc([noc, TT], BF16) for _ in range(2)]; d_y = [newdep(), newdep()]
            sgate = ua.alloc([noc, TT]); d_sg = [newdep() for _ in range(noc)]
            qt = ua.alloc([TT], BF16); d_qt = newdep()
            kt = ua.alloc([TT], BF16); d_kt = newdep()
            ktm = ua.alloc([nh, TT], BF16); d_ktm = newdep()
            ktok = ua.alloc([4, 128], BF16); d_ktok = newdep()
            V = ua.alloc([4, nh * 64], BF16); d_V = newdep()
            Vbd = ua.alloc([2, nh, 4, 64], BF16); d_Vbd = newdep()
            A = [ua.alloc([nh, 128], BF16) for _ in range(2)]; d_A = [newdep(), newdep()]
            T1 = ua.alloc([TT]); d_T1 = newdep()
            T2 = ua.alloc([TT]); d_T2 = newdep()
            T3 = ua.alloc([TT]); d_T3 = newdep()
            T4 = ua.alloc([TT]); d_T4 = newdep()
            T5 = ua.alloc([TT]); d_T5 = newdep()
            T6 = ua.alloc([TT]); d_T6 = newdep()
            sm = ua.alloc([4, 16]); d_sm = newdep()
            emh = ua.alloc([4, 16]); d_emh = newdep()
            cUe = ua.alloc([64, 9]); d_cUe = newdep()
            decf = ua.alloc([64, 9]); d_decf = newdep()
            Sbd = ua.alloc([8, nh * 64], BF16); d_Sbd = newdep()
            osq = ua.alloc([TT], BF16); d_osq = newdep()
            P.dve(lambda e: e.memset(dece, 0.0), writes=[d_dece])
            P.dve(lambda e: e.memset(ext, 0.0), writes=[d_ext])
            ai = 0
            for tt in range(NTT):
                tsl = slice(tt * TT, (tt + 1) * TT)
                yi = tt % 2
                for oc in range(noc):
                    bg_ = rot_bank(); proj_fm(wv, dw, gate_col + oc * 128, tt, bg_)
                    P.act(lambda e, bg_=bg_: e.activation(out=T1, in_=ps[bg_][:, :], func=AF.Exp, scale=-1.0), reads=[d_ps[bg_]], writes=[d_T1])
                    P.act(lambda e: e.activation(out=T2, in_=T1, func=AF.Ln, bias=1.0, scale=1.0), reads=[d_T1], writes=[d_T2])
                    P.act(lambda e: e.activation(out=T3, in_=T2, func=AF.Exp, scale=-1.0), reads=[d_T2], writes=[d_T3])
                    P.dve(lambda e, oc=oc, bg_=bg_: e.tensor_tensor(out=sgate[:, oc, :], in0=ps[bg_][:, :], in1=T3, op=ALU.mult),
                          reads=[d_ps[bg_], d_T3], writes=[d_sg[oc]])
                if kind == "gla":
                    bgl = rot_bank(); proj_fm(wv, dw, 512, tt, bgl, M=16)
                    P.act(lambda e, bgl=bgl: e.activation(out=T5[0:16, :], in_=ps[bgl][0:16, :], func=AF.Copy), reads=[d_ps[bgl]], writes=[d_T5])
                    bz = rot_bank()
                    P.pe(lambda e, bz=bz: e.matmul(ps[bz][:, :], lhsT=wg_s[0:16, l, :], rhs=T5[0:16, :], start=True, stop=True),
                         reads=[d_T5, d_par], writes=[d_ps[bz]])
                    P.act(lambda e, bz=bz: e.activation(out=T1, in_=ps[bz][:, :], func=AF.Exp, scale=-1.0, bias=nbg_s[:, l:l + 1]),
                          reads=[d_ps[bz], d_setup], writes=[d_T1])
                    P.act(lambda e: e.activation(out=T2, in_=T1, func=AF.Ln, bias=1.0, scale=1.0), reads=[d_T1], writes=[d_T2])
                    src_l, d_src = T2, d_T2
                else:
                    bf_ = rot_bank(); proj_fm(wv, dw, fcol, tt, bf_)
                    P.act(lambda e, bf_=bf_: e.activation(out=T1, in_=ps[bf_][:, :], func=AF.Exp, scale=-1.0), reads=[d_ps[bf_]], writes=[d_T1])
                    P.act(lambda e: e.activation(out=T2, in_=T1, func=AF.Ln, bias=1.0, scale=1.0), reads=[d_T1], writes=[d_T2])
                    P.act(lambda e: e.activation(out=T3, in_=T1, func=AF.Ln, bias=1.0, scale=lb_s[:, g, l:l + 1]), reads=[d_T1, d_setup], writes=[d_T3])
                    P.dve(lambda e, bf_=bf_: e.tensor_tensor(out=T6, in0=ps[bf_][:, :], in1=T2, op=ALU.add), reads=[d_ps[bf_], d_T2], writes=[d_T6])
                    P.act(lambda e: e.activation(out=T6, in_=T6, func=AF.Exp, scale=-1.0), reads=[d_T6], writes=[d_T6])
                    P.dve(lambda e: e.tensor_tensor(out=T3, in0=T3, in1=T2, op=ALU.subtract), reads=[d_T3, d_T2], writes=[d_T3])
                    src_l, d_src = T3, d_T3
                P.dve(lambda e, src_l=src_l: e.tensor_tensor_scan(out=T4, data0=m32[:], data1=src_l, initial=0.0, op0=ALU.mult, op1=ALU.add),
                      reads=[d_src, d_const], writes=[d_T4])
                b3 = T4.rearrange("p (n k) -> p n k", k=CH)
                b4 = T4.rearrange("p (a n k) -> p a n k", a=2, k=CH)
                P.dve(lambda e, b3=b3: e.tensor_tensor(out=T5.rearrange("p (n k) -> p n k", k=CH), in0=b3,
                                                       in1=b3[:, :, CH // 2:CH // 2 + 1].to_broadcast([128, 16, CH]), op=ALU.subtract),
                      reads=[d_T4, d_T5], writes=[d_T5])
                P.act(lambda e: e.activation(out=T1, in_=T5, func=AF.Exp, scale=sc), reads=[d_T5, d_T1], writes=[d_T1])
                P.act(lambda e: e.activation(out=T2, in_=T5, func=AF.Exp, scale=-sc), reads=[d_T5, d_T2], writes=[d_T2])
                bmid, blast = b3[:, :, CH // 2], b3[:, :, CH - 1]
                P.act(lambda e, bmid=bmid: e.activation(out=sm[:, 0, :], in_=bmid, func=AF.Exp, scale=sc), reads=[d_T4], writes=[d_sm])
                P.act(lambda e, b4=b4: e.activation(out=dece[:, :, 1:9], in_=b4[:, :, :, CH - 1], func=AF.Exp, scale=sc), reads=[d_T4], writes=[d_dece])
                P.dve(lambda e, bmid=bmid, blast=blast: e.tensor_tensor(out=sm[:, 2, :], in0=blast, in1=bmid, op=ALU.subtract), reads=[d_T4, d_sm], writes=[d_sm])
                P.act(lambda e: e.activation(out=sm[:, 1, :], in_=sm[:, 2, :], func=AF.Exp, scale=sc), reads=[d_sm], writes=[d_sm])
                P.dve(lambda e: e.tensor_tensor(out=emh[:, 0:nh, :], in0=bc_mid(sm[:, 0, :], nh), in1=bc_last(hmask[:, 0:nh], 16), op=ALU.mult),
                      reads=[d_sm, d_const], writes=[d_emh])
                bq = rot_bank(); proj_fm(wv, dw, qc, tt, bq)
                if kind == "gla":
                    P.dve(lambda e, bq=bq: e.scalar_tensor_tensor(out=qt, in0=ps[bq][:, :], scalar=float(32 ** -0.5), in1=T1, op0=ALU.mult, op1=ALU.mult),
                          reads=[d_ps[bq], d_T1], writes=[d_qt])
                    bk_ = rot_bank(); proj_fm(wv, dw, kc, tt, bk_)
                    P.dve(lambda e, bk_=bk_: e.tensor_tensor(out=kt, in0=ps[bk_][:, :], in1=T2, op=ALU.mult), reads=[d_ps[bk_], d_T2], writes=[d_kt])
                else:
                    P.dve(lambda e, bq=bq: e.tensor_tensor(out=qt, in0=ps[bq][:, :], in1=T1, op=ALU.mult), reads=[d_ps[bq], d_T1], writes=[d_qt])
                    P.dve(lambda e: e.scalar_tensor_tensor(out=kt, in0=T6, scalar=oml_s[:, g, l:l + 1], in1=T2, op0=ALU.mult, op1=ALU.mult),
                          reads=[d_T6, d_T2, d_setup], writes=[d_kt])
                for hh in range(nh):
                    P.dve(lambda e, hh=hh: e.tensor_scalar(out=ktm[:, hh, :], in0=kt, scalar1=hmask[:, hh:hh + 1], scalar2=None, op0=ALU.mult),
                          reads=[d_kt, d_const], writes=[d_ktm])
                for sub in range(4):
                    P.pe(lambda e, sub=sub: e.transpose(psT[:, sub * 128:(sub + 1) * 128], kt[:, sub * 128:(sub + 1) * 128], ident_bf[:]),
                         reads=[d_kt, d_const], writes=[d_psT])
                P.act(lambda e: e.activation(out=ktok.rearrange("p a t -> p (a t)"), in_=psT[:, 0:512], func=AF.Copy), reads=[d_psT], writes=[d_ktok])
                for half in range(2):
                    bv = rot_bank()
                    for s2 in range(2):
                        proj_tok(wv, dw, vc, nh * 64, tt, half * 2 + s2, bv, s2 * 256)
                    P.act(lambda e, bv=bv, half=half: e.activation(out=V[:, half * 2:half * 2 + 2, :],
                                                                   in_=ps[bv][:, :].rearrange("p (s c) -> p s c", s=2)[:, :, 0:nh * 64], func=AF.Copy),
                          reads=[d_ps[bv]], writes=[d_V])
                    for s2 in range(2):
                        for n_ in range(4):
                            P.pool(lambda e, s2=s2, n_=n_, half=half: e.tensor_scalar(
                                out=Vbd[:, s2, :, n_, :], in0=V[:, half * 2 + s2, :].rearrange("p (h v) -> p h v", h=nh),
                                scalar1=cmask[:, n_:n_ + 1], scalar2=1.0, op0=ALU.mult, op1=ALU.mult),
                                reads=[d_V, d_const], writes=[d_Vbd])
                    for s2 in range(2):
                        sub = half * 2 + s2
                        for hh in range(nh):
                            kw = {"tile_position": (0, 96)} if hh * dk == 96 else {}
                            P.pe(lambda e, s2=s2, sub=sub, hh=hh, kw=kw: e.matmul(
                                ps[BU][hh * dk:(hh + 1) * dk, s2 * 256:(s2 + 1) * 256], lhsT=ktok[:, sub, hh * dk:(hh + 1) * dk],
                                rhs=Vbd[:, s2, hh, :, :].rearrange("p n v -> p (n v)"), start=True, stop=True, **kw),
                                reads=[d_ktok, d_Vbd], writes=[d_ps[BU]])
                    n0 = half * 8
                    P.dve(lambda e, n0=n0: e.tensor_tensor(out=cUe[:, :, 1:9], in0=ps[BU][:, :].rearrange("p (n v) -> p v n", v=64),
                                                           in1=bc_mid(sm[:, 1, n0:n0 + 8], 64), op=ALU.mult),
                          reads=[d_ps[BU], d_sm], writes=[d_cUe])
                    P.dve(lambda e: e.tensor_copy(out=cUe[:, :, 0:1], in_=ext[:, :, 8:9]), reads=[d_ext, d_cUe], writes=[d_cUe])
                    P.dve(lambda e, half=half: e.tensor_copy(out=decf, in_=bc_mid(dece[:, half, :], 64)), reads=[d_dece], writes=[d_decf])
                    P.dve(lambda e: e.tensor_tensor_scan(out=ext.rearrange("p v n -> p (v n)"), data0=decf.rearrange("p v n -> p (v n)"),
                                                         data1=cUe.rearrange("p v n -> p (v n)"), initial=0.0, op0=ALU.mult, op1=ALU.add),
                          reads=[d_cUe, d_decf], writes=[d_ext])
                    for hh in range(nh):
                        P.dve(lambda e, hh=hh, n0=n0: e.tensor_tensor(out=Sbd[:, :, hh * 64:(hh + 1) * 64], in0=ext[:, :, 0:8].rearrange("p v n -> p n v"),
                                                                      in1=bc_last(emh[:, hh, n0:n0 + 8], 64), op=ALU.mult),
                              reads=[d_ext, d_emh], writes=[d_Sbd])
                    for s2 in range(2):
                        sub = half * 2 + s2
                        ssl = slice(sub * 128, (sub + 1) * 128)
                        for hh in range(nh):
                            P.pe(lambda e, hh=hh, ssl=ssl: e.matmul(ps[BA][:, hh * 128:(hh + 1) * 128], lhsT=ktm[:, hh, ssl], rhs=qt[:, ssl], start=True, stop=True),
                                 reads=[d_ktm, d_qt], writes=[d_ps[BA]])
                        a_ = ai % 2
                        ai += 1
                        P.dve(lambda e, a_=a_: e.tensor_tensor(out=A[a_], in0=ps[BA][:, 0:nh * 128].rearrange("p (h i) -> p h i", h=nh),
                                                               in1=bc_mid(maskBD[:], nh), op=ALU.mult),
                              reads=[d_ps[BA], d_const], writes=[d_A[a_]])
                        for hh in range(nh):
                            oc, hl = hh // 2, hh % 2
                            P.pe(lambda e, a_=a_, hh=hh, oc=oc, hl=hl, sub=sub, ssl=ssl: e.matmul(
                                ps[BO[oc]][64 * hl:64 * hl + 64, ssl], lhsT=V[:, sub, hh * 64:(hh + 1) * 64], rhs=A[a_][:, hh, :], start=True, stop=False),
                                reads=[d_V, d_A[a_]], writes=[d_ps[BO[oc]]])
                        for n_ in range(4):
                            nn = s2 * 4 + n_
                            csl = slice(sub * 128 + n_ * 32, sub * 128 + n_ * 32 + 32)
                            for oc in range(noc):
                                P.pe(lambda e, nn=nn, oc=oc, csl=csl, n_=n_: e.matmul(ps[BO[oc]][:, csl], lhsT=Sbd[:, nn, oc * 128:(oc + 1) * 128], rhs=qt[:, csl],
                                                                                  start=False, stop=(n_ == 3)),
                                     reads=[d_Sbd, d_qt], writes=[d_ps[BO[oc]]])
                for oc in range(noc):
                    P.act(lambda e, oc=oc: e.activation(out=osq, in_=ps[BO[oc]][:, :], func=AF.Square), reads=[d_ps[BO[oc]]], writes=[d_osq])
                    bs_ = rot_bank()
                    P.pe(lambda e, bs_=bs_: e.matmul(ps[bs_][:, :], lhsT=blk_bf[:], rhs=osq, start=True, stop=True), reads=[d_osq, d_const], writes=[d_ps[bs_]])
                    P.act(lambda e, bs_=bs_: e.activation(out=T1, in_=ps[bs_][:, :], func=AF.Ln, scale=1.0 / HD, bias=EPS), reads=[d_ps[bs_], d_T1], writes=[d_T1])
                    P.act(lambda e: e.activation(out=T2, in_=T1, func=AF.Exp, scale=-0.5), reads=[d_T1, d_T2], writes=[d_T2])
                    P.dve(lambda e, oc=oc: e.tensor_tensor(out=T3, in0=ps[BO[oc]][:, :], in1=T2, op=ALU.mult), reads=[d_ps[BO[oc]], d_T2, d_T3], writes=[d_T3])
                    P.dve(lambda e, oc=oc, yi=yi: e.scalar_tensor_tensor(out=y[yi][:, oc, :], in0=T3, scalar=ng[:, l:l + 1], in1=sgate[:, oc, :],
                                                                         op0=ALU.mult, op1=ALU.mult),
                          reads=[d_T3, d_sg[oc], d_par], writes=[d_y[yi]])
                out_proj(l, b, wo, dwo, y[yi], d_y[yi], tt, nk=noc)

        def ffn_run(l, b, segs, st_):
            hid, d_hid, sa, d_sa, tq, d_tq = st_["hid"], st_["d_hid"], st_["sa"], st_["d_sa"], st_["tq"], st_["d_tq"]
            slabs = []
            for gi, (w1d, w3d, w2d, dff, cf) in enumerate(segs):
                for si in range((dff + SLAB - 1) // SLAB):
                    slabs.append((gi, si, w1d, w3d, w2d, dff))
            loaded = {}

            def issue(j):
                gi, si, w1d, w3d, w2d, dff = slabs[j]
                f0 = si * SLAB
                sw = min(SLAB, dff - f0)
                s_ = ring_next()
                w1v = ring[s_][:, 0:1024].bitcast(BF16).rearrange("p (c n) -> p c n", c=DC)[:, :, 0:sw]
                w3v = ring[s_][:, 1024:2048].bitcast(BF16).rearrange("p (c n) -> p c n", c=DC)[:, :, 0:sw]
                w2v = ring[s_][:, 2048:3072].bitcast(BF16).rearrange("p (c n) -> p c n", c=2)[:, 0:sw // 128, :]
                P.dma("pool", lambda e: e.dma_start(out=w1v, in_=w1d[:, f0:f0 + sw].rearrange("(c p) n -> p c n", p=128)), writes=[d_ring[s_][0]])
                P.dma("pool", lambda e: e.dma_start(out=w3v, in_=w3d[:, f0:f0 + sw].rearrange("(c p) n -> p c n", p=128)), writes=[d_ring[s_][1]])
                P.dma("pool", lambda e: e.dma_start(out=w2v, in_=w2d[f0:f0 + sw, :].rearrange("(c p) n -> p c n", p=128)), writes=[d_ring[s_][2]])
                loaded[j] = (s_, sw, w1v, w3v, w2v)

            def stage_a(j, tt, comb):
                s_, sw, w1v, w3v, w2v = loaded[j]
                nfc = sw // 128
                tsl = slice(tt * TT, (tt + 1) * TT)
                hi = st_["k"] % 2
                st_["k"] += 1
                for fc in range(nfc):
                    pa = (st_["p"] % 2) * 2
                    st_["p"] += 1
                    for (wv_, dr, bk) in ((w1v, d_ring[s_][0], pa), (w3v, d_ring[s_][1], pa + 1)):
                        for c in range(DC):
                            P.pe(lambda e, c=c: e.matmul(ps[bk][:, :], lhsT=wv_[:, c, fc * 128:(fc + 1) * 128], rhs=h[:, c, tsl],
                                                         start=(c == 0), stop=(c == DC - 1)),
                                 reads=[dr, d_h[c][tt]], writes=[d_ps[bk]])
                    qi = st_["q"] % 3
                    st_["q"] += 1
                    P.act(lambda e: e.activation(out=sa[qi], in_=ps[pa][:, :], func=AF.Silu), reads=[d_ps[pa]], writes=[d_sa[qi]])
                    if comb is not None:
                        cb_, d_cb = comb
                        P.pool(lambda e: e.tensor_tensor(out=tq[qi], in0=sa[qi], in1=cb_[:, tsl], op=ALU.mult), reads=[d_sa[qi], d_cb], writes=[d_tq[qi]])
                        src_, dsrc = tq[qi], d_tq[qi]
                    else:
                        src_, dsrc = sa[qi], d_sa[qi]
                    P.dve(lambda e: e.tensor_tensor(out=hid[hi][:, fc, :], in0=ps[pa + 1][:, :], in1=src_, op=ALU.mult),
                          reads=[d_ps[pa + 1], dsrc], writes=[d_hid[hi][fc]])
                return (j, tt, hi)

            def stage_b(tok_):
                j, tt, hi = tok_
                s_, sw, w1v, w3v, w2v = loaded[j]
                nfc = sw // 128
                tsl = slice(tt * TT, (tt + 1) * TT)
                for co in range(DC):
                    bk = 4 + st_["o"] % 3
                    st_["o"] += 1
                    for fc in range(nfc):
                        P.pe(lambda e: e.matmul(ps[bk][:, :], lhsT=w2v[:, fc, co * 128:(co + 1) * 128], rhs=hid[hi][:, fc, :],
                                                start=(fc == 0), stop=(fc == nfc - 1)),
                             reads=[d_ring[s_][2], d_hid[hi][fc]], writes=[d_ps[bk]])
                    P.dve(lambda e: e.scalar_tensor_tensor(out=x[:, co, tsl], in0=ps[bk][:, :], scalar=modv[:, l, 40 + co, b:b + 1],
                                                           in1=x[:, co, tsl], op0=ALU.mult, op1=ALU.add),
                          reads=[d_ps[bk], d_x[co][tt], d_setup], writes=[d_x[co][tt]])

            issue(0)
            if len(slabs) > 1:
                issue(1)
            pend = None
            comb = None
            for j, (gi, si, _, _, _, _) in enumerate(slabs):
                if si == 0:
                    cf = segs[gi][4]
                    comb = cf() if cf is not None else None
                for tt in range(NTT):
                    cur = stage_a(j, tt, comb)
                    if pend is not None:
                        stage_b(pend)
                    if tt == 0 and j >= 1 and j + 1 < len(slabs):
                        issue(j + 1)
                    pend = cur
            stage_b(pend)

        def ffn_state():
            st_ = dict(k=0, p=0, q=0, o=0)
            st_["hid"] = [ua.alloc([SLAB // 128, TT], BF16) for _ in range(2)]
            st_["d_hid"] = [[newdep() for _ in range(4)] for _ in range(2)]
            st_["sa"] = [ua.alloc([TT]) for _ in range(3)]
            st_["d_sa"] = [newdep() for _ in range(3)]
            st_["tq"] = [ua.alloc([TT]) for _ in range(3)]
            st_["d_tq"] = [newdep() for _ in range(3)]
            return st_

        def dense_ffn(l, b):
            phase_switch()
            st_ = ffn_state()
            idx = l // 2
            ffn_run(l, b, [(ffn_w1[idx], ffn_w3[idx], ffn_w2[idx], DFF, None)], st_)

        def moe_ffn(l, b):
            phase_switch()
            idx = l // 2
            st_ = ffn_state()
            NS = T // 128
            lg = ua.alloc([NS, NE]); d_lg = newdep()
            top = ua.alloc([NS, 8]); d_top = newdep()
            wts = ua.alloc([4, NS]); d_w = newdep()
            cmb = ua.alloc([NS, NE]); d_cmb = newdep()
            cm2 = ua.alloc([NS, NE]); d_cm2 = newdep()
            combT = ua.alloc([T]); d_cT = newdep()
            cbc = [ua.alloc([T]) for _ in range(2)]; d_cbc = [newdep(), newdep()]
            for s in range(NS):
                tt = (s * 128) // TT
                for c in range(DC):
                    P.pe(lambda e, s=s, c=c: e.matmul(ps[0][:, s * NE:(s + 1) * NE], lhsT=h[:, c, s * 128:(s + 1) * 128], rhs=wr_s[:, idx, c, :],
                                                      start=(c == 0), stop=(c == DC - 1)),
                         reads=[d_h[c][tt], d_par], writes=[d_ps[0]])
            P.dve(lambda e: e.tensor_copy(out=lg.rearrange("p s e -> p (s e)"), in_=ps[0][:, 0:NS * NE]), reads=[d_ps[0]], writes=[d_lg])
            for s in range(NS):
                P.dve(lambda e, s=s: e.max(out=top[:, s, :], in_=lg[:, s, :]), reads=[d_lg], writes=[d_top])
            m1 = top[:, :, 0]
            m2 = top[:, :, 1]
            P.dve(lambda e: e.tensor_tensor(out=wts[:, 0, :], in0=m2, in1=m1, op=ALU.subtract), reads=[d_top], writes=[d_w])
            P.act(lambda e: e.activation(out=wts[:, 0, :], in_=wts[:, 0, :], func=AF.Exp), reads=[d_w], writes=[d_w])
            P.dve(lambda e: e.tensor_scalar(out=wts[:, 1, :], in0=wts[:, 0, :], scalar1=1.0, scalar2=None, op0=ALU.add), reads=[d_w], writes=[d_w])
            P.dve(lambda e: e.reciprocal(out=wts[:, 1, :], in_=wts[:, 1, :]), reads=[d_w], writes=[d_w])
            P.dve(lambda e: e.tensor_tensor(out=wts[:, 2, :], in0=wts[:, 0, :], in1=wts[:, 1, :], op=ALU.mult), reads=[d_w], writes=[d_w])
            P.dve(lambda e: e.tensor_tensor(out=cmb, in0=lg, in1=bc_last(m1, NE), op=ALU.is_equal), reads=[d_lg, d_top], writes=[d_cmb])
            P.dve(lambda e: e.tensor_tensor(out=cmb, in0=cmb, in1=bc_last(wts[:, 1, :], NE), op=ALU.mult), reads=[d_cmb, d_w], writes=[d_cmb])
            P.dve(lambda e: e.tensor_tensor(out=cm2, in0=lg, in1=bc_last(m2, NE), op=ALU.is_equal), reads=[d_lg, d_top], writes=[d_cm2])
            P.dve(lambda e: e.tensor_tensor(out=cm2, in0=cm2, in1=bc_last(wts[:, 2, :], NE), op=ALU.mult), reads=[d_cm2, d_w], writes=[d_cm2])
            P.dve(lambda e: e.tensor_tensor(out=cmb, in0=cmb, in1=cm2, op=ALU.add), reads=[d_cmb, d_cm2], writes=[d_cmb])
            for s in range(NS):
                bk = 1 + (s // 4) % 2
                P.pe(lambda e, s=s, bk=bk: e.transpose(ps[bk][0:NE, (s % 4) * 128:(s % 4 + 1) * 128], cmb[:, s, :], ident_f[:]),
                     reads=[d_cmb, d_const], writes=[d_ps[bk]])
                if s % 4 == 3:
                    P.act(lambda e, s=s, bk=bk: e.activation(out=combT[0:NE, (s - 3) * 128:(s + 1) * 128], in_=ps[bk][0:NE, :], func=AF.Copy),
                          reads=[d_ps[bk]], writes=[d_cT])
            def make_comb(ex):
                def cf():
                    ci = ex % 2
                    for tt in range(NTT):
                        bk = 1 + tt % 2
                        P.pe(lambda e: e.matmul(ps[bk][:, :], lhsT=sel[0:NE, ex, :], rhs=combT[0:NE, tt * TT:(tt + 1) * TT], start=True, stop=True),
                             reads=[d_cT, d_const], writes=[d_ps[bk]])
                        P.act(lambda e: e.activation(out=cbc[ci][:, tt * TT:(tt + 1) * TT], in_=ps[bk][:, :], func=AF.Copy),
                              reads=[d_ps[bk]], writes=[d_cbc[ci]])
                    return (cbc[ci], d_cbc[ci])
                return cf
            ffn_run(l, b, [(moe_w1[idx, ex], moe_w3[idx, ex], moe_w2[idx, ex], DFE, make_comb(ex)) for ex in range(NE)], st_)

        for b in range(NB):
            for c in range(DC):
                for tt in range(NTT):
                    P.dma("sp", lambda e, b=b, c=c, tt=tt: e.dma_start(out=x[:, c, tt * TT:(tt + 1) * TT], in_=xT[b, c * 128:(c + 1) * 128, tt * TT:(tt + 1) * TT]),
                          writes=[d_x[c][tt]])
            for l in range(L):
                if not diag_skip_mixers:
                    rmsnorm_to_h(l, b, gsm, 0)
                    conv_pass(l, b)
                    sgu_pass(l, b)
                    recur_pass(l, b, "gla")
                    recur_pass(l, b, "hgrn", 0)
                    recur_pass(l, b, "hgrn", 1)
                rmsnorm_to_h(l, b, gsf, 24)
                if l % 2 == 0:
                    dense_ffn(l, b)
                else:
                    moe_ffn(l, b)
            phase_switch()
            sq = [ua.alloc([TT], BF16) for _ in range(3)]; d_sq = [newdep() for _ in range(3)]
            lnv = ua.alloc([TT]); rstd = ua.alloc([TT]); d_ln = newdep(); d_rs = newdep()
            k = 0
            for tt in range(NTT):
                tsl = slice(tt * TT, (tt + 1) * TT)
                bk = rot_bank()
                for c in range(DC):
                    i = k % 3
                    k += 1
                    P.act(lambda e, c=c, i=i, tsl=tsl: e.activation(out=sq[i], in_=x[:, c, tsl], func=AF.Square), reads=[d_x[c][tt]], writes=[d_sq[i]])
                    P.pe(lambda e, c=c, i=i, bk=bk: e.matmul(ps[bk][:, :], lhsT=ones_bf[:], rhs=sq[i], start=(c == 0), stop=(c == DC - 1)),
                         reads=[d_sq[i], d_const], writes=[d_ps[bk]])
                P.act(lambda e, bk=bk: e.activation(out=lnv, in_=ps[bk][:, :], func=AF.Ln, scale=1.0 / D, bias=EPS), reads=[d_ps[bk]], writes=[d_ln])
                P.act(lambda e: e.activation(out=rstd, in_=lnv, func=AF.Exp, scale=-0.5), reads=[d_ln], writes=[d_rs])
                for c in range(DC):
                    P.dve(lambda e, c=c, tsl=tsl: e.scalar_tensor_tensor(out=x[:, c, tsl], in0=x[:, c, tsl], scalar=gfin_s[:, c:c + 1], in1=rstd,
                                                                         op0=ALU.mult, op1=ALU.mult),
                          reads=[d_x[c][tt], d_rs, d_par], writes=[d_x[c][tt]])
                    o = P.dma("sp", lambda e, b=b, c=c, tsl=tsl: e.dma_start(out=outT[b, c * 128:(c + 1) * 128, tsl], in_=x[:, c, tsl]),
                              reads=[d_x[c][tt]], writes=[Dep()], dep=d_x[c][tt])
                    P.final_waits.append(o)
        P.emit()
    return nc


def _fm(v):
    v = np.asarray(v, np.float32)
    lead = v.shape[:-1]
    n = v.shape[-1] // 128
    v = v.reshape(lead + (n, 128))
    return np.ascontiguousarray(np.moveaxis(v, -1, 0))


_PROG_CACHE = {}


def kernel(x, c, norm_mix_g, norm_ffn_g, final_norm_g, w_ada, b_ada, w_in, w_out, conv_w,
           sgu_norm_g, sgu_w, sgu_b, gla_w_gate, gla_b_gate, gla_norm_g, hgrn_lower_bounds,
           hgrn_norm_g, ffn_w1, ffn_w3, ffn_w2, moe_router, moe_w1, moe_w3, moe_w2, n_cores=8):
    f = lambda a: np.ascontiguousarray(np.asarray(a, dtype=np.float32))
    x = f(x)
    B, T, _ = x.shape
    L = w_in.shape[0]
    n_moe = L // 2
    NB = B // n_cores
    key = (NB, T, L, n_moe)
    if key not in _PROG_CACHE:
        _PROG_CACHE[key] = build_program(NB, T, L, n_moe)
    nc = _PROG_CACHE[key]
    c = f(c)
    shared = {
        "g_mix": _fm(norm_mix_g), "g_ffn": _fm(norm_ffn_g), "g_fin": _fm(final_norm_g),
        "w_ada": f(w_ada), "b_ada": _fm(b_ada), "w_in": f(w_in), "w_out": f(w_out),
        "conv_w": _fm(conv_w), "sgu_g": _fm(sgu_norm_g),
        "sgu_wT": np.ascontiguousarray(f(sgu_w).transpose(3, 0, 1, 2)),
        "sgu_bf": np.ascontiguousarray(np.repeat(f(sgu_b).reshape(L, 2, 2, 1, 128), 64, axis=3).reshape(L, 2, 128, 128).transpose(2, 0, 1, 3)),
        "gla_wg": np.ascontiguousarray(f(gla_w_gate).transpose(1, 0, 2)),
        "gla_bg": np.ascontiguousarray(f(gla_b_gate).T),
        "gla_ng": np.ascontiguousarray(np.tile(f(gla_norm_g), (1, 2)).T),
        "hg_lb": np.ascontiguousarray(f(hgrn_lower_bounds).reshape(L, 2, 128).transpose(2, 1, 0)),
        "hg_ng": np.ascontiguousarray(np.tile(f(hgrn_norm_g), (1, 2)).T),
        "ffn_w1": f(ffn_w1), "ffn_w3": f(ffn_w3), "ffn_w2": f(ffn_w2),
        "moe_r": f(moe_router), "moe_w1": f(moe_w1), "moe_w3": f(moe_w3), "moe_w2": f(moe_w2),
    }
    in_maps = []
    for i in range(n_cores):
        xb = x[i * NB:(i + 1) * NB]
        m = dict(shared)
        m["xT"] = np.ascontiguousarray(xb.transpose(0, 2, 1))
        m["cT"] = np.ascontiguousarray(c[i * NB:(i + 1) * NB].T.reshape(DC, 128, NB).transpose(1, 0, 2))
        in_maps.append(m)
    res = run_bass_kernel_spmd(nc, in_maps, core_ids=list(range(n_cores)))
    out = np.empty((B, T, D), np.float32)
    for i in range(n_cores):
        out[i * NB:(i + 1) * NB] = res.results[i]["outT"].transpose(0, 2, 1)
    return out
```

```python
import contextlib
import types
import numpy as np
import concourse.bass as bass
import concourse.mybir as mybir
from concourse.bass_utils import run_bass_kernel_spmd

F32 = mybir.dt.float32
BF16 = mybir.dt.bfloat16
AF = mybir.ActivationFunctionType
ALU = mybir.AluOpType
AX = mybir.AxisListType

D = 1024
DC = 8
GW = 256
HD = 64
NE = 8
DFF = 2816
DFE = 3584
INW = 3088
EPS = 1e-6
TT = 512
CH = 32
SLAB = 256


class Dep:
    __slots__ = ("name", "last_writer", "readers", "dma_sem", "dma_count")

    def __init__(self, name="", after=None):
        self.name = name
        self.last_writer = after
        self.readers = []
        self.dma_sem = None
        self.dma_count = 0


class Op:
    __slots__ = ("eng", "fn", "deps", "signaled", "is_dma", "dep_obj", "sem_val")

    def __init__(self, eng, fn, is_dma=False):
        self.eng = eng
        self.fn = fn
        self.deps = []
        self.signaled = False
        self.is_dma = is_dma
        self.dep_obj = None
        self.sem_val = None


ENGS = ("pe", "act", "dve", "pool", "sp")


def _freeze(fn):
    if fn.__closure__ is None:
        return fn
    cells = []
    for c in fn.__closure__:
        try:
            cells.append(types.CellType(c.cell_contents))
        except ValueError:
            cells.append(c)
    g = types.FunctionType(fn.__code__, fn.__globals__, fn.__name__, fn.__defaults__, tuple(cells))
    g.__kwdefaults__ = fn.__kwdefaults__
    return g


class Prog:
    def __init__(self, nc):
        self.nc = nc
        self.ops = {e: [] for e in ENGS}
        self.n_dma_sems = 0
        self.final_waits = []

    def _collect(self, op, reads, writes, same_engine_sync=True):
        deps = []
        for d in reads:
            w = d.last_writer
            if w is not None:
                deps.append(w)
        for d in writes:
            w = d.last_writer
            if w is not None:
                if not (op.is_dma and w.is_dma):
                    deps.append(w)
            deps.extend(d.readers)
        out = []
        seen = set()
        for w in deps:
            if w is op or id(w) in seen:
                continue
            if (not w.is_dma) and w.eng == op.eng and not same_engine_sync:
                continue
            seen.add(id(w))
            out.append(w)
        op.deps = out
        for w in out:
            w.signaled = True
        for d in writes:
            d.last_writer = op
            d.readers = []
        for d in reads:
            if not op.is_dma and d.readers:
                d.readers = [r for r in d.readers if r.is_dma or r.eng != op.eng]
            d.readers.append(op)

    def op(self, eng, fn, reads=(), writes=()):
        o = Op(eng, _freeze(fn))
        self._collect(o, reads, writes, same_engine_sync=(eng != "pe"))
        self.ops[eng].append(o)
        return o

    def dma(self, queue, fn, reads=(), writes=(), dep=None):
        o = Op(queue, _freeze(fn), is_dma=True)
        d = dep if dep is not None else writes[0]
        if d.dma_sem is None:
            d.dma_sem = self.n_dma_sems
            self.n_dma_sems += 1
        d.dma_count += 1
        o.dep_obj = d
        o.sem_val = 16 * d.dma_count
        o.signaled = True
        self._collect(o, reads, writes)
        self.ops[queue].append(o)
        return o

    def pe(self, fn, reads=(), writes=()):
        return self.op("pe", fn, reads, writes)

    def act(self, fn, reads=(), writes=()):
        return self.op("act", fn, reads, writes)

    def dve(self, fn, reads=(), writes=()):
        return self.op("dve", fn, reads, writes)

    def pool(self, fn, reads=(), writes=()):
        return self.op("pool", fn, reads, writes)

    def barrier(self, deps):
        return self.op("dve", lambda e: e.memset(self.scratch, 0.0), reads=(), writes=list(deps) + [self.scratch_dep])

    def emit(self):
        nc = self.nc
        for e in ENGS:
            cnt = 0
            for o in self.ops[e]:
                if o.is_dma:
                    continue
                if o.signaled:
                    cnt += 1
                    o.sem_val = cnt
        with contextlib.ExitStack() as st:
            eng_sems = {e: st.enter_context(nc.semaphore("s_" + e)) for e in ENGS}
            dma_sems = [st.enter_context(nc.semaphore("d%d" % i)) for i in range(self.n_dma_sems)]
            block = st.enter_context(nc.Block())

            def tok(o):
                if o.is_dma:
                    return ("d", o.dep_obj.dma_sem), dma_sems[o.dep_obj.dma_sem], o.sem_val
                return ("e", o.eng), eng_sems[o.eng], o.sem_val

            def run(ename, engine):
                known = {}
                for o in self.ops[ename]:
                    need = {}
                    for w in o.deps:
                        k, s, v = tok(w)
                        if known.get(k, 0) >= v:
                            continue
                        if k not in need or need[k][1] < v:
                            need[k] = (s, v)
                    for k, (s, v) in need.items():
                        engine.wait_ge(s, v)
                        known[k] = v
                    ins = o.fn(engine)
                    if o.is_dma:
                        ins.then_inc(dma_sems[o.dep_obj.dma_sem], 16)
                    elif o.signaled:
                        ins.then_inc(eng_sems[ename], 1)
                if ename == "sp":
                    for w in self.final_waits:
                        k, s, v = tok(w)
                        engine.wait_ge(s, v)

            @block.tensor
            def _(t):
                run("pe", t)

            @block.scalar
            def _(t):
                run("act", t)

            @block.vector
            def _(t):
                run("dve", t)

            @block.gpsimd
            def _(t):
                run("pool", t)

            @block.sync
            def _(t):
                run("sp", t)


class Arena:
    def __init__(self, tensor, words):
        self.t = tensor
        self.words = words
        self.off = 0
        self.marks = []

    def alloc(self, shape, dtype=F32):
        n = int(np.prod(shape))
        w = n if dtype == F32 else (n + 1) // 2
        w = (w + 7) // 8 * 8
        assert self.off + w <= self.words, ("arena overflow", self.off, w, self.words)
        v = self.t[:, self.off:self.off + w]
        self.off += w
        if dtype != F32:
            v = v.bitcast(dtype)
        v = v[:, 0:n]
        if len(shape) == 2:
            v = v.rearrange("p (a b) -> p a b", a=shape[0])
        elif len(shape) == 3:
            v = v.rearrange("p (a b c) -> p a b c", a=shape[0], b=shape[1])
        elif len(shape) == 4:
            v = v.rearrange("p (a b c d) -> p a b c d", a=shape[0], b=shape[1], c=shape[2])
        return v

    def mark(self):
        return self.off

    def reset(self, m):
        self.off = m


def bc_last(ap, n):
    shp = list(ap.shape)
    return ap.unsqueeze(len(shp)).to_broadcast(shp + [n])


def bc_mid(ap, n):
    shp = list(ap.shape)
    return ap.unsqueeze(1).to_broadcast([shp[0], n] + shp[1:])


def build_program(NB, T, L, n_moe, diag_skip_mixers=False):
    NTT = T // TT
    n_dense = (L + 1) // 2
    nc = bass.Bass("TRN2", target_bir_lowering=False)

    def din(name, shape):
        return nc.dram_tensor(name, list(shape), F32, kind="ExternalInput").ap()

    xT = din("xT", [NB, D, T])
    cT = din("cT", [128, DC, NB])
    g_mix = din("g_mix", [128, L, DC])
    g_ffn = din("g_ffn", [128, L, DC])
    g_fin = din("g_fin", [128, DC])
    w_ada = din("w_ada", [L, D, 6 * D])
    b_ada = din("b_ada", [128, L, 48])
    w_in = din("w_in", [L, D, INW])
    w_out = din("w_out", [L, D, D])
    conv_w = din("conv_w", [128, L, 3, 2])
    sgu_g = din("sgu_g", [128, L, 2])
    sgu_wT = din("sgu_wT", [128, L, 4, 128])
    sgu_bf = din("sgu_bf", [128, L, 2, 128])
    gla_wg = din("gla_wg", [16, L, 128])
    gla_bg = din("gla_bg", [128, L])
    gla_ng = din("gla_ng", [128, L])
    hg_lb = din("hg_lb", [128, 2, L])
    hg_ng = din("hg_ng", [128, L])
    ffn_w1 = din("ffn_w1", [n_dense, D, DFF])
    ffn_w3 = din("ffn_w3", [n_dense, D, DFF])
    ffn_w2 = din("ffn_w2", [n_dense, DFF, D])
    moe_r = din("moe_r", [max(n_moe, 1), D, NE])
    moe_w1 = din("moe_w1", [max(n_moe, 1), NE, D, DFE])
    moe_w3 = din("moe_w3", [max(n_moe, 1), NE, D, DFE])
    moe_w2 = din("moe_w2", [max(n_moe, 1), NE, DFE, D])
    outT = nc.dram_tensor("outT", [NB, D, T], F32, kind="ExternalOutput").ap()

    P = Prog(nc)
    with contextlib.ExitStack() as st:
        def sb(name, shape, dt=F32):
            return st.enter_context(nc.sbuf_tensor(name, list(shape), dt))

        x = sb("x", [128, DC, T])
        h = sb("h", [128, DC, T], BF16)
        RINGW = 4352
        ring = [sb("ring%d" % i, [128, RINGW]) for i in range(2)]
        UW = 13696
        ureg = sb("ureg", [128, UW])
        ones_bf = sb("ones_bf", [128, 128], BF16)
        blk_bf = sb("blk_bf", [128, 128], BF16)
        ident_bf = sb("ident_bf", [128, 128], BF16)
        ident_f = sb("ident_f", [128, 128])
        ones_f = sb("ones_f", [128, 128])
        maskBD = sb("maskBD", [128, 128])
        m32 = sb("m32", [128, TT])
        hmask4 = sb("hmask4", [128, 4])
        hmask2 = sb("hmask2", [128, 2])
        cmask = sb("cmask", [128, 4])
        sel = sb("sel", [8, NE, 128])
        scratch = sb("scratch", [128, 8])
        P.scratch = scratch[:, 0:1]
        P.scratch_dep = Dep("scratch")
        cond = sb("cond", [128, DC, NB])
        gmix_s = sb("gmix_s", [128, L, DC])
        gffn_s = sb("gffn_s", [128, L, DC])
        gfin_s = sb("gfin_s", [128, DC])
        bada_s = sb("bada_s", [128, L, 48])
        modv = sb("modv", [128, L, 48, NB])
        gsm = sb("gsm", [128, L, DC, NB])
        gsf = sb("gsf", [128, L, DC, NB])
        convw_s = sb("convw_s", [128, L, 3, 2])
        sgug_s = sb("sgug_s", [128, L, 2])
        sguw_s = sb("sguw_s", [128, L, 4, 128], BF16)
        sgub_s = sb("sgub_s", [128, L, 2, 128])
        wg_s = sb("wg_s", [16, L, 128])
        nbg_s = sb("nbg_s", [128, L])
        glang_s = sb("glang_s", [128, L])
        hgng_s = sb("hgng_s", [128, L])
        lbe = sb("lbe", [128, 2, L])
        lb_s = sb("lb_s", [128, 2, L])
        oml_s = sb("oml_s", [128, 2, L])
        lbt = sb("lbt", [128, 2, 2])
        wr_s = sb("wr_s", [128, max(n_moe, 1), DC, NE], BF16)

        ps = [st.enter_context(nc.psum_tensor("ps%d" % i, [128, 512], F32)) for i in range(7)]
        psT = st.enter_context(nc.psum_tensor("psT", [128, 1024], BF16))
        d_ps = [Dep("ps%d" % i) for i in range(7)]
        d_psT = Dep("psT")

        d_x = [[Dep("x%d_%d" % (c, t)) for t in range(NTT)] for c in range(DC)]
        d_h = [[Dep("h%d_%d" % (c, t)) for t in range(NTT)] for c in range(DC)]
        d_ring = [[Dep("ring%d_%d" % (i, j)) for j in range(3)] for i in range(2)]
        d_const = Dep("const")
        d_par = Dep("par")
        d_setup = Dep("setup")

        C = [d_const]
        P.pool(lambda e: e.memset(ones_bf[:], 1.0), writes=C)
        P.pool(lambda e: e.memset(ones_f[:], 1.0), writes=C)
        P.pool(lambda e: e.memset(blk_bf[:], 0.0), writes=C)
        P.pool(lambda e: e.memset(blk_bf[0:64, 0:64], 1.0), reads=C, writes=C)
        P.pool(lambda e: e.memset(blk_bf[64:128, 64:128], 1.0), reads=C, writes=C)
        P.pool(lambda e: e.affine_select(out=ident_bf[:], in_=ones_bf[:], pattern=[[-1, 128]], compare_op=ALU.is_equal,
                                          fill=0.0, base=0, channel_multiplier=1), reads=C, writes=C)
        P.pool(lambda e: e.affine_select(out=ident_f[:], in_=ones_f[:], pattern=[[-1, 128]], compare_op=ALU.is_equal,
                                          fill=0.0, base=0, channel_multiplier=1), reads=C, writes=C)
        P.pool(lambda e: e.affine_select(out=maskBD[:], in_=ones_f[:], pattern=[[1, 128]], compare_op=ALU.is_ge,
                                          fill=0.0, base=0, channel_multiplier=-1), reads=C, writes=C)
        for bl in range(1, 4):
            P.pool(lambda e, bl=bl: e.affine_select(out=maskBD[:, 32 * bl:32 * bl + 32], in_=maskBD[:, 32 * bl:32 * bl + 32],
                                                    pattern=[[0, 32]], compare_op=ALU.is_ge, fill=0.0, base=-32 * bl,
                                                    channel_multiplier=1), reads=C, writes=C)
        P.pool(lambda e: e.memset(m32[:], 1.0), reads=C, writes=C)
        P.pool(lambda e: e.memset(m32[:].rearrange("p (n k) -> p n k", k=CH)[:, :, 0:1], 0.0), reads=C, writes=C)
        for (msk, nh, dk) in ((hmask4, 4, 32), (hmask2, 2, 64), (cmask, 4, 32)):
            P.pool(lambda e, msk=msk, nh=nh, dk=dk: e.affine_select(out=msk[:], in_=ones_f[:, 0:nh], pattern=[[-dk, nh]], compare_op=ALU.is_ge,
                                                                    fill=0.0, base=0, channel_multiplier=1), reads=C, writes=C)
            P.pool(lambda e, msk=msk, nh=nh, dk=dk: e.affine_select(out=msk[:], in_=msk[:], pattern=[[dk, nh]], compare_op=ALU.is_ge,
                                                                    fill=0.0, base=dk - 1, channel_multiplier=-1), reads=C, writes=C)
        P.pool(lambda e: e.affine_select(out=sel[:], in_=bc_mid(ones_f[0:8, :], NE), pattern=[[-1, NE], [0, 128]], compare_op=ALU.is_equal,
                                          fill=0.0, base=0, channel_multiplier=1), reads=C, writes=C)

        Wp = [d_par]
        for dst, src in ((cond, cT), (gmix_s, g_mix), (gffn_s, g_ffn), (gfin_s, g_fin), (bada_s, b_ada), (convw_s, conv_w),
                         (sgug_s, sgu_g), (sgub_s, sgu_bf), (wg_s, gla_wg), (nbg_s, gla_bg), (glang_s, gla_ng),
                         (hgng_s, hg_ng), (lbe, hg_lb)):
            P.dma("sp", lambda e, dst=dst, src=src: e.dma_start(out=dst[:], in_=src), writes=Wp)
        P.dma("pool", lambda e: e.dma_start(out=sguw_s[:], in_=sgu_wT), writes=Wp)
        if n_moe:
            for m in range(n_moe):
                P.dma("pool", lambda e, m=m: e.dma_start(out=wr_s[:, m], in_=moe_r[m].rearrange("(c p) e -> p c e", p=128)), writes=Wp)
        S = [d_setup]
        R_ = [d_par, d_const]
        for l in range(L):
            for hh in range(4):
                P.pool(lambda e, l=l, hh=hh: e.affine_select(out=sguw_s[:, l, hh, :], in_=sguw_s[:, l, hh, :], pattern=[[1, 128]],
                                                             compare_op=ALU.is_ge, fill=0.0, base=0, channel_multiplier=-1),
                       reads=R_, writes=S)
        P.act(lambda e: e.activation(out=cond[:], in_=cond[:], func=AF.Silu), reads=R_, writes=S)
        P.dve(lambda e: e.tensor_scalar(out=nbg_s[:], in0=nbg_s[:], scalar1=-1.0, scalar2=None, op0=ALU.mult), reads=R_, writes=S)
        P.act(lambda e: e.activation(out=lbe[:], in_=lbe[:], func=AF.Exp), reads=R_ + S, writes=S)
        P.dve(lambda e: e.tensor_reduce(out=lbt[:, :, 0:1], in_=lbe[:], axis=AX.X, op=ALU.add), reads=S, writes=S)
        P.dve(lambda e: e.reciprocal(out=lbt[:, :, 1:2], in_=lbt[:, :, 0:1]), reads=S, writes=S)
        P.dve(lambda e: e.tensor_tensor(out=lbe[:], in0=lbe[:], in1=lbt[:, :, 1:2].to_broadcast([128, 2, L]), op=ALU.mult), reads=S, writes=S)
        P.dve(lambda e: e.memset(lb_s[:, :, 0:1], 0.0), reads=S, writes=S)
        for l in range(1, L):
            P.dve(lambda e, l=l: e.tensor_tensor(out=lb_s[:, :, l:l + 1], in0=lb_s[:, :, l - 1:l], in1=lbe[:, :, l:l + 1], op=ALU.add), reads=S, writes=S)
        P.dve(lambda e: e.tensor_scalar(out=oml_s[:], in0=lb_s[:], scalar1=-1.0, scalar2=1.0, op0=ALU.mult, op1=ALU.add), reads=S, writes=S)

        ADW = 512
        rv = [ring[i][:, 0:DC * ADW].rearrange("p (c n) -> p c n", c=DC) for i in range(2)]
        step = 0
        d_ada = [Dep("ada0"), Dep("ada1")]
        for l in range(L):
            for pc in range(6 * D // ADW):
                s_ = step % 2
                step += 1
                P.dma("sp", lambda e, l=l, pc=pc, s_=s_: e.dma_start(out=rv[s_], in_=w_ada[l][:, pc * ADW:(pc + 1) * ADW].rearrange("(c p) n -> p c n", p=128)),
                      writes=[d_ada[s_]])
                for jj in range(ADW // 128):
                    j = pc * (ADW // 128) + jj
                    for c in range(DC):
                        P.pe(lambda e, s_=s_, jj=jj, j=j, c=c: e.matmul(ps[0][:, j * NB:(j + 1) * NB], lhsT=rv[s_][:, c, jj * 128:(jj + 1) * 128],
                                                                      rhs=cond[:, c, :], start=(c == 0), stop=(c == DC - 1)),
                             reads=[d_ada[s_], d_setup], writes=[d_ps[0]])
            P.dve(lambda e, l=l: e.tensor_tensor(out=modv[:, l], in0=ps[0][:, 0:48 * NB].rearrange("p (j b) -> p j b", b=NB),
                                                 in1=bc_last(bada_s[:, l, :], NB), op=ALU.add), reads=[d_ps[0], d_par], writes=S)
            for (gdst, gsrc, j0) in ((gsm, gmix_s, 8), (gsf, gffn_s, 32)):
                P.dve(lambda e, l=l, gdst=gdst, j0=j0: e.tensor_scalar(out=gdst[:, l], in0=modv[:, l, j0:j0 + 8, :], scalar1=1.0, scalar2=None, op0=ALU.add),
                      reads=S, writes=S)
                P.dve(lambda e, l=l, gdst=gdst, gsrc=gsrc: e.tensor_tensor(out=gdst[:, l], in0=gdst[:, l], in1=bc_last(gsrc[:, l, :], NB), op=ALU.mult),
                      reads=S + [d_par], writes=S)

        ada_done = P.barrier(d_ada)
        for i in range(2):
            for j in range(3):
                d_ring[i][j].last_writer = ada_done
        CONSTS = [d_const, d_par, d_setup]

        ua = Arena(ureg, UW)
        phase = {"deps": [], "after": None}

        def newdep(name=""):
            d_ = Dep(name, after=phase["after"])
            phase["deps"].append(d_)
            return d_

        def phase_switch():
            if phase["deps"]:
                phase["after"] = P.barrier(phase["deps"])
            phase["deps"] = []
            ua.reset(0)

        rot = {"i": 0}

        def rot_bank(n=3):
            i = rot["i"] % n
            rot["i"] += 1
            return i

        def rmsnorm_to_h(l, b, gs_t, sh_j0):
            phase_switch()
            sq = [ua.alloc([TT], BF16) for _ in range(3)]
            d_sq = [newdep() for _ in range(3)]
            lnv = ua.alloc([TT]); rstd = ua.alloc([TT])
            d_ln = newdep(); d_rs = newdep()
            tmp = [ua.alloc([TT]) for _ in range(3)]
            d_tmp = [newdep() for _ in range(3)]
            k = 0
            for tt in range(NTT):
                tsl = slice(tt * TT, (tt + 1) * TT)
                bk = rot_bank()
                for c in range(DC):
                    i = k % 3
                    k += 1
                    P.act(lambda e, c=c, i=i, tsl=tsl: e.activation(out=sq[i], in_=x[:, c, tsl], func=AF.Square),
                          reads=[d_x[c][tt]], writes=[d_sq[i]])
                    P.pe(lambda e, c=c, i=i, bk=bk: e.matmul(ps[bk][:, :], lhsT=ones_bf[:], rhs=sq[i], start=(c == 0), stop=(c == DC - 1)),
                         reads=[d_sq[i], d_const], writes=[d_ps[bk]])
                P.act(lambda e, bk=bk: e.activation(out=lnv, in_=ps[bk][:, :], func=AF.Ln, scale=1.0 / D, bias=EPS),
                      reads=[d_ps[bk]], writes=[d_ln])
                P.act(lambda e: e.activation(out=rstd, in_=lnv, func=AF.Exp, scale=-0.5), reads=[d_ln], writes=[d_rs])
                for c in range(DC):
                    i = k % 3
                    k += 1
                    P.dve(lambda e, c=c, i=i, tsl=tsl: e.tensor_tensor(out=tmp[i], in0=x[:, c, tsl], in1=rstd, op=ALU.mult),
                          reads=[d_x[c][tt], d_rs], writes=[d_tmp[i]])
                    P.act(lambda e, c=c, i=i, tsl=tsl: e.activation(out=h[:, c, tsl], in_=tmp[i], func=AF.Identity,
                                                                    scale=gs_t[:, l, c, b:b + 1], bias=modv[:, l, sh_j0 + c, b:b + 1]),
                          reads=[d_tmp[i], d_setup], writes=[d_h[c][tt]])

        ring_state = {"n": 0}

        def ring_next():
            s_ = ring_state["n"] % 2
            ring_state["n"] += 1
            return s_

        WO_OFF = 3328

        def load_mixer_weights(l, ranges, r0, nrows):
            s_ = ring_next()
            ncols = sum(n for _, n in ranges)
            assert DC * ncols // 2 <= WO_OFF
            wv = ring[s_][:, 0:DC * ncols // 2].bitcast(BF16).rearrange("p (c n) -> p c n", c=DC)
            nkc = nrows // 128
            wo = ring[s_][:, WO_OFF:WO_OFF + nkc * D // 2].bitcast(BF16).rearrange("p (c n) -> p c n", c=nkc)
            nwords = DC * ncols // 2
            span = [d_ring[s_][j] for j in range(3) if nwords > (0, 1024, 2048)[j]]
            off = 0
            for (c0, n) in ranges:
                P.dma("pool", lambda e, c0=c0, n=n, off=off: e.dma_start(out=wv[:, :, off:off + n],
                                                                        in_=w_in[l][:, c0:c0 + n].rearrange("(c p) n -> p c n", p=128)),
                      writes=span, dep=d_ring[s_][0])
                off += n
            P.dma("pool", lambda e: e.dma_start(out=wo, in_=w_out[l][r0:r0 + nrows, :].rearrange("(c p) n -> p c n", p=128)),
                  writes=[d_ring[s_][2]], dep=d_ring[s_][2])
            dwo_ = [d_ring[s_][2]]
            return wv, wo, span, dwo_

        def proj_fm(wv, dw, col, tt, bk, M=128):
            tsl = slice(tt * TT, (tt + 1) * TT)
            for c in range(DC):
                P.pe(lambda e, c=c: e.matmul(ps[bk][0:M, :], lhsT=wv[:, c, col:col + M], rhs=h[:, c, tsl], start=(c == 0), stop=(c == DC - 1)),
                     reads=dw + [d_h[c][tt]], writes=[d_ps[bk]])

        def proj_tok(wv, dw, col, ncols, tt, sub, bk, off):
            t0 = tt * TT + sub * 128
            for c in range(DC):
                P.pe(lambda e, c=c: e.matmul(ps[bk][:, off:off + ncols], lhsT=h[:, c, t0:t0 + 128], rhs=wv[:, c, col:col + ncols],
                                             start=(c == 0), stop=(c == DC - 1)),
                     reads=dw + [d_h[c][tt]], writes=[d_ps[bk]])

        def out_proj(l, b, wo, dwo, y, d_y, tt, nk=2):
            tsl = slice(tt * TT, (tt + 1) * TT)
            for co in range(DC):
                bk = rot_bank()
                for kc in range(nk):
                    P.pe(lambda e, co=co, kc=kc, bk=bk: e.matmul(ps[bk][:, :], lhsT=wo[:, kc, co * 128:(co + 1) * 128], rhs=y[:, kc, :],
                                                                 start=(kc == 0), stop=(kc == nk - 1)),
                         reads=dwo + [d_y], writes=[d_ps[bk]])
                P.dve(lambda e, co=co, bk=bk: e.scalar_tensor_tensor(out=x[:, co, tsl], in0=ps[bk][:, :], scalar=modv[:, l, 16 + co, b:b + 1],
                                                                     in1=x[:, co, tsl], op0=ALU.mult, op1=ALU.add),
                      reads=[d_ps[bk], d_x[co][tt], d_setup], writes=[d_x[co][tt]])

        def conv_pass(l, b):
            phase_switch()
            wv, wo, dw, dwo = load_mixer_weights(l, [(0, 768)], 0, 256)
            z = ua.alloc([2, TT + 2]); d_z = [newdep(), newdep()]
            cxs = [ua.alloc([TT]) for _ in range(2)]; d_cxs = [newdep(), newdep()]
            acc = [ua.alloc([TT]) for _ in range(2)]; d_acc = [newdep(), newdep()]
            y = [ua.alloc([2, TT], BF16) for _ in range(2)]; d_y = [newdep(), newdep()]
            for ch in range(2):
                P.dve(lambda e, ch=ch: e.memset(z[:, ch, 0:2], 0.0), writes=[d_z[ch]])
            for tt in range(NTT):
                yi = tt % 2
                for ch in range(2):
                    b_cc = rot_bank(); proj_fm(wv, dw, 256 + ch * 128, tt, b_cc)
                    b_cx = rot_bank(); proj_fm(wv, dw, 512 + ch * 128, tt, b_cx)
                    P.act(lambda e, ch=ch, b_cx=b_cx: e.activation(out=cxs[ch], in_=ps[b_cx][:, :], func=AF.Copy),
                          reads=[d_ps[b_cx]], writes=[d_cxs[ch]])
                    P.dve(lambda e, ch=ch, b_cc=b_cc: e.tensor_tensor(out=z[:, ch, 2:TT + 2], in0=ps[b_cc][:, :], in1=cxs[ch], op=ALU.mult),
                          reads=[d_ps[b_cc], d_cxs[ch]], writes=[d_z[ch]])
                    P.dve(lambda e, ch=ch: e.tensor_scalar(out=acc[ch], in0=z[:, ch, 0:TT], scalar1=convw_s[:, l, 0, ch:ch + 1], scalar2=None, op0=ALU.mult),
                          reads=[d_z[ch], d_par], writes=[d_acc[ch]])
                    for kk in (1, 2):
                        P.dve(lambda e, ch=ch, kk=kk: e.scalar_tensor_tensor(out=acc[ch], in0=z[:, ch, kk:TT + kk], scalar=convw_s[:, l, kk, ch:ch + 1],
                                                                             in1=acc[ch], op0=ALU.mult, op1=ALU.add),
                              reads=[d_z[ch], d_par, d_acc[ch]], writes=[d_acc[ch]])
                    P.dve(lambda e, ch=ch: e.tensor_copy(out=z[:, ch, 0:2], in_=z[:, ch, TT:TT + 2]), reads=[d_z[ch]], writes=[d_z[ch]])
                    b_cb = rot_bank(); proj_fm(wv, dw, ch * 128, tt, b_cb)
                    P.dve(lambda e, ch=ch, b_cb=b_cb, yi=yi: e.tensor_tensor(out=y[yi][:, ch, :], in0=ps[b_cb][:, :], in1=acc[ch], op=ALU.mult),
                          reads=[d_ps[b_cb], d_acc[ch]], writes=[d_y[yi]])
                out_proj(l, b, wo, dwo, y[yi], d_y[yi], tt)

        def sgu_pass(l, b):
            phase_switch()
            wv, wo, dw, dwo = load_mixer_weights(l, [(768, 512)], 256, 256)
            vn = [ua.alloc([256], BF16) for _ in range(2)]; d_vn = [newdep(), newdep()]
            stats = ua.alloc([2, 6]); mv = ua.alloc([2, 2]); rs = ua.alloc([2, 2]); d_st = [newdep(), newdep()]
            t1 = [ua.alloc([TT]) for _ in range(2)]; d_t1 = [newdep(), newdep()]
            y = [ua.alloc([2, TT], BF16) for _ in range(2)]; d_y = [newdep(), newdep()]
            k = 0
            for tt in range(NTT):
                yi = tt % 2
                bm = [3, 4]
                for sub in range(4):
                    i = k % 2
                    k += 1
                    bv = rot_bank()
                    proj_tok(wv, dw, 256, 256, tt, sub, bv, 0)
                    P.dve(lambda e, i=i, bv=bv: e.bn_stats(out=stats[:, i, :], in_=ps[bv][:, 0:256]), reads=[d_ps[bv]], writes=[d_st[i]])
                    P.dve(lambda e, i=i: e.bn_aggr(out=mv[:, i, :], in_=stats[:, i, :]), reads=[d_st[i]], writes=[d_st[i]])
                    P.act(lambda e, i=i: e.activation(out=rs[:, i, 0:1], in_=mv[:, i, 1:2], func=AF.Ln, bias=EPS, scale=1.0), reads=[d_st[i]], writes=[d_st[i]])
                    P.act(lambda e, i=i: e.activation(out=rs[:, i, 1:2], in_=rs[:, i, 0:1], func=AF.Exp, scale=-0.5), reads=[d_st[i]], writes=[d_st[i]])
                    P.dve(lambda e, i=i, bv=bv: e.tensor_scalar(out=vn[i], in0=ps[bv][:, 0:256], scalar1=mv[:, i, 0:1], scalar2=rs[:, i, 1:2],
                                                                op0=ALU.subtract, op1=ALU.mult),
                          reads=[d_ps[bv], d_st[i]], writes=[d_vn[i]])
                    for hh in range(4):
                        cc, hl = hh // 2, hh % 2
                        P.pe(lambda e, i=i, hh=hh, cc=cc, hl=hl, sub=sub: e.matmul(ps[bm[cc]][64 * hl:64 * hl + 64, sub * 128:(sub + 1) * 128],
                                                                                   lhsT=vn[i][:, hh * 64:(hh + 1) * 64], rhs=sguw_s[:, l, hh, :],
                                                                                   start=True, stop=True),
                             reads=[d_vn[i], d_setup], writes=[d_ps[bm[cc]]])
                for cc in range(2):
                    bu = rot_bank(); proj_fm(wv, dw, cc * 128, tt, bu)
                    P.dve(lambda e, cc=cc: e.scalar_tensor_tensor(out=t1[cc].rearrange("p (a t) -> p a t", a=4),
                                                                  in0=ps[bm[cc]][:, :].rearrange("p (a t) -> p a t", a=4),
                                                                  scalar=sgug_s[:, l, cc:cc + 1], in1=bc_mid(sgub_s[:, l, cc, :], 4),
                                                                  op0=ALU.mult, op1=ALU.add),
                          reads=[d_ps[bm[cc]], d_par], writes=[d_t1[cc]])
                    P.dve(lambda e, cc=cc, bu=bu, yi=yi: e.tensor_tensor(out=y[yi][:, cc, :], in0=ps[bu][:, :], in1=t1[cc], op=ALU.mult),
                          reads=[d_ps[bu], d_t1[cc]], writes=[d_y[yi]])
                out_proj(l, b, wo, dwo, y[yi], d_y[yi], tt)

        def recur_pass(l, b, kind, g=0):
            phase_switch()
            if kind == "gla":
                wv, wo, dw, dwo = load_mixer_weights(l, [(1280, 784)], 512, 256)
                nh, dk, sc, hmask, ng = 4, 32, -1.0 / 16.0, hmask4, glang_s
                qc, kc, vc, gate_col, noc = 0, 128, 256, 528, 2
            else:
                base = 2064
                wv, wo, dw, dwo = load_mixer_weights(l, [(base + g * 128, 128), (base + 256 + g * 128, 128), (base + 512 + g * 128, 128),
                                                        (base + 768 + g * 128, 128)], 768 + g * 128, 128)
                nh, dk, sc, hmask, ng = 2, 64, 1.0, hmask2, hgng_s
                qc, fcol, vc, gate_col, noc = 0, 128, 256, 384, 1
            BO = [5, 6]
            BA, BU = 3, 4
            ext = ua.alloc([64, 9]); d_ext = newdep()
            dece = ua.alloc([2, 9]); d_dece = newdep()
            y = [ua.alloc([noc, TT], BF16) for _ in range(2)]; d_y = [newdep(), newdep()]
            sgate = ua.alloc([noc, TT]); d_sg = [newdep() for _ in range(noc)]
            qt = ua.alloc([TT], BF16); d_qt = newdep()
            kt = ua.alloc([TT], BF16); d_kt = newdep()
            ktm = ua.alloc([nh, TT], BF16); d_ktm = newdep()
            ktok = ua.alloc([4, 128], BF16); d_ktok = newdep()
            V = ua.alloc([4, nh * 64], BF16); d_V = newdep()
            Vbd = ua.alloc([2, nh, 4, 64], BF16); d_Vbd = newdep()
            A = [ua.alloc([nh, 128], BF16) for _ in range(2)]; d_A = [newdep(), newdep()]
            T1 = ua.alloc([TT]); d_T1 = newdep()
            T2 = ua.alloc([TT]); d_T2 = newdep()
            T3 = ua.alloc([TT]); d_T3 = newdep()
            T4 = ua.alloc([TT]); d_T4 = newdep()
            T5 = ua.alloc([TT]); d_T5 = newdep()
            T6 = ua.alloc([TT]); d_T6 = newdep()
            sm = ua.alloc([4, 16]); d_sm = newdep()
            emh = ua.alloc([4, 16]); d_emh = newdep()
            cUe = ua.alloc([64, 9]); d_cUe = newdep()
            decf = ua.alloc([64, 9]); d_decf = newdep()
            Sbd = ua.alloc([8, nh * 64], BF16); d_Sbd = newdep()
            osq = ua.alloc([TT], BF16); d_osq = newdep()
            P.dve(lambda e: e.memset(dece, 0.0), writes=[d_dece])
            P.dve(lambda e: e.memset(ext, 0.0), writes=[d_ext])
            ai = 0
            for tt in range(NTT):
                tsl = slice(tt * TT, (tt + 1) * TT)
                yi = tt % 2
                for oc in range(noc):
                    bg_ = rot_bank(); proj_fm(wv, dw, gate_col + oc * 128, tt, bg_)
                    P.act(lambda e, bg_=bg_: e.activation(out=T1, in_=ps[bg_][:, :], func=AF.Exp, scale=-1.0), reads=[d_ps[bg_]], writes=[d_T1])
                    P.act(lambda e: e.activation(out=T2, in_=T1, func=AF.Ln, bias=1.0, scale=1.0), reads=[d_T1], writes=[d_T2])
                    P.act(lambda e: e.activation(out=T3, in_=T2, func=AF.Exp, scale=-1.0), reads=[d_T2], writes=[d_T3])
                    P.dve(lambda e, oc=oc, bg_=bg_: e.tensor_tensor(out=sgate[:, oc, :], in0=ps[bg_][:, :], in1=T3, op=ALU.mult),
                          reads=[d_ps[bg_], d_T3], writes=[d_sg[oc]])
                if kind == "gla":
                    bgl = rot_bank(); proj_fm(wv, dw, 512, tt, bgl, M=16)
                    P.act(lambda e, bgl=bgl: e.activation(out=T5[0:16, :], in_=ps[bgl][0:16, :], func=AF.Copy), reads=[d_ps[bgl]], writes=[d_T5])
                    bz = rot_bank()
                    P.pe(lambda e, bz=bz: e.matmul(ps[bz][:, :], lhsT=wg_s[0:16, l, :], rhs=T5[0:16, :], start=True, stop=True),
                         reads=[d_T5, d_par], writes=[d_ps[bz]])
                    P.act(lambda e, bz=bz: e.activation(out=T1, in_=ps[bz][:, :], func=AF.Exp, scale=-1.0, bias=nbg_s[:, l:l + 1]),
                          reads=[d_ps[bz], d_setup], writes=[d_T1])
                    P.act(lambda e: e.activation(out=T2, in_=T1, func=AF.Ln, bias=1.0, scale=1.0), reads=[d_T1], writes=[d_T2])
                    src_l, d_src = T2, d_T2
                else:
                    bf_ = rot_bank(); proj_fm(wv, dw, fcol, tt, bf_)
                    P.act(lambda e, bf_=bf_: e.activation(out=T1, in_=ps[bf_][:, :], func=AF.Exp, scale=-1.0), reads=[d_ps[bf_]], writes=[d_T1])
                    P.act(lambda e: e.activation(out=T2, in_=T1, func=AF.Ln, bias=1.0, scale=1.0), reads=[d_T1], writes=[d_T2])
                    P.act(lambda e: e.activation(out=T3, in_=T1, func=AF.Ln, bias=1.0, scale=lb_s[:, g, l:l + 1]), reads=[d_T1, d_setup], writes=[d_T3])
                    P.dve(lambda e, bf_=bf_: e.tensor_tensor(out=T6, in0=ps[bf_][:, :], in1=T2, op=ALU.add), reads=[d_ps[bf_], d_T2], writes=[d_T6])
                    P.act(lambda e: e.activation(out=T6, in_=T6, func=AF.Exp, scale=-1.0), reads=[d_T6], writes=[d_T6])
                    P.dve(lambda e: e.tensor_tensor(out=T3, in0=T3, in1=T2, op=ALU.subtract), reads=[d_T3, d_T2], writes=[d_T3])
                    src_l, d_src = T3, d_T3
                P.dve(lambda e, src_l=src_l: e.tensor_tensor_scan(out=T4, data0=m32[:], data1=src_l, initial=0.0, op0=ALU.mult, op1=ALU.add),
                      reads=[d_src, d_const], writes=[d_T4])
                b3 = T4.rearrange("p (n k) -> p n k", k=CH)
                b4 = T4.rearrange("p (a n k) -> p a n k", a=2, k=CH)
                P.dve(lambda e, b3=b3: e.tensor_tensor(out=T5.rearrange("p (n k) -> p n k", k=CH), in0=b3,
                                                       in1=b3[:, :, CH // 2:CH // 2 + 1].to_broadcast([128, 16, CH]), op=ALU.subtract),
                      reads=[d_T4, d_T5], writes=[d_T5])
                P.act(lambda e: e.activation(out=T1, in_=T5, func=AF.Exp, scale=sc), reads=[d_T5, d_T1], writes=[d_T1])
                P.act(lambda e: e.activation(out=T2, in_=T5, func=AF.Exp, scale=-sc), reads=[d_T5, d_T2], writes=[d_T2])
                bmid, blast = b3[:, :, CH // 2], b3[:, :, CH - 1]
                P.act(lambda e, bmid=bmid: e.activation(out=sm[:, 0, :], in_=bmid, func=AF.Exp, scale=sc), reads=[d_T4], writes=[d_sm])
                P.act(lambda e, b4=b4: e.activation(out=dece[:, :, 1:9], in_=b4[:, :, :, CH - 1], func=AF.Exp, scale=sc), reads=[d_T4], writes=[d_dece])
                P.dve(lambda e, bmid=bmid, blast=blast: e.tensor_tensor(out=sm[:, 2, :], in0=blast, in1=bmid, op=ALU.subtract), reads=[d_T4, d_sm], writes=[d_sm])
                P.act(lambda e: e.activation(out=sm[:, 1, :], in_=sm[:, 2, :], func=AF.Exp, scale=sc), reads=[d_sm], writes=[d_sm])
                P.dve(lambda e: e.tensor_tensor(out=emh[:, 0:nh, :], in0=bc_mid(sm[:, 0, :], nh), in1=bc_last(hmask[:, 0:nh], 16), op=ALU.mult),
                      reads=[d_sm, d_const], writes=[d_emh])
                bq = rot_bank(); proj_fm(wv, dw, qc, tt, bq)
                if kind == "gla":
                    P.dve(lambda e, bq=bq: e.scalar_tensor_tensor(out=qt, in0=ps[bq][:, :], scalar=float(32 ** -0.5), in1=T1, op0=ALU.mult, op1=ALU.mult),
                          reads=[d_ps[bq], d_T1], writes=[d_qt])
                    bk_ = rot_bank(); proj_fm(wv, dw, kc, tt, bk_)
                    P.dve(lambda e, bk_=bk_: e.tensor_tensor(out=kt, in0=ps[bk_][:, :], in1=T2, op=ALU.mult), reads=[d_ps[bk_], d_T2], writes=[d_kt])
                else:
                    P.dve(lambda e, bq=bq: e.tensor_tensor(out=qt, in0=ps[bq][:, :], in1=T1, op=ALU.mult), reads=[d_ps[bq], d_T1], writes=[d_qt])
                    P.dve(lambda e: e.scalar_tensor_tensor(out=kt, in0=T6, scalar=oml_s[:, g, l:l + 1], in1=T2, op0=ALU.mult, op1=ALU.mult),
                          reads=[d_T6, d_T2, d_setup], writes=[d_kt])
                for hh in range(nh):
                    P.dve(lambda e, hh=hh: e.tensor_scalar(out=ktm[:, hh, :], in0=kt, scalar1=hmask[:, hh:hh + 1], scalar2=None, op0=ALU.mult),
                          reads=[d_kt, d_const], writes=[d_ktm])
                for sub in range(4):
                    P.pe(lambda e, sub=sub: e.transpose(psT[:, sub * 128:(sub + 1) * 128], kt[:, sub * 128:(sub + 1) * 128], ident_bf[:]),
                         reads=[d_kt, d_const], writes=[d_psT])
                P.act(lambda e: e.activation(out=ktok.rearrange("p a t -> p (a t)"), in_=psT[:, 0:512], func=AF.Copy), reads=[d_psT], writes=[d_ktok])
                for half in range(2):
                    bv = rot_bank()
                    for s2 in range(2):
                        proj_tok(wv, dw, vc, nh * 64, tt, half * 2 + s2, bv, s2 * 256)
                    P.act(lambda e, bv=bv, half=half: e.activation(out=V[:, half * 2:half * 2 + 2, :],
                                                                   in_=ps[bv][:, :].rearrange("p (s c) -> p s c", s=2)[:, :, 0:nh * 64], func=AF.Copy),
                          reads=[d_ps[bv]], writes=[d_V])
                    for s2 in range(2):
                        for n_ in range(4):
                            P.pool(lambda e, s2=s2, n_=n_, half=half: e.tensor_scalar(
                                out=Vbd[:, s2, :, n_, :], in0=V[:, half * 2 + s2, :].rearrange("p (h v) -> p h v", h=nh),
                                scalar1=cmask[:, n_:n_ + 1], scalar2=1.0, op0=ALU.mult, op1=ALU.mult),
                                reads=[d_V, d_const], writes=[d_Vbd])
                    for s2 in range(2):
                        sub = half * 2 + s2
                        for hh in range(nh):
                            kw = {"tile_position": (0, 96)} if hh * dk == 96 else {}
                            P.pe(lambda e, s2=s2, sub=sub, hh=hh, kw=kw: e.matmul(
                                ps[BU][hh * dk:(hh + 1) * dk, s2 * 256:(s2 + 1) * 256], lhsT=ktok[:, sub, hh * dk:(hh + 1) * dk],
                                rhs=Vbd[:, s2, hh, :, :].rearrange("p n v -> p (n v)"), start=True, stop=True, **kw),
                                reads=[d_ktok, d_Vbd], writes=[d_ps[BU]])
                    n0 = half * 8
                    P.dve(lambda e, n0=n0: e.tensor_tensor(out=cUe[:, :, 1:9], in0=ps[BU][:, :].rearrange("p (n v) -> p v n", v=64),
                                                           in1=bc_mid(sm[:, 1, n0:n0 + 8], 64), op=ALU.mult),
                          reads=[d_ps[BU], d_sm], writes=[d_cUe])
                    P.dve(lambda e: e.tensor_copy(out=cUe[:, :, 0:1], in_=ext[:, :, 8:9]), reads=[d_ext, d_cUe], writes=[d_cUe])
                    P.dve(lambda e, half=half: e.tensor_copy(out=decf, in_=bc_mid(dece[:, half, :], 64)), reads=[d_dece], writes=[d_decf])
                    P.dve(lambda e: e.tensor_tensor_scan(out=ext.rearrange("p v n -> p (v n)"), data0=decf.rearrange("p v n -> p (v n)"),
                                                         data1=cUe.rearrange("p v n -> p (v n)"), initial=0.0, op0=ALU.mult, op1=ALU.add),
                          reads=[d_cUe, d_decf], writes=[d_ext])
                    for hh in range(nh):
                        P.dve(lambda e, hh=hh, n0=n0: e.tensor_tensor(out=Sbd[:, :, hh * 64:(hh + 1) * 64], in0=ext[:, :, 0:8].rearrange("p v n -> p n v"),
                                                                      in1=bc_last(emh[:, hh, n0:n0 + 8], 64), op=ALU.mult),
                              reads=[d_ext, d_emh], writes=[d_Sbd])
                    for s2 in range(2):
                        sub = half * 2 + s2
                        ssl = slice(sub * 128, (sub + 1) * 128)
                        for hh in range(nh):
                            P.pe(lambda e, hh=hh, ssl=ssl: e.matmul(ps[BA][:, hh * 128:(hh + 1) * 128], lhsT=ktm[:, hh, ssl], rhs=qt[:, ssl], start=True, stop=True),
                                 reads=[d_ktm, d_qt], writes=[d_ps[BA]])
                        a_ = ai % 2
                        ai += 1
                        P.dve(lambda e, a_=a_: e.tensor_tensor(out=A[a_], in0=ps[BA][:, 0:nh * 128].rearrange("p (h i) -> p h i", h=nh),
                                                               in1=bc_mid(maskBD[:], nh), op=ALU.mult),
                              reads=[d_ps[BA], d_const], writes=[d_A[a_]])
                        for hh in range(nh):
                            oc, hl = hh // 2, hh % 2
                            P.pe(lambda e, a_=a_, hh=hh, oc=oc, hl=hl, sub=sub, ssl=ssl: e.matmul(
                                ps[BO[oc]][64 * hl:64 * hl + 64, ssl], lhsT=V[:, sub, hh * 64:(hh + 1) * 64], rhs=A[a_][:, hh, :], start=True, stop=False),
                                reads=[d_V, d_A[a_]], writes=[d_ps[BO[oc]]])
                        for n_ in range(4):
                            nn = s2 * 4 + n_
                            csl = slice(sub * 128 + n_ * 32, sub * 128 + n_ * 32 + 32)
                            for oc in range(noc):
                                P.pe(lambda e, nn=nn, oc=oc, csl=csl, n_=n_: e.matmul(ps[BO[oc]][:, csl], lhsT=Sbd[:, nn, oc * 128:(oc + 1) * 128], rhs=qt[:, csl],
                                                                                  start=False, stop=(n_ == 3)),
                                     reads=[d_Sbd, d_qt], writes=[d_ps[BO[oc]]])
                for oc in range(noc):
                    P.act(lambda e, oc=oc: e.activation(out=osq, in_=ps[BO[oc]][:, :], func=AF.Square), reads=[d_ps[BO[oc]]], writes=[d_osq])
                    bs_ = rot_bank()
                    P.pe(lambda e, bs_=bs_: e.matmul(ps[bs_][:, :], lhsT=blk_bf[:], rhs=osq, start=True, stop=True), reads=[d_osq, d_const], writes=[d_ps[bs_]])
                    P.act(lambda e, bs_=bs_: e.activation(out=T1, in_=ps[bs_][:, :], func=AF.Ln, scale=1.0 / HD, bias=EPS), reads=[d_ps[bs_], d_T1], writes=[d_T1])
                    P.act(lambda e: e.activation(out=T2, in_=T1, func=AF.Exp, scale=-0.5), reads=[d_T1, d_T2], writes=[d_T2])
                    P.dve(lambda e, oc=oc: e.tensor_tensor(out=T3, in0=ps[BO[oc]][:, :], in1=T2, op=ALU.mult), reads=[d_ps[BO[oc]], d_T2, d_T3], writes=[d_T3])
                    P.dve(lambda e, oc=oc, yi=yi: e.scalar_tensor_tensor(out=y[yi][:, oc, :], in0=T3, scalar=ng[:, l:l + 1], in1=sgate[:, oc, :],
                                                                         op0=ALU.mult, op1=ALU.mult),
                          reads=[d_T3, d_sg[oc], d_par], writes=[d_y[yi]])
                out_proj(l, b, wo, dwo, y[yi], d_y[yi], tt, nk=noc)

        def ffn_run(l, b, segs, st_):
            hid, d_hid, sa, d_sa, tq, d_tq = st_["hid"], st_["d_hid"], st_["sa"], st_["d_sa"], st_["tq"], st_["d_tq"]
            slabs = []
            for gi, (w1d, w3d, w2d, dff, cf) in enumerate(segs):
                for si in range((dff + SLAB - 1) // SLAB):
                    slabs.append((gi, si, w1d, w3d, w2d, dff))
            loaded = {}

            def issue(j):
                gi, si, w1d, w3d, w2d, dff = slabs[j]
                f0 = si * SLAB
                sw = min(SLAB, dff - f0)
                s_ = ring_next()
                w1v = ring[s_][:, 0:1024].bitcast(BF16).rearrange("p (c n) -> p c n", c=DC)[:, :, 0:sw]
                w3v = ring[s_][:, 1024:2048].bitcast(BF16).rearrange("p (c n) -> p c n", c=DC)[:, :, 0:sw]
                w2v = ring[s_][:, 2048:3072].bitcast(BF16).rearrange("p (c n) -> p c n", c=2)[:, 0:sw // 128, :]
                P.dma("pool", lambda e: e.dma_start(out=w1v, in_=w1d[:, f0:f0 + sw].rearrange("(c p) n -> p c n", p=128)), writes=[d_ring[s_][0]])
                P.dma("pool", lambda e: e.dma_start(out=w3v, in_=w3d[:, f0:f0 + sw].rearrange("(c p) n -> p c n", p=128)), writes=[d_ring[s_][1]])
                P.dma("pool", lambda e: e.dma_start(out=w2v, in_=w2d[f0:f0 + sw, :].rearrange("(c p) n -> p c n", p=128)), writes=[d_ring[s_][2]])
                loaded[j] = (s_, sw, w1v, w3v, w2v)

            def stage_a(j, tt, comb):
                s_, sw, w1v, w3v, w2v = loaded[j]
                nfc = sw // 128
                tsl = slice(tt * TT, (tt + 1) * TT)
                hi = st_["k"] % 2
                st_["k"] += 1
                for fc in range(nfc):
                    pa = (st_["p"] % 2) * 2
                    st_["p"] += 1
                    for (wv_, dr, bk) in ((w1v, d_ring[s_][0], pa), (w3v, d_ring[s_][1], pa + 1)):
                        for c in range(DC):
                            P.pe(lambda e, c=c: e.matmul(ps[bk][:, :], lhsT=wv_[:, c, fc * 128:(fc + 1) * 128], rhs=h[:, c, tsl],
                                                         start=(c == 0), stop=(c == DC - 1)),
                                 reads=[dr, d_h[c][tt]], writes=[d_ps[bk]])
                        if bk == pa:
                            yield None
                    qi = st_["q"] % 3
                    st_["q"] += 1
                    P.act(lambda e: e.activation(out=sa[qi], in_=ps[pa][:, :], func=AF.Silu), reads=[d_ps[pa]], writes=[d_sa[qi]])
                    if comb is not None:
                        cb_, d_cb = comb
                        P.pool(lambda e: e.tensor_tensor(out=tq[qi], in0=sa[qi], in1=cb_[:, tsl], op=ALU.mult), reads=[d_sa[qi], d_cb], writes=[d_tq[qi]])
                        src_, dsrc = tq[qi], d_tq[qi]
                    else:
                        src_, dsrc = sa[qi], d_sa[qi]
                    P.dve(lambda e: e.tensor_tensor(out=hid[hi][:, fc, :], in0=ps[pa + 1][:, :], in1=src_, op=ALU.mult),
                          reads=[d_ps[pa + 1], dsrc], writes=[d_hid[hi][fc]])
                    yield None
                st_["last"] = (j, tt, hi)

            def stage_b(tok_):
                j, tt, hi = tok_
                s_, sw, w1v, w3v, w2v = loaded[j]
                nfc = sw // 128
                tsl = slice(tt * TT, (tt + 1) * TT)
                for co in range(DC):
                    bk = 4 + st_["o"] % 3
                    st_["o"] += 1
                    for fc in range(nfc):
                        P.pe(lambda e: e.matmul(ps[bk][:, :], lhsT=w2v[:, fc, co * 128:(co + 1) * 128], rhs=hid[hi][:, fc, :],
                                                start=(fc == 0), stop=(fc == nfc - 1)),
                             reads=[d_ring[s_][2], d_hid[hi][fc]], writes=[d_ps[bk]])
                    P.dve(lambda e: e.scalar_tensor_tensor(out=x[:, co, tsl], in0=ps[bk][:, :], scalar=modv[:, l, 40 + co, b:b + 1],
                                                           in1=x[:, co, tsl], op0=ALU.mult, op1=ALU.add),
                          reads=[d_ps[bk], d_x[co][tt], d_setup], writes=[d_x[co][tt]])
                    yield None

            def run_interleaved(ga, gb):
                a_live, b_live = ga is not None, gb is not None
                while a_live or b_live:
                    if a_live:
                        a_live = next(ga, "end") != "end"
                    for _ in range(2):
                        if b_live:
                            b_live = next(gb, "end") != "end"

            issue(0)
            if len(slabs) > 1:
                issue(1)
            pend = None
            comb = None
            for j, (gi, si, _, _, _, _) in enumerate(slabs):
                if si == 0:
                    cf = segs[gi][4]
                    comb = cf() if cf is not None else None
                for tt in range(NTT):
                    run_interleaved(stage_a(j, tt, comb), stage_b(pend) if pend is not None else None)
                    cur = st_["last"]
                    if tt == 0 and j >= 1 and j + 1 < len(slabs):
                        issue(j + 1)
                    pend = cur
            run_interleaved(None, stage_b(pend))

        def ffn_state():
            st_ = dict(k=0, p=0, q=0, o=0)
            st_["hid"] = [ua.alloc([SLAB // 128, TT], BF16) for _ in range(2)]
            st_["d_hid"] = [[newdep() for _ in range(4)] for _ in range(2)]
            st_["sa"] = [ua.alloc([TT]) for _ in range(3)]
            st_["d_sa"] = [newdep() for _ in range(3)]
            st_["tq"] = [ua.alloc([TT]) for _ in range(3)]
            st_["d_tq"] = [newdep() for _ in range(3)]
            return st_

        def dense_ffn(l, b):
            phase_switch()
            st_ = ffn_state()
            idx = l // 2
            ffn_run(l, b, [(ffn_w1[idx], ffn_w3[idx], ffn_w2[idx], DFF, None)], st_)

        def moe_ffn(l, b):
            phase_switch()
            idx = l // 2
            st_ = ffn_state()
            NS = T // 128
            lg = ua.alloc([NS, NE]); d_lg = newdep()
            top = ua.alloc([NS, 8]); d_top = newdep()
            wts = ua.alloc([4, NS]); d_w = newdep()
            cmb = ua.alloc([NS, NE]); d_cmb = newdep()
            cm2 = ua.alloc([NS, NE]); d_cm2 = newdep()
            combT = ua.alloc([T]); d_cT = newdep()
            cbc = [ua.alloc([T]) for _ in range(2)]; d_cbc = [newdep(), newdep()]
            for s in range(NS):
                tt = (s * 128) // TT
                for c in range(DC):
                    P.pe(lambda e, s=s, c=c: e.matmul(ps[0][:, s * NE:(s + 1) * NE], lhsT=h[:, c, s * 128:(s + 1) * 128], rhs=wr_s[:, idx, c, :],
                                                      start=(c == 0), stop=(c == DC - 1)),
                         reads=[d_h[c][tt], d_par], writes=[d_ps[0]])
            P.dve(lambda e: e.tensor_copy(out=lg.rearrange("p s e -> p (s e)"), in_=ps[0][:, 0:NS * NE]), reads=[d_ps[0]], writes=[d_lg])
            for s in range(NS):
                P.dve(lambda e, s=s: e.max(out=top[:, s, :], in_=lg[:, s, :]), reads=[d_lg], writes=[d_top])
            m1 = top[:, :, 0]
            m2 = top[:, :, 1]
            P.dve(lambda e: e.tensor_tensor(out=wts[:, 0, :], in0=m2, in1=m1, op=ALU.subtract), reads=[d_top], writes=[d_w])
            P.act(lambda e: e.activation(out=wts[:, 0, :], in_=wts[:, 0, :], func=AF.Exp), reads=[d_w], writes=[d_w])
            P.dve(lambda e: e.tensor_scalar(out=wts[:, 1, :], in0=wts[:, 0, :], scalar1=1.0, scalar2=None, op0=ALU.add), reads=[d_w], writes=[d_w])
            P.dve(lambda e: e.reciprocal(out=wts[:, 1, :], in_=wts[:, 1, :]), reads=[d_w], writes=[d_w])
            P.dve(lambda e: e.tensor_tensor(out=wts[:, 2, :], in0=wts[:, 0, :], in1=wts[:, 1, :], op=ALU.mult), reads=[d_w], writes=[d_w])
            P.dve(lambda e: e.tensor_tensor(out=cmb, in0=lg, in1=bc_last(m1, NE), op=ALU.is_equal), reads=[d_lg, d_top], writes=[d_cmb])
            P.dve(lambda e: e.tensor_tensor(out=cmb, in0=cmb, in1=bc_last(wts[:, 1, :], NE), op=ALU.mult), reads=[d_cmb, d_w], writes=[d_cmb])
            P.dve(lambda e: e.tensor_tensor(out=cm2, in0=lg, in1=bc_last(m2, NE), op=ALU.is_equal), reads=[d_lg, d_top], writes=[d_cm2])
            P.dve(lambda e: e.tensor_tensor(out=cm2, in0=cm2, in1=bc_last(wts[:, 2, :], NE), op=ALU.mult), reads=[d_cm2, d_w], writes=[d_cm2])
            P.dve(lambda e: e.tensor_tensor(out=cmb, in0=cmb, in1=cm2, op=ALU.add), reads=[d_cmb, d_cm2], writes=[d_cmb])
            for s in range(NS):
                bk = 1 + (s // 4) % 2
                P.pe(lambda e, s=s, bk=bk: e.transpose(ps[bk][0:NE, (s % 4) * 128:(s % 4 + 1) * 128], cmb[:, s, :], ident_f[:]),
                     reads=[d_cmb, d_const], writes=[d_ps[bk]])
                if s % 4 == 3:
                    P.act(lambda e, s=s, bk=bk: e.activation(out=combT[0:NE, (s - 3) * 128:(s + 1) * 128], in_=ps[bk][0:NE, :], func=AF.Copy),
                          reads=[d_ps[bk]], writes=[d_cT])
            def make_comb(ex):
                def cf():
                    ci = ex % 2
                    for tt in range(NTT):
                        bk = 1 + tt % 2
                        P.pe(lambda e: e.matmul(ps[bk][:, :], lhsT=sel[0:NE, ex, :], rhs=combT[0:NE, tt * TT:(tt + 1) * TT], start=True, stop=True),
                             reads=[d_cT, d_const], writes=[d_ps[bk]])
                        P.act(lambda e: e.activation(out=cbc[ci][:, tt * TT:(tt + 1) * TT], in_=ps[bk][:, :], func=AF.Copy),
                              reads=[d_ps[bk]], writes=[d_cbc[ci]])
                    return (cbc[ci], d_cbc[ci])
                return cf
            ffn_run(l, b, [(moe_w1[idx, ex], moe_w3[idx, ex], moe_w2[idx, ex], DFE, make_comb(ex)) for ex in range(NE)], st_)

        for b in range(NB):
            for c in range(DC):
                for tt in range(NTT):
                    P.dma("sp", lambda e, b=b, c=c, tt=tt: e.dma_start(out=x[:, c, tt * TT:(tt + 1) * TT], in_=xT[b, c * 128:(c + 1) * 128, tt * TT:(tt + 1) * TT]),
                          writes=[d_x[c][tt]])
            for l in range(L):
                if not diag_skip_mixers:
                    rmsnorm_to_h(l, b, gsm, 0)
                    conv_pass(l, b)
                    sgu_pass(l, b)
                    recur_pass(l, b, "gla")
                    recur_pass(l, b, "hgrn", 0)
                    recur_pass(l, b, "hgrn", 1)
                rmsnorm_to_h(l, b, gsf, 24)
                if l % 2 == 0:
                    dense_ffn(l, b)
                else:
                    moe_ffn(l, b)
            phase_switch()
            sq = [ua.alloc([TT], BF16) for _ in range(3)]; d_sq = [newdep() for _ in range(3)]
            lnv = ua.alloc([TT]); rstd = ua.alloc([TT]); d_ln = newdep(); d_rs = newdep()
            k = 0
            for tt in range(NTT):
                tsl = slice(tt * TT, (tt + 1) * TT)
                bk = rot_bank()
                for c in range(DC):
                    i = k % 3
                    k += 1
                    P.act(lambda e, c=c, i=i, tsl=tsl: e.activation(out=sq[i], in_=x[:, c, tsl], func=AF.Square), reads=[d_x[c][tt]], writes=[d_sq[i]])
                    P.pe(lambda e, c=c, i=i, bk=bk: e.matmul(ps[bk][:, :], lhsT=ones_bf[:], rhs=sq[i], start=(c == 0), stop=(c == DC - 1)),
                         reads=[d_sq[i], d_const], writes=[d_ps[bk]])
                P.act(lambda e, bk=bk: e.activation(out=lnv, in_=ps[bk][:, :], func=AF.Ln, scale=1.0 / D, bias=EPS), reads=[d_ps[bk]], writes=[d_ln])
                P.act(lambda e: e.activation(out=rstd, in_=lnv, func=AF.Exp, scale=-0.5), reads=[d_ln], writes=[d_rs])
                for c in range(DC):
                    P.dve(lambda e, c=c, tsl=tsl: e.scalar_tensor_tensor(out=x[:, c, tsl], in0=x[:, c, tsl], scalar=gfin_s[:, c:c + 1], in1=rstd,
                                                                         op0=ALU.mult, op1=ALU.mult),
                          reads=[d_x[c][tt], d_rs, d_par], writes=[d_x[c][tt]])
                    o = P.dma("sp", lambda e, b=b, c=c, tsl=tsl: e.dma_start(out=outT[b, c * 128:(c + 1) * 128, tsl], in_=x[:, c, tsl]),
                              reads=[d_x[c][tt]], writes=[Dep()], dep=d_x[c][tt])
                    P.final_waits.append(o)
        P.emit()
    return nc


def _fm(v):
    v = np.asarray(v, np.float32)
    lead = v.shape[:-1]
    n = v.shape[-1] // 128
    v = v.reshape(lead + (n, 128))
    return np.ascontiguousarray(np.moveaxis(v, -1, 0))


_PROG_CACHE = {}


def kernel(x, c, norm_mix_g, norm_ffn_g, final_norm_g, w_ada, b_ada, w_in, w_out, conv_w,
           sgu_norm_g, sgu_w, sgu_b, gla_w_gate, gla_b_gate, gla_norm_g, hgrn_lower_bounds,
           hgrn_norm_g, ffn_w1, ffn_w3, ffn_w2, moe_router, moe_w1, moe_w3, moe_w2, n_cores=8):
    f = lambda a: np.ascontiguousarray(np.asarray(a, dtype=np.float32))
    x = f(x)
    B, T, _ = x.shape
    L = w_in.shape[0]
    n_moe = L // 2
    NB = B // n_cores
    key = (NB, T, L, n_moe)
    if key not in _PROG_CACHE:
        _PROG_CACHE[key] = build_program(NB, T, L, n_moe)
    nc = _PROG_CACHE[key]
    c = f(c)
    shared = {
        "g_mix": _fm(norm_mix_g), "g_ffn": _fm(norm_ffn_g), "g_fin": _fm(final_norm_g),
        "w_ada": f(w_ada), "b_ada": _fm(b_ada), "w_in": f(w_in), "w_out": f(w_out),
        "conv_w": _fm(conv_w), "sgu_g": _fm(sgu_norm_g),
        "sgu_wT": np.ascontiguousarray(f(sgu_w).transpose(3, 0, 1, 2)),
        "sgu_bf": np.ascontiguousarray(np.repeat(f(sgu_b).reshape(L, 2, 2, 1, 128), 64, axis=3).reshape(L, 2, 128, 128).transpose(2, 0, 1, 3)),
        "gla_wg": np.ascontiguousarray(f(gla_w_gate).transpose(1, 0, 2)),
        "gla_bg": np.ascontiguousarray(f(gla_b_gate).T),
        "gla_ng": np.ascontiguousarray(np.tile(f(gla_norm_g), (1, 2)).T),
        "hg_lb": np.ascontiguousarray(f(hgrn_lower_bounds).reshape(L, 2, 128).transpose(2, 1, 0)),
        "hg_ng": np.ascontiguousarray(np.tile(f(hgrn_norm_g), (1, 2)).T),
        "ffn_w1": f(ffn_w1), "ffn_w3": f(ffn_w3), "ffn_w2": f(ffn_w2),
        "moe_r": f(moe_router), "moe_w1": f(moe_w1), "moe_w3": f(moe_w3), "moe_w2": f(moe_w2),
    }
    in_maps = []
    for i in range(n_cores):
        xb = x[i * NB:(i + 1) * NB]
        m = dict(shared)
        m["xT"] = np.ascontiguousarray(xb.transpose(0, 2, 1))
        m["cT"] = np.ascontiguousarray(c[i * NB:(i + 1) * NB].T.reshape(DC, 128, NB).transpose(1, 0, 2))
        in_maps.append(m)
    res = run_bass_kernel_spmd(nc, in_maps, core_ids=list(range(n_cores)))
    out = np.empty((B, T, D), np.float32)
    for i in range(n_cores):
        out[i * NB:(i + 1) * NB] = res.results[i]["outT"].transpose(0, 2, 1)
    return out
```

```python
import contextlib
import types
import numpy as np
import concourse.bass as bass
import concourse.mybir as mybir
from concourse.bass_utils import run_bass_kernel_spmd

F32 = mybir.dt.float32
BF16 = mybir.dt.bfloat16
AF = mybir.ActivationFunctionType
ALU = mybir.AluOpType
AX = mybir.AxisListType

D = 1024
DC = 8
GW = 256
HD = 64
NE = 8
DFF = 2816
DFE = 3584
INW = 3088
EPS = 1e-6
TT = 512
CH = 32
SLAB = 256


class Dep:
    __slots__ = ("name", "last_writer", "readers", "dma_sem", "dma_count")

    def __init__(self, name="", after=None):
        self.name = name
        self.last_writer = after
        self.readers = []
        self.dma_sem = None
        self.dma_count = 0


class Op:
    __slots__ = ("eng", "fn", "deps", "signaled", "is_dma", "dep_obj", "sem_val")

    def __init__(self, eng, fn, is_dma=False):
        self.eng = eng
        self.fn = fn
        self.deps = []
        self.signaled = False
        self.is_dma = is_dma
        self.dep_obj = None
        self.sem_val = None


ENGS = ("pe", "act", "dve", "pool", "sp")


def _freeze(fn):
    if fn.__closure__ is None:
        return fn
    cells = []
    for c in fn.__closure__:
        try:
            cells.append(types.CellType(c.cell_contents))
        except ValueError:
            cells.append(c)
    g = types.FunctionType(fn.__code__, fn.__globals__, fn.__name__, fn.__defaults__, tuple(cells))
    g.__kwdefaults__ = fn.__kwdefaults__
    return g


class Prog:
    def __init__(self, nc):
        self.nc = nc
        self.ops = {e: [] for e in ENGS}
        self.n_dma_sems = 0
        self.final_waits = []

    def _collect(self, op, reads, writes, same_engine_sync=True):
        deps = []
        for d in reads:
            w = d.last_writer
            if w is not None:
                deps.append(w)
        for d in writes:
            w = d.last_writer
            if w is not None:
                if not (op.is_dma and w.is_dma):
                    deps.append(w)
            deps.extend(d.readers)
        out = []
        seen = set()
        for w in deps:
            if w is op or id(w) in seen:
                continue
            if (not w.is_dma) and w.eng == op.eng and not same_engine_sync:
                continue
            seen.add(id(w))
            out.append(w)
        op.deps = out
        for w in out:
            w.signaled = True
        for d in writes:
            d.last_writer = op
            d.readers = []
        for d in reads:
            if not op.is_dma and d.readers:
                d.readers = [r for r in d.readers if r.is_dma or r.eng != op.eng]
            d.readers.append(op)

    def op(self, eng, fn, reads=(), writes=()):
        o = Op(eng, _freeze(fn))
        self._collect(o, reads, writes, same_engine_sync=(eng != "pe"))
        self.ops[eng].append(o)
        return o

    def dma(self, queue, fn, reads=(), writes=(), dep=None):
        o = Op(queue, _freeze(fn), is_dma=True)
        d = dep if dep is not None else writes[0]
        if d.dma_sem is None:
            d.dma_sem = self.n_dma_sems
            self.n_dma_sems += 1
        d.dma_count += 1
        o.dep_obj = d
        o.sem_val = 16 * d.dma_count
        o.signaled = True
        self._collect(o, reads, writes)
        self.ops[queue].append(o)
        return o

    def pe(self, fn, reads=(), writes=()):
        return self.op("pe", fn, reads, writes)

    def act(self, fn, reads=(), writes=()):
        return self.op("act", fn, reads, writes)

    def dve(self, fn, reads=(), writes=()):
        return self.op("dve", fn, reads, writes)

    def pool(self, fn, reads=(), writes=()):
        return self.op("pool", fn, reads, writes)

    def barrier(self, deps):
        return self.op("dve", lambda e: e.memset(self.scratch, 0.0), reads=(), writes=list(deps) + [self.scratch_dep])

    def emit(self):
        nc = self.nc
        for e in ENGS:
            cnt = 0
            for o in self.ops[e]:
                if o.is_dma:
                    continue
                if o.signaled:
                    cnt += 1
                    o.sem_val = cnt
        with contextlib.ExitStack() as st:
            eng_sems = {e: st.enter_context(nc.semaphore("s_" + e)) for e in ENGS}
            dma_sems = [st.enter_context(nc.semaphore("d%d" % i)) for i in range(self.n_dma_sems)]
            block = st.enter_context(nc.Block())

            def tok(o):
                if o.is_dma:
                    return ("d", o.dep_obj.dma_sem), dma_sems[o.dep_obj.dma_sem], o.sem_val
                return ("e", o.eng), eng_sems[o.eng], o.sem_val

            def run(ename, engine):
                known = {}
                for o in self.ops[ename]:
                    need = {}
                    for w in o.deps:
                        k, s, v = tok(w)
                        if known.get(k, 0) >= v:
                            continue
                        if k not in need or need[k][1] < v:
                            need[k] = (s, v)
                    for k, (s, v) in need.items():
                        engine.wait_ge(s, v)
                        known[k] = v
                    ins = o.fn(engine)
                    if o.is_dma:
                        ins.then_inc(dma_sems[o.dep_obj.dma_sem], 16)
                    elif o.signaled:
                        ins.then_inc(eng_sems[ename], 1)
                if ename == "sp":
                    for w in self.final_waits:
                        k, s, v = tok(w)
                        engine.wait_ge(s, v)

            @block.tensor
            def _(t):
                run("pe", t)

            @block.scalar
            def _(t):
                run("act", t)

            @block.vector
            def _(t):
                run("dve", t)

            @block.gpsimd
            def _(t):
                run("pool", t)

            @block.sync
            def _(t):
                run("sp", t)


class Arena:
    def __init__(self, tensor, words):
        self.t = tensor
        self.words = words
        self.off = 0
        self.marks = []

    def alloc(self, shape, dtype=F32):
        n = int(np.prod(shape))
        w = n if dtype == F32 else (n + 1) // 2
        w = (w + 7) // 8 * 8
        assert self.off + w <= self.words, ("arena overflow", self.off, w, self.words)
        v = self.t[:, self.off:self.off + w]
        self.off += w
        if dtype != F32:
            v = v.bitcast(dtype)
        v = v[:, 0:n]
        if len(shape) == 2:
            v = v.rearrange("p (a b) -> p a b", a=shape[0])
        elif len(shape) == 3:
            v = v.rearrange("p (a b c) -> p a b c", a=shape[0], b=shape[1])
        elif len(shape) == 4:
            v = v.rearrange("p (a b c d) -> p a b c d", a=shape[0], b=shape[1], c=shape[2])
        return v

    def mark(self):
        return self.off

    def reset(self, m):
        self.off = m


def bc_last(ap, n):
    shp = list(ap.shape)
    return ap.unsqueeze(len(shp)).to_broadcast(shp + [n])


def bc_mid(ap, n):
    shp = list(ap.shape)
    return ap.unsqueeze(1).to_broadcast([shp[0], n] + shp[1:])


def build_program(NB, T, L, n_moe, diag_skip_mixers=False):
    NTT = T // TT
    n_dense = (L + 1) // 2
    nc = bass.Bass("TRN2", target_bir_lowering=False)

    def din(name, shape):
        return nc.dram_tensor(name, list(shape), F32, kind="ExternalInput").ap()

    xT = din("xT", [NB, D, T])
    cT = din("cT", [128, DC, NB])
    g_mix = din("g_mix", [128, L, DC])
    g_ffn = din("g_ffn", [128, L, DC])
    g_fin = din("g_fin", [128, DC])
    w_ada = din("w_ada", [L, D, 6 * D])
    b_ada = din("b_ada", [128, L, 48])
    w_in = din("w_in", [L, D, INW])
    w_out = din("w_out", [L, D, D])
    conv_w = din("conv_w", [128, L, 3, 2])
    sgu_g = din("sgu_g", [128, L, 2])
    sgu_wT = din("sgu_wT", [128, L, 4, 128])
    sgu_bf = din("sgu_bf", [128, L, 2, 128])
    gla_wg = din("gla_wg", [16, L, 128])
    gla_bg = din("gla_bg", [128, L])
    gla_ng = din("gla_ng", [128, L])
    hg_lb = din("hg_lb", [128, 2, L])
    hg_ng = din("hg_ng", [128, L])
    ffn_w1 = din("ffn_w1", [n_dense, D, DFF])
    ffn_w3 = din("ffn_w3", [n_dense, D, DFF])
    ffn_w2 = din("ffn_w2", [n_dense, DFF, D])
    moe_r = din("moe_r", [max(n_moe, 1), D, NE])
    moe_w1 = din("moe_w1", [max(n_moe, 1), NE, D, DFE])
    moe_w3 = din("moe_w3", [max(n_moe, 1), NE, D, DFE])
    moe_w2 = din("moe_w2", [max(n_moe, 1), NE, DFE, D])
    outT = nc.dram_tensor("outT", [NB, D, T], F32, kind="ExternalOutput").ap()

    P = Prog(nc)
    with contextlib.ExitStack() as st:
        def sb(name, shape, dt=F32):
            return st.enter_context(nc.sbuf_tensor(name, list(shape), dt))

        x = sb("x", [128, DC, T])
        h = sb("h", [128, DC, T], BF16)
        RINGW = 4352
        ring = [sb("ring%d" % i, [128, RINGW]) for i in range(2)]
        UW = 13696
        ureg = sb("ureg", [128, UW])
        ones_bf = sb("ones_bf", [128, 128], BF16)
        blk_bf = sb("blk_bf", [128, 128], BF16)
        ident_bf = sb("ident_bf", [128, 128], BF16)
        ident_f = sb("ident_f", [128, 128])
        ones_f = sb("ones_f", [128, 128])
        maskBD = sb("maskBD", [128, 128])
        m32 = sb("m32", [128, TT])
        hmask4 = sb("hmask4", [128, 4])
        hmask2 = sb("hmask2", [128, 2])
        cmask = sb("cmask", [128, 4])
        sel = sb("sel", [8, NE, 128])
        scratch = sb("scratch", [128, 8])
        P.scratch = scratch[:, 0:1]
        P.scratch_dep = Dep("scratch")
        cond = sb("cond", [128, DC, NB])
        gmix_s = sb("gmix_s", [128, L, DC])
        gffn_s = sb("gffn_s", [128, L, DC])
        gfin_s = sb("gfin_s", [128, DC])
        bada_s = sb("bada_s", [128, L, 48])
        modv = sb("modv", [128, L, 48, NB])
        gsm = sb("gsm", [128, L, DC, NB])
        gsf = sb("gsf", [128, L, DC, NB])
        convw_s = sb("convw_s", [128, L, 3, 2])
        sgug_s = sb("sgug_s", [128, L, 2])
        sguw_s = sb("sguw_s", [128, L, 4, 128], BF16)
        sgub_s = sb("sgub_s", [128, L, 2, 128])
        wg_s = sb("wg_s", [16, L, 128])
        nbg_s = sb("nbg_s", [128, L])
        glang_s = sb("glang_s", [128, L])
        hgng_s = sb("hgng_s", [128, L])
        lbe = sb("lbe", [128, 2, L])
        lb_s = sb("lb_s", [128, 2, L])
        oml_s = sb("oml_s", [128, 2, L])
        lbt = sb("lbt", [128, 2, 2])
        wr_s = sb("wr_s", [128, max(n_moe, 1), DC, NE], BF16)

        ps = [st.enter_context(nc.psum_tensor("ps%d" % i, [128, 512], F32)) for i in range(7)]
        psT = st.enter_context(nc.psum_tensor("psT", [128, 1024], BF16))
        d_ps = [Dep("ps%d" % i) for i in range(7)]
        d_psT = Dep("psT")

        d_x = [[Dep("x%d_%d" % (c, t)) for t in range(NTT)] for c in range(DC)]
        d_h = [[Dep("h%d_%d" % (c, t)) for t in range(NTT)] for c in range(DC)]
        d_ring = [[Dep("ring%d_%d" % (i, j)) for j in range(3)] for i in range(2)]
        d_const = Dep("const")
        d_par = Dep("par")
        d_setup = Dep("setup")

        C = [d_const]
        P.pool(lambda e: e.memset(ones_bf[:], 1.0), writes=C)
        P.pool(lambda e: e.memset(ones_f[:], 1.0), writes=C)
        P.pool(lambda e: e.memset(blk_bf[:], 0.0), writes=C)
        P.pool(lambda e: e.memset(blk_bf[0:64, 0:64], 1.0), reads=C, writes=C)
        P.pool(lambda e: e.memset(blk_bf[64:128, 64:128], 1.0), reads=C, writes=C)
        P.pool(lambda e: e.affine_select(out=ident_bf[:], in_=ones_bf[:], pattern=[[-1, 128]], compare_op=ALU.is_equal,
                                          fill=0.0, base=0, channel_multiplier=1), reads=C, writes=C)
        P.pool(lambda e: e.affine_select(out=ident_f[:], in_=ones_f[:], pattern=[[-1, 128]], compare_op=ALU.is_equal,
                                          fill=0.0, base=0, channel_multiplier=1), reads=C, writes=C)
        P.pool(lambda e: e.affine_select(out=maskBD[:], in_=ones_f[:], pattern=[[1, 128]], compare_op=ALU.is_ge,
                                          fill=0.0, base=0, channel_multiplier=-1), reads=C, writes=C)
        for bl in range(1, 4):
            P.pool(lambda e, bl=bl: e.affine_select(out=maskBD[:, 32 * bl:32 * bl + 32], in_=maskBD[:, 32 * bl:32 * bl + 32],
                                                    pattern=[[0, 32]], compare_op=ALU.is_ge, fill=0.0, base=-32 * bl,
                                                    channel_multiplier=1), reads=C, writes=C)
        P.pool(lambda e: e.memset(m32[:], 1.0), reads=C, writes=C)
        P.pool(lambda e: e.memset(m32[:].rearrange("p (n k) -> p n k", k=CH)[:, :, 0:1], 0.0), reads=C, writes=C)
        for (msk, nh, dk) in ((hmask4, 4, 32), (hmask2, 2, 64), (cmask, 4, 32)):
            P.pool(lambda e, msk=msk, nh=nh, dk=dk: e.affine_select(out=msk[:], in_=ones_f[:, 0:nh], pattern=[[-dk, nh]], compare_op=ALU.is_ge,
                                                                    fill=0.0, base=0, channel_multiplier=1), reads=C, writes=C)
            P.pool(lambda e, msk=msk, nh=nh, dk=dk: e.affine_select(out=msk[:], in_=msk[:], pattern=[[dk, nh]], compare_op=ALU.is_ge,
                                                                    fill=0.0, base=dk - 1, channel_multiplier=-1), reads=C, writes=C)
        P.pool(lambda e: e.affine_select(out=sel[:], in_=bc_mid(ones_f[0:8, :], NE), pattern=[[-1, NE], [0, 128]], compare_op=ALU.is_equal,
                                          fill=0.0, base=0, channel_multiplier=1), reads=C, writes=C)

        Wp = [d_par]
        for dst, src in ((cond, cT), (gmix_s, g_mix), (gffn_s, g_ffn), (gfin_s, g_fin), (bada_s, b_ada), (convw_s, conv_w),
                         (sgug_s, sgu_g), (sgub_s, sgu_bf), (wg_s, gla_wg), (nbg_s, gla_bg), (glang_s, gla_ng),
                         (hgng_s, hg_ng), (lbe, hg_lb)):
            P.dma("sp", lambda e, dst=dst, src=src: e.dma_start(out=dst[:], in_=src), writes=Wp)
        P.dma("pool", lambda e: e.dma_start(out=sguw_s[:], in_=sgu_wT), writes=Wp)
        if n_moe:
            for m in range(n_moe):
                P.dma("pool", lambda e, m=m: e.dma_start(out=wr_s[:, m], in_=moe_r[m].rearrange("(c p) e -> p c e", p=128)), writes=Wp)
        S = [d_setup]
        R_ = [d_par, d_const]
        for l in range(L):
            for hh in range(4):
                P.pool(lambda e, l=l, hh=hh: e.affine_select(out=sguw_s[:, l, hh, :], in_=sguw_s[:, l, hh, :], pattern=[[1, 128]],
                                                             compare_op=ALU.is_ge, fill=0.0, base=0, channel_multiplier=-1),
                       reads=R_, writes=S)
        P.act(lambda e: e.activation(out=cond[:], in_=cond[:], func=AF.Silu), reads=R_, writes=S)
        P.dve(lambda e: e.tensor_scalar(out=nbg_s[:], in0=nbg_s[:], scalar1=-1.0, scalar2=None, op0=ALU.mult), reads=R_, writes=S)
        P.act(lambda e: e.activation(out=lbe[:], in_=lbe[:], func=AF.Exp), reads=R_ + S, writes=S)
        P.dve(lambda e: e.tensor_reduce(out=lbt[:, :, 0:1], in_=lbe[:], axis=AX.X, op=ALU.add), reads=S, writes=S)
        P.dve(lambda e: e.reciprocal(out=lbt[:, :, 1:2], in_=lbt[:, :, 0:1]), reads=S, writes=S)
        P.dve(lambda e: e.tensor_tensor(out=lbe[:], in0=lbe[:], in1=lbt[:, :, 1:2].to_broadcast([128, 2, L]), op=ALU.mult), reads=S, writes=S)
        P.dve(lambda e: e.memset(lb_s[:, :, 0:1], 0.0), reads=S, writes=S)
        for l in range(1, L):
            P.dve(lambda e, l=l: e.tensor_tensor(out=lb_s[:, :, l:l + 1], in0=lb_s[:, :, l - 1:l], in1=lbe[:, :, l:l + 1], op=ALU.add), reads=S, writes=S)
        P.dve(lambda e: e.tensor_scalar(out=oml_s[:], in0=lb_s[:], scalar1=-1.0, scalar2=1.0, op0=ALU.mult, op1=ALU.add), reads=S, writes=S)

        ADW = 512
        rv = [ring[i][:, 0:DC * ADW].rearrange("p (c n) -> p c n", c=DC) for i in range(2)]
        step = 0
        d_ada = [Dep("ada0"), Dep("ada1")]
        for l in range(L):
            for pc in range(6 * D // ADW):
                s_ = step % 2
                step += 1
                P.dma("sp", lambda e, l=l, pc=pc, s_=s_: e.dma_start(out=rv[s_], in_=w_ada[l][:, pc * ADW:(pc + 1) * ADW].rearrange("(c p) n -> p c n", p=128)),
                      writes=[d_ada[s_]])
                for jj in range(ADW // 128):
                    j = pc * (ADW // 128) + jj
                    for c in range(DC):
                        P.pe(lambda e, s_=s_, jj=jj, j=j, c=c: e.matmul(ps[0][:, j * NB:(j + 1) * NB], lhsT=rv[s_][:, c, jj * 128:(jj + 1) * 128],
                                                                      rhs=cond[:, c, :], start=(c == 0), stop=(c == DC - 1)),
                             reads=[d_ada[s_], d_setup], writes=[d_ps[0]])
            P.dve(lambda e, l=l: e.tensor_tensor(out=modv[:, l], in0=ps[0][:, 0:48 * NB].rearrange("p (j b) -> p j b", b=NB),
                                                 in1=bc_last(bada_s[:, l, :], NB), op=ALU.add), reads=[d_ps[0], d_par], writes=S)
            for (gdst, gsrc, j0) in ((gsm, gmix_s, 8), (gsf, gffn_s, 32)):
                P.dve(lambda e, l=l, gdst=gdst, j0=j0: e.tensor_scalar(out=gdst[:, l], in0=modv[:, l, j0:j0 + 8, :], scalar1=1.0, scalar2=None, op0=ALU.add),
                      reads=S, writes=S)
                P.dve(lambda e, l=l, gdst=gdst, gsrc=gsrc: e.tensor_tensor(out=gdst[:, l], in0=gdst[:, l], in1=bc_last(gsrc[:, l, :], NB), op=ALU.mult),
                      reads=S + [d_par], writes=S)

        ada_done = P.barrier(d_ada)
        for i in range(2):
            for j in range(3):
                d_ring[i][j].last_writer = ada_done
        CONSTS = [d_const, d_par, d_setup]

        ua = Arena(ureg, UW)
        phase = {"deps": [], "after": None}

        def newdep(name=""):
            d_ = Dep(name, after=phase["after"])
            phase["deps"].append(d_)
            return d_

        def phase_switch():
            if phase["deps"]:
                phase["after"] = P.barrier(phase["deps"])
            phase["deps"] = []
            ua.reset(0)

        rot = {"i": 0}

        def rot_bank(n=3):
            i = rot["i"] % n
            rot["i"] += 1
            return i

        def rmsnorm_to_h(l, b, gs_t, sh_j0):
            phase_switch()
            sq = [ua.alloc([TT], BF16) for _ in range(3)]
            d_sq = [newdep() for _ in range(3)]
            lnv = ua.alloc([TT]); rstd = ua.alloc([TT])
            d_ln = newdep(); d_rs = newdep()
            tmp = [ua.alloc([TT]) for _ in range(3)]
            d_tmp = [newdep() for _ in range(3)]
            k = 0
            for tt in range(NTT):
                tsl = slice(tt * TT, (tt + 1) * TT)
                bk = rot_bank()
                for c in range(DC):
                    i = k % 3
                    k += 1
                    P.act(lambda e, c=c, i=i, tsl=tsl: e.activation(out=sq[i], in_=x[:, c, tsl], func=AF.Square),
                          reads=[d_x[c][tt]], writes=[d_sq[i]])
                    P.pe(lambda e, c=c, i=i, bk=bk: e.matmul(ps[bk][:, :], lhsT=ones_bf[:], rhs=sq[i], start=(c == 0), stop=(c == DC - 1)),
                         reads=[d_sq[i], d_const], writes=[d_ps[bk]])
                P.act(lambda e, bk=bk: e.activation(out=lnv, in_=ps[bk][:, :], func=AF.Ln, scale=1.0 / D, bias=EPS),
                      reads=[d_ps[bk]], writes=[d_ln])
                P.act(lambda e: e.activation(out=rstd, in_=lnv, func=AF.Exp, scale=-0.5), reads=[d_ln], writes=[d_rs])
                for c in range(DC):
                    i = k % 3
                    k += 1
                    P.dve(lambda e, c=c, i=i, tsl=tsl: e.tensor_tensor(out=tmp[i], in0=x[:, c, tsl], in1=rstd, op=ALU.mult),
                          reads=[d_x[c][tt], d_rs], writes=[d_tmp[i]])
                    P.act(lambda e, c=c, i=i, tsl=tsl: e.activation(out=h[:, c, tsl], in_=tmp[i], func=AF.Identity,
                                                                    scale=gs_t[:, l, c, b:b + 1], bias=modv[:, l, sh_j0 + c, b:b + 1]),
                          reads=[d_tmp[i], d_setup], writes=[d_h[c][tt]])

        ring_state = {"n": 0}

        def ring_next():
            s_ = ring_state["n"] % 2
            ring_state["n"] += 1
            return s_

        WO_OFF = 3328
        preloaded = {}
        plan = {"next": {}}

        def take_weights(key, loader):
            w = preloaded.pop(key) if key in preloaded else loader()
            nxt = plan["next"].get(key)
            if nxt is not None and nxt[1] is not None:
                preloaded[nxt[0]] = nxt[1]()
            return w

        def load_mixer_weights(l, ranges, r0, nrows):
            s_ = ring_next()
            ncols = sum(n for _, n in ranges)
            assert DC * ncols // 2 <= WO_OFF
            wv = ring[s_][:, 0:DC * ncols // 2].bitcast(BF16).rearrange("p (c n) -> p c n", c=DC)
            nkc = nrows // 128
            wo = ring[s_][:, WO_OFF:WO_OFF + nkc * D // 2].bitcast(BF16).rearrange("p (c n) -> p c n", c=nkc)
            nwords = DC * ncols // 2
            span = [d_ring[s_][j] for j in range(3) if nwords > (0, 1024, 2048)[j]]
            off = 0
            for (c0, n) in ranges:
                P.dma("pool", lambda e, c0=c0, n=n, off=off: e.dma_start(out=wv[:, :, off:off + n],
                                                                        in_=w_in[l][:, c0:c0 + n].rearrange("(c p) n -> p c n", p=128)),
                      writes=span, dep=d_ring[s_][0])
                off += n
            P.dma("pool", lambda e: e.dma_start(out=wo, in_=w_out[l][r0:r0 + nrows, :].rearrange("(c p) n -> p c n", p=128)),
                  writes=[d_ring[s_][2]], dep=d_ring[s_][2])
            dwo_ = [d_ring[s_][2]]
            return wv, wo, span, dwo_

        def proj_fm(wv, dw, col, tt, bk, M=128):
            tsl = slice(tt * TT, (tt + 1) * TT)
            for c in range(DC):
                P.pe(lambda e, c=c: e.matmul(ps[bk][0:M, :], lhsT=wv[:, c, col:col + M], rhs=h[:, c, tsl], start=(c == 0), stop=(c == DC - 1)),
                     reads=dw + [d_h[c][tt]], writes=[d_ps[bk]])

        def proj_tok(wv, dw, col, ncols, tt, sub, bk, off):
            t0 = tt * TT + sub * 128
            for c in range(DC):
                P.pe(lambda e, c=c: e.matmul(ps[bk][:, off:off + ncols], lhsT=h[:, c, t0:t0 + 128], rhs=wv[:, c, col:col + ncols],
                                             start=(c == 0), stop=(c == DC - 1)),
                     reads=dw + [d_h[c][tt]], writes=[d_ps[bk]])

        def out_proj(l, b, wo, dwo, y, d_y, tt, nk=2):
            tsl = slice(tt * TT, (tt + 1) * TT)
            for co in range(DC):
                bk = rot_bank()
                for kc in range(nk):
                    P.pe(lambda e, co=co, kc=kc, bk=bk: e.matmul(ps[bk][:, :], lhsT=wo[:, kc, co * 128:(co + 1) * 128], rhs=y[:, kc, :],
                                                                 start=(kc == 0), stop=(kc == nk - 1)),
                         reads=dwo + [d_y], writes=[d_ps[bk]])
                P.dve(lambda e, co=co, bk=bk: e.scalar_tensor_tensor(out=x[:, co, tsl], in0=ps[bk][:, :], scalar=modv[:, l, 16 + co, b:b + 1],
                                                                     in1=x[:, co, tsl], op0=ALU.mult, op1=ALU.add),
                      reads=[d_ps[bk], d_x[co][tt], d_setup], writes=[d_x[co][tt]])

        def conv_pass(l, b):
            phase_switch()
            wv, wo, dw, dwo = take_weights(("conv", b, l), lambda: load_mixer_weights(l, [(0, 768)], 0, 256))
            z = ua.alloc([2, TT + 2]); d_z = [newdep(), newdep()]
            cxs = [ua.alloc([TT]) for _ in range(2)]; d_cxs = [newdep(), newdep()]
            acc = [ua.alloc([TT]) for _ in range(2)]; d_acc = [newdep(), newdep()]
            y = [ua.alloc([2, TT], BF16) for _ in range(2)]; d_y = [newdep(), newdep()]
            for ch in range(2):
                P.dve(lambda e, ch=ch: e.memset(z[:, ch, 0:2], 0.0), writes=[d_z[ch]])
            deferred = []
            for tt in range(NTT):
                yi = tt % 2
                for ch in range(2):
                    b_cc = rot_bank(); proj_fm(wv, dw, 256 + ch * 128, tt, b_cc)
                    b_cx = rot_bank(); proj_fm(wv, dw, 512 + ch * 128, tt, b_cx)
                    P.act(lambda e, ch=ch, b_cx=b_cx: e.activation(out=cxs[ch], in_=ps[b_cx][:, :], func=AF.Copy),
                          reads=[d_ps[b_cx]], writes=[d_cxs[ch]])
                    P.dve(lambda e, ch=ch, b_cc=b_cc: e.tensor_tensor(out=z[:, ch, 2:TT + 2], in0=ps[b_cc][:, :], in1=cxs[ch], op=ALU.mult),
                          reads=[d_ps[b_cc], d_cxs[ch]], writes=[d_z[ch]])
                    if ch == 0 and deferred:
                        out_proj(*deferred.pop())
                    P.dve(lambda e, ch=ch: e.tensor_scalar(out=acc[ch], in0=z[:, ch, 0:TT], scalar1=convw_s[:, l, 0, ch:ch + 1], scalar2=None, op0=ALU.mult),
                          reads=[d_z[ch], d_par], writes=[d_acc[ch]])
                    for kk in (1, 2):
                        P.dve(lambda e, ch=ch, kk=kk: e.scalar_tensor_tensor(out=acc[ch], in0=z[:, ch, kk:TT + kk], scalar=convw_s[:, l, kk, ch:ch + 1],
                                                                             in1=acc[ch], op0=ALU.mult, op1=ALU.add),
                              reads=[d_z[ch], d_par, d_acc[ch]], writes=[d_acc[ch]])
                    P.dve(lambda e, ch=ch: e.tensor_copy(out=z[:, ch, 0:2], in_=z[:, ch, TT:TT + 2]), reads=[d_z[ch]], writes=[d_z[ch]])
                    b_cb = rot_bank(); proj_fm(wv, dw, ch * 128, tt, b_cb)
                    P.dve(lambda e, ch=ch, b_cb=b_cb, yi=yi: e.tensor_tensor(out=y[yi][:, ch, :], in0=ps[b_cb][:, :], in1=acc[ch], op=ALU.mult),
                          reads=[d_ps[b_cb], d_acc[ch]], writes=[d_y[yi]])
                deferred.append((l, b, wo, dwo, y[yi], d_y[yi], tt))
            out_proj(*deferred.pop())

        def sgu_pass(l, b):
            phase_switch()
            wv, wo, dw, dwo = take_weights(("sgu", b, l), lambda: load_mixer_weights(l, [(768, 512)], 256, 256))
            vn = [ua.alloc([256], BF16) for _ in range(2)]; d_vn = [newdep(), newdep()]
            stats = ua.alloc([2, 6]); mv = ua.alloc([2, 2]); rs = ua.alloc([2, 2]); d_st = [newdep(), newdep()]
            t1 = [ua.alloc([TT]) for _ in range(2)]; d_t1 = [newdep(), newdep()]
            y = [ua.alloc([2, TT], BF16) for _ in range(2)]; d_y = [newdep(), newdep()]
            k = 0
            deferred = []
            for tt in range(NTT):
                yi = tt % 2
                bm = [3, 4]
                for sub in range(4):
                    i = k % 2
                    k += 1
                    bv = rot_bank()
                    proj_tok(wv, dw, 256, 256, tt, sub, bv, 0)
                    P.dve(lambda e, i=i, bv=bv: e.bn_stats(out=stats[:, i, :], in_=ps[bv][:, 0:256]), reads=[d_ps[bv]], writes=[d_st[i]])
                    P.dve(lambda e, i=i: e.bn_aggr(out=mv[:, i, :], in_=stats[:, i, :]), reads=[d_st[i]], writes=[d_st[i]])
                    P.act(lambda e, i=i: e.activation(out=rs[:, i, 0:1], in_=mv[:, i, 1:2], func=AF.Ln, bias=EPS, scale=1.0), reads=[d_st[i]], writes=[d_st[i]])
                    P.act(lambda e, i=i: e.activation(out=rs[:, i, 1:2], in_=rs[:, i, 0:1], func=AF.Exp, scale=-0.5), reads=[d_st[i]], writes=[d_st[i]])
                    P.dve(lambda e, i=i, bv=bv: e.tensor_scalar(out=vn[i], in0=ps[bv][:, 0:256], scalar1=mv[:, i, 0:1], scalar2=rs[:, i, 1:2],
                                                                op0=ALU.subtract, op1=ALU.mult),
                          reads=[d_ps[bv], d_st[i]], writes=[d_vn[i]])
                    if sub == 0 and deferred:
                        out_proj(*deferred.pop())
                    for hh in range(4):
                        cc, hl = hh // 2, hh % 2
                        P.pe(lambda e, i=i, hh=hh, cc=cc, hl=hl, sub=sub: e.matmul(ps[bm[cc]][64 * hl:64 * hl + 64, sub * 128:(sub + 1) * 128],
                                                                                   lhsT=vn[i][:, hh * 64:(hh + 1) * 64], rhs=sguw_s[:, l, hh, :],
                                                                                   start=True, stop=True),
                             reads=[d_vn[i], d_setup], writes=[d_ps[bm[cc]]])
                for cc in range(2):
                    bu = rot_bank(); proj_fm(wv, dw, cc * 128, tt, bu)
                    P.dve(lambda e, cc=cc: e.scalar_tensor_tensor(out=t1[cc].rearrange("p (a t) -> p a t", a=4),
                                                                  in0=ps[bm[cc]][:, :].rearrange("p (a t) -> p a t", a=4),
                                                                  scalar=sgug_s[:, l, cc:cc + 1], in1=bc_mid(sgub_s[:, l, cc, :], 4),
                                                                  op0=ALU.mult, op1=ALU.add),
                          reads=[d_ps[bm[cc]], d_par], writes=[d_t1[cc]])
                    P.dve(lambda e, cc=cc, bu=bu, yi=yi: e.tensor_tensor(out=y[yi][:, cc, :], in0=ps[bu][:, :], in1=t1[cc], op=ALU.mult),
                          reads=[d_ps[bu], d_t1[cc]], writes=[d_y[yi]])
                deferred.append((l, b, wo, dwo, y[yi], d_y[yi], tt))
            out_proj(*deferred.pop())

        def recur_pass(l, b, kind, g=0):
            phase_switch()
            if kind == "gla":
                wv, wo, dw, dwo = take_weights(("gla", b, l), lambda: load_mixer_weights(l, [(1280, 784)], 512, 256))
                nh, dk, sc, hmask, ng = 4, 32, -1.0 / 16.0, hmask4, glang_s
                qc, kc, vc, gate_col, noc = 0, 128, 256, 528, 2
            else:
                base = 2064
                wv, wo, dw, dwo = take_weights(("hg%d" % g, b, l), lambda: load_mixer_weights(
                    l, [(base + g * 128, 128), (base + 256 + g * 128, 128), (base + 512 + g * 128, 128), (base + 768 + g * 128, 128)], 768 + g * 128, 128))
                nh, dk, sc, hmask, ng = 2, 64, 1.0, hmask2, hgng_s
                qc, fcol, vc, gate_col, noc = 0, 128, 256, 384, 1
            BO = [5, 6]
            BA, BU = 3, 4
            ext = ua.alloc([64, 9]); d_ext = newdep()
            dece = ua.alloc([2, 9]); d_dece = newdep()
            y = [ua.alloc([noc, TT], BF16) for _ in range(2)]; d_y = [newdep(), newdep()]
            sgate = ua.alloc([noc, TT]); d_sg = [newdep() for _ in range(noc)]
            qt = ua.alloc([TT], BF16); d_qt = newdep()
            kt = ua.alloc([TT], BF16); d_kt = newdep()
            ktm = ua.alloc([nh, TT], BF16); d_ktm = newdep()
            ktok = ua.alloc([4, 128], BF16); d_ktok = newdep()
            V = ua.alloc([4, nh * 64], BF16); d_V = newdep()
            Vbd = ua.alloc([2, nh, 4, 64], BF16); d_Vbd = newdep()
            A = [ua.alloc([nh, 128], BF16) for _ in range(2)]; d_A = [newdep(), newdep()]
            T1 = ua.alloc([TT]); d_T1 = newdep()
            T2 = ua.alloc([TT]); d_T2 = newdep()
            T3 = ua.alloc([TT]); d_T3 = newdep()
            T4 = ua.alloc([TT]); d_T4 = newdep()
            T5 = ua.alloc([TT]); d_T5 = newdep()
            T6 = ua.alloc([TT]); d_T6 = newdep()
            sm = ua.alloc([4, 16]); d_sm = newdep()
            emh = ua.alloc([4, 16]); d_emh = newdep()
            cUe = ua.alloc([64, 9]); d_cUe = newdep()
            decf = ua.alloc([64, 9]); d_decf = newdep()
            Sbd = ua.alloc([8, nh * 64], BF16); d_Sbd = newdep()
            osq = ua.alloc([TT], BF16); d_osq = newdep()
            P.dve(lambda e: e.memset(dece, 0.0), writes=[d_dece])
            P.dve(lambda e: e.memset(ext, 0.0), writes=[d_ext])
            ai = 0
            deferred = []
            for tt in range(NTT):
                tsl = slice(tt * TT, (tt + 1) * TT)
                yi = tt % 2
                for oc in range(noc):
                    bg_ = rot_bank(); proj_fm(wv, dw, gate_col + oc * 128, tt, bg_)
                    P.act(lambda e, bg_=bg_: e.activation(out=T1, in_=ps[bg_][:, :], func=AF.Exp, scale=-1.0), reads=[d_ps[bg_]], writes=[d_T1])
                    P.act(lambda e: e.activation(out=T2, in_=T1, func=AF.Ln, bias=1.0, scale=1.0), reads=[d_T1], writes=[d_T2])
                    P.act(lambda e: e.activation(out=T3, in_=T2, func=AF.Exp, scale=-1.0), reads=[d_T2], writes=[d_T3])
                    P.dve(lambda e, oc=oc, bg_=bg_: e.tensor_tensor(out=sgate[:, oc, :], in0=ps[bg_][:, :], in1=T3, op=ALU.mult),
                          reads=[d_ps[bg_], d_T3], writes=[d_sg[oc]])
                if kind == "gla":
                    bgl = rot_bank(); proj_fm(wv, dw, 512, tt, bgl, M=16)
                    P.act(lambda e, bgl=bgl: e.activation(out=T5[0:16, :], in_=ps[bgl][0:16, :], func=AF.Copy), reads=[d_ps[bgl]], writes=[d_T5])
                    bz = rot_bank()
                    P.pe(lambda e, bz=bz: e.matmul(ps[bz][:, :], lhsT=wg_s[0:16, l, :], rhs=T5[0:16, :], start=True, stop=True),
                         reads=[d_T5, d_par], writes=[d_ps[bz]])
                    P.act(lambda e, bz=bz: e.activation(out=T1, in_=ps[bz][:, :], func=AF.Exp, scale=-1.0, bias=nbg_s[:, l:l + 1]),
                          reads=[d_ps[bz], d_setup], writes=[d_T1])
                    P.act(lambda e: e.activation(out=T2, in_=T1, func=AF.Ln, bias=1.0, scale=1.0), reads=[d_T1], writes=[d_T2])
                    src_l, d_src = T2, d_T2
                else:
                    bf_ = rot_bank(); proj_fm(wv, dw, fcol, tt, bf_)
                    P.act(lambda e, bf_=bf_: e.activation(out=T1, in_=ps[bf_][:, :], func=AF.Exp, scale=-1.0), reads=[d_ps[bf_]], writes=[d_T1])
                    P.act(lambda e: e.activation(out=T2, in_=T1, func=AF.Ln, bias=1.0, scale=1.0), reads=[d_T1], writes=[d_T2])
                    P.act(lambda e: e.activation(out=T3, in_=T1, func=AF.Ln, bias=1.0, scale=lb_s[:, g, l:l + 1]), reads=[d_T1, d_setup], writes=[d_T3])
                    P.dve(lambda e, bf_=bf_: e.tensor_tensor(out=T6, in0=ps[bf_][:, :], in1=T2, op=ALU.add), reads=[d_ps[bf_], d_T2], writes=[d_T6])
                    P.act(lambda e: e.activation(out=T6, in_=T6, func=AF.Exp, scale=-1.0), reads=[d_T6], writes=[d_T6])
                    P.dve(lambda e: e.tensor_tensor(out=T3, in0=T3, in1=T2, op=ALU.subtract), reads=[d_T3, d_T2], writes=[d_T3])
                    src_l, d_src = T3, d_T3
                if deferred:
                    out_proj(*deferred.pop(), **{"nk": noc})
                P.dve(lambda e, src_l=src_l: e.tensor_tensor_scan(out=T4, data0=m32[:], data1=src_l, initial=0.0, op0=ALU.mult, op1=ALU.add),
                      reads=[d_src, d_const], writes=[d_T4])
                b3 = T4.rearrange("p (n k) -> p n k", k=CH)
                b4 = T4.rearrange("p (a n k) -> p a n k", a=2, k=CH)
                P.dve(lambda e, b3=b3: e.tensor_tensor(out=T5.rearrange("p (n k) -> p n k", k=CH), in0=b3,
                                                       in1=b3[:, :, CH // 2:CH // 2 + 1].to_broadcast([128, 16, CH]), op=ALU.subtract),
                      reads=[d_T4, d_T5], writes=[d_T5])
                P.act(lambda e: e.activation(out=T1, in_=T5, func=AF.Exp, scale=sc), reads=[d_T5, d_T1], writes=[d_T1])
                P.act(lambda e: e.activation(out=T2, in_=T5, func=AF.Exp, scale=-sc), reads=[d_T5, d_T2], writes=[d_T2])
                bmid, blast = b3[:, :, CH // 2], b3[:, :, CH - 1]
                P.act(lambda e, bmid=bmid: e.activation(out=sm[:, 0, :], in_=bmid, func=AF.Exp, scale=sc), reads=[d_T4], writes=[d_sm])
                P.act(lambda e, b4=b4: e.activation(out=dece[:, :, 1:9], in_=b4[:, :, :, CH - 1], func=AF.Exp, scale=sc), reads=[d_T4], writes=[d_dece])
                P.dve(lambda e, bmid=bmid, blast=blast: e.tensor_tensor(out=sm[:, 2, :], in0=blast, in1=bmid, op=ALU.subtract), reads=[d_T4, d_sm], writes=[d_sm])
                P.act(lambda e: e.activation(out=sm[:, 1, :], in_=sm[:, 2, :], func=AF.Exp, scale=sc), reads=[d_sm], writes=[d_sm])
                P.dve(lambda e: e.tensor_tensor(out=emh[:, 0:nh, :], in0=bc_mid(sm[:, 0, :], nh), in1=bc_last(hmask[:, 0:nh], 16), op=ALU.mult),
                      reads=[d_sm, d_const], writes=[d_emh])
                bq = rot_bank(); proj_fm(wv, dw, qc, tt, bq)
                if kind == "gla":
                    P.dve(lambda e, bq=bq: e.scalar_tensor_tensor(out=qt, in0=ps[bq][:, :], scalar=float(32 ** -0.5), in1=T1, op0=ALU.mult, op1=ALU.mult),
                          reads=[d_ps[bq], d_T1], writes=[d_qt])
                    bk_ = rot_bank(); proj_fm(wv, dw, kc, tt, bk_)
                    P.dve(lambda e, bk_=bk_: e.tensor_tensor(out=kt, in0=ps[bk_][:, :], in1=T2, op=ALU.mult), reads=[d_ps[bk_], d_T2], writes=[d_kt])
                else:
                    P.dve(lambda e, bq=bq: e.tensor_tensor(out=qt, in0=ps[bq][:, :], in1=T1, op=ALU.mult), reads=[d_ps[bq], d_T1], writes=[d_qt])
                    P.dve(lambda e: e.scalar_tensor_tensor(out=kt, in0=T6, scalar=oml_s[:, g, l:l + 1], in1=T2, op0=ALU.mult, op1=ALU.mult),
                          reads=[d_T6, d_T2, d_setup], writes=[d_kt])
                for hh in range(nh):
                    P.dve(lambda e, hh=hh: e.tensor_scalar(out=ktm[:, hh, :], in0=kt, scalar1=hmask[:, hh:hh + 1], scalar2=None, op0=ALU.mult),
                          reads=[d_kt, d_const], writes=[d_ktm])
                for sub in range(4):
                    P.pe(lambda e, sub=sub: e.transpose(psT[:, sub * 128:(sub + 1) * 128], kt[:, sub * 128:(sub + 1) * 128], ident_bf[:]),
                         reads=[d_kt, d_const], writes=[d_psT])
                P.act(lambda e: e.activation(out=ktok.rearrange("p a t -> p (a t)"), in_=psT[:, 0:512], func=AF.Copy), reads=[d_psT], writes=[d_ktok])
                for half in range(2):
                    bv = rot_bank()
                    for s2 in range(2):
                        proj_tok(wv, dw, vc, nh * 64, tt, half * 2 + s2, bv, s2 * 256)
                    P.act(lambda e, bv=bv, half=half: e.activation(out=V[:, half * 2:half * 2 + 2, :],
                                                                   in_=ps[bv][:, :].rearrange("p (s c) -> p s c", s=2)[:, :, 0:nh * 64], func=AF.Copy),
                          reads=[d_ps[bv]], writes=[d_V])
                    for s2 in range(2):
                        for n_ in range(4):
                            P.pool(lambda e, s2=s2, n_=n_, half=half: e.tensor_scalar(
                                out=Vbd[:, s2, :, n_, :], in0=V[:, half * 2 + s2, :].rearrange("p (h v) -> p h v", h=nh),
                                scalar1=cmask[:, n_:n_ + 1], scalar2=1.0, op0=ALU.mult, op1=ALU.mult),
                                reads=[d_V, d_const], writes=[d_Vbd])
                    for s2 in range(2):
                        sub = half * 2 + s2
                        for hh in range(nh):
                            kw = {"tile_position": (0, 96)} if hh * dk == 96 else {}
                            P.pe(lambda e, s2=s2, sub=sub, hh=hh, kw=kw: e.matmul(
                                ps[BU][hh * dk:(hh + 1) * dk, s2 * 256:(s2 + 1) * 256], lhsT=ktok[:, sub, hh * dk:(hh + 1) * dk],
                                rhs=Vbd[:, s2, hh, :, :].rearrange("p n v -> p (n v)"), start=True, stop=True, **kw),
                                reads=[d_ktok, d_Vbd], writes=[d_ps[BU]])
                    n0 = half * 8
                    P.dve(lambda e, n0=n0: e.tensor_tensor(out=cUe[:, :, 1:9], in0=ps[BU][:, :].rearrange("p (n v) -> p v n", v=64),
                                                           in1=bc_mid(sm[:, 1, n0:n0 + 8], 64), op=ALU.mult),
                          reads=[d_ps[BU], d_sm], writes=[d_cUe])
                    P.dve(lambda e: e.tensor_copy(out=cUe[:, :, 0:1], in_=ext[:, :, 8:9]), reads=[d_ext, d_cUe], writes=[d_cUe])
                    P.dve(lambda e, half=half: e.tensor_copy(out=decf, in_=bc_mid(dece[:, half, :], 64)), reads=[d_dece], writes=[d_decf])
                    P.dve(lambda e: e.tensor_tensor_scan(out=ext.rearrange("p v n -> p (v n)"), data0=decf.rearrange("p v n -> p (v n)"),
                                                         data1=cUe.rearrange("p v n -> p (v n)"), initial=0.0, op0=ALU.mult, op1=ALU.add),
                          reads=[d_cUe, d_decf], writes=[d_ext])
                    for hh in range(nh):
                        P.dve(lambda e, hh=hh, n0=n0: e.tensor_tensor(out=Sbd[:, :, hh * 64:(hh + 1) * 64], in0=ext[:, :, 0:8].rearrange("p v n -> p n v"),
                                                                      in1=bc_last(emh[:, hh, n0:n0 + 8], 64), op=ALU.mult),
                              reads=[d_ext, d_emh], writes=[d_Sbd])
                    for s2 in range(2):
                        sub = half * 2 + s2
                        ssl = slice(sub * 128, (sub + 1) * 128)
                        for hh in range(nh):
                            P.pe(lambda e, hh=hh, ssl=ssl: e.matmul(ps[BA][:, hh * 128:(hh + 1) * 128], lhsT=ktm[:, hh, ssl], rhs=qt[:, ssl], start=True, stop=True),
                                 reads=[d_ktm, d_qt], writes=[d_ps[BA]])
                        a_ = ai % 2
                        ai += 1
                        P.dve(lambda e, a_=a_: e.tensor_tensor(out=A[a_], in0=ps[BA][:, 0:nh * 128].rearrange("p (h i) -> p h i", h=nh),
                                                               in1=bc_mid(maskBD[:], nh), op=ALU.mult),
                              reads=[d_ps[BA], d_const], writes=[d_A[a_]])
                        for hh in range(nh):
                            oc, hl = hh // 2, hh % 2
                            P.pe(lambda e, a_=a_, hh=hh, oc=oc, hl=hl, sub=sub, ssl=ssl: e.matmul(
                                ps[BO[oc]][64 * hl:64 * hl + 64, ssl], lhsT=V[:, sub, hh * 64:(hh + 1) * 64], rhs=A[a_][:, hh, :], start=True, stop=False),
                                reads=[d_V, d_A[a_]], writes=[d_ps[BO[oc]]])
                        for n_ in range(4):
                            nn = s2 * 4 + n_
                            csl = slice(sub * 128 + n_ * 32, sub * 128 + n_ * 32 + 32)
                            for oc in range(noc):
                                P.pe(lambda e, nn=nn, oc=oc, csl=csl, n_=n_: e.matmul(ps[BO[oc]][:, csl], lhsT=Sbd[:, nn, oc * 128:(oc + 1) * 128], rhs=qt[:, csl],
                                                                                  start=False, stop=(n_ == 3)),
                                     reads=[d_Sbd, d_qt], writes=[d_ps[BO[oc]]])
                for oc in range(noc):
                    P.act(lambda e, oc=oc: e.activation(out=osq, in_=ps[BO[oc]][:, :], func=AF.Square), reads=[d_ps[BO[oc]]], writes=[d_osq])
                    bs_ = rot_bank()
                    P.pe(lambda e, bs_=bs_: e.matmul(ps[bs_][:, :], lhsT=blk_bf[:], rhs=osq, start=True, stop=True), reads=[d_osq, d_const], writes=[d_ps[bs_]])
                    P.act(lambda e, bs_=bs_: e.activation(out=T1, in_=ps[bs_][:, :], func=AF.Ln, scale=1.0 / HD, bias=EPS), reads=[d_ps[bs_], d_T1], writes=[d_T1])
                    P.act(lambda e: e.activation(out=T2, in_=T1, func=AF.Exp, scale=-0.5), reads=[d_T1, d_T2], writes=[d_T2])
                    P.dve(lambda e, oc=oc: e.tensor_tensor(out=T3, in0=ps[BO[oc]][:, :], in1=T2, op=ALU.mult), reads=[d_ps[BO[oc]], d_T2, d_T3], writes=[d_T3])
                    P.dve(lambda e, oc=oc, yi=yi: e.scalar_tensor_tensor(out=y[yi][:, oc, :], in0=T3, scalar=ng[:, l:l + 1], in1=sgate[:, oc, :],
                                                                         op0=ALU.mult, op1=ALU.mult),
                          reads=[d_T3, d_sg[oc], d_par], writes=[d_y[yi]])
                deferred.append((l, b, wo, dwo, y[yi], d_y[yi], tt))
            out_proj(*deferred.pop(), **{"nk": noc})

        def ffn_issue(slab):
            gi, si, w1d, w3d, w2d, dff = slab
            f0 = si * SLAB
            sw = min(SLAB, dff - f0)
            s_ = ring_next()
            w1v = ring[s_][:, 0:1024].bitcast(BF16).rearrange("p (c n) -> p c n", c=DC)[:, :, 0:sw]
            w3v = ring[s_][:, 1024:2048].bitcast(BF16).rearrange("p (c n) -> p c n", c=DC)[:, :, 0:sw]
            w2v = ring[s_][:, 2048:3072].bitcast(BF16).rearrange("p (c n) -> p c n", c=2)[:, 0:sw // 128, :]
            P.dma("pool", lambda e: e.dma_start(out=w1v, in_=w1d[:, f0:f0 + sw].rearrange("(c p) n -> p c n", p=128)), writes=[d_ring[s_][0]])
            P.dma("pool", lambda e: e.dma_start(out=w3v, in_=w3d[:, f0:f0 + sw].rearrange("(c p) n -> p c n", p=128)), writes=[d_ring[s_][1]])
            P.dma("pool", lambda e: e.dma_start(out=w2v, in_=w2d[f0:f0 + sw, :].rearrange("(c p) n -> p c n", p=128)), writes=[d_ring[s_][2]])
            return (s_, sw, w1v, w3v, w2v)

        def ffn_first_slab(l):
            idx = l // 2
            if l % 2 == 0:
                return (0, 0, ffn_w1[idx], ffn_w3[idx], ffn_w2[idx], DFF)
            return (0, 0, moe_w1[idx, 0], moe_w3[idx, 0], moe_w2[idx, 0], DFE)

        def ffn_run(l, b, segs, st_):
            hid, d_hid, sa, d_sa, tq, d_tq = st_["hid"], st_["d_hid"], st_["sa"], st_["d_sa"], st_["tq"], st_["d_tq"]
            slabs = []
            for gi, (w1d, w3d, w2d, dff, cf) in enumerate(segs):
                for si in range((dff + SLAB - 1) // SLAB):
                    slabs.append((gi, si, w1d, w3d, w2d, dff))
            loaded = {}

            def issue(j):
                if j == 0 and ("ffn", b, l) in preloaded:
                    loaded[0] = preloaded.pop(("ffn", b, l))
                    return
                loaded[j] = ffn_issue(slabs[j])

            def _unused(j):
                gi, si, w1d, w3d, w2d, dff = slabs[j]
                f0 = si * SLAB
                sw = min(SLAB, dff - f0)
                s_ = ring_next()
                w1v = ring[s_][:, 0:1024].bitcast(BF16).rearrange("p (c n) -> p c n", c=DC)[:, :, 0:sw]
                w3v = ring[s_][:, 1024:2048].bitcast(BF16).rearrange("p (c n) -> p c n", c=DC)[:, :, 0:sw]
                w2v = ring[s_][:, 2048:3072].bitcast(BF16).rearrange("p (c n) -> p c n", c=2)[:, 0:sw // 128, :]
                P.dma("pool", lambda e: e.dma_start(out=w1v, in_=w1d[:, f0:f0 + sw].rearrange("(c p) n -> p c n", p=128)), writes=[d_ring[s_][0]])
                P.dma("pool", lambda e: e.dma_start(out=w3v, in_=w3d[:, f0:f0 + sw].rearrange("(c p) n -> p c n", p=128)), writes=[d_ring[s_][1]])
                P.dma("pool", lambda e: e.dma_start(out=w2v, in_=w2d[f0:f0 + sw, :].rearrange("(c p) n -> p c n", p=128)), writes=[d_ring[s_][2]])
                loaded[j] = (s_, sw, w1v, w3v, w2v)

            def stage_a(j, tt, comb):
                s_, sw, w1v, w3v, w2v = loaded[j]
                nfc = sw // 128
                tsl = slice(tt * TT, (tt + 1) * TT)
                hi = st_["k"] % 2
                st_["k"] += 1
                for fc in range(nfc):
                    pa = (st_["p"] % 2) * 2
                    st_["p"] += 1
                    for (wv_, dr, bk) in ((w1v, d_ring[s_][0], pa), (w3v, d_ring[s_][1], pa + 1)):
                        for c in range(DC):
                            P.pe(lambda e, c=c: e.matmul(ps[bk][:, :], lhsT=wv_[:, c, fc * 128:(fc + 1) * 128], rhs=h[:, c, tsl],
                                                         start=(c == 0), stop=(c == DC - 1)),
                                 reads=[dr, d_h[c][tt]], writes=[d_ps[bk]])
                        if bk == pa:
                            yield None
                    qi = st_["q"] % 3
                    st_["q"] += 1
                    P.act(lambda e: e.activation(out=sa[qi], in_=ps[pa][:, :], func=AF.Silu), reads=[d_ps[pa]], writes=[d_sa[qi]])
                    if comb is not None:
                        cb_, d_cb = comb
                        P.pool(lambda e: e.tensor_tensor(out=tq[qi], in0=sa[qi], in1=cb_[:, tsl], op=ALU.mult), reads=[d_sa[qi], d_cb], writes=[d_tq[qi]])
                        src_, dsrc = tq[qi], d_tq[qi]
                    else:
                        src_, dsrc = sa[qi], d_sa[qi]
                    P.dve(lambda e: e.tensor_tensor(out=hid[hi][:, fc, :], in0=ps[pa + 1][:, :], in1=src_, op=ALU.mult),
                          reads=[d_ps[pa + 1], dsrc], writes=[d_hid[hi][fc]])
                    yield None
                st_["last"] = (j, tt, hi)

            def stage_b(tok_):
                j, tt, hi = tok_
                s_, sw, w1v, w3v, w2v = loaded[j]
                nfc = sw // 128
                tsl = slice(tt * TT, (tt + 1) * TT)
                for co in range(DC):
                    bk = 4 + st_["o"] % 3
                    st_["o"] += 1
                    for fc in range(nfc):
                        P.pe(lambda e: e.matmul(ps[bk][:, :], lhsT=w2v[:, fc, co * 128:(co + 1) * 128], rhs=hid[hi][:, fc, :],
                                                start=(fc == 0), stop=(fc == nfc - 1)),
                             reads=[d_ring[s_][2], d_hid[hi][fc]], writes=[d_ps[bk]])
                    P.dve(lambda e: e.scalar_tensor_tensor(out=x[:, co, tsl], in0=ps[bk][:, :], scalar=modv[:, l, 40 + co, b:b + 1],
                                                           in1=x[:, co, tsl], op0=ALU.mult, op1=ALU.add),
                          reads=[d_ps[bk], d_x[co][tt], d_setup], writes=[d_x[co][tt]])
                    yield None

            def run_interleaved(ga, gb):
                a_live, b_live = ga is not None, gb is not None
                while a_live or b_live:
                    if a_live:
                        a_live = next(ga, "end") != "end"
                    for _ in range(2):
                        if b_live:
                            b_live = next(gb, "end") != "end"

            issue(0)
            if len(slabs) > 1:
                issue(1)
            pend = None
            comb = None
            for j, (gi, si, _, _, _, _) in enumerate(slabs):
                if si == 0:
                    cf = segs[gi][4]
                    comb = cf() if cf is not None else None
                for tt in range(NTT):
                    run_interleaved(stage_a(j, tt, comb), stage_b(pend) if pend is not None else None)
                    cur = st_["last"]
                    if tt == 0 and j >= 1 and j + 1 < len(slabs):
                        issue(j + 1)
                    elif tt == 0 and j >= 1 and j + 1 == len(slabs):
                        nxt = plan["next"].get(("ffn", b, l))
                        if nxt is not None:
                            preloaded[nxt[0]] = nxt[1]()
                    pend = cur
            run_interleaved(None, stage_b(pend))

        def ffn_state():
            st_ = dict(k=0, p=0, q=0, o=0)
            st_["hid"] = [ua.alloc([SLAB // 128, TT], BF16) for _ in range(2)]
            st_["d_hid"] = [[newdep() for _ in range(4)] for _ in range(2)]
            st_["sa"] = [ua.alloc([TT]) for _ in range(3)]
            st_["d_sa"] = [newdep() for _ in range(3)]
            st_["tq"] = [ua.alloc([TT]) for _ in range(3)]
            st_["d_tq"] = [newdep() for _ in range(3)]
            return st_

        def dense_ffn(l, b):
            phase_switch()
            st_ = ffn_state()
            idx = l // 2
            ffn_run(l, b, [(ffn_w1[idx], ffn_w3[idx], ffn_w2[idx], DFF, None)], st_)

        def moe_ffn(l, b):
            phase_switch()
            idx = l // 2
            st_ = ffn_state()
            NS = T // 128
            lg = ua.alloc([NS, NE]); d_lg = newdep()
            top = ua.alloc([NS, 8]); d_top = newdep()
            wts = ua.alloc([4, NS]); d_w = newdep()
            cmb = ua.alloc([NS, NE]); d_cmb = newdep()
            cm2 = ua.alloc([NS, NE]); d_cm2 = newdep()
            combT = ua.alloc([T]); d_cT = newdep()
            cbc = [ua.alloc([T]) for _ in range(2)]; d_cbc = [newdep(), newdep()]
            for s in range(NS):
                tt = (s * 128) // TT
                for c in range(DC):
                    P.pe(lambda e, s=s, c=c: e.matmul(ps[0][:, s * NE:(s + 1) * NE], lhsT=h[:, c, s * 128:(s + 1) * 128], rhs=wr_s[:, idx, c, :],
                                                      start=(c == 0), stop=(c == DC - 1)),
                         reads=[d_h[c][tt], d_par], writes=[d_ps[0]])
            P.dve(lambda e: e.tensor_copy(out=lg.rearrange("p s e -> p (s e)"), in_=ps[0][:, 0:NS * NE]), reads=[d_ps[0]], writes=[d_lg])
            for s in range(NS):
                P.dve(lambda e, s=s: e.max(out=top[:, s, :], in_=lg[:, s, :]), reads=[d_lg], writes=[d_top])
            m1 = top[:, :, 0]
            m2 = top[:, :, 1]
            P.dve(lambda e: e.tensor_tensor(out=wts[:, 0, :], in0=m2, in1=m1, op=ALU.subtract), reads=[d_top], writes=[d_w])
            P.act(lambda e: e.activation(out=wts[:, 0, :], in_=wts[:, 0, :], func=AF.Exp), reads=[d_w], writes=[d_w])
            P.dve(lambda e: e.tensor_scalar(out=wts[:, 1, :], in0=wts[:, 0, :], scalar1=1.0, scalar2=None, op0=ALU.add), reads=[d_w], writes=[d_w])
            P.dve(lambda e: e.reciprocal(out=wts[:, 1, :], in_=wts[:, 1, :]), reads=[d_w], writes=[d_w])
            P.dve(lambda e: e.tensor_tensor(out=wts[:, 2, :], in0=wts[:, 0, :], in1=wts[:, 1, :], op=ALU.mult), reads=[d_w], writes=[d_w])
            P.dve(lambda e: e.tensor_tensor(out=cmb, in0=lg, in1=bc_last(m1, NE), op=ALU.is_equal), reads=[d_lg, d_top], writes=[d_cmb])
            P.dve(lambda e: e.tensor_tensor(out=cmb, in0=cmb, in1=bc_last(wts[:, 1, :], NE), op=ALU.mult), reads=[d_cmb, d_w], writes=[d_cmb])
            P.dve(lambda e: e.tensor_tensor(out=cm2, in0=lg, in1=bc_last(m2, NE), op=ALU.is_equal), reads=[d_lg, d_top], writes=[d_cm2])
            P.dve(lambda e: e.tensor_tensor(out=cm2, in0=cm2, in1=bc_last(wts[:, 2, :], NE), op=ALU.mult), reads=[d_cm2, d_w], writes=[d_cm2])
            P.dve(lambda e: e.tensor_tensor(out=cmb, in0=cmb, in1=cm2, op=ALU.add), reads=[d_cmb, d_cm2], writes=[d_cmb])
            for s in range(NS):
                bk = 1 + (s // 4) % 2
                P.pe(lambda e, s=s, bk=bk: e.transpose(ps[bk][0:NE, (s % 4) * 128:(s % 4 + 1) * 128], cmb[:, s, :], ident_f[:]),
                     reads=[d_cmb, d_const], writes=[d_ps[bk]])
                if s % 4 == 3:
                    P.act(lambda e, s=s, bk=bk: e.activation(out=combT[0:NE, (s - 3) * 128:(s + 1) * 128], in_=ps[bk][0:NE, :], func=AF.Copy),
                          reads=[d_ps[bk]], writes=[d_cT])
            def make_comb(ex):
                def cf():
                    ci = ex % 2
                    for tt in range(NTT):
                        bk = 1 + tt % 2
                        P.pe(lambda e: e.matmul(ps[bk][:, :], lhsT=sel[0:NE, ex, :], rhs=combT[0:NE, tt * TT:(tt + 1) * TT], start=True, stop=True),
                             reads=[d_cT, d_const], writes=[d_ps[bk]])
                        P.act(lambda e: e.activation(out=cbc[ci][:, tt * TT:(tt + 1) * TT], in_=ps[bk][:, :], func=AF.Copy),
                              reads=[d_ps[bk]], writes=[d_cbc[ci]])
                    return (cbc[ci], d_cbc[ci])
                return cf
            ffn_run(l, b, [(moe_w1[idx, ex], moe_w3[idx, ex], moe_w2[idx, ex], DFE, make_comb(ex)) for ex in range(NE)], st_)

        def _mk_loader(kind, l_):
            base = 2064
            if kind == "conv":
                return lambda: load_mixer_weights(l_, [(0, 768)], 0, 256)
            if kind == "sgu":
                return lambda: load_mixer_weights(l_, [(768, 512)], 256, 256)
            if kind == "gla":
                return lambda: load_mixer_weights(l_, [(1280, 784)], 512, 256)
            if kind in ("hg0", "hg1"):
                g_ = int(kind[2])
                return lambda: load_mixer_weights(l_, [(base + g_ * 128, 128), (base + 256 + g_ * 128, 128), (base + 512 + g_ * 128, 128),
                                                        (base + 768 + g_ * 128, 128)], 768 + g_ * 128, 128)
            return lambda: ffn_issue(ffn_first_slab(l_))

        if not diag_skip_mixers:
            order = [(k, b_, l_) for b_ in range(NB) for l_ in range(L) for k in ("conv", "sgu", "gla", "hg0", "hg1", "ffn")]
            for i_, key_ in enumerate(order[:-1]):
                nk = order[i_ + 1]
                plan["next"][key_] = (nk, _mk_loader(nk[0], nk[2]))

        for b in range(NB):
            for c in range(DC):
                for tt in range(NTT):
                    P.dma("sp", lambda e, b=b, c=c, tt=tt: e.dma_start(out=x[:, c, tt * TT:(tt + 1) * TT], in_=xT[b, c * 128:(c + 1) * 128, tt * TT:(tt + 1) * TT]),
                          writes=[d_x[c][tt]])
            for l in range(L):
                if not diag_skip_mixers:
                    rmsnorm_to_h(l, b, gsm, 0)
                    conv_pass(l, b)
                    sgu_pass(l, b)
                    recur_pass(l, b, "gla")
                    recur_pass(l, b, "hgrn", 0)
                    recur_pass(l, b, "hgrn", 1)
                rmsnorm_to_h(l, b, gsf, 24)
                if l % 2 == 0:
                    dense_ffn(l, b)
                else:
                    moe_ffn(l, b)
            phase_switch()
            sq = [ua.alloc([TT], BF16) for _ in range(3)]; d_sq = [newdep() for _ in range(3)]
            lnv = ua.alloc([TT]); rstd = ua.alloc([TT]); d_ln = newdep(); d_rs = newdep()
            k = 0
            for tt in range(NTT):
                tsl = slice(tt * TT, (tt + 1) * TT)
                bk = rot_bank()
                for c in range(DC):
                    i = k % 3
                    k += 1
                    P.act(lambda e, c=c, i=i, tsl=tsl: e.activation(out=sq[i], in_=x[:, c, tsl], func=AF.Square), reads=[d_x[c][tt]], writes=[d_sq[i]])
                    P.pe(lambda e, c=c, i=i, bk=bk: e.matmul(ps[bk][:, :], lhsT=ones_bf[:], rhs=sq[i], start=(c == 0), stop=(c == DC - 1)),
                         reads=[d_sq[i], d_const], writes=[d_ps[bk]])
                P.act(lambda e, bk=bk: e.activation(out=lnv, in_=ps[bk][:, :], func=AF.Ln, scale=1.0 / D, bias=EPS), reads=[d_ps[bk]], writes=[d_ln])
                P.act(lambda e: e.activation(out=rstd, in_=lnv, func=AF.Exp, scale=-0.5), reads=[d_ln], writes=[d_rs])
                for c in range(DC):
                    P.dve(lambda e, c=c, tsl=tsl: e.scalar_tensor_tensor(out=x[:, c, tsl], in0=x[:, c, tsl], scalar=gfin_s[:, c:c + 1], in1=rstd,
                                                                         op0=ALU.mult, op1=ALU.mult),
                          reads=[d_x[c][tt], d_rs, d_par], writes=[d_x[c][tt]])
                    o = P.dma("sp", lambda e, b=b, c=c, tsl=tsl: e.dma_start(out=outT[b, c * 128:(c + 1) * 128, tsl], in_=x[:, c, tsl]),
                              reads=[d_x[c][tt]], writes=[Dep()], dep=d_x[c][tt])
                    P.final_waits.append(o)
        P.emit()
    return nc


def _fm(v):
    v = np.asarray(v, np.float32)
    lead = v.shape[:-1]
    n = v.shape[-1] // 128
    v = v.reshape(lead + (n, 128))
    return np.ascontiguousarray(np.moveaxis(v, -1, 0))


_PROG_CACHE = {}


def kernel(x, c, norm_mix_g, norm_ffn_g, final_norm_g, w_ada, b_ada, w_in, w_out, conv_w,
           sgu_norm_g, sgu_w, sgu_b, gla_w_gate, gla_b_gate, gla_norm_g, hgrn_lower_bounds,
           hgrn_norm_g, ffn_w1, ffn_w3, ffn_w2, moe_router, moe_w1, moe_w3, moe_w2, n_cores=8):
    f = lambda a: np.ascontiguousarray(np.asarray(a, dtype=np.float32))
    x = f(x)
    B, T, _ = x.shape
    L = w_in.shape[0]
    n_moe = L // 2
    NB = B // n_cores
    key = (NB, T, L, n_moe)
    if key not in _PROG_CACHE:
        _PROG_CACHE[key] = build_program(NB, T, L, n_moe)
    nc = _PROG_CACHE[key]
    c = f(c)
    shared = {
        "g_mix": _fm(norm_mix_g), "g_ffn": _fm(norm_ffn_g), "g_fin": _fm(final_norm_g),
        "w_ada": f(w_ada), "b_ada": _fm(b_ada), "w_in": f(w_in), "w_out": f(w_out),
        "conv_w": _fm(conv_w), "sgu_g": _fm(sgu_norm_g),
        "sgu_wT": np.ascontiguousarray(f(sgu_w).transpose(3, 0, 1, 2)),
        "sgu_bf": np.ascontiguousarray(np.repeat(f(sgu_b).reshape(L, 2, 2, 1, 128), 64, axis=3).reshape(L, 2, 128, 128).transpose(2, 0, 1, 3)),
        "gla_wg": np.ascontiguousarray(f(gla_w_gate).transpose(1, 0, 2)),
        "gla_bg": np.ascontiguousarray(f(gla_b_gate).T),
        "gla_ng": np.ascontiguousarray(np.tile(f(gla_norm_g), (1, 2)).T),
        "hg_lb": np.ascontiguousarray(f(hgrn_lower_bounds).reshape(L, 2, 128).transpose(2, 1, 0)),
        "hg_ng": np.ascontiguousarray(np.tile(f(hgrn_norm_g), (1, 2)).T),
        "ffn_w1": f(ffn_w1), "ffn_w3": f(ffn_w3), "ffn_w2": f(ffn_w2),
        "moe_r": f(moe_router), "moe_w1": f(moe_w1), "moe_w3": f(moe_w3), "moe_w2": f(moe_w2),
    }
    in_maps = []
    for i in range(n_cores):
        xb = x[i * NB:(i + 1) * NB]
        m = dict(shared)
        m["xT"] = np.ascontiguousarray(xb.transpose(0, 2, 1))
        m["cT"] = np.ascontiguousarray(c[i * NB:(i + 1) * NB].T.reshape(DC, 128, NB).transpose(1, 0, 2))
        in_maps.append(m)
    res = run_bass_kernel_spmd(nc, in_maps, core_ids=list(range(n_cores)))
    out = np.empty((B, T, D), np.float32)
    for i in range(n_cores):
        out[i * NB:(i + 1) * NB] = res.results[i]["outT"].transpose(0, 2, 1)
    return out
```

```python
import contextlib
import types
import numpy as np
import concourse.bass as bass
import concourse.mybir as mybir
from concourse.bass_utils import run_bass_kernel_spmd

F32 = mybir.dt.float32
BF16 = mybir.dt.bfloat16
AF = mybir.ActivationFunctionType
ALU = mybir.AluOpType
AX = mybir.AxisListType

D = 1024
DC = 8
GW = 256
HD = 64
NE = 8
DFF = 2816
DFE = 3584
INW = 3088
EPS = 1e-6
TT = 512
CH = 32
SLAB = 256


class Dep:
    __slots__ = ("name", "last_writer", "readers", "dma_sem", "dma_count")

    def __init__(self, name="", after=None):
        self.name = name
        self.last_writer = after
        self.readers = []
        self.dma_sem = None
        self.dma_count = 0


class Op:
    __slots__ = ("eng", "fn", "deps", "signaled", "is_dma", "dep_obj", "sem_val")

    def __init__(self, eng, fn, is_dma=False):
        self.eng = eng
        self.fn = fn
        self.deps = []
        self.signaled = False
        self.is_dma = is_dma
        self.dep_obj = None
        self.sem_val = None


ENGS = ("pe", "act", "dve", "pool", "sp")


def _freeze(fn):
    if fn.__closure__ is None:
        return fn
    cells = []
    for c in fn.__closure__:
        try:
            cells.append(types.CellType(c.cell_contents))
        except ValueError:
            cells.append(c)
    g = types.FunctionType(fn.__code__, fn.__globals__, fn.__name__, fn.__defaults__, tuple(cells))
    g.__kwdefaults__ = fn.__kwdefaults__
    return g


class Prog:
    def __init__(self, nc):
        self.nc = nc
        self.ops = {e: [] for e in ENGS}
        self.n_dma_sems = 0
        self.final_waits = []

    def _collect(self, op, reads, writes, same_engine_sync=True):
        deps = []
        for d in reads:
            w = d.last_writer
            if w is not None:
                deps.append(w)
        for d in writes:
            w = d.last_writer
            if w is not None:
                if not (op.is_dma and w.is_dma):
                    deps.append(w)
            deps.extend(d.readers)
        out = []
        seen = set()
        for w in deps:
            if w is op or id(w) in seen:
                continue
            if (not w.is_dma) and w.eng == op.eng and not same_engine_sync:
                continue
            seen.add(id(w))
            out.append(w)
        op.deps = out
        for w in out:
            w.signaled = True
        for d in writes:
            d.last_writer = op
            d.readers = []
        for d in reads:
            if not op.is_dma and d.readers:
                d.readers = [r for r in d.readers if r.is_dma or r.eng != op.eng]
            d.readers.append(op)

    def op(self, eng, fn, reads=(), writes=()):
        o = Op(eng, _freeze(fn))
        self._collect(o, reads, writes, same_engine_sync=(eng != "pe"))
        self.ops[eng].append(o)
        return o

    def dma(self, queue, fn, reads=(), writes=(), dep=None):
        o = Op(queue, _freeze(fn), is_dma=True)
        d = dep if dep is not None else writes[0]
        if d.dma_sem is None:
            d.dma_sem = self.n_dma_sems
            self.n_dma_sems += 1
        d.dma_count += 1
        o.dep_obj = d
        o.sem_val = 16 * d.dma_count
        o.signaled = True
        self._collect(o, reads, writes)
        self.ops[queue].append(o)
        return o

    def pe(self, fn, reads=(), writes=()):
        return self.op("pe", fn, reads, writes)

    def act(self, fn, reads=(), writes=()):
        return self.op("act", fn, reads, writes)

    def dve(self, fn, reads=(), writes=()):
        return self.op("dve", fn, reads, writes)

    def pool(self, fn, reads=(), writes=()):
        return self.op("pool", fn, reads, writes)

    def barrier(self, deps):
        return self.op("dve", lambda e: e.memset(self.scratch, 0.0), reads=(), writes=list(deps) + [self.scratch_dep])

    def emit(self):
        nc = self.nc
        for e in ENGS:
            cnt = 0
            for o in self.ops[e]:
                if o.is_dma:
                    continue
                if o.signaled:
                    cnt += 1
                    o.sem_val = cnt
        with contextlib.ExitStack() as st:
            eng_sems = {e: st.enter_context(nc.semaphore("s_" + e)) for e in ENGS}
            dma_sems = [st.enter_context(nc.semaphore("d%d" % i)) for i in range(self.n_dma_sems)]
            block = st.enter_context(nc.Block())

            def tok(o):
                if o.is_dma:
                    return ("d", o.dep_obj.dma_sem), dma_sems[o.dep_obj.dma_sem], o.sem_val
                return ("e", o.eng), eng_sems[o.eng], o.sem_val

            def run(ename, engine):
                known = {}
                for o in self.ops[ename]:
                    need = {}
                    for w in o.deps:
                        k, s, v = tok(w)
                        if known.get(k, 0) >= v:
                            continue
                        if k not in need or need[k][1] < v:
                            need[k] = (s, v)
                    for k, (s, v) in need.items():
                        engine.wait_ge(s, v)
                        known[k] = v
                    ins = o.fn(engine)
                    if o.is_dma:
                        ins.then_inc(dma_sems[o.dep_obj.dma_sem], 16)
                    elif o.signaled:
                        ins.then_inc(eng_sems[ename], 1)
                if ename == "sp":
                    for w in self.final_waits:
                        k, s, v = tok(w)
                        engine.wait_ge(s, v)

            @block.tensor
            def _(t):
                run("pe", t)

            @block.scalar
            def _(t):
                run("act", t)

            @block.vector
            def _(t):
                run("dve", t)

            @block.gpsimd
            def _(t):
                run("pool", t)

            @block.sync
            def _(t):
                run("sp", t)


class Arena:
    def __init__(self, tensor, words):
        self.t = tensor
        self.words = words
        self.off = 0
        self.marks = []

    def alloc(self, shape, dtype=F32):
        n = int(np.prod(shape))
        w = n if dtype == F32 else (n + 1) // 2
        w = (w + 7) // 8 * 8
        assert self.off + w <= self.words, ("arena overflow", self.off, w, self.words)
        v = self.t[:, self.off:self.off + w]
        self.off += w
        if dtype != F32:
            v = v.bitcast(dtype)
        v = v[:, 0:n]
        if len(shape) == 2:
            v = v.rearrange("p (a b) -> p a b", a=shape[0])
        elif len(shape) == 3:
            v = v.rearrange("p (a b c) -> p a b c", a=shape[0], b=shape[1])
        elif len(shape) == 4:
            v = v.rearrange("p (a b c d) -> p a b c d", a=shape[0], b=shape[1], c=shape[2])
        return v

    def mark(self):
        return self.off

    def reset(self, m):
        self.off = m


def bc_last(ap, n):
    shp = list(ap.shape)
    return ap.unsqueeze(len(shp)).to_broadcast(shp + [n])


def bc_mid(ap, n):
    shp = list(ap.shape)
    return ap.unsqueeze(1).to_broadcast([shp[0], n] + shp[1:])


def build_program(NB, T, L, n_moe, diag_skip_mixers=False):
    NTT = T // TT
    n_dense = (L + 1) // 2
    nc = bass.Bass("TRN2", target_bir_lowering=False)

    def din(name, shape):
        return nc.dram_tensor(name, list(shape), F32, kind="ExternalInput").ap()

    xT = din("xT", [NB, D, T])
    cT = din("cT", [128, DC, NB])
    g_mix = din("g_mix", [128, L, DC])
    g_ffn = din("g_ffn", [128, L, DC])
    g_fin = din("g_fin", [128, DC])
    w_ada = din("w_ada", [L, D, 6 * D])
    b_ada = din("b_ada", [128, L, 48])
    w_in = din("w_in", [L, D, INW])
    w_out = din("w_out", [L, D, D])
    conv_w = din("conv_w", [128, L, 3, 2])
    sgu_g = din("sgu_g", [128, L, 2])
    sgu_wT = din("sgu_wT", [128, L, 4, 128])
    sgu_bf = din("sgu_bf", [128, L, 2, 128])
    gla_wg = din("gla_wg", [16, L, 128])
    gla_bg = din("gla_bg", [128, L])
    gla_ng = din("gla_ng", [128, L])
    hg_lb = din("hg_lb", [128, 2, L])
    hg_ng = din("hg_ng", [128, L])
    ffn_w1 = din("ffn_w1", [n_dense, D, DFF])
    ffn_w3 = din("ffn_w3", [n_dense, D, DFF])
    ffn_w2 = din("ffn_w2", [n_dense, DFF, D])
    moe_r = din("moe_r", [max(n_moe, 1), D, NE])
    moe_w1 = din("moe_w1", [max(n_moe, 1), NE, D, DFE])
    moe_w3 = din("moe_w3", [max(n_moe, 1), NE, D, DFE])
    moe_w2 = din("moe_w2", [max(n_moe, 1), NE, DFE, D])
    outT = nc.dram_tensor("outT", [NB, D, T], F32, kind="ExternalOutput").ap()

    P = Prog(nc)
    with contextlib.ExitStack() as st:
        def sb(name, shape, dt=F32):
            return st.enter_context(nc.sbuf_tensor(name, list(shape), dt))

        x = sb("x", [128, DC, T])
        h = sb("h", [128, DC, T], BF16)
        RINGW = 4352
        ring = [sb("ring%d" % i, [128, RINGW]) for i in range(2)]
        UW = 13696
        ureg = sb("ureg", [128, UW])
        ones_bf = sb("ones_bf", [128, 128], BF16)
        blk_bf = sb("blk_bf", [128, 128], BF16)
        ident_bf = sb("ident_bf", [128, 128], BF16)
        ident_f = sb("ident_f", [128, 128])
        ones_f = sb("ones_f", [128, 128])
        maskBD = sb("maskBD", [128, 128])
        m32 = sb("m32", [128, TT])
        hmask4 = sb("hmask4", [128, 4])
        hmask2 = sb("hmask2", [128, 2])
        cmask = sb("cmask", [128, 4])
        sel = sb("sel", [8, NE, 128])
        scratch = sb("scratch", [128, 8])
        P.scratch = scratch[:, 0:1]
        P.scratch_dep = Dep("scratch")
        cond = sb("cond", [128, DC, NB])
        gmix_s = sb("gmix_s", [128, L, DC])
        gffn_s = sb("gffn_s", [128, L, DC])
        gfin_s = sb("gfin_s", [128, DC])
        bada_s = sb("bada_s", [128, L, 48])
        modv = sb("modv", [128, L, 48, NB])
        gsm = sb("gsm", [128, L, DC, NB])
        gsf = sb("gsf", [128, L, DC, NB])
        convw_s = sb("convw_s", [128, L, 3, 2])
        sgug_s = sb("sgug_s", [128, L, 2])
        sguw_s = sb("sguw_s", [128, L, 4, 128], BF16)
        sgub_s = sb("sgub_s", [128, L, 2, 128])
        wg_s = sb("wg_s", [16, L, 128])
        nbg_s = sb("nbg_s", [128, L])
        glang_s = sb("glang_s", [128, L])
        hgng_s = sb("hgng_s", [128, L])
        lbe = sb("lbe", [128, 2, L])
        lb_s = sb("lb_s", [128, 2, L])
        oml_s = sb("oml_s", [128, 2, L])
        lbt = sb("lbt", [128, 2, 2])
        wr_s = sb("wr_s", [128, max(n_moe, 1), DC, NE], BF16)

        ps = [st.enter_context(nc.psum_tensor("ps%d" % i, [128, 512], F32)) for i in range(7)]
        psT = st.enter_context(nc.psum_tensor("psT", [128, 1024], BF16))
        d_ps = [Dep("ps%d" % i) for i in range(7)]
        d_psT = Dep("psT")

        d_x = [[Dep("x%d_%d" % (c, t)) for t in range(NTT)] for c in range(DC)]
        d_h = [[Dep("h%d_%d" % (c, t)) for t in range(NTT)] for c in range(DC)]
        d_ring = [[Dep("ring%d_%d" % (i, j)) for j in range(3)] for i in range(2)]
        d_const = Dep("const")
        d_par = Dep("par")
        d_setup = Dep("setup")

        C = [d_const]
        P.pool(lambda e: e.memset(ones_bf[:], 1.0), writes=C)
        P.pool(lambda e: e.memset(ones_f[:], 1.0), writes=C)
        P.pool(lambda e: e.memset(blk_bf[:], 0.0), writes=C)
        P.pool(lambda e: e.memset(blk_bf[0:64, 0:64], 1.0), reads=C, writes=C)
        P.pool(lambda e: e.memset(blk_bf[64:128, 64:128], 1.0), reads=C, writes=C)
        P.pool(lambda e: e.affine_select(out=ident_bf[:], in_=ones_bf[:], pattern=[[-1, 128]], compare_op=ALU.is_equal,
                                          fill=0.0, base=0, channel_multiplier=1), reads=C, writes=C)
        P.pool(lambda e: e.affine_select(out=ident_f[:], in_=ones_f[:], pattern=[[-1, 128]], compare_op=ALU.is_equal,
                                          fill=0.0, base=0, channel_multiplier=1), reads=C, writes=C)
        P.pool(lambda e: e.affine_select(out=maskBD[:], in_=ones_f[:], pattern=[[1, 128]], compare_op=ALU.is_ge,
                                          fill=0.0, base=0, channel_multiplier=-1), reads=C, writes=C)
        for bl in range(1, 4):
            P.pool(lambda e, bl=bl: e.affine_select(out=maskBD[:, 32 * bl:32 * bl + 32], in_=maskBD[:, 32 * bl:32 * bl + 32],
                                                    pattern=[[0, 32]], compare_op=ALU.is_ge, fill=0.0, base=-32 * bl,
                                                    channel_multiplier=1), reads=C, writes=C)
        P.pool(lambda e: e.memset(m32[:], 1.0), reads=C, writes=C)
        P.pool(lambda e: e.memset(m32[:].rearrange("p (n k) -> p n k", k=CH)[:, :, 0:1], 0.0), reads=C, writes=C)
        for (msk, nh, dk) in ((hmask4, 4, 32), (hmask2, 2, 64), (cmask, 4, 32)):
            P.pool(lambda e, msk=msk, nh=nh, dk=dk: e.affine_select(out=msk[:], in_=ones_f[:, 0:nh], pattern=[[-dk, nh]], compare_op=ALU.is_ge,
                                                                    fill=0.0, base=0, channel_multiplier=1), reads=C, writes=C)
            P.pool(lambda e, msk=msk, nh=nh, dk=dk: e.affine_select(out=msk[:], in_=msk[:], pattern=[[dk, nh]], compare_op=ALU.is_ge,
                                                                    fill=0.0, base=dk - 1, channel_multiplier=-1), reads=C, writes=C)
        P.pool(lambda e: e.affine_select(out=sel[:], in_=bc_mid(ones_f[0:8, :], NE), pattern=[[-1, NE], [0, 128]], compare_op=ALU.is_equal,
                                          fill=0.0, base=0, channel_multiplier=1), reads=C, writes=C)

        Wp = [d_par]
        for dst, src in ((cond, cT), (gmix_s, g_mix), (gffn_s, g_ffn), (gfin_s, g_fin), (bada_s, b_ada), (convw_s, conv_w),
                         (sgug_s, sgu_g), (sgub_s, sgu_bf), (wg_s, gla_wg), (nbg_s, gla_bg), (glang_s, gla_ng),
                         (hgng_s, hg_ng), (lbe, hg_lb)):
            P.dma("sp", lambda e, dst=dst, src=src: e.dma_start(out=dst[:], in_=src), writes=Wp)
        P.dma("pool", lambda e: e.dma_start(out=sguw_s[:], in_=sgu_wT), writes=Wp)
        if n_moe:
            for m in range(n_moe):
                P.dma("pool", lambda e, m=m: e.dma_start(out=wr_s[:, m], in_=moe_r[m].rearrange("(c p) e -> p c e", p=128)), writes=Wp)
        S = [d_setup]
        R_ = [d_par, d_const]
        for l in range(L):
            for hh in range(4):
                P.pool(lambda e, l=l, hh=hh: e.affine_select(out=sguw_s[:, l, hh, :], in_=sguw_s[:, l, hh, :], pattern=[[1, 128]],
                                                             compare_op=ALU.is_ge, fill=0.0, base=0, channel_multiplier=-1),
                       reads=R_, writes=S)
        P.act(lambda e: e.activation(out=cond[:], in_=cond[:], func=AF.Silu), reads=R_, writes=S)
        P.dve(lambda e: e.tensor_scalar(out=nbg_s[:], in0=nbg_s[:], scalar1=-1.0, scalar2=None, op0=ALU.mult), reads=R_, writes=S)
        P.act(lambda e: e.activation(out=lbe[:], in_=lbe[:], func=AF.Exp), reads=R_ + S, writes=S)
        P.dve(lambda e: e.tensor_reduce(out=lbt[:, :, 0:1], in_=lbe[:], axis=AX.X, op=ALU.add), reads=S, writes=S)
        P.dve(lambda e: e.reciprocal(out=lbt[:, :, 1:2], in_=lbt[:, :, 0:1]), reads=S, writes=S)
        P.dve(lambda e: e.tensor_tensor(out=lbe[:], in0=lbe[:], in1=lbt[:, :, 1:2].to_broadcast([128, 2, L]), op=ALU.mult), reads=S, writes=S)
        P.dve(lambda e: e.memset(lb_s[:, :, 0:1], 0.0), reads=S, writes=S)
        for l in range(1, L):
            P.dve(lambda e, l=l: e.tensor_tensor(out=lb_s[:, :, l:l + 1], in0=lb_s[:, :, l - 1:l], in1=lbe[:, :, l:l + 1], op=ALU.add), reads=S, writes=S)
        P.dve(lambda e: e.tensor_scalar(out=oml_s[:], in0=lb_s[:], scalar1=-1.0, scalar2=1.0, op0=ALU.mult, op1=ALU.add), reads=S, writes=S)

        ADW = 512
        rv = [ring[i][:, 0:DC * ADW].rearrange("p (c n) -> p c n", c=DC) for i in range(2)]
        step = 0
        d_ada = [Dep("ada0"), Dep("ada1")]
        for l in range(L):
            for pc in range(6 * D // ADW):
                s_ = step % 2
                step += 1
                P.dma("sp", lambda e, l=l, pc=pc, s_=s_: e.dma_start(out=rv[s_], in_=w_ada[l][:, pc * ADW:(pc + 1) * ADW].rearrange("(c p) n -> p c n", p=128)),
                      writes=[d_ada[s_]])
                for jj in range(ADW // 128):
                    j = pc * (ADW // 128) + jj
                    for c in range(DC):
                        P.pe(lambda e, s_=s_, jj=jj, j=j, c=c: e.matmul(ps[0][:, j * NB:(j + 1) * NB], lhsT=rv[s_][:, c, jj * 128:(jj + 1) * 128],
                                                                      rhs=cond[:, c, :], start=(c == 0), stop=(c == DC - 1)),
                             reads=[d_ada[s_], d_setup], writes=[d_ps[0]])
            P.dve(lambda e, l=l: e.tensor_tensor(out=modv[:, l], in0=ps[0][:, 0:48 * NB].rearrange("p (j b) -> p j b", b=NB),
                                                 in1=bc_last(bada_s[:, l, :], NB), op=ALU.add), reads=[d_ps[0], d_par], writes=S)
            for (gdst, gsrc, j0) in ((gsm, gmix_s, 8), (gsf, gffn_s, 32)):
                P.dve(lambda e, l=l, gdst=gdst, j0=j0: e.tensor_scalar(out=gdst[:, l], in0=modv[:, l, j0:j0 + 8, :], scalar1=1.0, scalar2=None, op0=ALU.add),
                      reads=S, writes=S)
                P.dve(lambda e, l=l, gdst=gdst, gsrc=gsrc: e.tensor_tensor(out=gdst[:, l], in0=gdst[:, l], in1=bc_last(gsrc[:, l, :], NB), op=ALU.mult),
                      reads=S + [d_par], writes=S)

        ada_done = P.barrier(d_ada)
        for i in range(2):
            for j in range(3):
                d_ring[i][j].last_writer = ada_done
        CONSTS = [d_const, d_par, d_setup]

        ua = Arena(ureg, UW)
        phase = {"deps": [], "after": None}

        def newdep(name=""):
            d_ = Dep(name, after=phase["after"])
            phase["deps"].append(d_)
            return d_

        def phase_switch():
            if phase["deps"]:
                phase["after"] = P.barrier(phase["deps"])
            phase["deps"] = []
            ua.reset(0)

        rot = {"i": 0}

        def rot_bank(n=3):
            i = rot["i"] % n
            rot["i"] += 1
            return i

        def rmsnorm_to_h(l, b, gs_t, sh_j0):
            phase_switch()
            sq = [ua.alloc([TT], BF16) for _ in range(3)]
            d_sq = [newdep() for _ in range(3)]
            lnv = ua.alloc([TT]); rstd = ua.alloc([TT])
            d_ln = newdep(); d_rs = newdep()
            tmp = [ua.alloc([TT]) for _ in range(3)]
            d_tmp = [newdep() for _ in range(3)]
            k = 0
            for tt in range(NTT):
                tsl = slice(tt * TT, (tt + 1) * TT)
                bk = rot_bank()
                for c in range(DC):
                    i = k % 3
                    k += 1
                    P.act(lambda e, c=c, i=i, tsl=tsl: e.activation(out=sq[i], in_=x[:, c, tsl], func=AF.Square),
                          reads=[d_x[c][tt]], writes=[d_sq[i]])
                    P.pe(lambda e, c=c, i=i, bk=bk: e.matmul(ps[bk][:, :], lhsT=ones_bf[:], rhs=sq[i], start=(c == 0), stop=(c == DC - 1)),
                         reads=[d_sq[i], d_const], writes=[d_ps[bk]])
                P.act(lambda e, bk=bk: e.activation(out=lnv, in_=ps[bk][:, :], func=AF.Ln, scale=1.0 / D, bias=EPS),
                      reads=[d_ps[bk]], writes=[d_ln])
                P.act(lambda e: e.activation(out=rstd, in_=lnv, func=AF.Exp, scale=-0.5), reads=[d_ln], writes=[d_rs])
                for c in range(DC):
                    i = k % 3
                    k += 1
                    P.dve(lambda e, c=c, i=i, tsl=tsl: e.tensor_tensor(out=tmp[i], in0=x[:, c, tsl], in1=rstd, op=ALU.mult),
                          reads=[d_x[c][tt], d_rs], writes=[d_tmp[i]])
                    P.act(lambda e, c=c, i=i, tsl=tsl: e.activation(out=h[:, c, tsl], in_=tmp[i], func=AF.Identity,
                                                                    scale=gs_t[:, l, c, b:b + 1], bias=modv[:, l, sh_j0 + c, b:b + 1]),
                          reads=[d_tmp[i], d_setup], writes=[d_h[c][tt]])

        ring_state = {"n": 0}

        def ring_next():
            s_ = ring_state["n"] % 2
            ring_state["n"] += 1
            return s_

        WO_OFF = 3328
        preloaded = {}
        plan = {"next": {}}

        def take_weights(key, loader):
            w = preloaded.pop(key) if key in preloaded else loader()
            nxt = plan["next"].get(key)
            if nxt is not None and nxt[1] is not None:
                preloaded[nxt[0]] = nxt[1]()
            return w

        def load_mixer_weights(l, ranges, r0, nrows):
            s_ = ring_next()
            ncols = sum(n for _, n in ranges)
            assert DC * ncols // 2 <= WO_OFF
            wv = ring[s_][:, 0:DC * ncols // 2].bitcast(BF16).rearrange("p (c n) -> p c n", c=DC)
            nkc = nrows // 128
            wo = ring[s_][:, WO_OFF:WO_OFF + nkc * D // 2].bitcast(BF16).rearrange("p (c n) -> p c n", c=nkc)
            nwords = DC * ncols // 2
            span = [d_ring[s_][j] for j in range(3) if nwords > (0, 1024, 2048)[j]]
            off = 0
            for (c0, n) in ranges:
                P.dma("pool", lambda e, c0=c0, n=n, off=off: e.dma_start(out=wv[:, :, off:off + n],
                                                                        in_=w_in[l][:, c0:c0 + n].rearrange("(c p) n -> p c n", p=128)),
                      writes=span, dep=d_ring[s_][0])
                off += n
            P.dma("pool", lambda e: e.dma_start(out=wo, in_=w_out[l][r0:r0 + nrows, :].rearrange("(c p) n -> p c n", p=128)),
                  writes=[d_ring[s_][2]], dep=d_ring[s_][2])
            dwo_ = [d_ring[s_][2]]
            return wv, wo, span, dwo_

        def proj_fm(wv, dw, col, tt, bk, M=128):
            tsl = slice(tt * TT, (tt + 1) * TT)
            for c in range(DC):
                P.pe(lambda e, c=c: e.matmul(ps[bk][0:M, :], lhsT=wv[:, c, col:col + M], rhs=h[:, c, tsl], start=(c == 0), stop=(c == DC - 1)),
                     reads=dw + [d_h[c][tt]], writes=[d_ps[bk]])

        def proj_tok(wv, dw, col, ncols, tt, sub, bk, off):
            t0 = tt * TT + sub * 128
            for c in range(DC):
                P.pe(lambda e, c=c: e.matmul(ps[bk][:, off:off + ncols], lhsT=h[:, c, t0:t0 + 128], rhs=wv[:, c, col:col + ncols],
                                             start=(c == 0), stop=(c == DC - 1)),
                     reads=dw + [d_h[c][tt]], writes=[d_ps[bk]])

        def out_proj(l, b, wo, dwo, y, d_y, tt, nk=2):
            tsl = slice(tt * TT, (tt + 1) * TT)
            for co in range(DC):
                bk = rot_bank()
                for kc in range(nk):
                    P.pe(lambda e, co=co, kc=kc, bk=bk: e.matmul(ps[bk][:, :], lhsT=wo[:, kc, co * 128:(co + 1) * 128], rhs=y[:, kc, :],
                                                                 start=(kc == 0), stop=(kc == nk - 1)),
                         reads=dwo + [d_y], writes=[d_ps[bk]])
                P.dve(lambda e, co=co, bk=bk: e.scalar_tensor_tensor(out=x[:, co, tsl], in0=ps[bk][:, :], scalar=modv[:, l, 16 + co, b:b + 1],
                                                                     in1=x[:, co, tsl], op0=ALU.mult, op1=ALU.add),
                      reads=[d_ps[bk], d_x[co][tt], d_setup], writes=[d_x[co][tt]])

        def conv_pass(l, b):
            phase_switch()
            wv, wo, dw, dwo = take_weights(("conv", b, l), lambda: load_mixer_weights(l, [(0, 768)], 0, 256))
            z = ua.alloc([2, TT + 2]); d_z = [newdep(), newdep()]
            cxs = [ua.alloc([TT]) for _ in range(2)]; d_cxs = [newdep(), newdep()]
            acc = [ua.alloc([TT]) for _ in range(2)]; d_acc = [newdep(), newdep()]
            y = [ua.alloc([2, TT], BF16) for _ in range(2)]; d_y = [newdep(), newdep()]
            for ch in range(2):
                P.dve(lambda e, ch=ch: e.memset(z[:, ch, 0:2], 0.0), writes=[d_z[ch]])
            deferred = []
            for tt in range(NTT):
                yi = tt % 2
                for ch in range(2):
                    b_cc = rot_bank(); proj_fm(wv, dw, 256 + ch * 128, tt, b_cc)
                    b_cx = rot_bank(); proj_fm(wv, dw, 512 + ch * 128, tt, b_cx)
                    P.act(lambda e, ch=ch, b_cx=b_cx: e.activation(out=cxs[ch], in_=ps[b_cx][:, :], func=AF.Copy),
                          reads=[d_ps[b_cx]], writes=[d_cxs[ch]])
                    P.dve(lambda e, ch=ch, b_cc=b_cc: e.tensor_tensor(out=z[:, ch, 2:TT + 2], in0=ps[b_cc][:, :], in1=cxs[ch], op=ALU.mult),
                          reads=[d_ps[b_cc], d_cxs[ch]], writes=[d_z[ch]])
                    if ch == 0 and deferred:
                        out_proj(*deferred.pop())
                    P.dve(lambda e, ch=ch: e.tensor_scalar(out=acc[ch], in0=z[:, ch, 0:TT], scalar1=convw_s[:, l, 0, ch:ch + 1], scalar2=None, op0=ALU.mult),
                          reads=[d_z[ch], d_par], writes=[d_acc[ch]])
                    for kk in (1, 2):
                        P.dve(lambda e, ch=ch, kk=kk: e.scalar_tensor_tensor(out=acc[ch], in0=z[:, ch, kk:TT + kk], scalar=convw_s[:, l, kk, ch:ch + 1],
                                                                             in1=acc[ch], op0=ALU.mult, op1=ALU.add),
                              reads=[d_z[ch], d_par, d_acc[ch]], writes=[d_acc[ch]])
                    P.dve(lambda e, ch=ch: e.tensor_copy(out=z[:, ch, 0:2], in_=z[:, ch, TT:TT + 2]), reads=[d_z[ch]], writes=[d_z[ch]])
                    b_cb = rot_bank(); proj_fm(wv, dw, ch * 128, tt, b_cb)
                    P.dve(lambda e, ch=ch, b_cb=b_cb, yi=yi: e.tensor_tensor(out=y[yi][:, ch, :], in0=ps[b_cb][:, :], in1=acc[ch], op=ALU.mult),
                          reads=[d_ps[b_cb], d_acc[ch]], writes=[d_y[yi]])
                deferred.append((l, b, wo, dwo, y[yi], d_y[yi], tt))
            out_proj(*deferred.pop())

        def sgu_pass(l, b):
            phase_switch()
            wv, wo, dw, dwo = take_weights(("sgu", b, l), lambda: load_mixer_weights(l, [(768, 512)], 256, 256))
            vn = [ua.alloc([256], BF16) for _ in range(2)]; d_vn = [newdep(), newdep()]
            stats = ua.alloc([2, 6]); mv = ua.alloc([2, 2]); rs = ua.alloc([2, 2]); d_st = [newdep(), newdep()]
            t1 = [ua.alloc([TT]) for _ in range(2)]; d_t1 = [newdep(), newdep()]
            y = [ua.alloc([2, TT], BF16) for _ in range(2)]; d_y = [newdep(), newdep()]
            k = 0
            deferred = []
            for tt in range(NTT):
                yi = tt % 2
                bm = [3, 4]
                for sub in range(4):
                    i = k % 2
                    k += 1
                    bv = rot_bank()
                    proj_tok(wv, dw, 256, 256, tt, sub, bv, 0)
                    P.dve(lambda e, i=i, bv=bv: e.bn_stats(out=stats[:, i, :], in_=ps[bv][:, 0:256]), reads=[d_ps[bv]], writes=[d_st[i]])
                    P.dve(lambda e, i=i: e.bn_aggr(out=mv[:, i, :], in_=stats[:, i, :]), reads=[d_st[i]], writes=[d_st[i]])
                    P.act(lambda e, i=i: e.activation(out=rs[:, i, 0:1], in_=mv[:, i, 1:2], func=AF.Ln, bias=EPS, scale=1.0), reads=[d_st[i]], writes=[d_st[i]])
                    P.act(lambda e, i=i: e.activation(out=rs[:, i, 1:2], in_=rs[:, i, 0:1], func=AF.Exp, scale=-0.5), reads=[d_st[i]], writes=[d_st[i]])
                    P.dve(lambda e, i=i, bv=bv: e.tensor_scalar(out=vn[i], in0=ps[bv][:, 0:256], scalar1=mv[:, i, 0:1], scalar2=rs[:, i, 1:2],
                                                                op0=ALU.subtract, op1=ALU.mult),
                          reads=[d_ps[bv], d_st[i]], writes=[d_vn[i]])
                    if sub == 0 and deferred:
                        out_proj(*deferred.pop())
                    for hh in range(4):
                        cc, hl = hh // 2, hh % 2
                        P.pe(lambda e, i=i, hh=hh, cc=cc, hl=hl, sub=sub: e.matmul(ps[bm[cc]][64 * hl:64 * hl + 64, sub * 128:(sub + 1) * 128],
                                                                                   lhsT=vn[i][:, hh * 64:(hh + 1) * 64], rhs=sguw_s[:, l, hh, :],
                                                                                   start=True, stop=True),
                             reads=[d_vn[i], d_setup], writes=[d_ps[bm[cc]]])
                for cc in range(2):
                    bu = rot_bank(); proj_fm(wv, dw, cc * 128, tt, bu)
                    P.dve(lambda e, cc=cc: e.scalar_tensor_tensor(out=t1[cc].rearrange("p (a t) -> p a t", a=4),
                                                                  in0=ps[bm[cc]][:, :].rearrange("p (a t) -> p a t", a=4),
                                                                  scalar=sgug_s[:, l, cc:cc + 1], in1=bc_mid(sgub_s[:, l, cc, :], 4),
                                                                  op0=ALU.mult, op1=ALU.add),
                          reads=[d_ps[bm[cc]], d_par], writes=[d_t1[cc]])
                    P.dve(lambda e, cc=cc, bu=bu, yi=yi: e.tensor_tensor(out=y[yi][:, cc, :], in0=ps[bu][:, :], in1=t1[cc], op=ALU.mult),
                          reads=[d_ps[bu], d_t1[cc]], writes=[d_y[yi]])
                deferred.append((l, b, wo, dwo, y[yi], d_y[yi], tt))
            out_proj(*deferred.pop())

        def recur_pass(l, b, kind, g=0):
            phase_switch()
            if kind == "gla":
                wv, wo, dw, dwo = take_weights(("gla", b, l), lambda: load_mixer_weights(l, [(1280, 784)], 512, 256))
                nh, dk, sc, hmask, ng = 4, 32, -1.0 / 16.0, hmask4, glang_s
                qc, kc, vc, gate_col, noc = 0, 128, 256, 528, 2
            else:
                base = 2064
                wv, wo, dw, dwo = take_weights(("hg%d" % g, b, l), lambda: load_mixer_weights(
                    l, [(base + g * 128, 128), (base + 256 + g * 128, 128), (base + 512 + g * 128, 128), (base + 768 + g * 128, 128)], 768 + g * 128, 128))
                nh, dk, sc, hmask, ng = 2, 64, 1.0, hmask2, hgng_s
                qc, fcol, vc, gate_col, noc = 0, 128, 256, 384, 1
            BO = [5, 6]
            BA, BU = 3, 4
            ext = ua.alloc([64, 9]); d_ext = newdep()
            dece = ua.alloc([2, 9]); d_dece = newdep()
            y = [ua.alloc([noc, TT], BF16) for _ in range(2)]; d_y = [newdep(), newdep()]
            sgate = ua.alloc([noc, TT]); d_sg = [newdep() for _ in range(noc)]
            qt = ua.alloc([TT], BF16); d_qt = newdep()
            kt = ua.alloc([TT], BF16); d_kt = newdep()
            ktm = ua.alloc([nh, TT], BF16); d_ktm = newdep()
            ktok = ua.alloc([4, 128], BF16); d_ktok = newdep()
            V = ua.alloc([4, nh * 64], BF16); d_V = newdep()
            Vbd = ua.alloc([2, nh, 4, 64], BF16); d_Vbd = newdep()
            A = [ua.alloc([nh, 128], BF16) for _ in range(2)]; d_A = [newdep(), newdep()]
            T1 = ua.alloc([TT]); d_T1 = newdep()
            T2 = ua.alloc([TT]); d_T2 = newdep()
            T3 = ua.alloc([TT]); d_T3 = newdep()
            T4 = ua.alloc([TT]); d_T4 = newdep()
            T5 = ua.alloc([TT]); d_T5 = newdep()
            T6 = ua.alloc([TT]); d_T6 = newdep()
            sm = ua.alloc([4, 16]); d_sm = newdep()
            emh = ua.alloc([4, 16]); d_emh = newdep()
            cUe = ua.alloc([64, 9]); d_cUe = newdep()
            decf = ua.alloc([64, 9]); d_decf = newdep()
            Sbd = ua.alloc([8, nh * 64], BF16); d_Sbd = newdep()
            osq = ua.alloc([TT], BF16); d_osq = newdep()
            P.dve(lambda e: e.memset(dece, 0.0), writes=[d_dece])
            P.dve(lambda e: e.memset(ext, 0.0), writes=[d_ext])
            ai = 0
            deferred = []
            for tt in range(NTT):
                tsl = slice(tt * TT, (tt + 1) * TT)
                yi = tt % 2
                for half in range(2):
                    bv = rot_bank()
                    for s2 in range(2):
                        proj_tok(wv, dw, vc, nh * 64, tt, half * 2 + s2, bv, s2 * 256)
                    P.act(lambda e, bv=bv, half=half: e.activation(out=V[:, half * 2:half * 2 + 2, :],
                                                                   in_=ps[bv][:, :].rearrange("p (s c) -> p s c", s=2)[:, :, 0:nh * 64], func=AF.Copy),
                          reads=[d_ps[bv]], writes=[d_V])
                for oc in range(noc):
                    bg_ = rot_bank(); proj_fm(wv, dw, gate_col + oc * 128, tt, bg_)
                    P.act(lambda e, bg_=bg_: e.activation(out=T1, in_=ps[bg_][:, :], func=AF.Exp, scale=-1.0), reads=[d_ps[bg_]], writes=[d_T1])
                    P.act(lambda e: e.activation(out=T2, in_=T1, func=AF.Ln, bias=1.0, scale=1.0), reads=[d_T1], writes=[d_T2])
                    P.act(lambda e: e.activation(out=T3, in_=T2, func=AF.Exp, scale=-1.0), reads=[d_T2], writes=[d_T3])
                    P.dve(lambda e, oc=oc, bg_=bg_: e.tensor_tensor(out=sgate[:, oc, :], in0=ps[bg_][:, :], in1=T3, op=ALU.mult),
                          reads=[d_ps[bg_], d_T3], writes=[d_sg[oc]])
                if kind == "gla":
                    bgl = rot_bank(); proj_fm(wv, dw, 512, tt, bgl, M=16)
                    P.act(lambda e, bgl=bgl: e.activation(out=T5[0:16, :], in_=ps[bgl][0:16, :], func=AF.Copy), reads=[d_ps[bgl]], writes=[d_T5])
                    bz = rot_bank()
                    P.pe(lambda e, bz=bz: e.matmul(ps[bz][:, :], lhsT=wg_s[0:16, l, :], rhs=T5[0:16, :], start=True, stop=True),
                         reads=[d_T5, d_par], writes=[d_ps[bz]])
                    P.act(lambda e, bz=bz: e.activation(out=T1, in_=ps[bz][:, :], func=AF.Exp, scale=-1.0, bias=nbg_s[:, l:l + 1]),
                          reads=[d_ps[bz], d_setup], writes=[d_T1])
                    P.act(lambda e: e.activation(out=T2, in_=T1, func=AF.Ln, bias=1.0, scale=1.0), reads=[d_T1], writes=[d_T2])
                    src_l, d_src = T2, d_T2
                else:
                    bf_ = rot_bank(); proj_fm(wv, dw, fcol, tt, bf_)
                    P.act(lambda e, bf_=bf_: e.activation(out=T1, in_=ps[bf_][:, :], func=AF.Exp, scale=-1.0), reads=[d_ps[bf_]], writes=[d_T1])
                    P.act(lambda e: e.activation(out=T2, in_=T1, func=AF.Ln, bias=1.0, scale=1.0), reads=[d_T1], writes=[d_T2])
                    P.act(lambda e: e.activation(out=T3, in_=T1, func=AF.Ln, bias=1.0, scale=lb_s[:, g, l:l + 1]), reads=[d_T1, d_setup], writes=[d_T3])
                    P.dve(lambda e, bf_=bf_: e.tensor_tensor(out=T6, in0=ps[bf_][:, :], in1=T2, op=ALU.add), reads=[d_ps[bf_], d_T2], writes=[d_T6])
                    P.act(lambda e: e.activation(out=T6, in_=T6, func=AF.Exp, scale=-1.0), reads=[d_T6], writes=[d_T6])
                    P.dve(lambda e: e.tensor_tensor(out=T3, in0=T3, in1=T2, op=ALU.subtract), reads=[d_T3, d_T2], writes=[d_T3])
                    src_l, d_src = T3, d_T3
                if deferred:
                    out_proj(*deferred.pop(), **{"nk": noc})
                P.dve(lambda e, src_l=src_l: e.tensor_tensor_scan(out=T4, data0=m32[:], data1=src_l, initial=0.0, op0=ALU.mult, op1=ALU.add),
                      reads=[d_src, d_const], writes=[d_T4])
                b3 = T4.rearrange("p (n k) -> p n k", k=CH)
                b4 = T4.rearrange("p (a n k) -> p a n k", a=2, k=CH)
                P.dve(lambda e, b3=b3: e.tensor_tensor(out=T5.rearrange("p (n k) -> p n k", k=CH), in0=b3,
                                                       in1=b3[:, :, CH // 2:CH // 2 + 1].to_broadcast([128, 16, CH]), op=ALU.subtract),
                      reads=[d_T4, d_T5], writes=[d_T5])
                P.act(lambda e: e.activation(out=T1, in_=T5, func=AF.Exp, scale=sc), reads=[d_T5, d_T1], writes=[d_T1])
                P.act(lambda e: e.activation(out=T2, in_=T5, func=AF.Exp, scale=-sc), reads=[d_T5, d_T2], writes=[d_T2])
                bmid, blast = b3[:, :, CH // 2], b3[:, :, CH - 1]
                P.act(lambda e, bmid=bmid: e.activation(out=sm[:, 0, :], in_=bmid, func=AF.Exp, scale=sc), reads=[d_T4], writes=[d_sm])
                P.act(lambda e, b4=b4: e.activation(out=dece[:, :, 1:9], in_=b4[:, :, :, CH - 1], func=AF.Exp, scale=sc), reads=[d_T4], writes=[d_dece])
                P.dve(lambda e, bmid=bmid, blast=blast: e.tensor_tensor(out=sm[:, 2, :], in0=blast, in1=bmid, op=ALU.subtract), reads=[d_T4, d_sm], writes=[d_sm])
                P.act(lambda e: e.activation(out=sm[:, 1, :], in_=sm[:, 2, :], func=AF.Exp, scale=sc), reads=[d_sm], writes=[d_sm])
                P.dve(lambda e: e.tensor_tensor(out=emh[:, 0:nh, :], in0=bc_mid(sm[:, 0, :], nh), in1=bc_last(hmask[:, 0:nh], 16), op=ALU.mult),
                      reads=[d_sm, d_const], writes=[d_emh])
                bq = rot_bank(); proj_fm(wv, dw, qc, tt, bq)
                if kind == "gla":
                    P.dve(lambda e, bq=bq: e.scalar_tensor_tensor(out=qt, in0=ps[bq][:, :], scalar=float(32 ** -0.5), in1=T1, op0=ALU.mult, op1=ALU.mult),
                          reads=[d_ps[bq], d_T1], writes=[d_qt])
                    bk_ = rot_bank(); proj_fm(wv, dw, kc, tt, bk_)
                    P.dve(lambda e, bk_=bk_: e.tensor_tensor(out=kt, in0=ps[bk_][:, :], in1=T2, op=ALU.mult), reads=[d_ps[bk_], d_T2], writes=[d_kt])
                else:
                    P.dve(lambda e, bq=bq: e.tensor_tensor(out=qt, in0=ps[bq][:, :], in1=T1, op=ALU.mult), reads=[d_ps[bq], d_T1], writes=[d_qt])
                    P.dve(lambda e: e.scalar_tensor_tensor(out=kt, in0=T6, scalar=oml_s[:, g, l:l + 1], in1=T2, op0=ALU.mult, op1=ALU.mult),
                          reads=[d_T6, d_T2, d_setup], writes=[d_kt])
                for hh in range(nh):
                    P.pool(lambda e, hh=hh: e.tensor_scalar(out=ktm[:, hh, :], in0=kt, scalar1=hmask[:, hh:hh + 1], scalar2=1.0, op0=ALU.mult, op1=ALU.mult),
                           reads=[d_kt, d_const], writes=[d_ktm])
                for sub in range(4):
                    P.pe(lambda e, sub=sub: e.transpose(psT[:, sub * 128:(sub + 1) * 128], kt[:, sub * 128:(sub + 1) * 128], ident_bf[:]),
                         reads=[d_kt, d_const], writes=[d_psT])
                P.act(lambda e: e.activation(out=ktok.rearrange("p a t -> p (a t)"), in_=psT[:, 0:512], func=AF.Copy), reads=[d_psT], writes=[d_ktok])
                for half in range(2):
                    for s2 in range(2):
                        for n_ in range(4):
                            P.pool(lambda e, s2=s2, n_=n_, half=half: e.tensor_scalar(
                                out=Vbd[:, s2, :, n_, :], in0=V[:, half * 2 + s2, :].rearrange("p (h v) -> p h v", h=nh),
                                scalar1=cmask[:, n_:n_ + 1], scalar2=1.0, op0=ALU.mult, op1=ALU.mult),
                                reads=[d_V, d_const], writes=[d_Vbd])
                    for s2 in range(2):
                        sub = half * 2 + s2
                        for hh in range(nh):
                            kw = {"tile_position": (0, 96)} if hh * dk == 96 else {}
                            P.pe(lambda e, s2=s2, sub=sub, hh=hh, kw=kw: e.matmul(
                                ps[BU][hh * dk:(hh + 1) * dk, s2 * 256:(s2 + 1) * 256], lhsT=ktok[:, sub, hh * dk:(hh + 1) * dk],
                                rhs=Vbd[:, s2, hh, :, :].rearrange("p n v -> p (n v)"), start=True, stop=True, **kw),
                                reads=[d_ktok, d_Vbd], writes=[d_ps[BU]])
                    n0 = half * 8
                    P.dve(lambda e, n0=n0: e.tensor_tensor(out=cUe[:, :, 1:9], in0=ps[BU][:, :].rearrange("p (n v) -> p v n", v=64),
                                                           in1=bc_mid(sm[:, 1, n0:n0 + 8], 64), op=ALU.mult),
                          reads=[d_ps[BU], d_sm], writes=[d_cUe])
                    P.dve(lambda e: e.tensor_copy(out=cUe[:, :, 0:1], in_=ext[:, :, 8:9]), reads=[d_ext, d_cUe], writes=[d_cUe])
                    P.dve(lambda e, half=half: e.tensor_copy(out=decf, in_=bc_mid(dece[:, half, :], 64)), reads=[d_dece], writes=[d_decf])
                    P.dve(lambda e: e.tensor_tensor_scan(out=ext.rearrange("p v n -> p (v n)"), data0=decf.rearrange("p v n -> p (v n)"),
                                                         data1=cUe.rearrange("p v n -> p (v n)"), initial=0.0, op0=ALU.mult, op1=ALU.add),
                          reads=[d_cUe, d_decf], writes=[d_ext])
                    for hh in range(nh):
                        P.dve(lambda e, hh=hh, n0=n0: e.tensor_tensor(out=Sbd[:, :, hh * 64:(hh + 1) * 64], in0=ext[:, :, 0:8].rearrange("p v n -> p n v"),
                                                                      in1=bc_last(emh[:, hh, n0:n0 + 8], 64), op=ALU.mult),
                              reads=[d_ext, d_emh], writes=[d_Sbd])
                    for s2 in range(2):
                        sub = half * 2 + s2
                        ssl = slice(sub * 128, (sub + 1) * 128)
                        for hh in range(nh):
                            P.pe(lambda e, hh=hh, ssl=ssl: e.matmul(ps[BA][:, hh * 128:(hh + 1) * 128], lhsT=ktm[:, hh, ssl], rhs=qt[:, ssl], start=True, stop=True),
                                 reads=[d_ktm, d_qt], writes=[d_ps[BA]])
                        a_ = ai % 2
                        ai += 1
                        P.dve(lambda e, a_=a_: e.tensor_tensor(out=A[a_], in0=ps[BA][:, 0:nh * 128].rearrange("p (h i) -> p h i", h=nh),
                                                               in1=bc_mid(maskBD[:], nh), op=ALU.mult),
                              reads=[d_ps[BA], d_const], writes=[d_A[a_]])
                        for hh in range(nh):
                            oc, hl = hh // 2, hh % 2
                            P.pe(lambda e, a_=a_, hh=hh, oc=oc, hl=hl, sub=sub, ssl=ssl: e.matmul(
                                ps[BO[oc]][64 * hl:64 * hl + 64, ssl], lhsT=V[:, sub, hh * 64:(hh + 1) * 64], rhs=A[a_][:, hh, :], start=True, stop=False),
                                reads=[d_V, d_A[a_]], writes=[d_ps[BO[oc]]])
                        for n_ in range(4):
                            nn = s2 * 4 + n_
                            csl = slice(sub * 128 + n_ * 32, sub * 128 + n_ * 32 + 32)
                            for oc in range(noc):
                                P.pe(lambda e, nn=nn, oc=oc, csl=csl, n_=n_: e.matmul(ps[BO[oc]][:, csl], lhsT=Sbd[:, nn, oc * 128:(oc + 1) * 128], rhs=qt[:, csl],
                                                                                  start=False, stop=(n_ == 3)),
                                     reads=[d_Sbd, d_qt], writes=[d_ps[BO[oc]]])
                for oc in range(noc):
                    P.act(lambda e, oc=oc: e.activation(out=osq, in_=ps[BO[oc]][:, :], func=AF.Square), reads=[d_ps[BO[oc]]], writes=[d_osq])
                    bs_ = rot_bank()
                    P.pe(lambda e, bs_=bs_: e.matmul(ps[bs_][:, :], lhsT=blk_bf[:], rhs=osq, start=True, stop=True), reads=[d_osq, d_const], writes=[d_ps[bs_]])
                    P.act(lambda e, bs_=bs_: e.activation(out=T1, in_=ps[bs_][:, :], func=AF.Ln, scale=1.0 / HD, bias=EPS), reads=[d_ps[bs_], d_T1], writes=[d_T1])
                    P.act(lambda e: e.activation(out=T2, in_=T1, func=AF.Exp, scale=-0.5), reads=[d_T1, d_T2], writes=[d_T2])
                    P.dve(lambda e, oc=oc: e.tensor_tensor(out=T3, in0=ps[BO[oc]][:, :], in1=T2, op=ALU.mult), reads=[d_ps[BO[oc]], d_T2, d_T3], writes=[d_T3])
                    P.dve(lambda e, oc=oc, yi=yi: e.scalar_tensor_tensor(out=y[yi][:, oc, :], in0=T3, scalar=ng[:, l:l + 1], in1=sgate[:, oc, :],
                                                                         op0=ALU.mult, op1=ALU.mult),
                          reads=[d_T3, d_sg[oc], d_par], writes=[d_y[yi]])
                deferred.append((l, b, wo, dwo, y[yi], d_y[yi], tt))
            out_proj(*deferred.pop(), **{"nk": noc})

        def ffn_issue(slab):
            gi, si, w1d, w3d, w2d, dff = slab
            f0 = si * SLAB
            sw = min(SLAB, dff - f0)
            s_ = ring_next()
            w1v = ring[s_][:, 0:1024].bitcast(BF16).rearrange("p (c n) -> p c n", c=DC)[:, :, 0:sw]
            w3v = ring[s_][:, 1024:2048].bitcast(BF16).rearrange("p (c n) -> p c n", c=DC)[:, :, 0:sw]
            w2v = ring[s_][:, 2048:3072].bitcast(BF16).rearrange("p (c n) -> p c n", c=2)[:, 0:sw // 128, :]
            P.dma("pool", lambda e: e.dma_start(out=w1v, in_=w1d[:, f0:f0 + sw].rearrange("(c p) n -> p c n", p=128)), writes=[d_ring[s_][0]])
            P.dma("pool", lambda e: e.dma_start(out=w3v, in_=w3d[:, f0:f0 + sw].rearrange("(c p) n -> p c n", p=128)), writes=[d_ring[s_][1]])
            P.dma("pool", lambda e: e.dma_start(out=w2v, in_=w2d[f0:f0 + sw, :].rearrange("(c p) n -> p c n", p=128)), writes=[d_ring[s_][2]])
            return (s_, sw, w1v, w3v, w2v)

        def ffn_first_slab(l):
            idx = l // 2
            if l % 2 == 0:
                return (0, 0, ffn_w1[idx], ffn_w3[idx], ffn_w2[idx], DFF)
            return (0, 0, moe_w1[idx, 0], moe_w3[idx, 0], moe_w2[idx, 0], DFE)

        def ffn_run(l, b, segs, st_):
            hid, d_hid, sa, d_sa, tq, d_tq = st_["hid"], st_["d_hid"], st_["sa"], st_["d_sa"], st_["tq"], st_["d_tq"]
            slabs = []
            for gi, (w1d, w3d, w2d, dff, cf) in enumerate(segs):
                for si in range((dff + SLAB - 1) // SLAB):
                    slabs.append((gi, si, w1d, w3d, w2d, dff))
            loaded = {}

            def issue(j):
                if j == 0 and ("ffn", b, l) in preloaded:
                    loaded[0] = preloaded.pop(("ffn", b, l))
                    return
                loaded[j] = ffn_issue(slabs[j])

            def _unused(j):
                gi, si, w1d, w3d, w2d, dff = slabs[j]
                f0 = si * SLAB
                sw = min(SLAB, dff - f0)
                s_ = ring_next()
                w1v = ring[s_][:, 0:1024].bitcast(BF16).rearrange("p (c n) -> p c n", c=DC)[:, :, 0:sw]
                w3v = ring[s_][:, 1024:2048].bitcast(BF16).rearrange("p (c n) -> p c n", c=DC)[:, :, 0:sw]
                w2v = ring[s_][:, 2048:3072].bitcast(BF16).rearrange("p (c n) -> p c n", c=2)[:, 0:sw // 128, :]
                P.dma("pool", lambda e: e.dma_start(out=w1v, in_=w1d[:, f0:f0 + sw].rearrange("(c p) n -> p c n", p=128)), writes=[d_ring[s_][0]])
                P.dma("pool", lambda e: e.dma_start(out=w3v, in_=w3d[:, f0:f0 + sw].rearrange("(c p) n -> p c n", p=128)), writes=[d_ring[s_][1]])
                P.dma("pool", lambda e: e.dma_start(out=w2v, in_=w2d[f0:f0 + sw, :].rearrange("(c p) n -> p c n", p=128)), writes=[d_ring[s_][2]])
                loaded[j] = (s_, sw, w1v, w3v, w2v)

            def stage_a(j, tt, comb):
                s_, sw, w1v, w3v, w2v = loaded[j]
                nfc = sw // 128
                tsl = slice(tt * TT, (tt + 1) * TT)
                hi = st_["k"] % 2
                st_["k"] += 1
                for fc in range(nfc):
                    pa = (st_["p"] % 2) * 2
                    st_["p"] += 1
                    for (wv_, dr, bk) in ((w1v, d_ring[s_][0], pa), (w3v, d_ring[s_][1], pa + 1)):
                        for c in range(DC):
                            P.pe(lambda e, c=c: e.matmul(ps[bk][:, :], lhsT=wv_[:, c, fc * 128:(fc + 1) * 128], rhs=h[:, c, tsl],
                                                         start=(c == 0), stop=(c == DC - 1)),
                                 reads=[dr, d_h[c][tt]], writes=[d_ps[bk]])
                        if bk == pa:
                            yield None
                    qi = st_["q"] % 3
                    st_["q"] += 1
                    P.act(lambda e: e.activation(out=sa[qi], in_=ps[pa][:, :], func=AF.Silu), reads=[d_ps[pa]], writes=[d_sa[qi]])
                    if comb is not None:
                        cb_, d_cb = comb
                        P.pool(lambda e: e.tensor_tensor(out=tq[qi], in0=sa[qi], in1=cb_[:, tsl], op=ALU.mult), reads=[d_sa[qi], d_cb], writes=[d_tq[qi]])
                        src_, dsrc = tq[qi], d_tq[qi]
                    else:
                        src_, dsrc = sa[qi], d_sa[qi]
                    P.dve(lambda e: e.tensor_tensor(out=hid[hi][:, fc, :], in0=ps[pa + 1][:, :], in1=src_, op=ALU.mult),
                          reads=[d_ps[pa + 1], dsrc], writes=[d_hid[hi][fc]])
                    yield None
                st_["last"] = (j, tt, hi)

            def stage_b(tok_):
                j, tt, hi = tok_
                s_, sw, w1v, w3v, w2v = loaded[j]
                nfc = sw // 128
                tsl = slice(tt * TT, (tt + 1) * TT)
                for co in range(DC):
                    bk = 4 + st_["o"] % 3
                    st_["o"] += 1
                    for fc in range(nfc):
                        P.pe(lambda e: e.matmul(ps[bk][:, :], lhsT=w2v[:, fc, co * 128:(co + 1) * 128], rhs=hid[hi][:, fc, :],
                                                start=(fc == 0), stop=(fc == nfc - 1)),
                             reads=[d_ring[s_][2], d_hid[hi][fc]], writes=[d_ps[bk]])
                    P.dve(lambda e: e.scalar_tensor_tensor(out=x[:, co, tsl], in0=ps[bk][:, :], scalar=modv[:, l, 40 + co, b:b + 1],
                                                           in1=x[:, co, tsl], op0=ALU.mult, op1=ALU.add),
                          reads=[d_ps[bk], d_x[co][tt], d_setup], writes=[d_x[co][tt]])
                    yield None

            def run_interleaved(ga, gb):
                a_live, b_live = ga is not None, gb is not None
                while a_live or b_live:
                    if a_live:
                        a_live = next(ga, "end") != "end"
                    for _ in range(2):
                        if b_live:
                            b_live = next(gb, "end") != "end"

            issue(0)
            if len(slabs) > 1:
                issue(1)
            pend = None
            comb = None
            for j, (gi, si, _, _, _, _) in enumerate(slabs):
                if si == 0:
                    cf = segs[gi][4]
                    comb = cf() if cf is not None else None
                for tt in range(NTT):
                    run_interleaved(stage_a(j, tt, comb), stage_b(pend) if pend is not None else None)
                    cur = st_["last"]
                    if tt == 0 and j >= 1 and j + 1 < len(slabs):
                        issue(j + 1)
                    elif tt == 0 and j >= 1 and j + 1 == len(slabs):
                        nxt = plan["next"].get(("ffn", b, l))
                        if nxt is not None:
                            preloaded[nxt[0]] = nxt[1]()
                    pend = cur
            run_interleaved(None, stage_b(pend))

        def ffn_state():
            st_ = dict(k=0, p=0, q=0, o=0)
            st_["hid"] = [ua.alloc([SLAB // 128, TT], BF16) for _ in range(2)]
            st_["d_hid"] = [[newdep() for _ in range(4)] for _ in range(2)]
            st_["sa"] = [ua.alloc([TT]) for _ in range(3)]
            st_["d_sa"] = [newdep() for _ in range(3)]
            st_["tq"] = [ua.alloc([TT]) for _ in range(3)]
            st_["d_tq"] = [newdep() for _ in range(3)]
            return st_

        def dense_ffn(l, b):
            phase_switch()
            st_ = ffn_state()
            idx = l // 2
            ffn_run(l, b, [(ffn_w1[idx], ffn_w3[idx], ffn_w2[idx], DFF, None)], st_)

        def moe_ffn(l, b):
            phase_switch()
            idx = l // 2
            st_ = ffn_state()
            NS = T // 128
            lg = ua.alloc([NS, NE]); d_lg = newdep()
            top = ua.alloc([NS, 8]); d_top = newdep()
            wts = ua.alloc([4, NS]); d_w = newdep()
            cmb = ua.alloc([NS, NE]); d_cmb = newdep()
            cm2 = ua.alloc([NS, NE]); d_cm2 = newdep()
            combT = ua.alloc([T]); d_cT = newdep()
            cbc = [ua.alloc([T]) for _ in range(2)]; d_cbc = [newdep(), newdep()]
            for s in range(NS):
                tt = (s * 128) // TT
                for c in range(DC):
                    P.pe(lambda e, s=s, c=c: e.matmul(ps[0][:, s * NE:(s + 1) * NE], lhsT=h[:, c, s * 128:(s + 1) * 128], rhs=wr_s[:, idx, c, :],
                                                      start=(c == 0), stop=(c == DC - 1)),
                         reads=[d_h[c][tt], d_par], writes=[d_ps[0]])
            P.dve(lambda e: e.tensor_copy(out=lg.rearrange("p s e -> p (s e)"), in_=ps[0][:, 0:NS * NE]), reads=[d_ps[0]], writes=[d_lg])
            for s in range(NS):
                P.dve(lambda e, s=s: e.max(out=top[:, s, :], in_=lg[:, s, :]), reads=[d_lg], writes=[d_top])
            m1 = top[:, :, 0]
            m2 = top[:, :, 1]
            P.dve(lambda e: e.tensor_tensor(out=wts[:, 0, :], in0=m2, in1=m1, op=ALU.subtract), reads=[d_top], writes=[d_w])
            P.act(lambda e: e.activation(out=wts[:, 0, :], in_=wts[:, 0, :], func=AF.Exp), reads=[d_w], writes=[d_w])
            P.dve(lambda e: e.tensor_scalar(out=wts[:, 1, :], in0=wts[:, 0, :], scalar1=1.0, scalar2=None, op0=ALU.add), reads=[d_w], writes=[d_w])
            P.dve(lambda e: e.reciprocal(out=wts[:, 1, :], in_=wts[:, 1, :]), reads=[d_w], writes=[d_w])
            P.dve(lambda e: e.tensor_tensor(out=wts[:, 2, :], in0=wts[:, 0, :], in1=wts[:, 1, :], op=ALU.mult), reads=[d_w], writes=[d_w])
            P.dve(lambda e: e.tensor_tensor(out=cmb, in0=lg, in1=bc_last(m1, NE), op=ALU.is_equal), reads=[d_lg, d_top], writes=[d_cmb])
            P.dve(lambda e: e.tensor_tensor(out=cmb, in0=cmb, in1=bc_last(wts[:, 1, :], NE), op=ALU.mult), reads=[d_cmb, d_w], writes=[d_cmb])
            P.dve(lambda e: e.tensor_tensor(out=cm2, in0=lg, in1=bc_last(m2, NE), op=ALU.is_equal), reads=[d_lg, d_top], writes=[d_cm2])
            P.dve(lambda e: e.tensor_tensor(out=cm2, in0=cm2, in1=bc_last(wts[:, 2, :], NE), op=ALU.mult), reads=[d_cm2, d_w], writes=[d_cm2])
            P.dve(lambda e: e.tensor_tensor(out=cmb, in0=cmb, in1=cm2, op=ALU.add), reads=[d_cmb, d_cm2], writes=[d_cmb])
            for s in range(NS):
                bk = 1 + (s // 4) % 2
                P.pe(lambda e, s=s, bk=bk: e.transpose(ps[bk][0:NE, (s % 4) * 128:(s % 4 + 1) * 128], cmb[:, s, :], ident_f[:]),
                     reads=[d_cmb, d_const], writes=[d_ps[bk]])
                if s % 4 == 3:
                    P.act(lambda e, s=s, bk=bk: e.activation(out=combT[0:NE, (s - 3) * 128:(s + 1) * 128], in_=ps[bk][0:NE, :], func=AF.Copy),
                          reads=[d_ps[bk]], writes=[d_cT])
            def make_comb(ex):
                def cf():
                    ci = ex % 2
                    for tt in range(NTT):
                        bk = 1 + tt % 2
                        P.pe(lambda e: e.matmul(ps[bk][:, :], lhsT=sel[0:NE, ex, :], rhs=combT[0:NE, tt * TT:(tt + 1) * TT], start=True, stop=True),
                             reads=[d_cT, d_const], writes=[d_ps[bk]])
                        P.act(lambda e: e.activation(out=cbc[ci][:, tt * TT:(tt + 1) * TT], in_=ps[bk][:, :], func=AF.Copy),
                              reads=[d_ps[bk]], writes=[d_cbc[ci]])
                    return (cbc[ci], d_cbc[ci])
                return cf
            ffn_run(l, b, [(moe_w1[idx, ex], moe_w3[idx, ex], moe_w2[idx, ex], DFE, make_comb(ex)) for ex in range(NE)], st_)

        def _mk_loader(kind, l_):
            base = 2064
            if kind == "conv":
                return lambda: load_mixer_weights(l_, [(0, 768)], 0, 256)
            if kind == "sgu":
                return lambda: load_mixer_weights(l_, [(768, 512)], 256, 256)
            if kind == "gla":
                return lambda: load_mixer_weights(l_, [(1280, 784)], 512, 256)
            if kind in ("hg0", "hg1"):
                g_ = int(kind[2])
                return lambda: load_mixer_weights(l_, [(base + g_ * 128, 128), (base + 256 + g_ * 128, 128), (base + 512 + g_ * 128, 128),
                                                        (base + 768 + g_ * 128, 128)], 768 + g_ * 128, 128)
            return lambda: ffn_issue(ffn_first_slab(l_))

        if not diag_skip_mixers:
            order = [(k, b_, l_) for b_ in range(NB) for l_ in range(L) for k in ("conv", "sgu", "gla", "hg0", "hg1", "ffn")]
            for i_, key_ in enumerate(order[:-1]):
                nk = order[i_ + 1]
                plan["next"][key_] = (nk, _mk_loader(nk[0], nk[2]))

        for b in range(NB):
            for c in range(DC):
                for tt in range(NTT):
                    P.dma("sp", lambda e, b=b, c=c, tt=tt: e.dma_start(out=x[:, c, tt * TT:(tt + 1) * TT], in_=xT[b, c * 128:(c + 1) * 128, tt * TT:(tt + 1) * TT]),
                          writes=[d_x[c][tt]])
            for l in range(L):
                if not diag_skip_mixers:
                    rmsnorm_to_h(l, b, gsm, 0)
                    conv_pass(l, b)
                    sgu_pass(l, b)
                    recur_pass(l, b, "gla")
                    recur_pass(l, b, "hgrn", 0)
                    recur_pass(l, b, "hgrn", 1)
                rmsnorm_to_h(l, b, gsf, 24)
                if l % 2 == 0:
                    dense_ffn(l, b)
                else:
                    moe_ffn(l, b)
            phase_switch()
            sq = [ua.alloc([TT], BF16) for _ in range(3)]; d_sq = [newdep() for _ in range(3)]
            lnv = ua.alloc([TT]); rstd = ua.alloc([TT]); d_ln = newdep(); d_rs = newdep()
            k = 0
            for tt in range(NTT):
                tsl = slice(tt * TT, (tt + 1) * TT)
                bk = rot_bank()
                for c in range(DC):
                    i = k % 3
                    k += 1
                    P.act(lambda e, c=c, i=i, tsl=tsl: e.activation(out=sq[i], in_=x[:, c, tsl], func=AF.Square), reads=[d_x[c][tt]], writes=[d_sq[i]])
                    P.pe(lambda e, c=c, i=i, bk=bk: e.matmul(ps[bk][:, :], lhsT=ones_bf[:], rhs=sq[i], start=(c == 0), stop=(c == DC - 1)),
                         reads=[d_sq[i], d_const], writes=[d_ps[bk]])
                P.act(lambda e, bk=bk: e.activation(out=lnv, in_=ps[bk][:, :], func=AF.Ln, scale=1.0 / D, bias=EPS), reads=[d_ps[bk]], writes=[d_ln])
                P.act(lambda e: e.activation(out=rstd, in_=lnv, func=AF.Exp, scale=-0.5), reads=[d_ln], writes=[d_rs])
                for c in range(DC):
                    P.dve(lambda e, c=c, tsl=tsl: e.scalar_tensor_tensor(out=x[:, c, tsl], in0=x[:, c, tsl], scalar=gfin_s[:, c:c + 1], in1=rstd,
                                                                         op0=ALU.mult, op1=ALU.mult),
                          reads=[d_x[c][tt], d_rs, d_par], writes=[d_x[c][tt]])
                    o = P.dma("sp", lambda e, b=b, c=c, tsl=tsl: e.dma_start(out=outT[b, c * 128:(c + 1) * 128, tsl], in_=x[:, c, tsl]),
                              reads=[d_x[c][tt]], writes=[Dep()], dep=d_x[c][tt])
                    P.final_waits.append(o)
        P.emit()
    return nc


def _fm(v):
    v = np.asarray(v, np.float32)
    lead = v.shape[:-1]
    n = v.shape[-1] // 128
    v = v.reshape(lead + (n, 128))
    return np.ascontiguousarray(np.moveaxis(v, -1, 0))


_PROG_CACHE = {}


def kernel(x, c, norm_mix_g, norm_ffn_g, final_norm_g, w_ada, b_ada, w_in, w_out, conv_w,
           sgu_norm_g, sgu_w, sgu_b, gla_w_gate, gla_b_gate, gla_norm_g, hgrn_lower_bounds,
           hgrn_norm_g, ffn_w1, ffn_w3, ffn_w2, moe_router, moe_w1, moe_w3, moe_w2, n_cores=8):
    f = lambda a: np.ascontiguousarray(np.asarray(a, dtype=np.float32))
    x = f(x)
    B, T, _ = x.shape
    L = w_in.shape[0]
    n_moe = L // 2
    NB = B // n_cores
    key = (NB, T, L, n_moe)
    if key not in _PROG_CACHE:
        _PROG_CACHE[key] = build_program(NB, T, L, n_moe)
    nc = _PROG_CACHE[key]
    c = f(c)
    shared = {
        "g_mix": _fm(norm_mix_g), "g_ffn": _fm(norm_ffn_g), "g_fin": _fm(final_norm_g),
        "w_ada": f(w_ada), "b_ada": _fm(b_ada), "w_in": f(w_in), "w_out": f(w_out),
        "conv_w": _fm(conv_w), "sgu_g": _fm(sgu_norm_g),
        "sgu_wT": np.ascontiguousarray(f(sgu_w).transpose(3, 0, 1, 2)),
        "sgu_bf": np.ascontiguousarray(np.repeat(f(sgu_b).reshape(L, 2, 2, 1, 128), 64, axis=3).reshape(L, 2, 128, 128).transpose(2, 0, 1, 3)),
        "gla_wg": np.ascontiguousarray(f(gla_w_gate).transpose(1, 0, 2)),
        "gla_bg": np.ascontiguousarray(f(gla_b_gate).T),
        "gla_ng": np.ascontiguousarray(np.tile(f(gla_norm_g), (1, 2)).T),
        "hg_lb": np.ascontiguousarray(f(hgrn_lower_bounds).reshape(L, 2, 128).transpose(2, 1, 0)),
        "hg_ng": np.ascontiguousarray(np.tile(f(hgrn_norm_g), (1, 2)).T),
        "ffn_w1": f(ffn_w1), "ffn_w3": f(ffn_w3), "ffn_w2": f(ffn_w2),
        "moe_r": f(moe_router), "moe_w1": f(moe_w1), "moe_w3": f(moe_w3), "moe_w2": f(moe_w2),
    }
    in_maps = []
    for i in range(n_cores):
        xb = x[i * NB:(i + 1) * NB]
        m = dict(shared)
        m["xT"] = np.ascontiguousarray(xb.transpose(0, 2, 1))
        m["cT"] = np.ascontiguousarray(c[i * NB:(i + 1) * NB].T.reshape(DC, 128, NB).transpose(1, 0, 2))
        in_maps.append(m)
    res = run_bass_kernel_spmd(nc, in_maps, core_ids=list(range(n_cores)))
    out = np.empty((B, T, D), np.float32)
    for i in range(n_cores):
        out[i * NB:(i + 1) * NB] = res.results[i]["outT"].transpose(0, 2, 1)
    return out
```

```python
import contextlib
import types
import numpy as np
import concourse.bass as bass
import concourse.mybir as mybir
from concourse.bass_utils import run_bass_kernel_spmd

F32 = mybir.dt.float32
BF16 = mybir.dt.bfloat16
AF = mybir.ActivationFunctionType
ALU = mybir.AluOpType
AX = mybir.AxisListType

D = 1024
DC = 8
GW = 256
HD = 64
NE = 8
DFF = 2816
DFE = 3584
INW = 3088
EPS = 1e-6
TT = 512
CH = 32
SLAB = 256


class Dep:
    __slots__ = ("name", "last_writer", "readers", "dma_sem", "dma_count")

    def __init__(self, name="", after=None):
        self.name = name
        self.last_writer = after
        self.readers = []
        self.dma_sem = None
        self.dma_count = 0


class Op:
    __slots__ = ("eng", "fn", "deps", "signaled", "is_dma", "dep_obj", "sem_val")

    def __init__(self, eng, fn, is_dma=False):
        self.eng = eng
        self.fn = fn
        self.deps = []
        self.signaled = False
        self.is_dma = is_dma
        self.dep_obj = None
        self.sem_val = None


ENGS = ("pe", "act", "dve", "pool", "sp")


def _freeze(fn):
    if fn.__closure__ is None:
        return fn
    cells = []
    for c in fn.__closure__:
        try:
            cells.append(types.CellType(c.cell_contents))
        except ValueError:
            cells.append(c)
    g = types.FunctionType(fn.__code__, fn.__globals__, fn.__name__, fn.__defaults__, tuple(cells))
    g.__kwdefaults__ = fn.__kwdefaults__
    return g


class Prog:
    def __init__(self, nc):
        self.nc = nc
        self.ops = {e: [] for e in ENGS}
        self.n_dma_sems = 0
        self.final_waits = []

    def _collect(self, op, reads, writes, same_engine_sync=True):
        deps = []
        for d in reads:
            w = d.last_writer
            if w is not None:
                deps.append(w)
        for d in writes:
            w = d.last_writer
            if w is not None:
                if not (op.is_dma and w.is_dma):
                    deps.append(w)
            deps.extend(d.readers)
        out = []
        seen = set()
        for w in deps:
            if w is op or id(w) in seen:
                continue
            if (not w.is_dma) and w.eng == op.eng and not same_engine_sync:
                continue
            seen.add(id(w))
            out.append(w)
        op.deps = out
        for w in out:
            w.signaled = True
        for d in writes:
            d.last_writer = op
            d.readers = []
        for d in reads:
            if not op.is_dma and d.readers:
                d.readers = [r for r in d.readers if r.is_dma or r.eng != op.eng]
            d.readers.append(op)

    def op(self, eng, fn, reads=(), writes=()):
        o = Op(eng, _freeze(fn))
        self._collect(o, reads, writes, same_engine_sync=(eng != "pe"))
        self.ops[eng].append(o)
        return o

    def dma(self, queue, fn, reads=(), writes=(), dep=None):
        o = Op(queue, _freeze(fn), is_dma=True)
        d = dep if dep is not None else writes[0]
        if d.dma_sem is None:
            d.dma_sem = self.n_dma_sems
            self.n_dma_sems += 1
        d.dma_count += 1
        o.dep_obj = d
        o.sem_val = 16 * d.dma_count
        o.signaled = True
        self._collect(o, reads, writes)
        self.ops[queue].append(o)
        return o

    def pe(self, fn, reads=(), writes=()):
        return self.op("pe", fn, reads, writes)

    def act(self, fn, reads=(), writes=()):
        return self.op("act", fn, reads, writes)

    def dve(self, fn, reads=(), writes=()):
        return self.op("dve", fn, reads, writes)

    def pool(self, fn, reads=(), writes=()):
        return self.op("pool", fn, reads, writes)

    def barrier(self, deps):
        return self.op("dve", lambda e: e.memset(self.scratch, 0.0), reads=(), writes=list(deps) + [self.scratch_dep])

    def emit(self):
        nc = self.nc
        for e in ENGS:
            cnt = 0
            for o in self.ops[e]:
                if o.is_dma:
                    continue
                if o.signaled:
                    cnt += 1
                    o.sem_val = cnt
        with contextlib.ExitStack() as st:
            eng_sems = {e: st.enter_context(nc.semaphore("s_" + e)) for e in ENGS}
            dma_sems = [st.enter_context(nc.semaphore("d%d" % i)) for i in range(self.n_dma_sems)]
            block = st.enter_context(nc.Block())

            def tok(o):
                if o.is_dma:
                    return ("d", o.dep_obj.dma_sem), dma_sems[o.dep_obj.dma_sem], o.sem_val
                return ("e", o.eng), eng_sems[o.eng], o.sem_val

            def run(ename, engine):
                known = {}
                for o in self.ops[ename]:
                    need = {}
                    for w in o.deps:
                        k, s, v = tok(w)
                        if known.get(k, 0) >= v:
                            continue
                        if k not in need or need[k][1] < v:
                            need[k] = (s, v)
                    for k, (s, v) in need.items():
                        engine.wait_ge(s, v)
                        known[k] = v
                    ins = o.fn(engine)
                    if o.is_dma:
                        ins.then_inc(dma_sems[o.dep_obj.dma_sem], 16)
                    elif o.signaled:
                        ins.then_inc(eng_sems[ename], 1)
                if ename == "sp":
                    for w in self.final_waits:
                        k, s, v = tok(w)
                        engine.wait_ge(s, v)

            @block.tensor
            def _(t):
                run("pe", t)

            @block.scalar
            def _(t):
                run("act", t)

            @block.vector
            def _(t):
                run("dve", t)

            @block.gpsimd
            def _(t):
                run("pool", t)

            @block.sync
            def _(t):
                run("sp", t)


class Arena:
    def __init__(self, tensor, words):
        self.t = tensor
        self.words = words
        self.off = 0
        self.marks = []

    def alloc(self, shape, dtype=F32):
        n = int(np.prod(shape))
        w = n if dtype == F32 else (n + 1) // 2
        w = (w + 7) // 8 * 8
        assert self.off + w <= self.words, ("arena overflow", self.off, w, self.words)
        v = self.t[:, self.off:self.off + w]
        self.off += w
        if dtype != F32:
            v = v.bitcast(dtype)
        v = v[:, 0:n]
        if len(shape) == 2:
            v = v.rearrange("p (a b) -> p a b", a=shape[0])
        elif len(shape) == 3:
            v = v.rearrange("p (a b c) -> p a b c", a=shape[0], b=shape[1])
        elif len(shape) == 4:
            v = v.rearrange("p (a b c d) -> p a b c d", a=shape[0], b=shape[1], c=shape[2])
        return v

    def mark(self):
        return self.off

    def reset(self, m):
        self.off = m


def bc_last(ap, n):
    shp = list(ap.shape)
    return ap.unsqueeze(len(shp)).to_broadcast(shp + [n])


def bc_mid(ap, n):
    shp = list(ap.shape)
    return ap.unsqueeze(1).to_broadcast([shp[0], n] + shp[1:])


def build_program(NB, T, L, n_moe, diag_skip_mixers=False):
    NTT = T // TT
    n_dense = (L + 1) // 2
    nc = bass.Bass("TRN2", target_bir_lowering=False)

    def din(name, shape):
        return nc.dram_tensor(name, list(shape), F32, kind="ExternalInput").ap()

    xT = din("xT", [NB, D, T])
    cT = din("cT", [128, DC, NB])
    g_mix = din("g_mix", [128, L, DC])
    g_ffn = din("g_ffn", [128, L, DC])
    g_fin = din("g_fin", [128, DC])
    w_ada = din("w_ada", [L, D, 6 * D])
    b_ada = din("b_ada", [128, L, 48])
    w_in = din("w_in", [L, D, INW])
    w_out = din("w_out", [L, D, D])
    conv_w = din("conv_w", [128, L, 3, 2])
    sgu_g = din("sgu_g", [128, L, 2])
    sgu_wT = din("sgu_wT", [128, L, 4, 128])
    sgu_bf = din("sgu_bf", [128, L, 2, 128])
    gla_wg = din("gla_wg", [16, L, 128])
    gla_bg = din("gla_bg", [128, L])
    gla_ng = din("gla_ng", [128, L])
    hg_lb = din("hg_lb", [128, 2, L])
    hg_ng = din("hg_ng", [128, L])
    ffn_w1 = din("ffn_w1", [n_dense, D, DFF])
    ffn_w3 = din("ffn_w3", [n_dense, D, DFF])
    ffn_w2 = din("ffn_w2", [n_dense, DFF, D])
    moe_r = din("moe_r", [max(n_moe, 1), D, NE])
    moe_w1 = din("moe_w1", [max(n_moe, 1), NE, D, DFE])
    moe_w3 = din("moe_w3", [max(n_moe, 1), NE, D, DFE])
    moe_w2 = din("moe_w2", [max(n_moe, 1), NE, DFE, D])
    outT = nc.dram_tensor("outT", [NB, D, T], F32, kind="ExternalOutput").ap()

    P = Prog(nc)
    with contextlib.ExitStack() as st:
        def sb(name, shape, dt=F32):
            return st.enter_context(nc.sbuf_tensor(name, list(shape), dt))

        x = sb("x", [128, DC, T])
        h = sb("h", [128, DC, T], BF16)
        RINGW = 4352
        ring = [sb("ring%d" % i, [128, RINGW]) for i in range(2)]
        UW = 13696
        ureg = sb("ureg", [128, UW])
        ones_bf = sb("ones_bf", [128, 128], BF16)
        blk_bf = sb("blk_bf", [128, 128], BF16)
        ident_bf = sb("ident_bf", [128, 128], BF16)
        ident_f = sb("ident_f", [128, 128])
        ones_f = sb("ones_f", [128, 128])
        maskBD = sb("maskBD", [128, 128])
        m32 = sb("m32", [128, TT])
        hmask4 = sb("hmask4", [128, 4])
        hmask2 = sb("hmask2", [128, 2])
        cmask = sb("cmask", [128, 4])
        sel = sb("sel", [8, NE, 128])
        scratch = sb("scratch", [128, 8])
        P.scratch = scratch[:, 0:1]
        P.scratch_dep = Dep("scratch")
        cond = sb("cond", [128, DC, NB])
        gmix_s = sb("gmix_s", [128, L, DC])
        gffn_s = sb("gffn_s", [128, L, DC])
        gfin_s = sb("gfin_s", [128, DC])
        bada_s = sb("bada_s", [128, L, 48])
        modv = sb("modv", [128, L, 48, NB])
        gsm = sb("gsm", [128, L, DC, NB])
        gsf = sb("gsf", [128, L, DC, NB])
        convw_s = sb("convw_s", [128, L, 3, 2])
        sgug_s = sb("sgug_s", [128, L, 2])
        sguw_s = sb("sguw_s", [128, L, 4, 128], BF16)
        sgub_s = sb("sgub_s", [128, L, 2, 128])
        wg_s = sb("wg_s", [16, L, 128])
        nbg_s = sb("nbg_s", [128, L])
        glang_s = sb("glang_s", [128, L])
        hgng_s = sb("hgng_s", [128, L])
        lbe = sb("lbe", [128, 2, L])
        lb_s = sb("lb_s", [128, 2, L])
        oml_s = sb("oml_s", [128, 2, L])
        lbt = sb("lbt", [128, 2, 2])
        wr_s = sb("wr_s", [128, max(n_moe, 1), DC, NE], BF16)

        ps = [st.enter_context(nc.psum_tensor("ps%d" % i, [128, 512], F32)) for i in range(7)]
        psT = st.enter_context(nc.psum_tensor("psT", [128, 1024], BF16))
        d_ps = [Dep("ps%d" % i) for i in range(7)]
        d_psT = Dep("psT")

        d_x = [[Dep("x%d_%d" % (c, t)) for t in range(NTT)] for c in range(DC)]
        d_h = [[Dep("h%d_%d" % (c, t)) for t in range(NTT)] for c in range(DC)]
        d_ring = [[Dep("ring%d_%d" % (i, j)) for j in range(3)] for i in range(2)]
        d_const = Dep("const")
        d_par = Dep("par")
        d_setup = Dep("setup")

        C = [d_const]
        P.pool(lambda e: e.memset(ones_bf[:], 1.0), writes=C)
        P.pool(lambda e: e.memset(ones_f[:], 1.0), writes=C)
        P.pool(lambda e: e.memset(blk_bf[:], 0.0), writes=C)
        P.pool(lambda e: e.memset(blk_bf[0:64, 0:64], 1.0), reads=C, writes=C)
        P.pool(lambda e: e.memset(blk_bf[64:128, 64:128], 1.0), reads=C, writes=C)
        P.pool(lambda e: e.affine_select(out=ident_bf[:], in_=ones_bf[:], pattern=[[-1, 128]], compare_op=ALU.is_equal,
                                          fill=0.0, base=0, channel_multiplier=1), reads=C, writes=C)
        P.pool(lambda e: e.affine_select(out=ident_f[:], in_=ones_f[:], pattern=[[-1, 128]], compare_op=ALU.is_equal,
                                          fill=0.0, base=0, channel_multiplier=1), reads=C, writes=C)
        P.pool(lambda e: e.affine_select(out=maskBD[:], in_=ones_f[:], pattern=[[1, 128]], compare_op=ALU.is_ge,
                                          fill=0.0, base=0, channel_multiplier=-1), reads=C, writes=C)
        for bl in range(1, 4):
            P.pool(lambda e, bl=bl: e.affine_select(out=maskBD[:, 32 * bl:32 * bl + 32], in_=maskBD[:, 32 * bl:32 * bl + 32],
                                                    pattern=[[0, 32]], compare_op=ALU.is_ge, fill=0.0, base=-32 * bl,
                                                    channel_multiplier=1), reads=C, writes=C)
        P.pool(lambda e: e.memset(m32[:], 1.0), reads=C, writes=C)
        P.pool(lambda e: e.memset(m32[:].rearrange("p (n k) -> p n k", k=CH)[:, :, 0:1], 0.0), reads=C, writes=C)
        for (msk, nh, dk) in ((hmask4, 4, 32), (hmask2, 2, 64), (cmask, 4, 32)):
            P.pool(lambda e, msk=msk, nh=nh, dk=dk: e.affine_select(out=msk[:], in_=ones_f[:, 0:nh], pattern=[[-dk, nh]], compare_op=ALU.is_ge,
                                                                    fill=0.0, base=0, channel_multiplier=1), reads=C, writes=C)
            P.pool(lambda e, msk=msk, nh=nh, dk=dk: e.affine_select(out=msk[:], in_=msk[:], pattern=[[dk, nh]], compare_op=ALU.is_ge,
                                                                    fill=0.0, base=dk - 1, channel_multiplier=-1), reads=C, writes=C)
        P.pool(lambda e: e.affine_select(out=sel[:], in_=bc_mid(ones_f[0:8, :], NE), pattern=[[-1, NE], [0, 128]], compare_op=ALU.is_equal,
                                          fill=0.0, base=0, channel_multiplier=1), reads=C, writes=C)

        Wp = [d_par]
        for dst, src in ((cond, cT), (gmix_s, g_mix), (gffn_s, g_ffn), (gfin_s, g_fin), (bada_s, b_ada), (convw_s, conv_w),
                         (sgug_s, sgu_g), (sgub_s, sgu_bf), (wg_s, gla_wg), (nbg_s, gla_bg), (glang_s, gla_ng),
                         (hgng_s, hg_ng), (lbe, hg_lb)):
            P.dma("sp", lambda e, dst=dst, src=src: e.dma_start(out=dst[:], in_=src), writes=Wp)
        P.dma("pool", lambda e: e.dma_start(out=sguw_s[:], in_=sgu_wT), writes=Wp)
        if n_moe:
            for m in range(n_moe):
                P.dma("pool", lambda e, m=m: e.dma_start(out=wr_s[:, m], in_=moe_r[m].rearrange("(c p) e -> p c e", p=128)), writes=Wp)
        S = [d_setup]
        R_ = [d_par, d_const]
        for l in range(L):
            for hh in range(4):
                P.pool(lambda e, l=l, hh=hh: e.affine_select(out=sguw_s[:, l, hh, :], in_=sguw_s[:, l, hh, :], pattern=[[1, 128]],
                                                             compare_op=ALU.is_ge, fill=0.0, base=0, channel_multiplier=-1),
                       reads=R_, writes=S)
        P.act(lambda e: e.activation(out=cond[:], in_=cond[:], func=AF.Silu), reads=R_, writes=S)
        P.dve(lambda e: e.tensor_scalar(out=nbg_s[:], in0=nbg_s[:], scalar1=-1.0, scalar2=None, op0=ALU.mult), reads=R_, writes=S)
        P.act(lambda e: e.activation(out=lbe[:], in_=lbe[:], func=AF.Exp), reads=R_ + S, writes=S)
        P.dve(lambda e: e.tensor_reduce(out=lbt[:, :, 0:1], in_=lbe[:], axis=AX.X, op=ALU.add), reads=S, writes=S)
        P.dve(lambda e: e.reciprocal(out=lbt[:, :, 1:2], in_=lbt[:, :, 0:1]), reads=S, writes=S)
        P.dve(lambda e: e.tensor_tensor(out=lbe[:], in0=lbe[:], in1=lbt[:, :, 1:2].to_broadcast([128, 2, L]), op=ALU.mult), reads=S, writes=S)
        P.dve(lambda e: e.memset(lb_s[:, :, 0:1], 0.0), reads=S, writes=S)
        for l in range(1, L):
            P.dve(lambda e, l=l: e.tensor_tensor(out=lb_s[:, :, l:l + 1], in0=lb_s[:, :, l - 1:l], in1=lbe[:, :, l:l + 1], op=ALU.add), reads=S, writes=S)
        P.dve(lambda e: e.tensor_scalar(out=oml_s[:], in0=lb_s[:], scalar1=-1.0, scalar2=1.0, op0=ALU.mult, op1=ALU.add), reads=S, writes=S)

        ADW = 512
        rv = [ring[i][:, 0:DC * ADW].rearrange("p (c n) -> p c n", c=DC) for i in range(2)]
        step = 0
        d_ada = [Dep("ada0"), Dep("ada1")]
        for l in range(L):
            for pc in range(6 * D // ADW):
                s_ = step % 2
                step += 1
                P.dma("sp", lambda e, l=l, pc=pc, s_=s_: e.dma_start(out=rv[s_], in_=w_ada[l][:, pc * ADW:(pc + 1) * ADW].rearrange("(c p) n -> p c n", p=128)),
                      writes=[d_ada[s_]])
                for jj in range(ADW // 128):
                    j = pc * (ADW // 128) + jj
                    for c in range(DC):
                        P.pe(lambda e, s_=s_, jj=jj, j=j, c=c: e.matmul(ps[0][:, j * NB:(j + 1) * NB], lhsT=rv[s_][:, c, jj * 128:(jj + 1) * 128],
                                                                      rhs=cond[:, c, :], start=(c == 0), stop=(c == DC - 1)),
                             reads=[d_ada[s_], d_setup], writes=[d_ps[0]])
            P.dve(lambda e, l=l: e.tensor_tensor(out=modv[:, l], in0=ps[0][:, 0:48 * NB].rearrange("p (j b) -> p j b", b=NB),
                                                 in1=bc_last(bada_s[:, l, :], NB), op=ALU.add), reads=[d_ps[0], d_par], writes=S)
            for (gdst, gsrc, j0) in ((gsm, gmix_s, 8), (gsf, gffn_s, 32)):
                P.dve(lambda e, l=l, gdst=gdst, j0=j0: e.tensor_scalar(out=gdst[:, l], in0=modv[:, l, j0:j0 + 8, :], scalar1=1.0, scalar2=None, op0=ALU.add),
                      reads=S, writes=S)
                P.dve(lambda e, l=l, gdst=gdst, gsrc=gsrc: e.tensor_tensor(out=gdst[:, l], in0=gdst[:, l], in1=bc_last(gsrc[:, l, :], NB), op=ALU.mult),
                      reads=S + [d_par], writes=S)

        ada_done = P.barrier(d_ada)
        for i in range(2):
            for j in range(3):
                d_ring[i][j].last_writer = ada_done
        CONSTS = [d_const, d_par, d_setup]

        ua = Arena(ureg, UW)
        phase = {"deps": [], "after": None}

        def newdep(name=""):
            d_ = Dep(name, after=phase["after"])
            phase["deps"].append(d_)
            return d_

        def phase_switch():
            if phase["deps"]:
                phase["after"] = P.barrier(phase["deps"])
            phase["deps"] = []
            ua.reset(0)

        rot = {"i": 0}

        def rot_bank(n=3):
            i = rot["i"] % n
            rot["i"] += 1
            return i

        def rmsnorm_to_h(l, b, gs_t, sh_j0):
            phase_switch()
            sq = [ua.alloc([TT], BF16) for _ in range(3)]
            d_sq = [newdep() for _ in range(3)]
            lnv = ua.alloc([TT]); rstd = ua.alloc([TT])
            d_ln = newdep(); d_rs = newdep()
            tmp = [ua.alloc([TT]) for _ in range(3)]
            d_tmp = [newdep() for _ in range(3)]
            k = 0
            for tt in range(NTT):
                tsl = slice(tt * TT, (tt + 1) * TT)
                bk = rot_bank()
                for c in range(DC):
                    i = k % 3
                    k += 1
                    P.act(lambda e, c=c, i=i, tsl=tsl: e.activation(out=sq[i], in_=x[:, c, tsl], func=AF.Square),
                          reads=[d_x[c][tt]], writes=[d_sq[i]])
                    P.pe(lambda e, c=c, i=i, bk=bk: e.matmul(ps[bk][:, :], lhsT=ones_bf[:], rhs=sq[i], start=(c == 0), stop=(c == DC - 1)),
                         reads=[d_sq[i], d_const], writes=[d_ps[bk]])
                P.act(lambda e, bk=bk: e.activation(out=lnv, in_=ps[bk][:, :], func=AF.Ln, scale=1.0 / D, bias=EPS),
                      reads=[d_ps[bk]], writes=[d_ln])
                P.act(lambda e: e.activation(out=rstd, in_=lnv, func=AF.Exp, scale=-0.5), reads=[d_ln], writes=[d_rs])
                for c in range(DC):
                    i = k % 3
                    k += 1
                    P.dve(lambda e, c=c, i=i, tsl=tsl: e.tensor_tensor(out=tmp[i], in0=x[:, c, tsl], in1=rstd, op=ALU.mult),
                          reads=[d_x[c][tt], d_rs], writes=[d_tmp[i]])
                    if c < 4:
                        P.act(lambda e, c=c, i=i, tsl=tsl: e.activation(out=h[:, c, tsl], in_=tmp[i], func=AF.Identity,
                                                                        scale=gs_t[:, l, c, b:b + 1], bias=modv[:, l, sh_j0 + c, b:b + 1]),
                              reads=[d_tmp[i], d_setup], writes=[d_h[c][tt]])
                    else:
                        P.dve(lambda e, c=c, i=i, tsl=tsl: e.tensor_scalar(out=h[:, c, tsl], in0=tmp[i], scalar1=gs_t[:, l, c, b:b + 1],
                                                                           scalar2=modv[:, l, sh_j0 + c, b:b + 1], op0=ALU.mult, op1=ALU.add),
                              reads=[d_tmp[i], d_setup], writes=[d_h[c][tt]])

        ring_state = {"n": 0}

        def ring_next():
            s_ = ring_state["n"] % 2
            ring_state["n"] += 1
            return s_

        WO_OFF = 3328
        preloaded = {}
        plan = {"next": {}}

        def take_weights(key, loader):
            w = preloaded.pop(key) if key in preloaded else loader()
            nxt = plan["next"].get(key)
            if nxt is not None and nxt[1] is not None:
                preloaded[nxt[0]] = nxt[1]()
            return w

        def load_mixer_weights(l, ranges, r0, nrows):
            s_ = ring_next()
            ncols = sum(n for _, n in ranges)
            assert DC * ncols // 2 <= WO_OFF
            wv = ring[s_][:, 0:DC * ncols // 2].bitcast(BF16).rearrange("p (c n) -> p c n", c=DC)
            nkc = nrows // 128
            wo = ring[s_][:, WO_OFF:WO_OFF + nkc * D // 2].bitcast(BF16).rearrange("p (c n) -> p c n", c=nkc)
            nwords = DC * ncols // 2
            span = [d_ring[s_][j] for j in range(3) if nwords > (0, 1024, 2048)[j]]
            off = 0
            for (c0, n) in ranges:
                P.dma("pool", lambda e, c0=c0, n=n, off=off: e.dma_start(out=wv[:, :, off:off + n],
                                                                        in_=w_in[l][:, c0:c0 + n].rearrange("(c p) n -> p c n", p=128)),
                      writes=span, dep=d_ring[s_][0])
                off += n
            P.dma("pool", lambda e: e.dma_start(out=wo, in_=w_out[l][r0:r0 + nrows, :].rearrange("(c p) n -> p c n", p=128)),
                  writes=[d_ring[s_][2]], dep=d_ring[s_][2])
            dwo_ = [d_ring[s_][2]]
            return wv, wo, span, dwo_

        def proj_fm(wv, dw, col, tt, bk, M=128):
            tsl = slice(tt * TT, (tt + 1) * TT)
            for c in range(DC):
                P.pe(lambda e, c=c: e.matmul(ps[bk][0:M, :], lhsT=wv[:, c, col:col + M], rhs=h[:, c, tsl], start=(c == 0), stop=(c == DC - 1)),
                     reads=dw + [d_h[c][tt]], writes=[d_ps[bk]])

        def proj_tok(wv, dw, col, ncols, tt, sub, bk, off):
            t0 = tt * TT + sub * 128
            for c in range(DC):
                P.pe(lambda e, c=c: e.matmul(ps[bk][:, off:off + ncols], lhsT=h[:, c, t0:t0 + 128], rhs=wv[:, c, col:col + ncols],
                                             start=(c == 0), stop=(c == DC - 1)),
                     reads=dw + [d_h[c][tt]], writes=[d_ps[bk]])

        def out_proj(l, b, wo, dwo, y, d_y, tt, nk=2):
            tsl = slice(tt * TT, (tt + 1) * TT)
            for co in range(DC):
                bk = rot_bank()
                for kc in range(nk):
                    P.pe(lambda e, co=co, kc=kc, bk=bk: e.matmul(ps[bk][:, :], lhsT=wo[:, kc, co * 128:(co + 1) * 128], rhs=y[:, kc, :],
                                                                 start=(kc == 0), stop=(kc == nk - 1)),
                         reads=dwo + [d_y], writes=[d_ps[bk]])
                P.dve(lambda e, co=co, bk=bk: e.scalar_tensor_tensor(out=x[:, co, tsl], in0=ps[bk][:, :], scalar=modv[:, l, 16 + co, b:b + 1],
                                                                     in1=x[:, co, tsl], op0=ALU.mult, op1=ALU.add),
                      reads=[d_ps[bk], d_x[co][tt], d_setup], writes=[d_x[co][tt]])

        def conv_pass(l, b):
            phase_switch()
            wv, wo, dw, dwo = take_weights(("conv", b, l), lambda: load_mixer_weights(l, [(0, 768)], 0, 256))
            z = ua.alloc([2, TT + 2]); d_z = [newdep(), newdep()]
            cxs = [ua.alloc([TT]) for _ in range(2)]; d_cxs = [newdep(), newdep()]
            acc = [ua.alloc([TT]) for _ in range(2)]; d_acc = [newdep(), newdep()]
            y = [ua.alloc([2, TT], BF16) for _ in range(2)]; d_y = [newdep(), newdep()]
            for ch in range(2):
                P.dve(lambda e, ch=ch: e.memset(z[:, ch, 0:2], 0.0), writes=[d_z[ch]])
            deferred = []
            for tt in range(NTT):
                yi = tt % 2
                for ch in range(2):
                    b_cc = rot_bank(); proj_fm(wv, dw, 256 + ch * 128, tt, b_cc)
                    b_cx = rot_bank(); proj_fm(wv, dw, 512 + ch * 128, tt, b_cx)
                    P.act(lambda e, ch=ch, b_cx=b_cx: e.activation(out=cxs[ch], in_=ps[b_cx][:, :], func=AF.Copy),
                          reads=[d_ps[b_cx]], writes=[d_cxs[ch]])
                    P.dve(lambda e, ch=ch, b_cc=b_cc: e.tensor_tensor(out=z[:, ch, 2:TT + 2], in0=ps[b_cc][:, :], in1=cxs[ch], op=ALU.mult),
                          reads=[d_ps[b_cc], d_cxs[ch]], writes=[d_z[ch]])
                    if ch == 0 and deferred:
                        out_proj(*deferred.pop())
                    P.dve(lambda e, ch=ch: e.tensor_scalar(out=acc[ch], in0=z[:, ch, 0:TT], scalar1=convw_s[:, l, 0, ch:ch + 1], scalar2=None, op0=ALU.mult),
                          reads=[d_z[ch], d_par], writes=[d_acc[ch]])
                    for kk in (1, 2):
                        P.dve(lambda e, ch=ch, kk=kk: e.scalar_tensor_tensor(out=acc[ch], in0=z[:, ch, kk:TT + kk], scalar=convw_s[:, l, kk, ch:ch + 1],
                                                                             in1=acc[ch], op0=ALU.mult, op1=ALU.add),
                              reads=[d_z[ch], d_par, d_acc[ch]], writes=[d_acc[ch]])
                    P.dve(lambda e, ch=ch: e.tensor_copy(out=z[:, ch, 0:2], in_=z[:, ch, TT:TT + 2]), reads=[d_z[ch]], writes=[d_z[ch]])
                    b_cb = rot_bank(); proj_fm(wv, dw, ch * 128, tt, b_cb)
                    P.dve(lambda e, ch=ch, b_cb=b_cb, yi=yi: e.tensor_tensor(out=y[yi][:, ch, :], in0=ps[b_cb][:, :], in1=acc[ch], op=ALU.mult),
                          reads=[d_ps[b_cb], d_acc[ch]], writes=[d_y[yi]])
                deferred.append((l, b, wo, dwo, y[yi], d_y[yi], tt))
            out_proj(*deferred.pop())

        def sgu_pass(l, b):
            phase_switch()
            wv, wo, dw, dwo = take_weights(("sgu", b, l), lambda: load_mixer_weights(l, [(768, 512)], 256, 256))
            vn = [ua.alloc([256], BF16) for _ in range(2)]; d_vn = [newdep(), newdep()]
            stats = ua.alloc([2, 6]); mv = ua.alloc([2, 2]); rs = ua.alloc([2, 2]); d_st = [newdep(), newdep()]
            t1 = [ua.alloc([TT]) for _ in range(2)]; d_t1 = [newdep(), newdep()]
            y = [ua.alloc([2, TT], BF16) for _ in range(2)]; d_y = [newdep(), newdep()]
            k = 0
            deferred = []
            for tt in range(NTT):
                yi = tt % 2
                bm = [3, 4]
                for sub in range(4):
                    i = k % 2
                    k += 1
                    bv = rot_bank()
                    proj_tok(wv, dw, 256, 256, tt, sub, bv, 0)
                    P.dve(lambda e, i=i, bv=bv: e.bn_stats(out=stats[:, i, :], in_=ps[bv][:, 0:256]), reads=[d_ps[bv]], writes=[d_st[i]])
                    P.dve(lambda e, i=i: e.bn_aggr(out=mv[:, i, :], in_=stats[:, i, :]), reads=[d_st[i]], writes=[d_st[i]])
                    P.act(lambda e, i=i: e.activation(out=rs[:, i, 0:1], in_=mv[:, i, 1:2], func=AF.Ln, bias=EPS, scale=1.0), reads=[d_st[i]], writes=[d_st[i]])
                    P.act(lambda e, i=i: e.activation(out=rs[:, i, 1:2], in_=rs[:, i, 0:1], func=AF.Exp, scale=-0.5), reads=[d_st[i]], writes=[d_st[i]])
                    P.dve(lambda e, i=i, bv=bv: e.tensor_scalar(out=vn[i], in0=ps[bv][:, 0:256], scalar1=mv[:, i, 0:1], scalar2=rs[:, i, 1:2],
                                                                op0=ALU.subtract, op1=ALU.mult),
                          reads=[d_ps[bv], d_st[i]], writes=[d_vn[i]])
                    if sub == 0 and deferred:
                        out_proj(*deferred.pop())
                    for hh in range(4):
                        cc, hl = hh // 2, hh % 2
                        P.pe(lambda e, i=i, hh=hh, cc=cc, hl=hl, sub=sub: e.matmul(ps[bm[cc]][64 * hl:64 * hl + 64, sub * 128:(sub + 1) * 128],
                                                                                   lhsT=vn[i][:, hh * 64:(hh + 1) * 64], rhs=sguw_s[:, l, hh, :],
                                                                                   start=True, stop=True),
                             reads=[d_vn[i], d_setup], writes=[d_ps[bm[cc]]])
                for cc in range(2):
                    bu = rot_bank(); proj_fm(wv, dw, cc * 128, tt, bu)
                    P.dve(lambda e, cc=cc: e.scalar_tensor_tensor(out=t1[cc].rearrange("p (a t) -> p a t", a=4),
                                                                  in0=ps[bm[cc]][:, :].rearrange("p (a t) -> p a t", a=4),
                                                                  scalar=sgug_s[:, l, cc:cc + 1], in1=bc_mid(sgub_s[:, l, cc, :], 4),
                                                                  op0=ALU.mult, op1=ALU.add),
                          reads=[d_ps[bm[cc]], d_par], writes=[d_t1[cc]])
                    P.dve(lambda e, cc=cc, bu=bu, yi=yi: e.tensor_tensor(out=y[yi][:, cc, :], in0=ps[bu][:, :], in1=t1[cc], op=ALU.mult),
                          reads=[d_ps[bu], d_t1[cc]], writes=[d_y[yi]])
                deferred.append((l, b, wo, dwo, y[yi], d_y[yi], tt))
            out_proj(*deferred.pop())

        def recur_pass(l, b, kind, g=0):
            phase_switch()
            if kind == "gla":
                wv, wo, dw, dwo = take_weights(("gla", b, l), lambda: load_mixer_weights(l, [(1280, 784)], 512, 256))
                nh, dk, sc, hmask, ng = 4, 32, -1.0 / 16.0, hmask4, glang_s
                qc, kc, vc, gate_col, noc = 0, 128, 256, 528, 2
            else:
                base = 2064
                wv, wo, dw, dwo = take_weights(("hg%d" % g, b, l), lambda: load_mixer_weights(
                    l, [(base + g * 128, 128), (base + 256 + g * 128, 128), (base + 512 + g * 128, 128), (base + 768 + g * 128, 128)], 768 + g * 128, 128))
                nh, dk, sc, hmask, ng = 2, 64, 1.0, hmask2, hgng_s
                qc, fcol, vc, gate_col, noc = 0, 128, 256, 384, 1
            BO = [5, 6]
            BA, BU = 3, 4
            ext = ua.alloc([64, 9]); d_ext = newdep()
            dece = ua.alloc([2, 9]); d_dece = newdep()
            y = [ua.alloc([noc, TT], BF16) for _ in range(2)]; d_y = [newdep(), newdep()]
            sgate = ua.alloc([noc, TT]); d_sg = [newdep() for _ in range(noc)]
            qt = ua.alloc([TT], BF16); d_qt = newdep()
            kt = ua.alloc([TT], BF16); d_kt = newdep()
            ktm = ua.alloc([nh, TT], BF16); d_ktm = newdep()
            ktok = ua.alloc([4, 128], BF16); d_ktok = newdep()
            V = ua.alloc([4, nh * 64], BF16); d_V = newdep()
            Vbd = ua.alloc([2, nh, 4, 64], BF16); d_Vbd = newdep()
            A = [ua.alloc([nh, 128], BF16) for _ in range(2)]; d_A = [newdep(), newdep()]
            T1 = ua.alloc([TT]); d_T1 = newdep()
            T2 = ua.alloc([TT]); d_T2 = newdep()
            T3 = ua.alloc([TT]); d_T3 = newdep()
            T4 = ua.alloc([TT]); d_T4 = newdep()
            T5 = ua.alloc([TT]); d_T5 = newdep()
            T6 = ua.alloc([TT]); d_T6 = newdep()
            sm = ua.alloc([4, 16]); d_sm = newdep()
            emh = ua.alloc([4, 16]); d_emh = newdep()
            cUe = ua.alloc([64, 9]); d_cUe = newdep()
            decf = ua.alloc([64, 9]); d_decf = newdep()
            Sbd = ua.alloc([8, nh * 64], BF16); d_Sbd = newdep()
            osq = ua.alloc([TT], BF16); d_osq = newdep()
            P.dve(lambda e: e.memset(dece, 0.0), writes=[d_dece])
            P.dve(lambda e: e.memset(ext, 0.0), writes=[d_ext])
            ai = 0
            deferred = []
            for tt in range(NTT):
                tsl = slice(tt * TT, (tt + 1) * TT)
                yi = tt % 2
                for half in range(2):
                    bv = rot_bank()
                    for s2 in range(2):
                        proj_tok(wv, dw, vc, nh * 64, tt, half * 2 + s2, bv, s2 * 256)
                    P.act(lambda e, bv=bv, half=half: e.activation(out=V[:, half * 2:half * 2 + 2, :],
                                                                   in_=ps[bv][:, :].rearrange("p (s c) -> p s c", s=2)[:, :, 0:nh * 64], func=AF.Copy),
                          reads=[d_ps[bv]], writes=[d_V])
                for oc in range(noc):
                    bg_ = rot_bank(); proj_fm(wv, dw, gate_col + oc * 128, tt, bg_)
                    P.act(lambda e, bg_=bg_: e.activation(out=T1, in_=ps[bg_][:, :], func=AF.Exp, scale=-1.0), reads=[d_ps[bg_]], writes=[d_T1])
                    P.act(lambda e: e.activation(out=T2, in_=T1, func=AF.Ln, bias=1.0, scale=1.0), reads=[d_T1], writes=[d_T2])
                    P.act(lambda e: e.activation(out=T3, in_=T2, func=AF.Exp, scale=-1.0), reads=[d_T2], writes=[d_T3])
                    P.dve(lambda e, oc=oc, bg_=bg_: e.tensor_tensor(out=sgate[:, oc, :], in0=ps[bg_][:, :], in1=T3, op=ALU.mult),
                          reads=[d_ps[bg_], d_T3], writes=[d_sg[oc]])
                if kind == "gla":
                    bgl = rot_bank(); proj_fm(wv, dw, 512, tt, bgl, M=16)
                    P.act(lambda e, bgl=bgl: e.activation(out=T5[0:16, :], in_=ps[bgl][0:16, :], func=AF.Copy), reads=[d_ps[bgl]], writes=[d_T5])
                    bz = rot_bank()
                    P.pe(lambda e, bz=bz: e.matmul(ps[bz][:, :], lhsT=wg_s[0:16, l, :], rhs=T5[0:16, :], start=True, stop=True),
                         reads=[d_T5, d_par], writes=[d_ps[bz]])
                    P.act(lambda e, bz=bz: e.activation(out=T1, in_=ps[bz][:, :], func=AF.Exp, scale=-1.0, bias=nbg_s[:, l:l + 1]),
                          reads=[d_ps[bz], d_setup], writes=[d_T1])
                    P.act(lambda e: e.activation(out=T2, in_=T1, func=AF.Ln, bias=1.0, scale=1.0), reads=[d_T1], writes=[d_T2])
                    src_l, d_src = T2, d_T2
                else:
                    bf_ = rot_bank(); proj_fm(wv, dw, fcol, tt, bf_)
                    P.act(lambda e, bf_=bf_: e.activation(out=T1, in_=ps[bf_][:, :], func=AF.Exp, scale=-1.0), reads=[d_ps[bf_]], writes=[d_T1])
                    P.act(lambda e: e.activation(out=T2, in_=T1, func=AF.Ln, bias=1.0, scale=1.0), reads=[d_T1], writes=[d_T2])
                    P.act(lambda e: e.activation(out=T3, in_=T1, func=AF.Ln, bias=1.0, scale=lb_s[:, g, l:l + 1]), reads=[d_T1, d_setup], writes=[d_T3])
                    P.dve(lambda e, bf_=bf_: e.tensor_tensor(out=T6, in0=ps[bf_][:, :], in1=T2, op=ALU.add), reads=[d_ps[bf_], d_T2], writes=[d_T6])
                    P.act(lambda e: e.activation(out=T6, in_=T6, func=AF.Exp, scale=-1.0), reads=[d_T6], writes=[d_T6])
                    P.dve(lambda e: e.tensor_tensor(out=T3, in0=T3, in1=T2, op=ALU.subtract), reads=[d_T3, d_T2], writes=[d_T3])
                    src_l, d_src = T3, d_T3
                if deferred:
                    out_proj(*deferred.pop(), **{"nk": noc})
                P.dve(lambda e, src_l=src_l: e.tensor_tensor_scan(out=T4, data0=m32[:], data1=src_l, initial=0.0, op0=ALU.mult, op1=ALU.add),
                      reads=[d_src, d_const], writes=[d_T4])
                b3 = T4.rearrange("p (n k) -> p n k", k=CH)
                b4 = T4.rearrange("p (a n k) -> p a n k", a=2, k=CH)
                P.dve(lambda e, b3=b3: e.tensor_tensor(out=T5.rearrange("p (n k) -> p n k", k=CH), in0=b3,
                                                       in1=b3[:, :, CH // 2:CH // 2 + 1].to_broadcast([128, 16, CH]), op=ALU.subtract),
                      reads=[d_T4, d_T5], writes=[d_T5])
                P.act(lambda e: e.activation(out=T1, in_=T5, func=AF.Exp, scale=sc), reads=[d_T5, d_T1], writes=[d_T1])
                P.act(lambda e: e.activation(out=T2, in_=T5, func=AF.Exp, scale=-sc), reads=[d_T5, d_T2], writes=[d_T2])
                bmid, blast = b3[:, :, CH // 2], b3[:, :, CH - 1]
                P.act(lambda e, bmid=bmid: e.activation(out=sm[:, 0, :], in_=bmid, func=AF.Exp, scale=sc), reads=[d_T4], writes=[d_sm])
                P.act(lambda e, b4=b4: e.activation(out=dece[:, :, 1:9], in_=b4[:, :, :, CH - 1], func=AF.Exp, scale=sc), reads=[d_T4], writes=[d_dece])
                P.dve(lambda e, bmid=bmid, blast=blast: e.tensor_tensor(out=sm[:, 2, :], in0=blast, in1=bmid, op=ALU.subtract), reads=[d_T4, d_sm], writes=[d_sm])
                P.act(lambda e: e.activation(out=sm[:, 1, :], in_=sm[:, 2, :], func=AF.Exp, scale=sc), reads=[d_sm], writes=[d_sm])
                P.dve(lambda e: e.tensor_tensor(out=emh[:, 0:nh, :], in0=bc_mid(sm[:, 0, :], nh), in1=bc_last(hmask[:, 0:nh], 16), op=ALU.mult),
                      reads=[d_sm, d_const], writes=[d_emh])
                bq = rot_bank(); proj_fm(wv, dw, qc, tt, bq)
                if kind == "gla":
                    P.dve(lambda e, bq=bq: e.scalar_tensor_tensor(out=qt, in0=ps[bq][:, :], scalar=float(32 ** -0.5), in1=T1, op0=ALU.mult, op1=ALU.mult),
                          reads=[d_ps[bq], d_T1], writes=[d_qt])
                    bk_ = rot_bank(); proj_fm(wv, dw, kc, tt, bk_)
                    P.dve(lambda e, bk_=bk_: e.tensor_tensor(out=kt, in0=ps[bk_][:, :], in1=T2, op=ALU.mult), reads=[d_ps[bk_], d_T2], writes=[d_kt])
                else:
                    P.dve(lambda e, bq=bq: e.tensor_tensor(out=qt, in0=ps[bq][:, :], in1=T1, op=ALU.mult), reads=[d_ps[bq], d_T1], writes=[d_qt])
                    P.dve(lambda e: e.scalar_tensor_tensor(out=kt, in0=T6, scalar=oml_s[:, g, l:l + 1], in1=T2, op0=ALU.mult, op1=ALU.mult),
                          reads=[d_T6, d_T2, d_setup], writes=[d_kt])
                for hh in range(nh):
                    P.pool(lambda e, hh=hh: e.tensor_scalar(out=ktm[:, hh, :], in0=kt, scalar1=hmask[:, hh:hh + 1], scalar2=1.0, op0=ALU.mult, op1=ALU.mult),
                           reads=[d_kt, d_const], writes=[d_ktm])
                for sub in range(4):
                    P.pe(lambda e, sub=sub: e.transpose(psT[:, sub * 128:(sub + 1) * 128], kt[:, sub * 128:(sub + 1) * 128], ident_bf[:]),
                         reads=[d_kt, d_const], writes=[d_psT])
                P.act(lambda e: e.activation(out=ktok.rearrange("p a t -> p (a t)"), in_=psT[:, 0:512], func=AF.Copy), reads=[d_psT], writes=[d_ktok])
                for half in range(2):
                    for s2 in range(2):
                        for n_ in range(4):
                            P.pool(lambda e, s2=s2, n_=n_, half=half: e.tensor_scalar(
                                out=Vbd[:, s2, :, n_, :], in0=V[:, half * 2 + s2, :].rearrange("p (h v) -> p h v", h=nh),
                                scalar1=cmask[:, n_:n_ + 1], scalar2=1.0, op0=ALU.mult, op1=ALU.mult),
                                reads=[d_V, d_const], writes=[d_Vbd])
                    a_of = {}
                    for s2 in range(2):
                        sub = half * 2 + s2
                        ssl = slice(sub * 128, (sub + 1) * 128)
                        bsc = BA if s2 == 0 else rot_bank()
                        for hh in range(nh):
                            P.pe(lambda e, hh=hh, ssl=ssl, bsc=bsc: e.matmul(ps[bsc][:, hh * 128:(hh + 1) * 128], lhsT=ktm[:, hh, ssl], rhs=qt[:, ssl], start=True, stop=True),
                                 reads=[d_ktm, d_qt], writes=[d_ps[bsc]])
                        a_ = ai % 2
                        ai += 1
                        a_of[s2] = a_
                        P.dve(lambda e, a_=a_, bsc=bsc: e.tensor_tensor(out=A[a_], in0=ps[bsc][:, 0:nh * 128].rearrange("p (h i) -> p h i", h=nh),
                                                                        in1=bc_mid(maskBD[:], nh), op=ALU.mult),
                              reads=[d_ps[bsc], d_const], writes=[d_A[a_]])
                    for s2 in range(2):
                        sub = half * 2 + s2
                        for hh in range(nh):
                            kw = {"tile_position": (0, 96)} if hh * dk == 96 else {}
                            P.pe(lambda e, s2=s2, sub=sub, hh=hh, kw=kw: e.matmul(
                                ps[BU][hh * dk:(hh + 1) * dk, s2 * 256:(s2 + 1) * 256], lhsT=ktok[:, sub, hh * dk:(hh + 1) * dk],
                                rhs=Vbd[:, s2, hh, :, :].rearrange("p n v -> p (n v)"), start=True, stop=True, **kw),
                                reads=[d_ktok, d_Vbd], writes=[d_ps[BU]])
                    n0 = half * 8
                    P.dve(lambda e, n0=n0: e.tensor_tensor(out=cUe[:, :, 1:9], in0=ps[BU][:, :].rearrange("p (n v) -> p v n", v=64),
                                                           in1=bc_mid(sm[:, 1, n0:n0 + 8], 64), op=ALU.mult),
                          reads=[d_ps[BU], d_sm], writes=[d_cUe])
                    P.dve(lambda e: e.tensor_copy(out=cUe[:, :, 0:1], in_=ext[:, :, 8:9]), reads=[d_ext, d_cUe], writes=[d_cUe])
                    P.dve(lambda e, half=half: e.tensor_copy(out=decf, in_=bc_mid(dece[:, half, :], 64)), reads=[d_dece], writes=[d_decf])
                    P.dve(lambda e: e.tensor_tensor_scan(out=ext.rearrange("p v n -> p (v n)"), data0=decf.rearrange("p v n -> p (v n)"),
                                                         data1=cUe.rearrange("p v n -> p (v n)"), initial=0.0, op0=ALU.mult, op1=ALU.add),
                          reads=[d_cUe, d_decf], writes=[d_ext])
                    for hh in range(nh):
                        P.dve(lambda e, hh=hh, n0=n0: e.tensor_tensor(out=Sbd[:, :, hh * 64:(hh + 1) * 64], in0=ext[:, :, 0:8].rearrange("p v n -> p n v"),
                                                                      in1=bc_last(emh[:, hh, n0:n0 + 8], 64), op=ALU.mult),
                              reads=[d_ext, d_emh], writes=[d_Sbd])
                    for s2 in range(2):
                        sub = half * 2 + s2
                        ssl = slice(sub * 128, (sub + 1) * 128)
                        a_ = a_of[s2]
                        for hh in range(nh):
                            oc, hl = hh // 2, hh % 2
                            P.pe(lambda e, a_=a_, hh=hh, oc=oc, hl=hl, sub=sub, ssl=ssl: e.matmul(
                                ps[BO[oc]][64 * hl:64 * hl + 64, ssl], lhsT=V[:, sub, hh * 64:(hh + 1) * 64], rhs=A[a_][:, hh, :], start=True, stop=False),
                                reads=[d_V, d_A[a_]], writes=[d_ps[BO[oc]]])
                        for n_ in range(4):
                            nn = s2 * 4 + n_
                            csl = slice(sub * 128 + n_ * 32, sub * 128 + n_ * 32 + 32)
                            for oc in range(noc):
                                P.pe(lambda e, nn=nn, oc=oc, csl=csl, n_=n_: e.matmul(ps[BO[oc]][:, csl], lhsT=Sbd[:, nn, oc * 128:(oc + 1) * 128], rhs=qt[:, csl],
                                                                                  start=False, stop=(n_ == 3)),
                                     reads=[d_Sbd, d_qt], writes=[d_ps[BO[oc]]])
                for oc in range(noc):
                    P.act(lambda e, oc=oc: e.activation(out=osq, in_=ps[BO[oc]][:, :], func=AF.Square), reads=[d_ps[BO[oc]]], writes=[d_osq])
                    bs_ = rot_bank()
                    P.pe(lambda e, bs_=bs_: e.matmul(ps[bs_][:, :], lhsT=blk_bf[:], rhs=osq, start=True, stop=True), reads=[d_osq, d_const], writes=[d_ps[bs_]])
                    P.act(lambda e, bs_=bs_: e.activation(out=T1, in_=ps[bs_][:, :], func=AF.Ln, scale=1.0 / HD, bias=EPS), reads=[d_ps[bs_], d_T1], writes=[d_T1])
                    P.act(lambda e: e.activation(out=T2, in_=T1, func=AF.Exp, scale=-0.5), reads=[d_T1, d_T2], writes=[d_T2])
                    P.dve(lambda e, oc=oc: e.tensor_tensor(out=T3, in0=ps[BO[oc]][:, :], in1=T2, op=ALU.mult), reads=[d_ps[BO[oc]], d_T2, d_T3], writes=[d_T3])
                    P.dve(lambda e, oc=oc, yi=yi: e.scalar_tensor_tensor(out=y[yi][:, oc, :], in0=T3, scalar=ng[:, l:l + 1], in1=sgate[:, oc, :],
                                                                         op0=ALU.mult, op1=ALU.mult),
                          reads=[d_T3, d_sg[oc], d_par], writes=[d_y[yi]])
                deferred.append((l, b, wo, dwo, y[yi], d_y[yi], tt))
            out_proj(*deferred.pop(), **{"nk": noc})

        def ffn_issue(slab):
            gi, si, w1d, w3d, w2d, dff = slab
            f0 = si * SLAB
            sw = min(SLAB, dff - f0)
            s_ = ring_next()
            w1v = ring[s_][:, 0:1024].bitcast(BF16).rearrange("p (c n) -> p c n", c=DC)[:, :, 0:sw]
            w3v = ring[s_][:, 1024:2048].bitcast(BF16).rearrange("p (c n) -> p c n", c=DC)[:, :, 0:sw]
            w2v = ring[s_][:, 2048:3072].bitcast(BF16).rearrange("p (c n) -> p c n", c=2)[:, 0:sw // 128, :]
            P.dma("pool", lambda e: e.dma_start(out=w1v, in_=w1d[:, f0:f0 + sw].rearrange("(c p) n -> p c n", p=128)), writes=[d_ring[s_][0]])
            P.dma("pool", lambda e: e.dma_start(out=w3v, in_=w3d[:, f0:f0 + sw].rearrange("(c p) n -> p c n", p=128)), writes=[d_ring[s_][1]])
            P.dma("pool", lambda e: e.dma_start(out=w2v, in_=w2d[f0:f0 + sw, :].rearrange("(c p) n -> p c n", p=128)), writes=[d_ring[s_][2]])
            return (s_, sw, w1v, w3v, w2v)

        def ffn_first_slab(l):
            idx = l // 2
            if l % 2 == 0:
                return (0, 0, ffn_w1[idx], ffn_w3[idx], ffn_w2[idx], DFF)
            return (0, 0, moe_w1[idx, 0], moe_w3[idx, 0], moe_w2[idx, 0], DFE)

        def ffn_run(l, b, segs, st_):
            hid, d_hid, sa, d_sa, tq, d_tq = st_["hid"], st_["d_hid"], st_["sa"], st_["d_sa"], st_["tq"], st_["d_tq"]
            slabs = []
            for gi, (w1d, w3d, w2d, dff, cf) in enumerate(segs):
                for si in range((dff + SLAB - 1) // SLAB):
                    slabs.append((gi, si, w1d, w3d, w2d, dff))
            loaded = {}

            def issue(j):
                if j == 0 and ("ffn", b, l) in preloaded:
                    loaded[0] = preloaded.pop(("ffn", b, l))
                    return
                loaded[j] = ffn_issue(slabs[j])

            def _unused(j):
                gi, si, w1d, w3d, w2d, dff = slabs[j]
                f0 = si * SLAB
                sw = min(SLAB, dff - f0)
                s_ = ring_next()
                w1v = ring[s_][:, 0:1024].bitcast(BF16).rearrange("p (c n) -> p c n", c=DC)[:, :, 0:sw]
                w3v = ring[s_][:, 1024:2048].bitcast(BF16).rearrange("p (c n) -> p c n", c=DC)[:, :, 0:sw]
                w2v = ring[s_][:, 2048:3072].bitcast(BF16).rearrange("p (c n) -> p c n", c=2)[:, 0:sw // 128, :]
                P.dma("pool", lambda e: e.dma_start(out=w1v, in_=w1d[:, f0:f0 + sw].rearrange("(c p) n -> p c n", p=128)), writes=[d_ring[s_][0]])
                P.dma("pool", lambda e: e.dma_start(out=w3v, in_=w3d[:, f0:f0 + sw].rearrange("(c p) n -> p c n", p=128)), writes=[d_ring[s_][1]])
                P.dma("pool", lambda e: e.dma_start(out=w2v, in_=w2d[f0:f0 + sw, :].rearrange("(c p) n -> p c n", p=128)), writes=[d_ring[s_][2]])
                loaded[j] = (s_, sw, w1v, w3v, w2v)

            def stage_a(j, tt, comb):
                s_, sw, w1v, w3v, w2v = loaded[j]
                nfc = sw // 128
                tsl = slice(tt * TT, (tt + 1) * TT)
                hi = st_["k"] % 2
                st_["k"] += 1
                for fc in range(nfc):
                    pa = (st_["p"] % 2) * 2
                    st_["p"] += 1
                    for (wv_, dr, bk) in ((w1v, d_ring[s_][0], pa), (w3v, d_ring[s_][1], pa + 1)):
                        for c in range(DC):
                            P.pe(lambda e, c=c: e.matmul(ps[bk][:, :], lhsT=wv_[:, c, fc * 128:(fc + 1) * 128], rhs=h[:, c, tsl],
                                                         start=(c == 0), stop=(c == DC - 1)),
                                 reads=[dr, d_h[c][tt]], writes=[d_ps[bk]])
                        if bk == pa:
                            yield None
                    qi = st_["q"] % 3
                    st_["q"] += 1
                    P.act(lambda e: e.activation(out=sa[qi], in_=ps[pa][:, :], func=AF.Silu), reads=[d_ps[pa]], writes=[d_sa[qi]])
                    if comb is not None:
                        cb_, d_cb = comb
                        P.pool(lambda e: e.tensor_tensor(out=tq[qi], in0=sa[qi], in1=cb_[:, tsl], op=ALU.mult), reads=[d_sa[qi], d_cb], writes=[d_tq[qi]])
                        src_, dsrc = tq[qi], d_tq[qi]
                    else:
                        src_, dsrc = sa[qi], d_sa[qi]
                    P.dve(lambda e: e.tensor_tensor(out=hid[hi][:, fc, :], in0=ps[pa + 1][:, :], in1=src_, op=ALU.mult),
                          reads=[d_ps[pa + 1], dsrc], writes=[d_hid[hi][fc]])
                    yield None
                st_["last"] = (j, tt, hi)

            def stage_b(tok_):
                j, tt, hi = tok_
                s_, sw, w1v, w3v, w2v = loaded[j]
                nfc = sw // 128
                tsl = slice(tt * TT, (tt + 1) * TT)
                for co in range(DC):
                    bk = 4 + st_["o"] % 3
                    st_["o"] += 1
                    for fc in range(nfc):
                        P.pe(lambda e: e.matmul(ps[bk][:, :], lhsT=w2v[:, fc, co * 128:(co + 1) * 128], rhs=hid[hi][:, fc, :],
                                                start=(fc == 0), stop=(fc == nfc - 1)),
                             reads=[d_ring[s_][2], d_hid[hi][fc]], writes=[d_ps[bk]])
                    P.dve(lambda e: e.scalar_tensor_tensor(out=x[:, co, tsl], in0=ps[bk][:, :], scalar=modv[:, l, 40 + co, b:b + 1],
                                                           in1=x[:, co, tsl], op0=ALU.mult, op1=ALU.add),
                          reads=[d_ps[bk], d_x[co][tt], d_setup], writes=[d_x[co][tt]])
                    yield None

            def run_interleaved(ga, gb):
                a_live, b_live = ga is not None, gb is not None
                while a_live or b_live:
                    if a_live:
                        a_live = next(ga, "end") != "end"
                    for _ in range(2):
                        if b_live:
                            b_live = next(gb, "end") != "end"

            issue(0)
            if len(slabs) > 1:
                issue(1)
            pend = None
            comb = None
            for j, (gi, si, _, _, _, _) in enumerate(slabs):
                if si == 0:
                    cf = segs[gi][4]
                    comb = cf() if cf is not None else None
                for tt in range(NTT):
                    run_interleaved(stage_a(j, tt, comb), stage_b(pend) if pend is not None else None)
                    cur = st_["last"]
                    if tt == 0 and j >= 1 and j + 1 < len(slabs):
                        issue(j + 1)
                    elif tt == 0 and j >= 1 and j + 1 == len(slabs):
                        nxt = plan["next"].get(("ffn", b, l))
                        if nxt is not None:
                            preloaded[nxt[0]] = nxt[1]()
                    pend = cur
            run_interleaved(None, stage_b(pend))

        def ffn_state():
            st_ = dict(k=0, p=0, q=0, o=0)
            st_["hid"] = [ua.alloc([SLAB // 128, TT], BF16) for _ in range(2)]
            st_["d_hid"] = [[newdep() for _ in range(4)] for _ in range(2)]
            st_["sa"] = [ua.alloc([TT]) for _ in range(3)]
            st_["d_sa"] = [newdep() for _ in range(3)]
            st_["tq"] = [ua.alloc([TT]) for _ in range(3)]
            st_["d_tq"] = [newdep() for _ in range(3)]
            return st_

        def dense_ffn(l, b):
            phase_switch()
            st_ = ffn_state()
            idx = l // 2
            ffn_run(l, b, [(ffn_w1[idx], ffn_w3[idx], ffn_w2[idx], DFF, None)], st_)

        def moe_ffn(l, b):
            phase_switch()
            idx = l // 2
            st_ = ffn_state()
            NS = T // 128
            lg = ua.alloc([NS, NE]); d_lg = newdep()
            top = ua.alloc([NS, 8]); d_top = newdep()
            wts = ua.alloc([4, NS]); d_w = newdep()
            cmb = ua.alloc([NS, NE]); d_cmb = newdep()
            cm2 = ua.alloc([NS, NE]); d_cm2 = newdep()
            combT = ua.alloc([T]); d_cT = newdep()
            cbc = [ua.alloc([T]) for _ in range(2)]; d_cbc = [newdep(), newdep()]
            for s in range(NS):
                tt = (s * 128) // TT
                for c in range(DC):
                    P.pe(lambda e, s=s, c=c: e.matmul(ps[0][:, s * NE:(s + 1) * NE], lhsT=h[:, c, s * 128:(s + 1) * 128], rhs=wr_s[:, idx, c, :],
                                                      start=(c == 0), stop=(c == DC - 1)),
                         reads=[d_h[c][tt], d_par], writes=[d_ps[0]])
            P.dve(lambda e: e.tensor_copy(out=lg.rearrange("p s e -> p (s e)"), in_=ps[0][:, 0:NS * NE]), reads=[d_ps[0]], writes=[d_lg])
            for s in range(NS):
                P.dve(lambda e, s=s: e.max(out=top[:, s, :], in_=lg[:, s, :]), reads=[d_lg], writes=[d_top])
            m1 = top[:, :, 0]
            m2 = top[:, :, 1]
            P.dve(lambda e: e.tensor_tensor(out=wts[:, 0, :], in0=m2, in1=m1, op=ALU.subtract), reads=[d_top], writes=[d_w])
            P.act(lambda e: e.activation(out=wts[:, 0, :], in_=wts[:, 0, :], func=AF.Exp), reads=[d_w], writes=[d_w])
            P.dve(lambda e: e.tensor_scalar(out=wts[:, 1, :], in0=wts[:, 0, :], scalar1=1.0, scalar2=None, op0=ALU.add), reads=[d_w], writes=[d_w])
            P.dve(lambda e: e.reciprocal(out=wts[:, 1, :], in_=wts[:, 1, :]), reads=[d_w], writes=[d_w])
            P.dve(lambda e: e.tensor_tensor(out=wts[:, 2, :], in0=wts[:, 0, :], in1=wts[:, 1, :], op=ALU.mult), reads=[d_w], writes=[d_w])
            P.dve(lambda e: e.tensor_tensor(out=cmb, in0=lg, in1=bc_last(m1, NE), op=ALU.is_equal), reads=[d_lg, d_top], writes=[d_cmb])
            P.dve(lambda e: e.tensor_tensor(out=cmb, in0=cmb, in1=bc_last(wts[:, 1, :], NE), op=ALU.mult), reads=[d_cmb, d_w], writes=[d_cmb])
            P.dve(lambda e: e.tensor_tensor(out=cm2, in0=lg, in1=bc_last(m2, NE), op=ALU.is_equal), reads=[d_lg, d_top], writes=[d_cm2])
            P.dve(lambda e: e.tensor_tensor(out=cm2, in0=cm2, in1=bc_last(wts[:, 2, :], NE), op=ALU.mult), reads=[d_cm2, d_w], writes=[d_cm2])
            P.dve(lambda e: e.tensor_tensor(out=cmb, in0=cmb, in1=cm2, op=ALU.add), reads=[d_cmb, d_cm2], writes=[d_cmb])
            for s in range(NS):
                bk = 1 + (s // 4) % 2
                P.pe(lambda e, s=s, bk=bk: e.transpose(ps[bk][0:NE, (s % 4) * 128:(s % 4 + 1) * 128], cmb[:, s, :], ident_f[:]),
                     reads=[d_cmb, d_const], writes=[d_ps[bk]])
                if s % 4 == 3:
                    P.act(lambda e, s=s, bk=bk: e.activation(out=combT[0:NE, (s - 3) * 128:(s + 1) * 128], in_=ps[bk][0:NE, :], func=AF.Copy),
                          reads=[d_ps[bk]], writes=[d_cT])
            def make_comb(ex):
                def cf():
                    ci = ex % 2
                    for tt in range(NTT):
                        bk = 1 + tt % 2
                        P.pe(lambda e: e.matmul(ps[bk][:, :], lhsT=sel[0:NE, ex, :], rhs=combT[0:NE, tt * TT:(tt + 1) * TT], start=True, stop=True),
                             reads=[d_cT, d_const], writes=[d_ps[bk]])
                        P.act(lambda e: e.activation(out=cbc[ci][:, tt * TT:(tt + 1) * TT], in_=ps[bk][:, :], func=AF.Copy),
                              reads=[d_ps[bk]], writes=[d_cbc[ci]])
                    return (cbc[ci], d_cbc[ci])
                return cf
            ffn_run(l, b, [(moe_w1[idx, ex], moe_w3[idx, ex], moe_w2[idx, ex], DFE, make_comb(ex)) for ex in range(NE)], st_)

        def _mk_loader(kind, l_):
            base = 2064
            if kind == "conv":
                return lambda: load_mixer_weights(l_, [(0, 768)], 0, 256)
            if kind == "sgu":
                return lambda: load_mixer_weights(l_, [(768, 512)], 256, 256)
            if kind == "gla":
                return lambda: load_mixer_weights(l_, [(1280, 784)], 512, 256)
            if kind in ("hg0", "hg1"):
                g_ = int(kind[2])
                return lambda: load_mixer_weights(l_, [(base + g_ * 128, 128), (base + 256 + g_ * 128, 128), (base + 512 + g_ * 128, 128),
                                                        (base + 768 + g_ * 128, 128)], 768 + g_ * 128, 128)
            return lambda: ffn_issue(ffn_first_slab(l_))

        if not diag_skip_mixers:
            order = [(k, b_, l_) for b_ in range(NB) for l_ in range(L) for k in ("conv", "sgu", "gla", "hg0", "hg1", "ffn")]
            for i_, key_ in enumerate(order[:-1]):
                nk = order[i_ + 1]
                plan["next"][key_] = (nk, _mk_loader(nk[0], nk[2]))

        for b in range(NB):
            for c in range(DC):
                for tt in range(NTT):
                    P.dma("sp", lambda e, b=b, c=c, tt=tt: e.dma_start(out=x[:, c, tt * TT:(tt + 1) * TT], in_=xT[b, c * 128:(c + 1) * 128, tt * TT:(tt + 1) * TT]),
                          writes=[d_x[c][tt]])
            for l in range(L):
                if not diag_skip_mixers:
                    rmsnorm_to_h(l, b, gsm, 0)
                    conv_pass(l, b)
                    sgu_pass(l, b)
                    recur_pass(l, b, "gla")
                    recur_pass(l, b, "hgrn", 0)
                    recur_pass(l, b, "hgrn", 1)
                rmsnorm_to_h(l, b, gsf, 24)
                if l % 2 == 0:
                    dense_ffn(l, b)
                else:
                    moe_ffn(l, b)
            phase_switch()
            sq = [ua.alloc([TT], BF16) for _ in range(3)]; d_sq = [newdep() for _ in range(3)]
            lnv = ua.alloc([TT]); rstd = ua.alloc([TT]); d_ln = newdep(); d_rs = newdep()
            k = 0
            for tt in range(NTT):
                tsl = slice(tt * TT, (tt + 1) * TT)
                bk = rot_bank()
                for c in range(DC):
                    i = k % 3
                    k += 1
                    P.act(lambda e, c=c, i=i, tsl=tsl: e.activation(out=sq[i], in_=x[:, c, tsl], func=AF.Square), reads=[d_x[c][tt]], writes=[d_sq[i]])
                    P.pe(lambda e, c=c, i=i, bk=bk: e.matmul(ps[bk][:, :], lhsT=ones_bf[:], rhs=sq[i], start=(c == 0), stop=(c == DC - 1)),
                         reads=[d_sq[i], d_const], writes=[d_ps[bk]])
                P.act(lambda e, bk=bk: e.activation(out=lnv, in_=ps[bk][:, :], func=AF.Ln, scale=1.0 / D, bias=EPS), reads=[d_ps[bk]], writes=[d_ln])
                P.act(lambda e: e.activation(out=rstd, in_=lnv, func=AF.Exp, scale=-0.5), reads=[d_ln], writes=[d_rs])
                for c in range(DC):
                    P.dve(lambda e, c=c, tsl=tsl: e.scalar_tensor_tensor(out=x[:, c, tsl], in0=x[:, c, tsl], scalar=gfin_s[:, c:c + 1], in1=rstd,
                                                                         op0=ALU.mult, op1=ALU.mult),
                          reads=[d_x[c][tt], d_rs, d_par], writes=[d_x[c][tt]])
                    o = P.dma("sp", lambda e, b=b, c=c, tsl=tsl: e.dma_start(out=outT[b, c * 128:(c + 1) * 128, tsl], in_=x[:, c, tsl]),
                              reads=[d_x[c][tt]], writes=[Dep()], dep=d_x[c][tt])
                    P.final_waits.append(o)
        P.emit()
    return nc


def _fm(v):
    v = np.asarray(v, np.float32)
    lead = v.shape[:-1]
    n = v.shape[-1] // 128
    v = v.reshape(lead + (n, 128))
    return np.ascontiguousarray(np.moveaxis(v, -1, 0))


_PROG_CACHE = {}


def kernel(x, c, norm_mix_g, norm_ffn_g, final_norm_g, w_ada, b_ada, w_in, w_out, conv_w,
           sgu_norm_g, sgu_w, sgu_b, gla_w_gate, gla_b_gate, gla_norm_g, hgrn_lower_bounds,
           hgrn_norm_g, ffn_w1, ffn_w3, ffn_w2, moe_router, moe_w1, moe_w3, moe_w2, n_cores=8):
    f = lambda a: np.ascontiguousarray(np.asarray(a, dtype=np.float32))
    x = f(x)
    B, T, _ = x.shape
    L = w_in.shape[0]
    n_moe = L // 2
    NB = B // n_cores
    key = (NB, T, L, n_moe)
    if key not in _PROG_CACHE:
        _PROG_CACHE[key] = build_program(NB, T, L, n_moe)
    nc = _PROG_CACHE[key]
    c = f(c)
    shared = {
        "g_mix": _fm(norm_mix_g), "g_ffn": _fm(norm_ffn_g), "g_fin": _fm(final_norm_g),
        "w_ada": f(w_ada), "b_ada": _fm(b_ada), "w_in": f(w_in), "w_out": f(w_out),
        "conv_w": _fm(conv_w), "sgu_g": _fm(sgu_norm_g),
        "sgu_wT": np.ascontiguousarray(f(sgu_w).transpose(3, 0, 1, 2)),
        "sgu_bf": np.ascontiguousarray(np.repeat(f(sgu_b).reshape(L, 2, 2, 1, 128), 64, axis=3).reshape(L, 2, 128, 128).transpose(2, 0, 1, 3)),
        "gla_wg": np.ascontiguousarray(f(gla_w_gate).transpose(1, 0, 2)),
        "gla_bg": np.ascontiguousarray(f(gla_b_gate).T),
        "gla_ng": np.ascontiguousarray(np.tile(f(gla_norm_g), (1, 2)).T),
        "hg_lb": np.ascontiguousarray(f(hgrn_lower_bounds).reshape(L, 2, 128).transpose(2, 1, 0)),
        "hg_ng": np.ascontiguousarray(np.tile(f(hgrn_norm_g), (1, 2)).T),
        "ffn_w1": f(ffn_w1), "ffn_w3": f(ffn_w3), "ffn_w2": f(ffn_w2),
        "moe_r": f(moe_router), "moe_w1": f(moe_w1), "moe_w3": f(moe_w3), "moe_w2": f(moe_w2),
    }
    in_maps = []
    for i in range(n_cores):
        xb = x[i * NB:(i + 1) * NB]
        m = dict(shared)
        m["xT"] = np.ascontiguousarray(xb.transpose(0, 2, 1))
        m["cT"] = np.ascontiguousarray(c[i * NB:(i + 1) * NB].T.reshape(DC, 128, NB).transpose(1, 0, 2))
        in_maps.append(m)
    res = run_bass_kernel_spmd(nc, in_maps, core_ids=list(range(n_cores)))
    out = np.empty((B, T, D), np.float32)
    for i in range(n_cores):
        out[i * NB:(i + 1) * NB] = res.results[i]["outT"].transpose(0, 2, 1)
    return out
```
